# Optimizing a Trainium2 kernel written in Bass

```python
import jax, jax.numpy as jnp
from jax import lax
import numpy as np

D_MODEL = 1024
BATCH = 8
SEQ = 2048
DEPTH = 4

N_MIXERS = 4
Q_BLOCK = 128
GATHER_CHUNK = 16
MAX_POS_OFFSET = 4096

MLA_HEADS = 8
MLA_NOPE = 128
MLA_ROPE = 64
MLA_V = 128
MLA_Q_RANK = 256
MLA_KV_RANK = 256
ROPE_BASE = 10000.0

MOBA_HEADS = 8
MOBA_HD = 128
MOBA_BLOCK = 256
MOBA_TOPK = 3

NSA_HEADS = 8
NSA_GROUPS = 2
NSA_HD = 128
NSA_CMP_LEN = 32
NSA_CMP_STRIDE = 16
NSA_SEL_BLOCK = 64
NSA_SEL_TOPN = 16
NSA_WINDOW = 512
NSA_FORCE_BONUS = 100.0

SB_HEADS = 8
SB_HD = 128

D_FF = 2816
CONV_W = 3
LN_EPS = 1e-5
RMS_EPS = 1e-6
NEG = -1e30
TINY = 1e-30
DEEPNORM_ALPHA = (2 * DEPTH) ** 0.25
DEEPNORM_BETA = (8 * DEPTH) ** -0.25

kernel_name = 'hybrid_mla_moba_nsa_stickbreaking_trunk'


def _count(kind):
    return len(range(kind, DEPTH, N_MIXERS))


def layer_norm(x, g, b):
    xf = x.astype(jnp.float32)
    mu = jnp.mean(xf, -1, keepdims=True)
    var = jnp.mean(jnp.square(xf - mu), -1, keepdims=True)
    return ((xf - mu) * lax.rsqrt(var + LN_EPS) * g.astype(jnp.float32) + b.astype(jnp.float32)).astype(x.dtype)


def rms_norm(x, g):
    xf = x.astype(jnp.float32)
    return (xf * lax.rsqrt(jnp.mean(jnp.square(xf), -1, keepdims=True) + RMS_EPS) * g.astype(jnp.float32)).astype(x.dtype)


def apply_rope(x, cos, sin):
    half = x.shape[-1] // 2
    x1 = x[..., :half].astype(jnp.float32)
    x2 = x[..., half:].astype(jnp.float32)
    return jnp.concatenate([x1 * cos - x2 * sin, x2 * cos + x1 * sin], -1).astype(x.dtype)


def alibi_slopes(n):
    return 2.0 ** (-8.0 * jnp.arange(1, n + 1, dtype=jnp.float32) / n)


def masked_softmax(s, mask):
    s = jnp.where(mask, s, NEG)
    p = jnp.where(mask, jnp.exp(s - jnp.max(s, -1, keepdims=True)), 0.0)
    return p / jnp.maximum(jnp.sum(p, -1, keepdims=True), TINY)


def split_heads3(proj, n_heads, hd):
    B, S, _ = proj.shape
    t = proj.reshape(B, S, 3, n_heads, hd).transpose(2, 0, 3, 1, 4)
    return t[0], t[1], t[2]


def merge_heads(o):
    B, H, S, D = o.shape
    return o.transpose(0, 2, 1, 3).reshape(B, S, H * D)


def causal_softmax_attention(q, k, v, scale):
    B, H, S, Dk = q.shape
    nqb = S // Q_BLOCK
    q_blocks = q.reshape(B, H, nqb, Q_BLOCK, Dk).transpose(2, 0, 1, 3, 4)
    k_idx = jnp.arange(S)

    def block(args):
        q_b, bi = args
        s = jnp.einsum('bhqd,bhkd->bhqk', q_b, k, preferred_element_type=jnp.float32) * scale
        q_idx = bi * Q_BLOCK + jnp.arange(Q_BLOCK)
        p = masked_softmax(s, k_idx[None, :] <= q_idx[:, None])
        return jnp.einsum('bhqk,bhkd->bhqd', p.astype(v.dtype), v)

    o = lax.map(block, (q_blocks, jnp.arange(nqb)))
    return o.transpose(1, 2, 0, 3, 4).reshape(B, H, S, v.shape[-1])


def mla_mixer(h, cos, sin, w_in, q_norm, w_uq, kv_norm, w_ukv, w_o):
    B, S, _ = h.shape
    H, DN, DR, DV = MLA_HEADS, MLA_NOPE, MLA_ROPE, MLA_V
    proj = h @ w_in
    c_q = rms_norm(proj[..., :MLA_Q_RANK], q_norm)
    c_kv = rms_norm(proj[..., MLA_Q_RANK:MLA_Q_RANK + MLA_KV_RANK], kv_norm)
    k_rope = apply_rope(proj[..., MLA_Q_RANK + MLA_KV_RANK:], cos, sin)
    q = (c_q @ w_uq).reshape(B, S, H, DN + DR).transpose(0, 2, 1, 3)
    q = jnp.concatenate([q[..., :DN], apply_rope(q[..., DN:], cos[:, None], sin[:, None])], -1)
    kv = (c_kv @ w_ukv).reshape(B, S, H, DN + DV).transpose(0, 2, 1, 3)
    k = jnp.concatenate([kv[..., :DN], jnp.broadcast_to(k_rope[:, None], (B, H, S, DR))], -1)
    v = kv[..., DN:]
    o = causal_softmax_attention(q, k, v, (DN + DR) ** -0.5)
    return merge_heads(o) @ w_o


def moba_mixer(h, positions, w_in, w_o):
    B, S, _ = h.shape
    H, D, BLK, C = MOBA_HEADS, MOBA_HD, MOBA_BLOCK, GATHER_CHUNK
    q, k, v = split_heads3(h @ w_in, H, D)
    nb = -(-S // BLK)
    pad = nb * BLK - S
    posf = positions.astype(jnp.float32)
    k_p = jnp.pad(k, ((0, 0), (0, 0), (0, pad), (0, 0)))
    v_p = jnp.pad(v, ((0, 0), (0, 0), (0, pad), (0, 0)))
    pos_p = jnp.pad(posf, ((0, 0), (0, pad)))
    k_blk = k_p.reshape(B, H, nb, BLK, D)
    v_blk = v_p.reshape(B, H, nb, BLK, D)
    pos_blk = pos_p.reshape(B, nb, BLK)
    k_mean = jnp.mean(k_blk.astype(jnp.float32), axis=3)
    slope = alibi_slopes(H)[None, :, None, None]
    kk = min(MOBA_TOPK, nb)
    nch = S // C
    scale = D ** -0.5
    b_ix = jnp.arange(B)[:, None, None, None]
    h_ix = jnp.arange(H)[None, :, None, None]
    blk_off = jnp.arange(BLK)
    sel_rank = jnp.arange(kk)

    def chunk(args):
        q_c, pq, ci = args
        start = ci * C
        q_idx = start + jnp.arange(C)
        own = start // BLK
        gate = jnp.einsum('bhqd,bhnd->bhqn', q_c.astype(jnp.float32), k_mean)
        gate = jnp.where(jnp.arange(nb) < own, gate, NEG)
        _, sel = lax.top_k(gate, kk)
        sel_valid = sel_rank < own
        k_sel = k_blk[b_ix, h_ix, sel]
        v_sel = v_blk[b_ix, h_ix, sel]
        kpos_sel = pos_blk[b_ix, sel]
        s_sel = jnp.einsum('bhqd,bhqnkd->bhqnk', q_c, k_sel, preferred_element_type=jnp.float32) * scale
        s_sel = s_sel - slope[..., None] * jnp.abs(pq[:, None, :, None, None] - kpos_sel)
        k_own = lax.dynamic_slice_in_dim(k_p, own * BLK, BLK, axis=2)
        v_own = lax.dynamic_slice_in_dim(v_p, own * BLK, BLK, axis=2)
        kpos_own = lax.dynamic_slice_in_dim(pos_p, own * BLK, BLK, axis=1)
        s_own = jnp.einsum('bhqd,bhkd->bhqk', q_c, k_own, preferred_element_type=jnp.float32) * scale
        s_own = s_own - slope * jnp.abs(pq[:, None, :, None] - kpos_own[:, None, None, :])
        mask_own = (own * BLK + blk_off)[None, :] <= q_idx[:, None]
        mask_sel = jnp.broadcast_to(jnp.repeat(sel_valid, BLK)[None, :], (C, kk * BLK))
        s = jnp.concatenate([s_sel.reshape(B, H, C, kk * BLK), s_own], -1)
        p = masked_softmax(s, jnp.concatenate([mask_sel, mask_own], -1))
        p_sel = p[..., :kk * BLK].reshape(B, H, C, kk, BLK).astype(v.dtype)
        p_own = p[..., kk * BLK:].astype(v.dtype)
        return (jnp.einsum('bhqnk,bhqnkd->bhqd', p_sel, v_sel)
                + jnp.einsum('bhqk,bhkd->bhqd', p_own, v_own))

    q_chunks = q.reshape(B, H, nch, C, D).transpose(2, 0, 1, 3, 4)
    pq_chunks = posf.reshape(B, nch, C).transpose(1, 0, 2)
    o = lax.map(chunk, (q_chunks, pq_chunks, jnp.arange(nch)))
    o = o.transpose(1, 2, 0, 3, 4).reshape(B, H, S, D)
    return merge_heads(o) @ w_o


def nsa_mixer(h, positions, w_in, cmp_pos, cmp_w1, cmp_w2, w_o):
    B, S, _ = h.shape
    H, G, D = NSA_HEADS, NSA_GROUPS, NSA_HD
    R = H // G
    L, ST, SB, W = NSA_CMP_LEN, NSA_CMP_STRIDE, NSA_SEL_BLOCK, NSA_WINDOW
    proj = h @ w_in
    q = proj[..., :H * D].reshape(B, S, G, R, D).transpose(0, 2, 3, 1, 4)
    kv = proj[..., H * D:H * D + 6 * G * D].reshape(B, S, 6, G, D).transpose(2, 0, 3, 1, 4)
    k_cmp_raw, v_cmp_raw, k_slc, v_slc, k_win, v_win = kv[0], kv[1], kv[2], kv[3], kv[4], kv[5]
    gates = jax.nn.sigmoid(proj[..., H * D + 6 * G * D:].astype(jnp.float32))
    gates = gates.reshape(B, S, 3, G, R).transpose(2, 0, 3, 4, 1)[..., None]
    posf = positions.astype(jnp.float32)
    slope = alibi_slopes(H).reshape(G, R)[None, :, :, None, None]
    scale = D ** -0.5
    q_idx_all = jnp.arange(S)

    nc = (S - L) // ST + 1
    c_start = jnp.arange(nc) * ST
    c_idx = c_start[:, None] + jnp.arange(L)[None, :]
    c_end = c_start + L - 1

    def compress(t, pe, w1, w2):
        blocks = t[:, :, c_idx] + pe
        return jax.nn.gelu(blocks.reshape(B, G, nc, L * D) @ w1) @ w2

    k_c = compress(k_cmp_raw, cmp_pos[0], cmp_w1[0], cmp_w2[0])
    v_c = compress(v_cmp_raw, cmp_pos[1], cmp_w1[1], cmp_w2[1])
    s_c = jnp.einsum('bgrqd,bgnd->bgrqn', q, k_c, preferred_element_type=jnp.float32) * scale
    s_c = s_c - slope * jnp.abs(posf[:, None, None, :, None] - posf[:, c_end][:, None, None, None, :])
    p_c = masked_softmax(s_c, c_end[None, :] <= q_idx_all[:, None])
    o_cmp = jnp.einsum('bgrqn,bgnd->bgrqd', p_c.astype(v_c.dtype), v_c)

    ns = S // SB
    j_start = jnp.arange(ns) * SB
    overlap = ((c_start[:, None] < j_start[None, :] + SB) & (c_end[:, None] >= j_start[None, :])).astype(jnp.float32)
    imp = jnp.einsum('bgrqn,nj->bgqj', p_c, overlap)
    q_blk = q_idx_all // SB
    jj = jnp.arange(ns)
    forced = ((jj[None, :] == 0) | (jj[None, :] == q_blk[:, None]) | (jj[None, :] == q_blk[:, None] - 1)).astype(jnp.float32)
    imp = jnp.where(jj[None, :] <= q_blk[:, None], imp + NSA_FORCE_BONUS * forced, NEG)
    topn = min(NSA_SEL_TOPN, ns)
    _, sel = lax.top_k(imp, topn)
    sel_valid = jnp.arange(topn)[None, :] <= q_blk[:, None]

    k_sb = k_slc.reshape(B, G, ns, SB, D)
    v_sb = v_slc.reshape(B, G, ns, SB, D)
    pos_sb = posf.reshape(B, ns, SB)
    C = GATHER_CHUNK
    nch = S // C
    b_ix = jnp.arange(B)[:, None, None, None]
    g_ix = jnp.arange(G)[None, :, None, None]
    sb_off = jnp.arange(SB)

    def sel_chunk(args):
        q_c, pq, sel_c, valid_c, ci = args
        q_i = ci * C + jnp.arange(C)
        k_g = k_sb[b_ix, g_ix, sel_c]
        v_g = v_sb[b_ix, g_ix, sel_c]
        kpos = pos_sb[b_ix, sel_c]
        k_i = sel_c[..., None] * SB + sb_off
        s = jnp.einsum('bgrqd,bgqnkd->bgrqnk', q_c, k_g, preferred_element_type=jnp.float32) * scale
        s = s - slope[..., None] * jnp.abs(pq[:, None, None, :, None, None] - kpos[:, :, None])
        mask = valid_c[None, None, :, :, None] & (k_i <= q_i[None, None, :, None, None])
        p = masked_softmax(s.reshape(B, G, R, C, topn * SB), mask.reshape(B, G, 1, C, topn * SB))
        return jnp.einsum('bgrqnk,bgqnkd->bgrqd', p.reshape(B, G, R, C, topn, SB).astype(v_g.dtype), v_g)

    o_sel = lax.map(sel_chunk, (
        q.reshape(B, G, R, nch, C, D).transpose(3, 0, 1, 2, 4, 5),
        posf.reshape(B, nch, C).transpose(1, 0, 2),
        sel.reshape(B, G, nch, C, topn).transpose(2, 0, 1, 3, 4),
        sel_valid.reshape(nch, C, topn),
        jnp.arange(nch)))
    o_sel = o_sel.transpose(1, 2, 3, 0, 4, 5).reshape(B, G, R, S, D)

    QB = Q_BLOCK
    nqb = S // QB
    span = W + QB
    band = jnp.arange(nqb)[:, None] * QB + jnp.arange(span)[None, :]
    k_w = jnp.pad(k_win, ((0, 0), (0, 0), (W, 0), (0, 0)))[:, :, band]
    v_w = jnp.pad(v_win, ((0, 0), (0, 0), (W, 0), (0, 0)))[:, :, band]
    kpos_w = jnp.pad(posf, ((0, 0), (W, 0)))[:, band]
    k_i = (band - W)[:, None, :]
    q_i = (jnp.arange(nqb)[:, None] * QB + jnp.arange(QB)[None, :])[:, :, None]
    mask_w = (k_i >= 0) & (k_i <= q_i) & (q_i - k_i < W)
    q_w = q.reshape(B, G, R, nqb, QB, D)
    s_w = jnp.einsum('bgriqd,bgikd->bgriqk', q_w, k_w, preferred_element_type=jnp.float32) * scale
    s_w = s_w - slope[..., None] * jnp.abs(posf.reshape(B, nqb, QB)[:, None, None, :, :, None] - kpos_w[:, None, None, :, None, :])
    p_w = masked_softmax(s_w, mask_w)
    o_win = jnp.einsum('bgriqk,bgikd->bgriqd', p_w.astype(v_w.dtype), v_w).reshape(B, G, R, S, D)

    o = gates[0] * o_cmp + gates[1] * o_sel + gates[2] * o_win
    o = o.transpose(0, 3, 1, 2, 4).reshape(B, S, H * D).astype(h.dtype)
    return o @ w_o


def stick_breaking_mixer(h, w_in, w_o):
    B, S, _ = h.shape
    H, D = SB_HEADS, SB_HD
    q, k, v = split_heads3(h @ w_in, H, D)
    scale = D ** -0.5
    nqb = S // Q_BLOCK
    k_idx = jnp.arange(S)

    def block(args):
        q_b, bi = args
        z = jnp.einsum('bhqd,bhkd->bhqk', q_b, k, preferred_element_type=jnp.float32) * scale
        q_idx = bi * Q_BLOCK + jnp.arange(Q_BLOCK)
        mask = k_idx[None, :] < q_idx[:, None]
        log_beta = jax.nn.log_sigmoid(z)
        log_1mb = jnp.where(mask, jax.nn.log_sigmoid(-z), 0.0)
        tail = lax.cumsum(log_1mb, axis=3, reverse=True) - log_1mb
        a = jnp.where(mask, jnp.exp(log_beta + tail), 0.0)
        return jnp.einsum('bhqk,bhkd->bhqd', a.astype(v.dtype), v)

    q_blocks = q.reshape(B, H, nqb, Q_BLOCK, D).transpose(2, 0, 1, 3, 4)
    o = lax.map(block, (q_blocks, jnp.arange(nqb)))
    o = o.transpose(1, 2, 0, 3, 4).reshape(B, H, S, D)
    return merge_heads(o) @ w_o


def conv_ffn(h, w_in, conv_w, conv_b, w_out):
    u = h @ w_in
    a, b = u[..., :D_FF], u[..., D_FF:]
    a = lax.conv_general_dilated(a, conv_w[:, None, :].astype(a.dtype), window_strides=(1,),
                                 padding=[(CONV_W - 1, 0)], dimension_numbers=('NWC', 'WIO', 'NWC'),
                                 feature_group_count=D_FF) + conv_b
    return (jax.nn.gelu(a) * b) @ w_out


def _w(key, shape, fan_in, scale=1.0):
    return jax.random.normal(key, shape, jnp.float32) * (scale * fan_in ** -0.5)


def _gain(key, shape):
    return 1.0 + 0.02 * jax.random.normal(key, shape, jnp.float32)


def _small(key, shape):
    return 0.02 * jax.random.normal(key, shape, jnp.float32)


def setup_inputs(seed: int = 0) -> dict:
    key = jax.random.key(seed)
    ks = jax.random.split(key, 32)
    D = D_MODEL
    nA, nB, nC, nD = _count(0), _count(1), _count(2), _count(3)
    mla_out = MLA_Q_RANK + MLA_KV_RANK + MLA_ROPE
    nsa_out = NSA_HEADS * NSA_HD + 6 * NSA_GROUPS * NSA_HD + 3 * NSA_HEADS
    mod_base = jnp.repeat(jnp.array([0.0, 0.0, 1.0, 0.0, 0.0, 1.0], jnp.float32), D)
    offset = jax.random.randint(ks[2], (BATCH, 1), 0, MAX_POS_OFFSET, dtype=jnp.int32)
    return {
        'x': jax.random.normal(ks[0], (BATCH, SEQ, D), jnp.float32),
        'c': jax.random.normal(ks[1], (BATCH, D), jnp.float32),
        'positions': offset + jnp.arange(SEQ, dtype=jnp.int32)[None, :],
        'mod_w': _w(ks[3], (DEPTH, D, 6 * D), D, 0.1),
        'mod_b': mod_base + _small(ks[4], (DEPTH, 6 * D)),
        'ln_g': _gain(ks[5], (DEPTH, 2, D)),
        'ln_b': _small(ks[6], (DEPTH, 2, D)),
        'ffn_w_in': _w(ks[7], (DEPTH, D, 2 * D_FF), D),
        'ffn_conv_w': _w(ks[8], (DEPTH, CONV_W, D_FF), CONV_W),
        'ffn_conv_b': _small(ks[9], (DEPTH, D_FF)),
        'ffn_w_out': _w(ks[10], (DEPTH, D_FF, D), D_FF, DEEPNORM_BETA),
        'mla_w_in': _w(ks[11], (nA, D, mla_out), D),
        'mla_q_norm': _gain(ks[12], (nA, MLA_Q_RANK)),
        'mla_w_uq': _w(ks[13], (nA, MLA_Q_RANK, MLA_HEADS * (MLA_NOPE + MLA_ROPE)), MLA_Q_RANK),
        'mla_kv_norm': _gain(ks[14], (nA, MLA_KV_RANK)),
        'mla_w_ukv': _w(ks[15], (nA, MLA_KV_RANK, MLA_HEADS * (MLA_NOPE + MLA_V)), MLA_KV_RANK),
        'mla_w_o': _w(ks[16], (nA, MLA_HEADS * MLA_V, D), MLA_HEADS * MLA_V, DEEPNORM_BETA),
        'moba_w_in': _w(ks[17], (nB, D, 3 * MOBA_HEADS * MOBA_HD), D),
        'moba_w_o': _w(ks[18], (nB, MOBA_HEADS * MOBA_HD, D), MOBA_HEADS * MOBA_HD, DEEPNORM_BETA),
        'nsa_w_in': _w(ks[19], (nC, D, nsa_out), D),
        'nsa_cmp_pos': _small(ks[20], (nC, 2, NSA_CMP_LEN, NSA_HD)),
        'nsa_cmp_w1': _w(ks[21], (nC, 2, NSA_CMP_LEN * NSA_HD, NSA_HD), NSA_CMP_LEN * NSA_HD),
        'nsa_cmp_w2': _w(ks[22], (nC, 2, NSA_HD, NSA_HD), NSA_HD),
        'nsa_w_o': _w(ks[23], (nC, NSA_HEADS * NSA_HD, D), NSA_HEADS * NSA_HD, DEEPNORM_BETA),
        'sb_w_in': _w(ks[24], (nD, D, 3 * SB_HEADS * SB_HD), D),
        'sb_w_o': _w(ks[25], (nD, SB_HEADS * SB_HD, D), SB_HEADS * SB_HD, DEEPNORM_BETA),
    }


def reference(x, c, positions, mod_w, mod_b, ln_g, ln_b, ffn_w_in, ffn_conv_w, ffn_conv_b, ffn_w_out,
              mla_w_in, mla_q_norm, mla_w_uq, mla_kv_norm, mla_w_ukv, mla_w_o,
              moba_w_in, moba_w_o, nsa_w_in, nsa_cmp_pos, nsa_cmp_w1, nsa_cmp_w2, nsa_w_o,
              sb_w_in, sb_w_o):
    B, S, D = x.shape
    half = MLA_ROPE // 2
    inv_freq = ROPE_BASE ** (-jnp.arange(half, dtype=jnp.float32) / half)
    ang = positions.astype(jnp.float32)[..., None] * inv_freq
    cos, sin = jnp.cos(ang), jnp.sin(ang)
    c_act = jax.nn.silu(c)
    for i in range(DEPTH):
        kind, j = i % N_MIXERS, i // N_MIXERS
        mod = (c_act @ mod_w[i] + mod_b[i]).reshape(B, 6, D)[:, :, None, :]
        hin = x * (1.0 + mod[:, 1]) + mod[:, 0]
        if kind == 0:
            y = mla_mixer(hin, cos, sin, mla_w_in[j], mla_q_norm[j], mla_w_uq[j], mla_kv_norm[j], mla_w_ukv[j], mla_w_o[j])
        elif kind == 1:
            y = moba_mixer(hin, positions, moba_w_in[j], moba_w_o[j])
        elif kind == 2:
            y = nsa_mixer(hin, positions, nsa_w_in[j], nsa_cmp_pos[j], nsa_cmp_w1[j], nsa_cmp_w2[j], nsa_w_o[j])
        else:
            y = stick_breaking_mixer(hin, sb_w_in[j], sb_w_o[j])
        x = layer_norm(DEEPNORM_ALPHA * x + mod[:, 2] * y, ln_g[i, 0], ln_b[i, 0])
        hin = x * (1.0 + mod[:, 4]) + mod[:, 3]
        y = conv_ffn(hin, ffn_w_in[i], ffn_conv_w[i], ffn_conv_b[i], ffn_w_out[i])
        x = layer_norm(DEEPNORM_ALPHA * x + mod[:, 5] * y, ln_g[i, 1], ln_b[i, 1])
    return x
```

```python
import numpy as np
import ml_dtypes
import concourse.bass as bass
import concourse.mybir as mybir
from concourse.bass_utils import run_bass_kernel_spmd

F32 = mybir.dt.float32
BF16 = mybir.dt.bfloat16
I32 = mybir.dt.int32
AF = mybir.ActivationFunctionType
ALU = mybir.AluOpType
AX = mybir.AxisListType

PE, ACT, DVE, POOL, SP = "tensor", "scalar", "vector", "gpsimd", "sync"
ENGINES = (PE, ACT, DVE, POOL, SP)


class Prog:
    def __init__(self, nc, es):
        self.nc = nc
        self.es = es
        self.sems = {}
        for e in ENGINES:
            self.sems[("eng", e)] = es.enter_context(nc.semaphore("s_" + e))
        self.counters = {e: 0 for e in ENGINES}
        self.chan_count = {}
        self.water = {e: {} for e in ENGINES}
        self.n_total = 0
        self._reset()

    def _reset(self):
        self.ops = []
        self.writers = {}
        self.readers = {}

    def add(self, eng, fn, reads=(), writes=(), dma=None):
        idx = len(self.ops)
        deps = set()
        src = eng if dma is None else ("dma", dma)
        for b in reads:
            deps.update(self.writers.get(b, {}).values())
        for b in writes:
            deps.update(self.writers.get(b, {}).values())
            deps.update(self.readers.get(b, {}).values())
        op = dict(eng=eng, fn=fn, deps=deps, dma=dma, ticket=None)
        if dma is not None:
            self.chan_count[dma] = self.chan_count.get(dma, 0) + 1
            op["dma_val"] = 16 * self.chan_count[dma]
        self.ops.append(op)
        for b in writes:
            self.writers.setdefault(b, {})[src] = idx
        for b in reads:
            self.readers.setdefault(b, {})[src] = idx
        return idx

    def flush(self):
        nc = self.nc
        ops = self.ops
        needed = set()
        last = {}
        for i, op in enumerate(ops):
            if op["dma"] is None:
                last[op["eng"]] = i
        needed.update(last.values())
        for op in ops:
            for d in op["deps"]:
                dop = ops[d]
                if dop["dma"] is None and not (dop["eng"] == PE and op["eng"] == PE and op["dma"] is None):
                    needed.add(d)
        for i, op in enumerate(ops):
            if op["dma"] is None and i in needed:
                self.counters[op["eng"]] += 1
                op["ticket"] = self.counters[op["eng"]]
        for c in self.chan_count:
            if ("dma", c) not in self.sems:
                self.sems[("dma", c)] = self.es.enter_context(nc.semaphore("d_" + str(c)))
        streams = {e: [] for e in ENGINES}
        for i, op in enumerate(ops):
            e = op["eng"]
            waits = {}
            for d in op["deps"]:
                dop = ops[d]
                if dop["dma"] is not None:
                    key, val = ("dma", dop["dma"]), dop["dma_val"]
                else:
                    if dop["eng"] == PE and e == PE and op["dma"] is None:
                        continue
                    key, val = ("eng", dop["eng"]), dop["ticket"]
                if val > waits.get(key, 0):
                    waits[key] = val
            wl = []
            for key, val in waits.items():
                if self.water[e].get(key, 0) >= val:
                    continue
                self.water[e][key] = val
                wl.append((key, val))
            streams[e].append((op, wl))
        final = [(("eng", e), self.counters[e]) for e in ENGINES] + \
                [(("dma", c), 16 * n) for c, n in self.chan_count.items()]
        sems = self.sems
        with nc.Block() as block:
            def mk(ename):
                def body(eng):
                    for op, wl in streams[ename]:
                        for key, val in wl:
                            eng.wait_ge(sems[key], val)
                        ins = op["fn"](eng)
                        if op["dma"] is not None:
                            ins.then_inc(sems[("dma", op["dma"])], 16)
                        elif op["ticket"] is not None:
                            ins.then_inc(sems[("eng", ename)], 1)
                    for key, val in final:
                        if val == 0 or self.water[ename].get(key, 0) >= val:
                            continue
                        eng.wait_ge(sems[key], val)
                        self.water[ename][key] = val
                return body
            block.tensor(mk(PE))
            block.scalar(mk(ACT))
            block.vector(mk(DVE))
            block.gpsimd(mk(POOL))
            block.sync(mk(SP))
        self.n_total += len(ops)
        self._reset()


S, D, NT, KC, DFF, NFC = 2048, 1024, 16, 8, 2816, 22
ALPHA = 8.0 ** 0.25
NEGB = -30000.0
PI = float(np.pi)


def host_consts():
    kp = np.arange(128)[:, None]
    qf = np.arange(128)[None, :]
    cb = np.zeros((128, 8, 128), np.float32)
    cb[:, 0] = np.eye(128)
    cb[:, 1] = np.where(kp <= qf, 0.0, NEGB)
    cb[:, 2] = np.where(kp > qf, 0.0, NEGB)
    cb[:, 3] = -(kp > qf).astype(np.float32)
    cb[:, 4] = 1.0
    cb[:, 5] = -1.0
    cb[:, 6] = (kp < qf).astype(np.float32)
    e8 = np.zeros((128, 8, 128), np.float32)
    for n in range(8):
        e8[n, n, :] = 1.0
    e32 = np.zeros((128, 16, 128), np.float32)
    for kt in range(16):
        for k in range(128):
            e32[2 * kt + k // 64, kt, k] = 1.0
    t_idx = np.arange(16)[None, :, None]
    n_idx = np.arange(32)[None, None, :]
    own = t_idx // 2
    moba_neg = np.broadcast_to(np.where(n_idx < own, 0.0, -1e30), (128, 16, 32)).astype(np.float32)
    moba_notown = np.broadcast_to((n_idx != own).astype(np.float32), (128, 16, 32)).astype(np.float32)
    invf = (np.float32(10000.0) ** (-np.arange(32, dtype=np.float32) / np.float32(32))).astype(np.float32)
    invf_bc = np.broadcast_to(invf[None, :], (128, 32)).astype(np.float32)
    q_idx = (np.arange(16)[None, :] * 128 + np.arange(128)[:, None])
    c_end = np.arange(127) * 16 + 31
    nsa_cmask = (c_end[None, None, :] <= q_idx[:, :, None]).astype(np.float32)
    qblk = q_idx // 64
    jj = np.arange(32)[None, None, :]
    valid = jj <= qblk[:, :, None]
    forced = (jj == 0) | (jj == qblk[:, :, None]) | (jj == qblk[:, :, None] - 1)
    nsa_bonus = np.where(valid, 100.0 * forced, -1e30).astype(np.float32)
    nsa_valid = valid.astype(np.float32)
    c_start = np.arange(127) * 16
    j_start = np.arange(32) * 64
    overlap = ((c_start[:, None] < j_start[None, :] + 64) & (c_end[:, None] >= j_start[None, :])).astype(np.float32)
    ov = np.zeros((128, 32), np.float32)
    ov[:127] = overlap
    bf = ml_dtypes.bfloat16
    return {
        "cb": cb.astype(bf), "e8": e8.reshape(128, 1024).astype(bf), "e32": e32.reshape(128, 2048).astype(bf),
        "moba_neg": moba_neg.reshape(128, 512), "moba_notown": moba_notown.reshape(128, 512),
        "invf": invf_bc, "nsa_cmask": nsa_cmask.reshape(128, 16 * 127).astype(bf), "nsa_bonus": nsa_bonus.reshape(128, 512),
        "nsa_valid": nsa_valid.reshape(128, 512), "ov": ov.astype(bf),
    }


WEIGHT_SPECS = [
    ("mod_w", [4, 1024, 6144]), ("mod_b", [4, 6144]), ("ln_g", [4, 2, 1024]), ("ln_b", [4, 2, 1024]),
    ("ffn_w_in", [4, 1024, 5632]), ("ffn_conv_w", [4, 3, 2816]), ("ffn_conv_b", [4, 2816]),
    ("ffn_w_out", [4, 2816, 1024]),
    ("mla_w_in", [1, 1024, 576]), ("mla_q_norm", [1, 256]), ("mla_w_uq", [1, 256, 1536]),
    ("mla_kv_norm", [1, 256]), ("mla_w_ukv", [1, 256, 2048]), ("mla_w_o", [1, 1024, 1024]),
    ("moba_w_in", [1, 1024, 3072]), ("moba_w_o", [1, 1024, 1024]),
    ("nsa_w_in", [1, 1024, 2584]), ("nsa_cmp_pos", [1, 2, 32, 128]), ("nsa_cmp_w1", [1, 2, 4096, 128]),
    ("nsa_cmp_w2", [1, 2, 128, 128]), ("nsa_w_o", [1, 1024, 1024]),
    ("sb_w_in", [1, 1024, 3072]), ("sb_w_o", [1, 1024, 1024]),
]
CONST_SPECS = [
    ("cb", [128, 8, 128], BF16), ("e8", [128, 1024], BF16), ("e32", [128, 2048], BF16),
    ("moba_neg", [128, 512], F32), ("moba_notown", [128, 512], F32), ("invf", [128, 32], F32),
    ("nsa_cmask", [128, 16 * 127], BF16), ("nsa_bonus", [128, 512], F32), ("nsa_valid", [128, 512], F32),
    ("ov", [128, 32], BF16),
]


class Ring:
    def __init__(self, name, n):
        self.vals = list(range(n)) if isinstance(n, int) else list(n)
        self.name, self.n, self.i = name, len(self.vals), 0

    def next(self):
        k = self.vals[self.i % self.n]
        self.i += 1
        return k


class Builder:
    def __init__(self, layers=(0, 1, 2, 3), taps=None):
        from contextlib import ExitStack
        self.layers = layers
        self.nc = nc = bass.Bass("TRN2", target_bir_lowering=False)
        self.es = ExitStack()
        self.P = Prog(nc, self.es)
        self.dr = {}
        self.dr["x"] = nc.dram_tensor("x", [S, D], F32, kind="ExternalInput").ap()
        self.dr["c"] = nc.dram_tensor("c", [1, D], F32, kind="ExternalInput").ap()
        self.dr["positions"] = nc.dram_tensor("positions", [1, S], I32, kind="ExternalInput").ap()
        for name, shp in WEIGHT_SPECS:
            self.dr[name] = nc.dram_tensor(name, shp, F32, kind="ExternalInput").ap()
        for name, shp, dt in CONST_SPECS:
            self.dr[name] = nc.dram_tensor(name, shp, dt, kind="ExternalInput").ap()
        self.dr["out"] = nc.dram_tensor("out", [S, D], F32, kind="ExternalOutput").ap()
        self.uid = 0

    def sb(self, st, name, shape, dt):
        self.uid += 1
        return st.enter_context(self.nc.sbuf_tensor(f"{name}_{self.uid}", shape, dt))

    def A(self, eng, fn, r=(), w=(), dma=None):
        return self.P.add(eng, fn, reads=r, writes=w, dma=dma)

    def psum_setup(self, st):
        self.uid += 1
        self.bank = [st.enter_context(self.nc.psum_tensor(f"bank{i}_{self.uid}", [128, 512], F32)) for i in range(8)]

    def bankbf(self, i):
        return self.bank[i][:].bitcast(BF16)

    def build(self):
        nc, A, dr = self.nc, self.A, self.dr
        from contextlib import ExitStack
        es = self.es
        self.psum_setup(es)
        self.x = self.sb(es, "x", [128, NT, D], F32)
        self.cb = self.sb(es, "cb", [128, 8, 128], BF16)
        self.posq = self.sb(es, "posq", [128, S], F32)
        self.posk = self.sb(es, "posk", [128, NT], F32)
        self.nposk = self.sb(es, "nposk", [128, NT], F32)
        self.crep = self.sb(es, "crep", [128, KC, 128], BF16)
        self.gate = self.sb(es, "gate", [128, D], F32)
        self.small = self.sb(es, "small", [128, 64], F32)
        x, cb = self.x, self.cb
        with ExitStack() as st:
            posi = self.sb(st, "posi", [128, S], I32)
            poski = self.sb(st, "poski", [128, NT], I32)
            cT = self.sb(st, "cT", [128, KC], F32)
            cA = self.sb(st, "cA", [128, KC], F32)
            A(SP, lambda e: e.dma_start(out=cb[:], in_=dr["cb"]), w=["cb"], dma="cb")
            A(SP, lambda e: e.dma_start(out=posi[:], in_=dr["positions"].partition_broadcast(128)), w=["posi"], dma="posi")
            A(SP, lambda e: e.dma_start(out=poski[:], in_=dr["positions"].rearrange("o (t p) -> p (o t)", p=128),
                                        allow_slow_non_contiguous=True), w=["poski"], dma="poski")
            A(SP, lambda e: e.dma_start(out=cT[:], in_=dr["c"].rearrange("o (k p) -> p (o k)", p=128),
                                        allow_slow_non_contiguous=True), w=["cT"], dma="cT")
            for t in range(NT):
                A(SP, lambda e, t=t: e.dma_start(out=x[:, t, :], in_=dr["x"][t * 128:(t + 1) * 128, :]),
                  w=[f"x{t}"], dma=f"x{t % 4}")
            A(DVE, lambda e: e.tensor_copy(out=self.posq[:], in_=posi[:]), r=["posi"], w=["posq"])
            A(DVE, lambda e: e.tensor_copy(out=self.posk[:], in_=poski[:]), r=["poski"], w=["posk"])
            A(DVE, lambda e: e.tensor_scalar(out=self.nposk[:], in0=self.posk[:], scalar1=-1.0, scalar2=None, op0=ALU.mult),
              r=["posk"], w=["posk"])
            A(ACT, lambda e: e.activation(out=cA[:], in_=cT[:], func=AF.Silu), r=["cT"], w=["cA"])
            for k in range(KC):
                A(DVE, lambda e, k=k: e.tensor_scalar(out=self.crep[:, k, :], in0=cb[:, 4, :], scalar1=cA[:, k:k + 1],
                                                      scalar2=None, op0=ALU.mult), r=["cA", "cb"], w=["crep"])
            self.P.flush()
        for li in self.layers:
            self.layer(li)
        for t in range(NT):
            A(SP, lambda e, t=t: e.dma_start(out=dr["out"][t * 128:(t + 1) * 128, :], in_=x[:, t, :]),
              r=[f"x{t}"], w=[f"out{t}"], dma=f"o{t % 4}")
        self.P.flush()
        return nc

    def compute_mod(self, st, li, half):
        A, dr = self.A, self.dr
        self.ms = self.sb(st, "ms", [128, 2, D], F32)
        wsl = [self.sb(st, f"modw{i}", [128, KC, 256], BF16) for i in range(2)]
        bsl = [self.sb(st, f"modb{i}", [128, 256], F32) for i in range(2)]
        for j in range(12):
            s = j % 2
            col0 = half * 3072 + j * 256
            A(POOL, lambda e, s=s, col0=col0: e.dma_start(
                out=wsl[s][:], in_=dr["mod_w"][li, :, col0:col0 + 256].rearrange("(k p) n -> p k n", p=128)),
              w=[f"modw{s}"], dma=f"modw{s}")
            A(SP, lambda e, s=s, col0=col0: e.dma_start(
                out=bsl[s][:], in_=dr["mod_b"][li:li + 1, col0:col0 + 256].partition_broadcast(128)),
              w=[f"modb{s}"], dma=f"modb{s}")
            pb = 6 + s
            for k in range(KC):
                A(PE, lambda e, s=s, k=k, pb=pb: e.matmul(self.bank[pb][:, 0:256], lhsT=self.crep[:, k, :],
                                                          rhs=wsl[s][:, k, :], start=(k == 0), stop=(k == KC - 1)),
                  r=["crep", f"modw{s}"], w=[f"B{pb}"])
            comp, off = (j * 256) // 1024, (j * 256) % 1024
            dst = self.ms[:, comp, off:off + 256] if comp < 2 else self.gate[:, off:off + 256]
            A(DVE, lambda e, s=s, pb=pb, dst=dst: e.tensor_tensor(out=dst, in0=self.bank[pb][:, 0:256], in1=bsl[s][:], op=ALU.add),
              r=[f"B{pb}", f"modb{s}"], w=["modt"])
        A(DVE, lambda e: e.tensor_scalar(out=self.ms[:, 1, :], in0=self.ms[:, 1, :], scalar1=1.0, scalar2=None,
                                         op0=ALU.add), r=["modt"], w=["modt"])

    def build_hT(self, st, hT):
        A, x, modt = self.A, self.x, self.ms
        tmp = [self.sb(st, f"htmp{i}", [128, D], F32) for i in range(2)]
        hb = [self.sb(st, f"hb{i}", [128, D], BF16) for i in range(2)]
        for t in range(NT):
            s = t % 2
            A(DVE, lambda e, t=t, s=s: e.tensor_tensor(out=tmp[s][:], in0=x[:, t, :], in1=modt[:, 1, :], op=ALU.mult),
              r=[f"x{t}", "modt"], w=[f"htmp{s}"])
            A(DVE, lambda e, s=s: e.tensor_tensor(out=hb[s][:], in0=tmp[s][:], in1=modt[:, 0, :], op=ALU.add),
              r=[f"htmp{s}", "modt"], w=[f"hb{s}"])
            pb = 6 + s
            for k in range(KC):
                A(PE, lambda e, s=s, k=k, pb=pb: e.transpose(out=self.bankbf(pb)[:, k * 128:(k + 1) * 128],
                                                             in_=hb[s][:, k * 128:(k + 1) * 128], identity=self.cb[:, 0, :]),
                  r=[f"hb{s}", "cb"], w=[f"B{pb}"])
            A(ACT, lambda e, t=t, pb=pb: e.activation(
                out=hT[:, :, t * 128:(t + 1) * 128], in_=self.bankbf(pb).rearrange("p (k n) -> p k n", k=KC), func=AF.Copy),
              r=[f"B{pb}"], w=[f"hT{t}"])
            A(ACT, lambda e, t=t: e.activation(out=x[:, t, :], in_=x[:, t, :], func=AF.Copy, scale=ALPHA),
              r=[f"x{t}"], w=[f"x{t}"])

    def layer_norm(self, st, li, which):
        A, x, dr = self.A, self.x, self.dr
        self.lng = self.sb(st, "lng", [128, D], F32)
        self.lnb = self.sb(st, "lnb", [128, D], F32)
        A(SP, lambda e: e.dma_start(out=self.lng[:], in_=dr["ln_g"][li, which:which + 1, :].partition_broadcast(128)),
          w=["lng"], dma="lng")
        A(SP, lambda e: e.dma_start(out=self.lnb[:], in_=dr["ln_b"][li, which:which + 1, :].partition_broadcast(128)),
          w=["lnb"], dma="lnb")
        stt = [self.sb(st, f"lnst{i}", [128, 16], F32) for i in range(2)]
        for t in range(NT):
            s = t % 2
            q = stt[s]
            A(DVE, lambda e, t=t, q=q: e.bn_stats(out=q[:, 0:6], in_=x[:, t, 0:512]), r=[f"x{t}"], w=[f"lnst{s}"])
            A(DVE, lambda e, t=t, q=q: e.bn_stats(out=q[:, 6:12], in_=x[:, t, 512:1024]), r=[f"x{t}"], w=[f"lnst{s}"])
            A(DVE, lambda e, q=q: e.bn_aggr(out=q[:, 12:14], in_=q[:, 0:12]), r=[f"lnst{s}"], w=[f"lnst{s}"])
            A(ACT, lambda e, q=q: e.activation(out=q[:, 14:15], in_=q[:, 13:14], func=AF.Ln, bias=1e-5, scale=1.0),
              r=[f"lnst{s}"], w=[f"lnst{s}"])
            A(ACT, lambda e, q=q: e.activation(out=q[:, 14:15], in_=q[:, 14:15], func=AF.Exp, scale=-0.5),
              r=[f"lnst{s}"], w=[f"lnst{s}"])
            A(DVE, lambda e, q=q: e.scalar_tensor_tensor(out=q[:, 15:16], in0=q[:, 12:13], scalar=-1.0, in1=q[:, 14:15],
                                                         op0=ALU.mult, op1=ALU.mult), r=[f"lnst{s}"], w=[f"lnst{s}"])
            A(ACT, lambda e, t=t, q=q: e.activation(out=x[:, t, :], in_=x[:, t, :], func=AF.Identity,
                                                    bias=q[:, 15:16], scale=q[:, 14:15]),
              r=[f"x{t}", f"lnst{s}"], w=[f"x{t}"])
            A(DVE, lambda e, t=t: e.tensor_tensor(out=x[:, t, :], in0=x[:, t, :], in1=self.lng[:], op=ALU.mult),
              r=[f"x{t}", "lng"], w=[f"x{t}"])
            A(DVE, lambda e, t=t: e.tensor_tensor(out=x[:, t, :], in0=x[:, t, :], in1=self.lnb[:], op=ALU.add),
              r=[f"x{t}", "lnb"], w=[f"x{t}"])

    def out_proj(self, wo, wkey, srcT, nj, w_dram_rows, src_keys):
        A, x = self.A, self.x
        A(POOL, lambda e: e.dma_start(out=wo[:, 0:nj, :], in_=w_dram_rows.rearrange("(j p) n -> p j n", p=128)),
          w=[wkey], dma=wkey)
        for j in range(nj):
            A(DVE, lambda e, j=j: e.tensor_tensor(out=wo[:, j, :], in0=wo[:, j, :], in1=self.gate[:], op=ALU.mult),
              r=[wkey, "modt"], w=[wkey])
        for t in range(NT):
            for n in range(2):
                pb = 4 + (2 * t + n) % 2
                for j in range(nj):
                    A(PE, lambda e, t=t, n=n, j=j, pb=pb: e.matmul(
                        self.bank[pb][:], lhsT=srcT[:, j, t * 128:(t + 1) * 128], rhs=wo[:, j, n * 512:(n + 1) * 512],
                        start=(j == 0), stop=(j == nj - 1)), r=[wkey] + src_keys, w=[f"B{pb}"])
                A(DVE, lambda e, t=t, n=n, pb=pb: e.tensor_tensor(
                    out=x[:, t, n * 512:(n + 1) * 512], in0=self.bank[pb][:], in1=x[:, t, n * 512:(n + 1) * 512], op=ALU.add),
                  r=[f"B{pb}", f"x{t}"], w=[f"x{t}"])

    def ffn(self, li):
        from contextlib import ExitStack
        A, dr, x = self.A, self.dr, self.x
        with ExitStack() as st:
            hT = self.sb(st, "hT", [128, KC, S], BF16)
            with ExitStack() as st0:
                self.compute_mod(st0, li, 1)
                self.build_hT(st0, hT)
                self.P.flush()
            hkeys = [f"hT{t}" for t in range(NT)]
            cw = self.sb(st, "cw", [128, 3, NFC], F32)
            cbias = self.sb(st, "cbias", [128, NFC], F32)
            for wi in range(3):
                A(SP, lambda e, wi=wi: e.dma_start(out=cw[:, wi, :], in_=dr["ffn_conv_w"][li, wi:wi + 1, :].rearrange("o (j p) -> p (o j)", p=128),
                                                   allow_slow_non_contiguous=True), w=["cw"], dma="cw")
            A(SP, lambda e: e.dma_start(out=cbias[:], in_=dr["ffn_conv_b"][li:li + 1, :].rearrange("o (j p) -> p (o j)", p=128),
                                        allow_slow_non_contiguous=True), w=["cbias"], dma="cbias")
            wab = [self.sb(st, f"wab{i}", [128, KC, 2, 128], BF16) for i in range(2)]
            aS = [self.sb(st, f"aS{i}", [128, S + 2], F32) for i in range(2)]
            tS = [self.sb(st, f"tS{i}", [128, 512], F32) for i in range(2)]
            gT = [self.sb(st, f"gT{i}", [128, 4, S], BF16) for i in range(2)]
            wof = [self.sb(st, f"wof{i}", [128, 4, D], BF16) for i in range(2)]
            for i in range(2):
                A(POOL, lambda e, i=i: e.memset(aS[i][:, 0:2], 0.0), w=[f"aS{i}"])
            groups = [(0, 4), (4, 4), (8, 4), (12, 4), (16, 4), (20, 2)]
            it = 0
            pending_out = None
            for gi, (f0, nj) in enumerate(groups):
                gs = gi % 2
                for jj in range(nj):
                    j = f0 + jj
                    ws = j % 2
                    for half in range(2):
                        c0 = half * DFF + j * 128
                        A(POOL, lambda e, ws=ws, half=half, c0=c0: e.dma_start(
                            out=wab[ws][:, :, half, :], in_=dr["ffn_w_in"][li, :, c0:c0 + 128].rearrange("(k p) n -> p k n", p=128)),
                          w=[f"wab{ws}"], dma=f"wab{ws}")
                    for c in range(4):
                        s2 = it % 2
                        it += 1
                        pa, pbk = (0, 1, 7)[(it - 1) % 3], (2, 3, 6)[(it - 1) % 3]
                        for half, pbank in ((0, pa), (1, pbk)):
                            for k in range(KC):
                                A(PE, lambda e, ws=ws, half=half, pbank=pbank, k=k, c=c: e.matmul(
                                    self.bank[pbank][:], lhsT=wab[ws][:, k, half, :], rhs=hT[:, k, c * 512:(c + 1) * 512],
                                    start=(k == 0), stop=(k == KC - 1)),
                                  r=[f"wab{ws}"] + hkeys[4 * c:4 * c + 4], w=[f"B{pbank}"])
                        a_ = aS[ws]
                        A(ACT, lambda e, a_=a_, pa=pa, c=c: e.activation(out=a_[:, 2 + c * 512:2 + (c + 1) * 512],
                                                                         in_=self.bank[pa][:], func=AF.Copy),
                          r=[f"B{pa}"], w=[f"aS{ws}"])
                        A(DVE, lambda e, a_=a_, s2=s2, j=j, c=c: e.tensor_scalar(
                            out=tS[s2][:], in0=a_[:, c * 512:(c + 1) * 512], scalar1=cw[:, 0, j:j + 1],
                            scalar2=cbias[:, j:j + 1], op0=ALU.mult, op1=ALU.add),
                          r=[f"aS{ws}", "cw", "cbias"], w=[f"tS{s2}"])
                        A(DVE, lambda e, a_=a_, s2=s2, j=j, c=c: e.scalar_tensor_tensor(
                            out=tS[s2][:], in0=a_[:, 1 + c * 512:1 + (c + 1) * 512], scalar=cw[:, 1, j:j + 1], in1=tS[s2][:],
                            op0=ALU.mult, op1=ALU.add), r=[f"aS{ws}", "cw", f"tS{s2}"], w=[f"tS{s2}"])
                        A(DVE, lambda e, a_=a_, s2=s2, j=j, c=c: e.scalar_tensor_tensor(
                            out=tS[s2][:], in0=a_[:, 2 + c * 512:2 + (c + 1) * 512], scalar=cw[:, 2, j:j + 1], in1=tS[s2][:],
                            op0=ALU.mult, op1=ALU.add), r=[f"aS{ws}", "cw", f"tS{s2}"], w=[f"tS{s2}"])
                        A(ACT, lambda e, s2=s2: e.activation(out=tS[s2][:], in_=tS[s2][:], func=AF.Gelu_apprx_tanh),
                          r=[f"tS{s2}"], w=[f"tS{s2}"])
                        A(DVE, lambda e, s2=s2, gs=gs, jj=jj, c=c, pbk=pbk: e.tensor_tensor(
                            out=gT[gs][:, jj, c * 512:(c + 1) * 512], in0=self.bank[pbk][:], in1=tS[s2][:], op=ALU.mult),
                          r=[f"tS{s2}", f"B{pbk}"], w=[f"gT{gs}"])
                if pending_out is not None:
                    self.out_proj(*pending_out)
                pending_out = (wof[gs], f"wof{gs}", gT[gs], nj, dr["ffn_w_out"][li, f0 * 128:(f0 + nj) * 128, :], [f"gT{gs}"])
            self.out_proj(*pending_out)
            self.layer_norm(st, li, 1)
            self.P.flush()

    def layer(self, li):
        if isinstance(li, tuple):
            self.ffn(li[1])
            return
        kind = li % 4
        if kind == 0:
            self.mla(li)
        elif kind == 1:
            self.moba(li)
        elif kind == 2:
            self.nsa(li)
        else:
            self.sbmix(li)
        self.ffn(li)


_CONSTS = None


def kernel(**inputs):
    return run_kernel(inputs, layers=(0, 1, 2, 3))


def run_kernel(inputs, layers, cores=8, x_override=None, trace=False):
    global _CONSTS
    if _CONSTS is None:
        _CONSTS = host_consts()
    b = Builder(layers=layers)
    nc = b.build()
    in_maps = []
    xs = inputs["x"] if x_override is None else x_override
    for ci in range(cores):
        m = {"x": np.ascontiguousarray(xs[ci], dtype=np.float32),
             "c": np.ascontiguousarray(inputs["c"][ci:ci + 1], dtype=np.float32),
             "positions": np.ascontiguousarray(inputs["positions"][ci:ci + 1], dtype=np.int32)}
        for name, _ in WEIGHT_SPECS:
            m[name] = np.ascontiguousarray(inputs[name], dtype=np.float32)
        for name, _, _ in CONST_SPECS:
            m[name] = _CONSTS[name]
        in_maps.append(m)
    res = run_bass_kernel_spmd(nc, in_maps, core_ids=list(range(cores)), **({'trace': True} if trace else {}))
    if trace:
        print('EXEC_TIME_NS', res.exec_time_ns)
    return np.stack([r["out"] for r in res.results], axis=0).astype(np.float32)


def _mixer_prologue(self, st, li):
    from contextlib import ExitStack
    hT = self.sb(st, "hT", [128, KC, S], BF16)
    with ExitStack() as st0:
        self.compute_mod(st0, li, 0)
        self.build_hT(st0, hT)
        self.P.flush()
    self.sring = Ring("s", 4)
    self.sring_s = None
    return hT, [f"hT{t}" for t in range(NT)]


def _proj_qkv(self, w3, h, wq, wkey, qT, qkey, kT, kkey, V, vkey, hT, hkeys, kmean=None):
    A = self.A
    for which in range(3):
        A(POOL, lambda e, which=which: e.dma_start(out=wq[:, :, which, :], in_=w3[which].rearrange("(k p) n -> p k n", p=128)),
          w=[wkey], dma=wkey)
    for which, (dst, dkey) in enumerate(((qT, qkey), (kT, kkey))):
        for c in range(4):
            pb = self.sring.next()
            for k in range(KC):
                A(PE, lambda e, which=which, k=k, c=c, pb=pb: e.matmul(
                    self.bank[pb][:], lhsT=wq[:, k, which, :], rhs=hT[:, k, c * 512:(c + 1) * 512],
                    start=(k == 0), stop=(k == KC - 1)), r=[wkey] + hkeys[4 * c:4 * c + 4], w=[f"B{pb}"])
            if which == 0:
                A(ACT, lambda e, c=c, pb=pb, dst=dst: e.activation(out=dst[:, c * 512:(c + 1) * 512], in_=self.bank[pb][:], func=AF.Copy),
                  r=[f"B{pb}"], w=[dkey])
            else:
                A(DVE, lambda e, c=c, pb=pb, dst=dst: e.tensor_copy(out=dst[:, c * 512:(c + 1) * 512], in_=self.bank[pb][:]),
                  r=[f"B{pb}"], w=[dkey])
                if kmean is not None:
                    A(DVE, lambda e, c=c, pb=pb: e.tensor_reduce(
                        out=kmean[:, 2 * c:2 * c + 2], in_=self.bank[pb][:].rearrange("p (b n) -> p b n", b=2), axis=AX.X, op=ALU.add),
                      r=[f"B{pb}"], w=["kmean"])
    for tg in range(4):
        pb = self.sring.next()
        for tt in range(4):
            t = tg * 4 + tt
            for k in range(KC):
                A(PE, lambda e, k=k, t=t, tt=tt, pb=pb: e.matmul(
                    self.bank[pb][:, tt * 128:(tt + 1) * 128], lhsT=hT[:, k, t * 128:(t + 1) * 128], rhs=wq[:, k, 2, :],
                    start=(k == 0), stop=(k == KC - 1)), r=[wkey, hkeys[t]], w=[f"B{pb}"])
        eng = ACT if tg % 2 == 0 else DVE
        if eng == ACT:
            A(ACT, lambda e, tg=tg, pb=pb: e.activation(out=V[:, tg * 4:(tg + 1) * 4, 0:128],
                                                        in_=self.bank[pb][:].rearrange("p (a n) -> p a n", a=4), func=AF.Copy),
              r=[f"B{pb}"], w=[vkey])
        else:
            A(DVE, lambda e, tg=tg, pb=pb: e.tensor_copy(out=V[:, tg * 4:(tg + 1) * 4, 0:128],
                                                         in_=self.bank[pb][:].rearrange("p (a n) -> p a n", a=4)),
              r=[f"B{pb}"], w=[vkey])


def _o_to_oT(self, src_ap, src_keys, otok, okey, oT, jj, t, scale_ap=None, scale_keys=()):
    A = self.A
    if scale_ap is None:
        A(ACT, lambda e: e.activation(out=otok[:], in_=src_ap, func=AF.Copy), r=list(src_keys), w=[okey])
    else:
        A(ACT, lambda e: e.activation(out=otok[:], in_=src_ap, func=AF.Copy, scale=scale_ap),
          r=list(src_keys) + list(scale_keys), w=[okey])
    pb = self.sring.next()
    A(PE, lambda e, pb=pb: e.transpose(out=self.bankbf(pb)[:, 0:128], in_=otok[:], identity=self.cb[:, 0, :]),
      r=[okey, "cb"], w=[f"B{pb}"])
    A(ACT, lambda e, pb=pb: e.activation(out=oT[:, jj, t * 128:(t + 1) * 128], in_=self.bankbf(pb)[:, 0:128], func=AF.Copy),
      r=[f"B{pb}"], w=["oT"])


def _sbmix(self, li):
    from contextlib import ExitStack
    A, dr, cb = self.A, self.dr, self.cb
    scale = 128.0 ** -0.5
    with ExitStack() as st:
        hT, hkeys = _mixer_prologue(self, st, li)
        wq1 = self.sb(st, "wq0", [128, KC, 3, 128], BF16)
        wq = [wq1, wq1]
        qT = [self.sb(st, f"qT{i}", [128, S], BF16) for i in range(2)]
        kT = [self.sb(st, f"kT{i}", [128, S], BF16) for i in range(2)]
        V = [self.sb(st, f"V{i}", [128, NT, 128], BF16) for i in range(2)]
        oT = self.sb(st, "oT", [128, 4, S], BF16)
        wo = self.sb(st, "wo", [128, 4, D], BF16)
        SPl = [self.sb(st, f"SPl{i}", [128, 512], F32) for i in range(2)]
        Lb = [self.sb(st, f"Lb{i}", [128, 512], BF16) for i in range(2)]
        Lsum = self.sb(st, "Lsum", [128, 512], BF16)
        T1 = [self.sb(st, f"T1{i}", [128, 512], F32) for i in range(3)]
        AT = [self.sb(st, f"AT{i}", [128, 512], BF16) for i in range(2)]
        otok = [self.sb(st, f"otok{i}", [128, 128], BF16) for i in range(2)]
        w_in = dr["sb_w_in"][0]
        self.atn_it = 0
        self.nls = 0
        self.oi = 0
        for h in range(8):
            s = h % 2
            w3 = [w_in[:, which * 1024 + h * 128: which * 1024 + (h + 1) * 128] for which in range(3)]
            _proj_qkv(self, w3, h, wq[s], "wq0", qT[s], f"qT{s}", kT[s], f"kT{s}", V[s], f"V{s}", hT, hkeys)
            stages = []
            for c in range(4):
                for kt in range(4 * c + 3, -1, -1):
                    sd = {}

                    def stage1(sd=sd, c=c, kt=kt, s=s):
                        r_ = kt - 4 * c
                        q0 = max(0, r_) * 128
                        diag = r_ >= 0
                        b = self.atn_it % 2
                        t3 = self.atn_it % 3
                        sd["b"], sd["t3"] = b, t3
                        self.atn_it += 1
                        pz = self.sring.next()
                        A(PE, lambda e: e.matmul(
                            self.bank[pz][:, q0:512], lhsT=kT[s][:, kt * 128:(kt + 1) * 128], rhs=qT[s][:, c * 512 + q0:(c + 1) * 512],
                            start=True, stop=True), r=[f"kT{s}", f"qT{s}"], w=[f"B{pz}"])
                        A(ACT, lambda e: e.activation(out=SPl[b][:, q0:512], in_=self.bank[pz][:, q0:512], func=AF.Exp, scale=scale),
                          r=[f"B{pz}"], w=[f"SPl{b}"])
                        A(ACT, lambda e: e.activation(out=Lb[b][:, q0:512], in_=SPl[b][:, q0:512], func=AF.Ln, bias=1.0, scale=1.0),
                          r=[f"SPl{b}"], w=[f"Lb{b}"])
                        A(DVE, lambda e: e.scalar_tensor_tensor(
                            out=T1[t3][:, q0:512], in0=self.bank[pz][:, q0:512], scalar=scale, in1=Lb[b][:, q0:512], op0=ALU.mult, op1=ALU.subtract),
                          r=[f"B{pz}", f"Lb{b}"], w=[f"T1{t3}"])
                        if diag:
                            A(DVE, lambda e: e.tensor_tensor(out=Lb[b][:, q0:q0 + 128], in0=Lb[b][:, q0:q0 + 128], in1=cb[:, 6, :], op=ALU.mult),
                              r=[f"Lb{b}", "cb"], w=[f"Lb{b}"])

                    def stage2a(sd=sd, c=c, kt=kt, s=s, h=h):
                        b, t3 = sd["b"], sd["t3"]
                        r_ = kt - 4 * c
                        q0 = max(0, r_) * 128
                        first = (kt == 4 * c + 3)
                        if first:
                            A(DVE, lambda e: e.memset(Lsum[:], 0.0), w=["Lsum"])
                        pt = self.sring.next()
                        A(PE, lambda e: e.matmul(
                            self.bank[pt][:, q0:512], lhsT=cb[:, 3, :], rhs=Lb[b][:, q0:512], start=True, stop=first),
                          r=["cb", f"Lb{b}"], w=[f"B{pt}"])
                        if not first:
                            A(PE, lambda e: e.matmul(
                                self.bank[pt][:, q0:512], lhsT=cb[:, 5, :], rhs=Lsum[:, q0:512], start=False, stop=True),
                              r=["cb", "Lsum"], w=[f"B{pt}"])
                        if kt > 0:
                            A(DVE, lambda e: e.tensor_tensor(out=Lsum[:, q0:512], in0=Lsum[:, q0:512], in1=Lb[b][:, q0:512], op=ALU.add),
                              r=["Lsum", f"Lb{b}"], w=["Lsum"])
                        A(DVE, lambda e: e.tensor_tensor(out=T1[t3][:, q0:512], in0=self.bank[pt][:, q0:512], in1=T1[t3][:, q0:512], op=ALU.add),
                          r=[f"B{pt}", f"T1{t3}"], w=[f"T1{t3}"])

                    def stage2b(sd=sd, c=c, kt=kt, s=s, h=h):
                        b, t3 = sd["b"], sd["t3"]
                        r_ = kt - 4 * c
                        q0 = max(0, r_) * 128
                        diag = r_ >= 0
                        A(ACT, lambda e: e.activation(out=AT[b][:, q0:512], in_=T1[t3][:, q0:512], func=AF.Exp),
                          r=[f"T1{t3}"], w=[f"AT{b}"])
                        if diag:
                            A(DVE, lambda e: e.tensor_tensor(out=AT[b][:, q0:q0 + 128], in0=AT[b][:, q0:q0 + 128], in1=cb[:, 6, :], op=ALU.mult),
                              r=[f"AT{b}", "cb"], w=[f"AT{b}"])
                        for i in range(q0 // 128, 4):
                            A(PE, lambda e, i=i: e.matmul(
                                self.bank[4 + i][:, 0:128], lhsT=AT[b][:, i * 128:(i + 1) * 128], rhs=V[s][:, kt, :],
                                start=(kt == 4 * c + i), stop=(kt == 0)), r=[f"AT{b}", f"V{s}"], w=[f"B{4 + i}"])
                        if kt == 0:
                            for i in range(4):
                                ob = self.oi % 2
                                self.oi += 1
                                _o_to_oT(self, self.bank[4 + i][:, 0:128], [f"B{4 + i}"], otok[ob], f"otok{ob}", oT, h % 4, 4 * c + i)
                    stages.append((stage1, stage2a, stage2b))
            nst = len(stages)
            for step in range(-2, nst):
                for si, off in enumerate((2, 1, 0)):
                    k = step + off
                    if 0 <= k < nst:
                        stages[k][si]()
            if h % 4 == 3:
                hg = h // 4
                self.out_proj(wo, "wo", oT, 4, dr["sb_w_o"][0, hg * 512:(hg + 1) * 512, :], ["oT"])
        self.layer_norm(st, li, 0)
        self.P.flush()


Builder.sbmix = _sbmix


def _attn_softmax(self, c_list, pairs, pkeys, V, vkey, scale, slope, items_of_chunk, maskmm, PT, TMP, DT, epi, far_ok=True):
    A, cb = self.A, self.cb
    nb = len(PT)
    stages = []
    for c in c_list:
        items = items_of_chunk(c)
        first_kt, last_kt = {}, {}
        for (kt, q0, q1, diag, far) in items:
            for i in range(q0 // 128, q1 // 128):
                first_kt.setdefault(i, kt)
                last_kt[i] = kt
        for idx, (kt, q0, q1, diag, far) in enumerate(items):
            s1, s2 = [], []

            st_ = {}

            def stage0(st_=st_, c=c, kt=kt, q0=q0, q1=q1):
                b = self.atn_it % nb
                self.atn_it += 1
                st_["b"] = b
                if slope is not None:
                    A(ACT, lambda e: e.activation(
                        out=DT[b][:, q0:q1], in_=self.posq[:, c * 512 + q0:c * 512 + q1], func=AF.Abs, bias=self.nposk[:, kt:kt + 1], scale=1.0),
                      r=["posq", "posk"], w=[f"DT{b}"])

            def stage1a(st_=st_, c=c, kt=kt, q0=q0, q1=q1, diag=diag, far=far):
                b = st_["b"]
                ps = (self.sring_s if (slope is None and self.sring_s is not None) else self.sring).next()
                st_["ps"] = ps
                mms = []
                for (kT_ap, qT_ap) in pairs:
                    mms.append((kT_ap[:, kt * 128:(kt + 1) * 128], qT_ap[:, c * 512 + q0:c * 512 + q1], q0, q1, list(pkeys)))
                if maskmm is not None:
                    lhs_fn, rhs_ap, mkeys = maskmm
                    mms.append((lhs_fn(kt), rhs_ap[:, c * 512 + q0:c * 512 + q1], q0, q1, list(mkeys)))
                if diag:
                    mms.append((cb[:, 0, :], cb[:, 1, :], q0, q0 + 128, ["cb"]))
                if far:
                    mms.append((cb[:, 0, :], cb[:, 2, :], q1 - 128, q1, ["cb"]))
                for mi, (lh, rh, a0, a1, keys) in enumerate(mms):
                    A(PE, lambda e, lh=lh, rh=rh, a0=a0, a1=a1, ps=ps, mi=mi, n=len(mms): e.matmul(
                        self.bank[ps][:, a0:a1], lhsT=lh, rhs=rh, start=(mi == 0), stop=(mi == n - 1)),
                      r=keys, w=[f"B{ps}"])
                if slope is not None:
                    A(DVE, lambda e: e.scalar_tensor_tensor(
                        out=TMP[b][:, q0:q1], in0=DT[b][:, q0:q1], scalar=-slope / scale, in1=self.bank[ps][:, q0:q1],
                        op0=ALU.mult, op1=ALU.add), r=[f"DT{b}", f"B{ps}"], w=[f"TMP{b}"])

            def stage1b(st_=st_, q0=q0, q1=q1):
                b, ps = st_["b"], st_["ps"]
                if slope is not None:
                    A(ACT, lambda e: e.activation(out=PT[b][:, q0:q1], in_=TMP[b][:, q0:q1], func=AF.Exp, scale=scale),
                      r=[f"TMP{b}"], w=[f"PT{b}"])
                else:
                    A(ACT, lambda e: e.activation(out=PT[b][:, q0:q1], in_=self.bank[ps][:, q0:q1], func=AF.Exp, scale=scale),
                      r=[f"B{ps}"], w=[f"PT{b}"])

            def stage2(st_=st_, c=c, kt=kt, q0=q0, q1=q1, last=(idx == len(items) - 1), first_kt=first_kt, last_kt=last_kt):
                b = st_["b"]
                for i in range(q0 // 128, q1 // 128):
                    A(PE, lambda e, i=i, s_=(kt == first_kt[i]), p_=(kt == last_kt[i]): e.matmul(
                        self.bank[4 + i][:, 0:129], lhsT=PT[b][:, i * 128:(i + 1) * 128], rhs=V[:, kt, 0:129], start=s_, stop=p_),
                      r=[f"PT{b}", vkey], w=[f"B{4 + i}"])
                if last:
                    for i in range(4):
                        if i in first_kt:
                            epi(c, i)
            stages.append((stage0, stage1a, stage1b, stage2))
    n = len(stages)
    offs = (3, 2, 1, 0)
    for step in range(-3, n):
        for si, off in enumerate(offs):
            k = step + off
            if 0 <= k < n:
                stages[k][si]()


def _causal_items(c):
    out = []
    for kt in range(0, 4 * c + 4):
        r_ = kt - 4 * c
        out.append((kt, max(0, r_) * 128, 512, r_ >= 0, False))
    return out


def _moba(self, li):
    from contextlib import ExitStack
    A, dr, cb = self.A, self.dr, self.cb
    scale = 128.0 ** -0.5
    self.atn_it = 0
    with ExitStack() as st:
        hT, hkeys = _mixer_prologue(self, st, li)
        wq = self.sb(st, "wq0", [128, KC, 3, 128], BF16)
        qT = [self.sb(st, f"qT{i}", [128, S], BF16) for i in range(2)]
        kT = [self.sb(st, f"kT{i}", [128, S], BF16) for i in range(2)]
        V = [self.sb(st, f"V{i}", [128, NT, 129], BF16) for i in range(2)]
        oT = self.sb(st, "oT", [128, 4, S], BF16)
        wo = self.sb(st, "wo", [128, 4, D], BF16)
        PT = [self.sb(st, f"PT{i}", [128, 512], BF16) for i in range(2)]
        TMP = [self.sb(st, f"TMP{i}", [128, 512], F32) for i in range(2)]
        DT = [self.sb(st, f"DT{i}", [128, 512], F32) for i in range(2)]
        otok = [self.sb(st, f"otok{i}", [128, 128], BF16) for i in range(2)]
        rden = [self.sb(st, f"rden{i}", [128, 1], F32) for i in range(2)]
        e8t = self.sb(st, "e8t", [128, 1024], BF16)
        mneg = self.sb(st, "mneg", [128, 512], F32)
        mnot = self.sb(st, "mnot", [128, 512], F32)
        kmean = self.sb(st, "kmean", [128, 32], F32)
        kmh = self.sb(st, "kmh", [128, 32], BF16)
        kml = self.sb(st, "kml", [128, 32], BF16)
        kmr = self.sb(st, "kmr", [128, 32], F32)
        gm = self.sb(st, "gm", [128, 512], F32)
        m8 = self.sb(st, "m8", [128, 128], F32)
        sel = self.sb(st, "sel", [128, 512], F32)
        nbb = self.sb(st, "nbb", [128, 512], BF16)
        selbT = self.sb(st, "selbT", [128, S], BF16)
        A(SP, lambda e: e.dma_start(out=e8t[:], in_=dr["e8"]), w=["e8t"], dma="e8t")
        A(SP, lambda e: e.dma_start(out=mneg[:], in_=dr["moba_neg"]), w=["mneg"], dma="mneg")
        A(SP, lambda e: e.dma_start(out=mnot[:], in_=dr["moba_notown"]), w=["mnot"], dma="mnot")
        for s in range(2):
            A(POOL, lambda e, s=s: e.memset(V[s][:, :, 128:129], 1.0), w=[f"V{s}"])
        A(POOL, lambda e: e.memset(kmean[:], 0.0), w=["kmean"])
        A(DVE, lambda e: e.memset(selbT[:], 0.0), w=["selbT"])
        w_in = dr["moba_w_in"][0]
        oi = [0]
        for h in range(8):
            s = h % 2
            slope = 2.0 ** (-(h + 1))
            w3 = [w_in[:, which * 1024 + h * 128: which * 1024 + (h + 1) * 128] for which in range(3)]
            _proj_qkv(self, w3, h, wq, "wq0", qT[s], f"qT{s}", kT[s], f"kT{s}", V[s], f"V{s}", hT, hkeys, kmean=kmean)
            A(DVE, lambda e: e.tensor_copy(out=kmh[:], in_=kmean[:]), r=["kmean"], w=["kmh"])
            A(DVE, lambda e: e.tensor_tensor(out=kmr[:], in0=kmean[:], in1=kmh[:], op=ALU.subtract), r=["kmean", "kmh"], w=["kmr"])
            A(DVE, lambda e: e.tensor_copy(out=kml[:], in_=kmr[:]), r=["kmr"], w=["kml"])
            pg = self.sring.next()
            for t in range(NT):
                A(PE, lambda e, t=t, s=s, pg=pg: e.matmul(self.bank[pg][:, t * 32:(t + 1) * 32], lhsT=qT[s][:, t * 128:(t + 1) * 128],
                                                          rhs=kmh[:], start=True, stop=False), r=[f"qT{s}", "kmh"], w=[f"B{pg}"])
                A(PE, lambda e, t=t, s=s, pg=pg: e.matmul(self.bank[pg][:, t * 32:(t + 1) * 32], lhsT=qT[s][:, t * 128:(t + 1) * 128],
                                                          rhs=kml[:], start=False, stop=True), r=[f"qT{s}", "kml"], w=[f"B{pg}"])
            A(DVE, lambda e, pg=pg: e.tensor_tensor(out=gm[:], in0=self.bank[pg][:], in1=mneg[:], op=ALU.add),
              r=[f"B{pg}", "mneg"], w=["gm"])
            for t in range(NT):
                A(DVE, lambda e, t=t: e.max(out=m8[:, t * 8:(t + 1) * 8], in_=gm[:, t * 32:(t + 1) * 32]), r=["gm"], w=["m8"])
            for t in range(NT):
                A(DVE, lambda e, t=t: e.tensor_scalar(out=sel[:, t * 32:(t + 1) * 32], in0=gm[:, t * 32:(t + 1) * 32],
                                                      scalar1=m8[:, t * 8 + 2:t * 8 + 3], scalar2=None, op0=ALU.is_ge),
                  r=["gm", "m8"], w=["sel"])
            A(DVE, lambda e: e.tensor_scalar(out=sel[:], in0=sel[:], scalar1=-NEGB, scalar2=NEGB, op0=ALU.mult, op1=ALU.add),
              r=["sel"], w=["sel"])
            A(DVE, lambda e: e.tensor_tensor(out=nbb[:], in0=sel[:], in1=mnot[:], op=ALU.mult), r=["sel", "mnot"], w=["nbb"])
            for half in range(2):
                pb = self.sring.next()
                for tt in range(8):
                    t = half * 8 + tt
                    A(PE, lambda e, t=t, tt=tt, pb=pb: e.transpose(out=self.bankbf(pb)[0:32, tt * 128:(tt + 1) * 128],
                                                                  in_=nbb[:, t * 32:(t + 1) * 32], identity=cb[:, 0, :]),
                      r=["nbb", "cb"], w=[f"B{pb}"])
                A(ACT, lambda e, half=half, pb=pb: e.activation(out=selbT[0:32, half * 1024:(half + 1) * 1024],
                                                                in_=self.bankbf(pb)[0:32, :], func=AF.Copy),
                  r=[f"B{pb}"], w=["selbT"])

            def epi(c, i, h=h, s=s):
                ob = oi[0] % 2
                oi[0] += 1
                A(DVE, lambda e, ob=ob, i=i: e.reciprocal(out=rden[ob][:], in_=self.bank[4 + i][:, 128:129]),
                  r=[f"B{4 + i}"], w=[f"rden{ob}"])
                _o_to_oT(self, self.bank[4 + i][:, 0:128], [f"B{4 + i}"], otok[ob], f"otok{ob}", oT, h % 4, 4 * c + i,
                         scale_ap=rden[ob][:, 0:1], scale_keys=[f"rden{ob}"])

            _attn_softmax(self, range(4), [(kT[s], qT[s])], [f"kT{s}", f"qT{s}"], V[s], f"V{s}", scale, slope, _causal_items,
                          (lambda kt: e8t[:, (kt // 2) * 128:(kt // 2 + 1) * 128], selbT, ["e8t", "selbT"]), PT, TMP, DT, epi)
            if h % 4 == 3:
                hg = h // 4
                self.out_proj(wo, "wo", oT, 4, dr["moba_w_o"][0, hg * 512:(hg + 1) * 512, :], ["oT"])
        self.layer_norm(st, li, 0)
        self.P.flush()


Builder.moba = _moba


def _rope_ops(self, x1, x2, cos, sin, o1, o2, R, rkeys, in_keys, okey):
    A = self.A
    A(DVE, lambda e: e.tensor_tensor(out=R[0], in0=x1, in1=cos, op=ALU.mult), r=in_keys + ["rope"], w=[rkeys[0]])
    A(DVE, lambda e: e.tensor_tensor(out=R[1], in0=x2, in1=sin, op=ALU.mult), r=in_keys + ["rope"], w=[rkeys[1]])
    A(DVE, lambda e: e.tensor_tensor(out=o1, in0=R[0], in1=R[1], op=ALU.subtract), r=[rkeys[0], rkeys[1]], w=[okey])
    A(DVE, lambda e: e.tensor_tensor(out=R[2], in0=x2, in1=cos, op=ALU.mult), r=in_keys + ["rope"], w=[rkeys[2]])
    A(DVE, lambda e: e.tensor_tensor(out=R[3], in0=x1, in1=sin, op=ALU.mult), r=in_keys + ["rope"], w=[rkeys[3]])
    A(DVE, lambda e: e.tensor_tensor(out=o2, in0=R[2], in1=R[3], op=ALU.add), r=[rkeys[2], rkeys[3]], w=[okey])


def _mla(self, li):
    from contextlib import ExitStack
    A, dr, cb = self.A, self.dr, self.cb
    scale = 192.0 ** -0.5
    self.atn_it = 0
    with ExitStack() as st:
        wuq = self.sb(st, "wuq", [128, 2, 1536], BF16)
        wukv = self.sb(st, "wukv", [128, 2, 2048], BF16)
        cos_t = self.sb(st, "cos_t", [128, NT, 32], F32)
        sin_t = self.sb(st, "sin_t", [128, NT, 32], F32)
        c_qT = self.sb(st, "c_qT", [128, 2, S], BF16)
        c_kvT = self.sb(st, "c_kvT", [128, 2, S], BF16)
        krT = self.sb(st, "krT", [128, S], BF16)
        gains = self.sb(st, "gains", [128, 4], F32)
        Rt = self.sb(st, "Rt", [128, 4, 128], F32)
        with ExitStack() as sth:
            hT, hkeys = _mixer_prologue(self, sth, li)
            w_in = self.sb(sth, "mlawin", [128, KC, 576], BF16)
            ang = self.sb(sth, "ang", [128, NT, 32], F32)
            invf = self.sb(sth, "invf", [128, 32], F32)
            junk = self.sb(sth, "junk", [128, 256], F32)
            ss = [self.sb(sth, f"ss{i}", [128, 4], F32) for i in range(2)]
            cn = [self.sb(sth, f"cn{i}", [128, 512], BF16) for i in range(2)]
            kr = [self.sb(sth, f"kr{i}", [128, 64], BF16) for i in range(2)]
            A(POOL, lambda e: e.dma_start(out=w_in[:], in_=dr["mla_w_in"][0].rearrange("(k p) n -> p k n", p=128)), w=["mlawin"], dma="mlawin")
            A(POOL, lambda e: e.dma_start(out=wuq[:], in_=dr["mla_w_uq"][0].rearrange("(k p) n -> p k n", p=128)), w=["wuq"], dma="wuq")
            for hf in range(2):
                A(POOL, lambda e, hf=hf: e.dma_start(out=wukv[:, :, hf * 1024:(hf + 1) * 1024],
                                                     in_=dr["mla_w_ukv"][0][:, hf * 1024:(hf + 1) * 1024].rearrange("(k p) n -> p k n", p=128)),
                  w=["wukv"], dma="wukv")
            A(SP, lambda e: e.dma_start(out=invf[:], in_=dr["invf"]), w=["invf"], dma="invf")
            A(SP, lambda e: e.dma_start(out=gains[:, 0:2], in_=dr["mla_q_norm"][0:1, :].rearrange("o (j p) -> p (o j)", p=128),
                                        allow_slow_non_contiguous=True), w=["gains"], dma="gains")
            A(SP, lambda e: e.dma_start(out=gains[:, 2:4], in_=dr["mla_kv_norm"][0:1, :].rearrange("o (j p) -> p (o j)", p=128),
                                        allow_slow_non_contiguous=True), w=["gains"], dma="gains")
            for t in range(NT):
                A(DVE, lambda e, t=t: e.tensor_scalar(out=ang[:, t, :], in0=invf[:], scalar1=self.posk[:, t:t + 1], scalar2=None, op0=ALU.mult),
                  r=["invf", "posk"], w=["ang"])
            ki = self.sb(sth, "ki", [128, NT, 32], I32)
            kf = self.sb(sth, "kf", [128, NT, 32], F32)
            mk = self.sb(sth, "mk", [128, NT, 32], F32)
            C1, C2 = 6.28125, 2 * np.pi - 6.28125
            for dst, shift in ((sin_t, 0.0), (cos_t, 0.5 * PI)):
                A(DVE, lambda e, dst=dst, shift=shift: e.tensor_scalar(out=dst[:], in0=ang[:], scalar1=shift, scalar2=None, op0=ALU.add), r=["ang"], w=["rope"])
                A(DVE, lambda e, dst=dst: e.tensor_scalar(out=kf[:], in0=dst[:], scalar1=float(1.0 / (2 * np.pi)), scalar2=None, op0=ALU.mult), r=["rope"], w=["kf"])
                A(DVE, lambda e: e.tensor_copy(out=ki[:], in_=kf[:]), r=["kf"], w=["ki"])
                A(DVE, lambda e: e.tensor_copy(out=kf[:], in_=ki[:]), r=["ki"], w=["kf"])
                A(DVE, lambda e, dst=dst: e.scalar_tensor_tensor(out=dst[:], in0=kf[:], scalar=-C1, in1=dst[:], op0=ALU.mult, op1=ALU.add), r=["kf", "rope"], w=["rope"])
                A(DVE, lambda e, dst=dst: e.scalar_tensor_tensor(out=dst[:], in0=kf[:], scalar=-C2, in1=dst[:], op0=ALU.mult, op1=ALU.add), r=["kf", "rope"], w=["rope"])
                A(DVE, lambda e, dst=dst: e.tensor_scalar(out=mk[:], in0=dst[:], scalar1=PI, scalar2=None, op0=ALU.is_gt), r=["rope"], w=["mk"])
                A(DVE, lambda e, dst=dst: e.scalar_tensor_tensor(out=dst[:], in0=mk[:], scalar=-2 * PI, in1=dst[:], op0=ALU.mult, op1=ALU.add), r=["mk", "rope"], w=["rope"])
                A(DVE, lambda e, dst=dst: e.tensor_scalar(out=mk[:], in0=dst[:], scalar1=-PI, scalar2=None, op0=ALU.is_lt), r=["rope"], w=["mk"])
                A(DVE, lambda e, dst=dst: e.scalar_tensor_tensor(out=dst[:], in0=mk[:], scalar=2 * PI, in1=dst[:], op0=ALU.mult, op1=ALU.add), r=["mk", "rope"], w=["rope"])
                A(DVE, lambda e, dst=dst: e.tensor_scalar(out=dst[:], in0=dst[:], scalar1=-3.1415925, scalar2=3.1415925, op0=ALU.max, op1=ALU.min), r=["rope"], w=["rope"])
            A(ACT, lambda e: e.activation(out=sin_t[:], in_=sin_t[:], func=AF.Sin), r=["rope"], w=["rope"])
            A(ACT, lambda e: e.activation(out=cos_t[:], in_=cos_t[:], func=AF.Sin), r=["rope"], w=["rope"])
            for t in range(NT):
                b = t % 2
                pa, pb2 = self.sring.next(), self.sring.next()
                for k in range(KC):
                    A(PE, lambda e, k=k, t=t, pa=pa: e.matmul(self.bank[pa][:], lhsT=hT[:, k, t * 128:(t + 1) * 128], rhs=w_in[:, k, 0:512],
                                                              start=(k == 0), stop=(k == KC - 1)), r=[hkeys[t], "mlawin"], w=[f"B{pa}"])
                for k in range(KC):
                    A(PE, lambda e, k=k, t=t, pb2=pb2: e.matmul(self.bank[pb2][:, 0:64], lhsT=hT[:, k, t * 128:(t + 1) * 128], rhs=w_in[:, k, 512:576],
                                                                start=(k == 0), stop=(k == KC - 1)), r=[hkeys[t], "mlawin"], w=[f"B{pb2}"])
                for j in range(2):
                    A(ACT, lambda e, j=j, pa=pa, b=b: e.activation(out=junk[:], in_=self.bank[pa][:, j * 256:(j + 1) * 256], func=AF.Square,
                                                                   accum_out=ss[b][:, j:j + 1]), r=[f"B{pa}"], w=["junk", f"ss{b}"])
                A(ACT, lambda e, b=b: e.activation(out=ss[b][:, 2:4], in_=ss[b][:, 0:2], func=AF.Ln, bias=1e-6, scale=1.0 / 256.0), r=[f"ss{b}"], w=[f"ss{b}"])
                A(ACT, lambda e, b=b: e.activation(out=ss[b][:, 2:4], in_=ss[b][:, 2:4], func=AF.Exp, scale=-0.5), r=[f"ss{b}"], w=[f"ss{b}"])
                for j in range(2):
                    A(DVE, lambda e, j=j, pa=pa, b=b: e.tensor_scalar(out=cn[b][:, j * 256:(j + 1) * 256], in0=self.bank[pa][:, j * 256:(j + 1) * 256],
                                                                      scalar1=ss[b][:, 2 + j:3 + j], scalar2=None, op0=ALU.mult),
                      r=[f"B{pa}", f"ss{b}"], w=[f"cn{b}"])
                pt = self.sring.next()
                for j in range(4):
                    A(PE, lambda e, j=j, b=b, pt=pt: e.transpose(out=self.bankbf(pt)[:, j * 128:(j + 1) * 128], in_=cn[b][:, j * 128:(j + 1) * 128],
                                                                 identity=cb[:, 0, :]), r=[f"cn{b}", "cb"], w=[f"B{pt}"])
                for j in range(4):
                    dst = c_qT if j < 2 else c_kvT
                    A(DVE, lambda e, j=j, t=t, pt=pt, dst=dst: e.tensor_scalar(
                        out=dst[:, j % 2, t * 128:(t + 1) * 128], in0=self.bankbf(pt)[:, j * 128:(j + 1) * 128], scalar1=gains[:, j:j + 1],
                        scalar2=None, op0=ALU.mult), r=[f"B{pt}", "gains"], w=["c_qT" if j < 2 else "c_kvT"])
                _rope_ops(self, self.bank[pb2][:, 0:32], self.bank[pb2][:, 32:64], cos_t[:, t, :], sin_t[:, t, :],
                          kr[b][:, 0:32], kr[b][:, 32:64], [Rt[:, i, 0:32] for i in range(4)], [f"Rt{i}" for i in range(4)], [f"B{pb2}"], f"kr{b}")
                pk = self.sring.next()
                A(PE, lambda e, b=b, pk=pk: e.transpose(out=self.bankbf(pk)[0:64, 0:128], in_=kr[b][:], identity=cb[:, 0, :]), r=[f"kr{b}", "cb"], w=[f"B{pk}"])
                A(ACT, lambda e, t=t, pk=pk: e.activation(out=krT[0:64, t * 128:(t + 1) * 128], in_=self.bankbf(pk)[0:64, 0:128], func=AF.Copy),
                  r=[f"B{pk}"], w=["krT"])
            self.P.flush()
        self.sring = Ring("m", [2, 3])
        self.sring_s = Ring("sc", [0, 1])
        qnT = [self.sb(st, f"qnT{i}", [128, S], BF16) for i in range(2)]
        qrT = [self.sb(st, f"qrT{i}", [128, S], BF16) for i in range(2)]
        knT = [self.sb(st, f"knT{i}", [128, S], BF16) for i in range(2)]
        V = [self.sb(st, f"V{i}", [128, NT, 129], BF16) for i in range(2)]
        oT = self.sb(st, "oT", [128, 4, S], BF16)
        wo = self.sb(st, "wo", [128, 4, D], BF16)
        PT = [self.sb(st, f"PT{i}", [128, 512], BF16) for i in range(2)]
        otok = [self.sb(st, f"otok{i}", [128, 128], BF16) for i in range(2)]
        rden = [self.sb(st, f"rden{i}", [128, 1], F32) for i in range(2)]
        qr = [self.sb(st, f"qr{i}", [128, 4, 64], BF16) for i in range(2)]
        for s in range(2):
            A(POOL, lambda e, s=s: e.memset(V[s][:, :, 128:129], 1.0), w=[f"V{s}"])
            A(DVE, lambda e, s=s: e.memset(qrT[s][64:128, :], 0.0), w=[f"qrT{s}"])
        A(DVE, lambda e: e.memset(krT[64:128, :], 0.0), w=["krT"])
        oi = [0]
        qi = 0
        for h in range(8):
            s = h % 2
            for c in range(4):
                pb = self.sring.next()
                for k in range(2):
                    A(PE, lambda e, k=k, c=c, pb=pb, h=h: e.matmul(self.bank[pb][:], lhsT=wuq[:, k, h * 192:h * 192 + 128], rhs=c_qT[:, k, c * 512:(c + 1) * 512],
                                                                   start=(k == 0), stop=(k == 1)), r=["wuq", "c_qT"], w=[f"B{pb}"])
                A(ACT, lambda e, c=c, pb=pb, s=s: e.activation(out=qnT[s][:, c * 512:(c + 1) * 512], in_=self.bank[pb][:], func=AF.Copy), r=[f"B{pb}"], w=[f"qnT{s}"])
                pb = self.sring.next()
                for k in range(2):
                    A(PE, lambda e, k=k, c=c, pb=pb, h=h: e.matmul(self.bank[pb][:], lhsT=wukv[:, k, h * 256:h * 256 + 128], rhs=c_kvT[:, k, c * 512:(c + 1) * 512],
                                                                   start=(k == 0), stop=(k == 1)), r=["wukv", "c_kvT"], w=[f"B{pb}"])
                A(DVE, lambda e, c=c, pb=pb, s=s: e.tensor_copy(out=knT[s][:, c * 512:(c + 1) * 512], in_=self.bank[pb][:]), r=[f"B{pb}"], w=[f"knT{s}"])
            for tg in range(4):
                pb = self.sring.next()
                for tt in range(4):
                    t = tg * 4 + tt
                    for k in range(2):
                        A(PE, lambda e, k=k, t=t, tt=tt, pb=pb, h=h: e.matmul(
                            self.bank[pb][:, tt * 128:(tt + 1) * 128], lhsT=c_kvT[:, k, t * 128:(t + 1) * 128], rhs=wukv[:, k, h * 256 + 128:h * 256 + 256],
                            start=(k == 0), stop=(k == 1)), r=["wukv", "c_kvT"], w=[f"B{pb}"])
                A(ACT, lambda e, tg=tg, pb=pb, s=s: e.activation(out=V[s][:, tg * 4:(tg + 1) * 4, 0:128],
                                                                 in_=self.bank[pb][:].rearrange("p (a n) -> p a n", a=4), func=AF.Copy), r=[f"B{pb}"], w=[f"V{s}"])
                pq = self.sring.next()
                for tt in range(4):
                    t = tg * 4 + tt
                    for k in range(2):
                        A(PE, lambda e, k=k, t=t, tt=tt, pq=pq, h=h: e.matmul(
                            self.bank[pq][:, tt * 64:(tt + 1) * 64], lhsT=c_qT[:, k, t * 128:(t + 1) * 128], rhs=wuq[:, k, h * 192 + 128:h * 192 + 192],
                            start=(k == 0), stop=(k == 1)), r=["wuq", "c_qT"], w=[f"B{pq}"])
                qb = qi % 2
                qi += 1
                xv = self.bank[pq][:, 0:256].rearrange("p (a n) -> p a n", a=4)
                _rope_ops(self, xv[:, :, 0:32], xv[:, :, 32:64], cos_t[:, tg * 4:(tg + 1) * 4, :], sin_t[:, tg * 4:(tg + 1) * 4, :],
                          qr[qb][:, :, 0:32], qr[qb][:, :, 32:64], [Rt[:, i, :].rearrange("p (a n) -> p a n", a=4) for i in range(4)],
                          [f"Rt{i}" for i in range(4)], [f"B{pq}"], f"qr{qb}")
                pt = self.sring.next()
                for tt in range(4):
                    A(PE, lambda e, tt=tt, qb=qb, pt=pt: e.transpose(out=self.bankbf(pt)[0:64, tt * 128:(tt + 1) * 128], in_=qr[qb][:, tt, :],
                                                                    identity=cb[:, 0, :]), r=[f"qr{qb}", "cb"], w=[f"B{pt}"])
                A(ACT, lambda e, tg=tg, pt=pt, s=s: e.activation(out=qrT[s][0:64, tg * 512:(tg + 1) * 512], in_=self.bankbf(pt)[0:64, 0:512], func=AF.Copy),
                  r=[f"B{pt}"], w=[f"qrT{s}"])

            def epi(c, i, h=h):
                ob = oi[0] % 2
                oi[0] += 1
                A(DVE, lambda e, ob=ob, i=i: e.reciprocal(out=rden[ob][:], in_=self.bank[4 + i][:, 128:129]), r=[f"B{4 + i}"], w=[f"rden{ob}"])
                _o_to_oT(self, self.bank[4 + i][:, 0:128], [f"B{4 + i}"], otok[ob], f"otok{ob}", oT, h % 4, 4 * c + i,
                         scale_ap=rden[ob][:, 0:1], scale_keys=[f"rden{ob}"])

            _attn_softmax(self, range(4), [(knT[s], qnT[s]), (krT, qrT[s])], [f"knT{s}", f"qnT{s}", "krT", f"qrT{s}"], V[s], f"V{s}",
                          scale, None, _causal_items, None, PT, None, None, epi)
            if h % 4 == 3:
                hg = h // 4
                self.out_proj(wo, "wo", oT, 4, dr["mla_w_o"][0, hg * 512:(hg + 1) * 512, :], ["oT"])
        self.layer_norm(st, li, 0)
        self.P.flush()


Builder.mla = _mla


def _proj_fm(self, wcols, wsl, wring, dst, dkey, hT, hkeys, use_act):
    A = self.A
    ws = wring.next()
    A(POOL, lambda e: e.dma_start(out=wsl[ws][:], in_=wcols.rearrange("(k p) n -> p k n", p=128)), w=[f"wsl{ws}"], dma=f"wsl{ws}")
    for c in range(4):
        pb = self.sring.next()
        for k in range(KC):
            A(PE, lambda e, k=k, c=c, pb=pb: e.matmul(self.bank[pb][:], lhsT=wsl[ws][:, k, :], rhs=hT[:, k, c * 512:(c + 1) * 512],
                                                      start=(k == 0), stop=(k == KC - 1)), r=[f"wsl{ws}"] + hkeys[4 * c:4 * c + 4], w=[f"B{pb}"])
        if use_act:
            A(ACT, lambda e, c=c, pb=pb: e.activation(out=dst[:, c * 512:(c + 1) * 512], in_=self.bank[pb][:], func=AF.Copy), r=[f"B{pb}"], w=[dkey])
        else:
            A(DVE, lambda e, c=c, pb=pb: e.tensor_copy(out=dst[:, c * 512:(c + 1) * 512], in_=self.bank[pb][:]), r=[f"B{pb}"], w=[dkey])


def _proj_tm(self, wcols, wsl, wring, V, vkey, hT, hkeys):
    A = self.A
    ws = wring.next()
    A(POOL, lambda e: e.dma_start(out=wsl[ws][:], in_=wcols.rearrange("(k p) n -> p k n", p=128)), w=[f"wsl{ws}"], dma=f"wsl{ws}")
    for tg in range(4):
        pb = self.sring.next()
        for tt in range(4):
            t = tg * 4 + tt
            for k in range(KC):
                A(PE, lambda e, k=k, t=t, tt=tt, pb=pb: e.matmul(self.bank[pb][:, tt * 128:(tt + 1) * 128], lhsT=hT[:, k, t * 128:(t + 1) * 128],
                                                                rhs=wsl[ws][:, k, :], start=(k == 0), stop=(k == KC - 1)),
                  r=[f"wsl{ws}", hkeys[t]], w=[f"B{pb}"])
        if tg % 2 == 0:
            A(ACT, lambda e, tg=tg, pb=pb: e.activation(out=V[:, tg * 4:(tg + 1) * 4, 0:128], in_=self.bank[pb][:].rearrange("p (a n) -> p a n", a=4),
                                                        func=AF.Copy), r=[f"B{pb}"], w=[vkey])
        else:
            A(DVE, lambda e, tg=tg, pb=pb: e.tensor_copy(out=V[:, tg * 4:(tg + 1) * 4, 0:128], in_=self.bank[pb][:].rearrange("p (a n) -> p a n", a=4)),
              r=[f"B{pb}"], w=[vkey])


def _win_items(c):
    out = []
    for kt in range(max(0, 4 * c - 4), 4 * c + 4):
        r_ = kt - 4 * c
        i_lo, i_hi = max(0, r_), min(3, r_ + 4)
        out.append((kt, i_lo * 128, (i_hi + 1) * 128, r_ >= 0, r_ + 4 <= 3))
    return out


def _nsa(self, li):
    from contextlib import ExitStack
    A, dr, cb = self.A, self.dr, self.cb
    scale = 128.0 ** -0.5
    self.atn_it = 0
    w_in = dr["nsa_w_in"][0]
    with ExitStack() as st:
        hT, hkeys = _mixer_prologue(self, st, li)
        oT = self.sb(st, "oT", [128, 4, S], BF16)
        gates = self.sb(st, "gates", [128, NT * 24], F32)
        wsl = [self.sb(st, f"wsl{i}", [128, KC, 128], BF16) for i in range(2)]
        wring = Ring("w", 2)
        with ExitStack() as sg:
            wg = self.sb(sg, "wg", [128, KC, 24], BF16)
            A(POOL, lambda e: e.dma_start(out=wg[:], in_=w_in[:, 2560:2584].rearrange("(k p) n -> p k n", p=128)), w=["wg"], dma="wg")
            pb = self.sring.next()
            for t in range(NT):
                for k in range(KC):
                    A(PE, lambda e, k=k, t=t, pb=pb: e.matmul(self.bank[pb][:, t * 24:(t + 1) * 24], lhsT=hT[:, k, t * 128:(t + 1) * 128], rhs=wg[:, k, :],
                                                              start=(k == 0), stop=(k == KC - 1)), r=["wg", hkeys[t]], w=[f"B{pb}"])
            A(ACT, lambda e, pb=pb: e.activation(out=gates[:], in_=self.bank[pb][:, 0:NT * 24], func=AF.Sigmoid), r=[f"B{pb}"], w=["gates"])
            self.P.flush()
        for g in range(2):
            with ExitStack() as sgp:
                qT = [self.sb(sgp, f"qT{i}", [128, S], BF16) for i in range(4)]
                kslT = self.sb(sgp, "kslT", [128, S], BF16)
                kwnT = self.sb(sgp, "kwnT", [128, S], BF16)
                vsl = self.sb(sgp, "vsl", [128, NT, 129], BF16)
                vwn = self.sb(sgp, "vwn", [128, NT, 129], BF16)
                ocs = [self.sb(sgp, f"ocs{i}", [128, NT, 128], BF16) for i in range(4)]
                nbT = self.sb(sgp, "nbT", [128, S], BF16)
                A(DVE, lambda e: e.memset(nbT[:], 0.0), w=["nbT"])
                A(POOL, lambda e: e.memset(vsl[:, :, 128:129], 1.0), w=["vsl"])
                A(POOL, lambda e: e.memset(vwn[:, :, 128:129], 1.0), w=["vwn"])
                for r in range(4):
                    hh = g * 4 + r
                    _proj_fm(self, w_in[:, hh * 128:(hh + 1) * 128], wsl, wring, qT[r], f"qT{r}", hT, hkeys, r % 2 == 0)
                kvc = lambda which: w_in[:, 1024 + which * 256 + g * 128: 1024 + which * 256 + (g + 1) * 128]
                _proj_fm(self, kvc(2), wsl, wring, kslT, "kslT", hT, hkeys, True)
                _proj_tm(self, kvc(3), wsl, wring, vsl, "vsl", hT, hkeys)
                _proj_fm(self, kvc(4), wsl, wring, kwnT, "kwnT", hT, hkeys, False)
                _proj_tm(self, kvc(5), wsl, wring, vwn, "vwn", hT, hkeys)
                with ExitStack() as sc:
                    kcT = self.sb(sc, "kcT", [128, 128], BF16)
                    vc = self.sb(sc, "vc", [128, 128], BF16)
                    with ExitStack() as sca:
                        rawT = [self.sb(sca, f"rawT{i}", [128, S], BF16) for i in range(2)]
                        w1b = self.sb(sca, "w1b", [128, 32, 128], BF16)
                        w2b = self.sb(sca, "w2b", [128, 128], BF16)
                        peT = self.sb(sca, "peT", [128, 32], F32)
                        peTb = self.sb(sca, "peTb", [128, 32], BF16)
                        b1 = self.sb(sca, "b1", [128, 1], F32)
                        g1 = self.sb(sca, "g1", [128, 128], BF16)
                        _proj_fm(self, kvc(0), wsl, wring, rawT[0], "rawT0", hT, hkeys, True)
                        _proj_fm(self, kvc(1), wsl, wring, rawT[1], "rawT1", hT, hkeys, False)
                        for which in range(2):
                            A(POOL, lambda e, which=which: e.dma_start(out=w1b[:], in_=dr["nsa_cmp_w1"][0, which].rearrange("(l d) f -> d l f", d=128)),
                              w=["w1b"], dma="w1b")
                            A(POOL, lambda e, which=which: e.dma_start(out=w2b[:], in_=dr["nsa_cmp_w2"][0, which]), w=["w2b"], dma="w2b")
                            A(SP, lambda e, which=which: e.dma_start(out=peT[:], in_=dr["nsa_cmp_pos"][0, which].rearrange("l d -> d l"),
                                                                     allow_slow_non_contiguous=True), w=["peT"], dma="peT")
                            A(DVE, lambda e: e.tensor_copy(out=peTb[:], in_=peT[:]), r=["peT"], w=["peTb"])
                            pz = self.sring.next()
                            for l in range(32):
                                A(PE, lambda e, l=l, pz=pz: e.matmul(self.bank[pz][:, 0:1], lhsT=w1b[:, l, :], rhs=peTb[:, l:l + 1], start=(l == 0), stop=(l == 31)),
                                  r=["w1b", "peTb"], w=[f"B{pz}"])
                            A(DVE, lambda e, pz=pz: e.tensor_copy(out=b1[:], in_=self.bank[pz][:, 0:1]), r=[f"B{pz}"], w=["b1"])
                            ph = self.sring.next()
                            for l in range(32):
                                A(PE, lambda e, l=l, ph=ph, which=which: e.matmul(self.bank[ph][:, 0:127], lhsT=w1b[:, l, :], rhs=rawT[which][:, l:l + 2017:16],
                                                                                 start=(l == 0), stop=(l == 31)), r=["w1b", f"rawT{which}"], w=[f"B{ph}"])
                            A(ACT, lambda e, ph=ph: e.activation(out=g1[:, 0:127], in_=self.bank[ph][:, 0:127], func=AF.Gelu_apprx_tanh, bias=b1[:, 0:1], scale=1.0),
                              r=[f"B{ph}", "b1"], w=["g1"])
                            po = self.sring.next()
                            if which == 0:
                                A(PE, lambda e, po=po: e.matmul(self.bank[po][:, 0:127], lhsT=w2b[:], rhs=g1[:, 0:127], start=True, stop=True), r=["w2b", "g1"], w=[f"B{po}"])
                                A(DVE, lambda e, po=po: e.tensor_copy(out=kcT[:, 0:127], in_=self.bank[po][:, 0:127]), r=[f"B{po}"], w=["kcT"])
                            else:
                                A(PE, lambda e, po=po: e.matmul(self.bank[po][0:127, 0:128], lhsT=g1[:, 0:127], rhs=w2b[:], start=True, stop=True), r=["w2b", "g1"], w=[f"B{po}"])
                                A(DVE, lambda e, po=po: e.tensor_copy(out=vc[0:127, :], in_=self.bank[po][0:127, 0:128]), r=[f"B{po}"], w=["vc"])
                        self.P.flush()
                    cmask = self.sb(sc, "cmask", [128, NT * 127], BF16)
                    ovt = self.sb(sc, "ovt", [128, 32], BF16)
                    zer = self.sb(sc, "zer", [128, 512], BF16)
                    Dc = [self.sb(sc, f"Dc{i}", [128, 127], F32) for i in range(2)]
                    tc_ = [self.sb(sc, f"tc{i}", [128, 127], F32) for i in range(2)]
                    pc = [self.sb(sc, f"pc{i}", [128, 127], F32) for i in range(2)]
                    pnb = [self.sb(sc, f"pnb{i}", [128, 127], BF16) for i in range(2)]
                    pnT = [self.sb(sc, f"pnT{i}", [128, 128], BF16) for i in range(2)]
                    sm = [self.sb(sc, f"sm{i}", [128, 8], F32) for i in range(2)]
                    impm = self.sb(sc, "impm", [128, 512], F32)
                    work = self.sb(sc, "work", [128, 512], F32)
                    nval = self.sb(sc, "nval", [128, 512], F32)
                    nbon = self.sb(sc, "nbon", [128, 512], F32)
                    m8a = self.sb(sc, "m8a", [128, 128], F32)
                    m8b = self.sb(sc, "m8b", [128, 128], F32)
                    selt = self.sb(sc, "selt", [128, 512], F32)
                    nbb = self.sb(sc, "nbb", [128, 512], BF16)
                    A(SP, lambda e: e.dma_start(out=cmask[:], in_=dr["nsa_cmask"]), w=["cmask"], dma="cmask")
                    A(SP, lambda e: e.dma_start(out=ovt[:], in_=dr["ov"]), w=["ovt"], dma="ovt")
                    A(SP, lambda e: e.dma_start(out=nval[:], in_=dr["nsa_valid"]), w=["nval"], dma="nval")
                    A(SP, lambda e: e.dma_start(out=nbon[:], in_=dr["nsa_bonus"]), w=["nbon"], dma="nbon")
                    A(POOL, lambda e: e.memset(zer[:], 0.0), w=["zer"])
                    A(PE, lambda e: e.matmul(self.bank[4][:], lhsT=cb[:, 4, :], rhs=zer[:], start=True, stop=False, skip_group_check=True), r=["cb", "zer"], w=["B4"])
                    ci = 0
                    for r in range(4):
                        hh = g * 4 + r
                        slope = 2.0 ** (-(hh + 1))
                        for t in range(NT):
                            b = ci % 2
                            ci += 1
                            pS = self.sring.next()
                            A(PE, lambda e, t=t, r=r, pS=pS: e.matmul(self.bank[pS][:, 0:127], lhsT=qT[r][:, t * 128:(t + 1) * 128], rhs=kcT[:, 0:127], start=True, stop=True),
                              r=[f"qT{r}", "kcT"], w=[f"B{pS}"])
                            A(ACT, lambda e, t=t, b=b: e.activation(out=Dc[b][:], in_=self.posq[:, 31:2048:16], func=AF.Abs, bias=self.nposk[:, t:t + 1], scale=1.0),
                              r=["posq", "posk"], w=[f"Dc{b}"])
                            A(DVE, lambda e, b=b, pS=pS, slope=slope: e.scalar_tensor_tensor(out=tc_[b][:], in0=Dc[b][:], scalar=-slope / scale, in1=self.bank[pS][:, 0:127],
                                                                                             op0=ALU.mult, op1=ALU.add), r=[f"Dc{b}", f"B{pS}"], w=[f"tc{b}"])
                            A(DVE, lambda e, b=b: e.reduce_max(out=sm[b][:, 0:1], in_=tc_[b][:], axis=AX.X), r=[f"tc{b}"], w=[f"sm{b}"])
                            A(DVE, lambda e, b=b: e.tensor_scalar(out=sm[b][:, 1:2], in0=sm[b][:, 0:1], scalar1=-scale, scalar2=None, op0=ALU.mult), r=[f"sm{b}"], w=[f"sm{b}"])
                            A(ACT, lambda e, b=b: e.activation(out=pc[b][:], in_=tc_[b][:], func=AF.Exp, bias=sm[b][:, 1:2], scale=scale), r=[f"tc{b}", f"sm{b}"], w=[f"pc{b}"])
                            A(DVE, lambda e, b=b, t=t: e.tensor_tensor(out=pc[b][:], in0=pc[b][:], in1=cmask[:, t * 127:(t + 1) * 127], op=ALU.mult), r=[f"pc{b}", "cmask"], w=[f"pc{b}"])
                            A(DVE, lambda e, b=b: e.reduce_sum(out=sm[b][:, 2:3], in_=pc[b][:], axis=AX.X), r=[f"pc{b}"], w=[f"sm{b}"])
                            A(DVE, lambda e, b=b: e.tensor_scalar(out=sm[b][:, 2:3], in0=sm[b][:, 2:3], scalar1=1e-30, scalar2=None, op0=ALU.max), r=[f"sm{b}"], w=[f"sm{b}"])
                            A(DVE, lambda e, b=b: e.reciprocal(out=sm[b][:, 3:4], in_=sm[b][:, 2:3]), r=[f"sm{b}"], w=[f"sm{b}"])
                            A(DVE, lambda e, b=b: e.tensor_scalar(out=pnb[b][:], in0=pc[b][:], scalar1=sm[b][:, 3:4], scalar2=None, op0=ALU.mult), r=[f"pc{b}", f"sm{b}"], w=[f"pnb{b}"])
                            pt = self.sring.next()
                            A(PE, lambda e, b=b, pt=pt: e.transpose(out=self.bankbf(pt)[0:127, 0:128], in_=pnb[b][:], identity=cb[:, 0, :]), r=[f"pnb{b}", "cb"], w=[f"B{pt}"])
                            A(ACT, lambda e, b=b, pt=pt: e.activation(out=pnT[b][0:127, :], in_=self.bankbf(pt)[0:127, 0:128], func=AF.Copy), r=[f"B{pt}"], w=[f"pnT{b}"])
                            po = self.sring.next()
                            A(PE, lambda e, b=b, po=po: e.matmul(self.bank[po][:, 0:128], lhsT=pnT[b][0:127, :], rhs=vc[0:127, :], start=True, stop=True), r=[f"pnT{b}", "vc"], w=[f"B{po}"])
                            A(PE, lambda e, b=b, t=t: e.matmul(self.bank[4][:, t * 32:(t + 1) * 32], lhsT=pnT[b][0:127, :], rhs=ovt[0:127, :], start=False, stop=False,
                                                               skip_group_check=True), r=[f"pnT{b}", "ovt"], w=["B4"])
                            A(ACT, lambda e, r=r, t=t, po=po, hh=hh: e.activation(out=ocs[r][:, t, :], in_=self.bank[po][:, 0:128], func=AF.Copy,
                                                                                  scale=gates[:, t * 24 + hh:t * 24 + hh + 1]), r=[f"B{po}", "gates"], w=[f"ocs{r}"])
                    A(DVE, lambda e: e.tensor_tensor(out=impm[:], in0=self.bank[4][:], in1=nval[:], op=ALU.mult), r=["B4", "nval"], w=["impm"])
                    A(DVE, lambda e: e.tensor_tensor(out=impm[:], in0=impm[:], in1=nbon[:], op=ALU.add), r=["impm", "nbon"], w=["impm"])
                    for t in range(NT):
                        sl = slice(t * 32, (t + 1) * 32)
                        s8 = slice(t * 8, (t + 1) * 8)
                        A(DVE, lambda e, sl=sl, s8=s8: e.max(out=m8a[:, s8], in_=impm[:, sl]), r=["impm"], w=["m8a"])
                        A(DVE, lambda e, sl=sl, s8=s8: e.match_replace(out=work[:, sl], in_to_replace=m8a[:, s8], in_values=impm[:, sl], imm_value=-3.0e38),
                          r=["impm", "m8a"], w=["work"])
                        A(DVE, lambda e, sl=sl, s8=s8: e.max(out=m8b[:, s8], in_=work[:, sl]), r=["work"], w=["m8b"])
                        A(DVE, lambda e, sl=sl, t=t: e.tensor_scalar(out=selt[:, sl], in0=impm[:, sl], scalar1=m8b[:, t * 8 + 7:t * 8 + 8], scalar2=None, op0=ALU.is_ge),
                          r=["impm", "m8b"], w=["selt"])
                    A(DVE, lambda e: e.tensor_tensor(out=selt[:], in0=selt[:], in1=nval[:], op=ALU.mult), r=["selt", "nval"], w=["selt"])
                    A(DVE, lambda e: e.tensor_scalar(out=nbb[:], in0=selt[:], scalar1=-NEGB, scalar2=NEGB, op0=ALU.mult, op1=ALU.add), r=["selt"], w=["nbb"])
                    for q4 in range(2):
                        pb = self.sring.next()
                        for tt in range(8):
                            t = q4 * 8 + tt
                            A(PE, lambda e, t=t, tt=tt, pb=pb: e.transpose(out=self.bankbf(pb)[0:32, tt * 128:(tt + 1) * 128], in_=nbb[:, t * 32:(t + 1) * 32],
                                                                          identity=cb[:, 0, :]), r=["nbb", "cb"], w=[f"B{pb}"])
                        A(ACT, lambda e, q4=q4, pb=pb: e.activation(out=nbT[0:32, q4 * 1024:(q4 + 1) * 1024], in_=self.bankbf(pb)[0:32, :], func=AF.Copy),
                          r=[f"B{pb}"], w=["nbT"])
                    self.P.flush()
                with ExitStack() as sw:
                    e32t = self.sb(sw, "e32t", [128, 2048], BF16)
                    PT = [self.sb(sw, f"PT{i}", [128, 512], BF16) for i in range(2)]
                    TMP = [self.sb(sw, f"TMP{i}", [128, 512], F32) for i in range(2)]
                    DT = [self.sb(sw, f"DT{i}", [128, 512], F32) for i in range(2)]
                    otok = [self.sb(sw, f"otok{i}", [128, 128], BF16) for i in range(2)]
                    rd = [self.sb(sw, f"rd{i}", [128, 2], F32) for i in range(2)]
                    A(SP, lambda e: e.dma_start(out=e32t[:], in_=dr["e32"]), w=["e32t"], dma="e32t")
                    oi = [0]
                    for r in range(4):
                        hh = g * 4 + r
                        slope = 2.0 ** (-(hh + 1))

                        def epi_sel(c, i, r=r, hh=hh):
                            ob = oi[0] % 2
                            oi[0] += 1
                            t = 4 * c + i
                            A(DVE, lambda e: e.reciprocal(out=rd[ob][:, 0:1], in_=self.bank[4 + i][:, 128:129]), r=[f"B{4 + i}"], w=[f"rd{ob}"])
                            A(DVE, lambda e: e.tensor_tensor(out=rd[ob][:, 1:2], in0=rd[ob][:, 0:1], in1=gates[:, t * 24 + 8 + hh:t * 24 + 9 + hh], op=ALU.mult),
                              r=[f"rd{ob}", "gates"], w=[f"rd{ob}"])
                            A(DVE, lambda e: e.scalar_tensor_tensor(out=ocs[r][:, t, :], in0=self.bank[4 + i][:, 0:128], scalar=rd[ob][:, 1:2], in1=ocs[r][:, t, :],
                                                                    op0=ALU.mult, op1=ALU.add), r=[f"B{4 + i}", f"rd{ob}", f"ocs{r}"], w=[f"ocs{r}"])

                        def epi_win(c, i, r=r, hh=hh):
                            ob = oi[0] % 2
                            oi[0] += 1
                            t = 4 * c + i
                            A(DVE, lambda e: e.reciprocal(out=rd[ob][:, 0:1], in_=self.bank[4 + i][:, 128:129]), r=[f"B{4 + i}"], w=[f"rd{ob}"])
                            A(DVE, lambda e: e.tensor_tensor(out=rd[ob][:, 1:2], in0=rd[ob][:, 0:1], in1=gates[:, t * 24 + 16 + hh:t * 24 + 17 + hh], op=ALU.mult),
                              r=[f"rd{ob}", "gates"], w=[f"rd{ob}"])
                            A(DVE, lambda e: e.scalar_tensor_tensor(out=otok[ob][:], in0=self.bank[4 + i][:, 0:128], scalar=rd[ob][:, 1:2], in1=ocs[r][:, t, :],
                                                                    op0=ALU.mult, op1=ALU.add), r=[f"B{4 + i}", f"rd{ob}", f"ocs{r}"], w=[f"otok{ob}"])
                            pb = self.sring.next()
                            A(PE, lambda e, pb=pb: e.transpose(out=self.bankbf(pb)[:, 0:128], in_=otok[ob][:], identity=cb[:, 0, :]), r=[f"otok{ob}", "cb"], w=[f"B{pb}"])
                            A(ACT, lambda e, pb=pb: e.activation(out=oT[:, r, t * 128:(t + 1) * 128], in_=self.bankbf(pb)[:, 0:128], func=AF.Copy), r=[f"B{pb}"], w=["oT"])

                        _attn_softmax(self, range(4), [(kslT, qT[r])], ["kslT", f"qT{r}"], vsl, "vsl", scale, slope, _causal_items,
                                      (lambda kt: e32t[:, kt * 128:(kt + 1) * 128], nbT, ["e32t", "nbT"]), PT, TMP, DT, epi_sel)
                        _attn_softmax(self, range(4), [(kwnT, qT[r])], ["kwnT", f"qT{r}"], vwn, "vwn", scale, slope, _win_items,
                                      None, PT, TMP, DT, epi_win)
                    self.P.flush()
            with ExitStack() as so:
                wo = self.sb(so, "wo", [128, 4, D], BF16)
                self.out_proj(wo, "wo", oT, 4, dr["nsa_w_o"][0, g * 512:(g + 1) * 512, :], ["oT"])
                self.P.flush()
        self.layer_norm(st, li, 0)
        self.P.flush()


Builder.nsa = _nsa
```

```python
import numpy as np
import ml_dtypes
import concourse.bass as bass
import concourse.mybir as mybir
from concourse.bass_utils import run_bass_kernel_spmd

F32 = mybir.dt.float32
BF16 = mybir.dt.bfloat16
I32 = mybir.dt.int32
AF = mybir.ActivationFunctionType
ALU = mybir.AluOpType
AX = mybir.AxisListType

PE, ACT, DVE, POOL, SP = "tensor", "scalar", "vector", "gpsimd", "sync"
ENGINES = (PE, ACT, DVE, POOL, SP)


class Prog:
    def __init__(self, nc, es):
        self.nc = nc
        self.es = es
        self.sems = {}
        for e in ENGINES:
            self.sems[("eng", e)] = es.enter_context(nc.semaphore("s_" + e))
        self.counters = {e: 0 for e in ENGINES}
        self.chan_count = {}
        self.water = {e: {} for e in ENGINES}
        self.n_total = 0
        self._reset()

    def _reset(self):
        self.ops = []
        self.writers = {}
        self.readers = {}

    def add(self, eng, fn, reads=(), writes=(), dma=None):
        idx = len(self.ops)
        deps = set()
        src = eng if dma is None else ("dma", dma)
        for b in reads:
            deps.update(self.writers.get(b, {}).values())
        for b in writes:
            deps.update(self.writers.get(b, {}).values())
            deps.update(self.readers.get(b, {}).values())
        op = dict(eng=eng, fn=fn, deps=deps, dma=dma, ticket=None)
        if dma is not None:
            self.chan_count[dma] = self.chan_count.get(dma, 0) + 1
            op["dma_val"] = 16 * self.chan_count[dma]
        self.ops.append(op)
        for b in writes:
            self.writers.setdefault(b, {})[src] = idx
        for b in reads:
            self.readers.setdefault(b, {})[src] = idx
        return idx

    def flush(self):
        nc = self.nc
        ops = self.ops
        needed = set()
        last = {}
        for i, op in enumerate(ops):
            if op["dma"] is None:
                last[op["eng"]] = i
        needed.update(last.values())
        for op in ops:
            for d in op["deps"]:
                dop = ops[d]
                if dop["dma"] is None and not (dop["eng"] == PE and op["eng"] == PE and op["dma"] is None):
                    needed.add(d)
        for i, op in enumerate(ops):
            if op["dma"] is None and i in needed:
                self.counters[op["eng"]] += 1
                op["ticket"] = self.counters[op["eng"]]
        for c in self.chan_count:
            if ("dma", c) not in self.sems:
                self.sems[("dma", c)] = self.es.enter_context(nc.semaphore("d_" + str(c)))
        streams = {e: [] for e in ENGINES}
        for i, op in enumerate(ops):
            e = op["eng"]
            waits = {}
            for d in op["deps"]:
                dop = ops[d]
                if dop["dma"] is not None:
                    key, val = ("dma", dop["dma"]), dop["dma_val"]
                else:
                    if dop["eng"] == PE and e == PE and op["dma"] is None:
                        continue
                    key, val = ("eng", dop["eng"]), dop["ticket"]
                if val > waits.get(key, 0):
                    waits[key] = val
            wl = []
            for key, val in waits.items():
                if self.water[e].get(key, 0) >= val:
                    continue
                self.water[e][key] = val
                wl.append((key, val))
            streams[e].append((op, wl))
        final = [(("eng", e), self.counters[e]) for e in ENGINES] + \
                [(("dma", c), 16 * n) for c, n in self.chan_count.items()]
        sems = self.sems
        with nc.Block() as block:
            def mk(ename):
                def body(eng):
                    for op, wl in streams[ename]:
                        for key, val in wl:
                            eng.wait_ge(sems[key], val)
                        ins = op["fn"](eng)
                        if op["dma"] is not None:
                            ins.then_inc(sems[("dma", op["dma"])], 16)
                        elif op["ticket"] is not None:
                            ins.then_inc(sems[("eng", ename)], 1)
                    for key, val in final:
                        if val == 0 or self.water[ename].get(key, 0) >= val:
                            continue
                        eng.wait_ge(sems[key], val)
                        self.water[ename][key] = val
                return body
            block.tensor(mk(PE))
            block.scalar(mk(ACT))
            block.vector(mk(DVE))
            block.gpsimd(mk(POOL))
            block.sync(mk(SP))
        self.n_total += len(ops)
        self._reset()


S, D, NT, KC, DFF, NFC = 2048, 1024, 16, 8, 2816, 22
ALPHA = 8.0 ** 0.25
NEGB = -30000.0
PI = float(np.pi)


def host_consts():
    kp = np.arange(128)[:, None]
    qf = np.arange(128)[None, :]
    cb = np.zeros((128, 8, 128), np.float32)
    cb[:, 0] = np.eye(128)
    cb[:, 1] = np.where(kp <= qf, 0.0, NEGB)
    cb[:, 2] = np.where(kp > qf, 0.0, NEGB)
    cb[:, 3] = -(kp > qf).astype(np.float32)
    cb[:, 4] = 1.0
    cb[:, 5] = -1.0
    cb[:, 6] = (kp < qf).astype(np.float32)
    e8 = np.zeros((128, 8, 128), np.float32)
    for n in range(8):
        e8[n, n, :] = 1.0
    e32 = np.zeros((128, 16, 128), np.float32)
    for kt in range(16):
        for k in range(128):
            e32[2 * kt + k // 64, kt, k] = 1.0
    t_idx = np.arange(16)[None, :, None]
    n_idx = np.arange(32)[None, None, :]
    own = t_idx // 2
    moba_neg = np.broadcast_to(np.where(n_idx < own, 0.0, -1e30), (128, 16, 32)).astype(np.float32)
    moba_notown = np.broadcast_to((n_idx != own).astype(np.float32), (128, 16, 32)).astype(np.float32)
    invf = (np.float32(10000.0) ** (-np.arange(32, dtype=np.float32) / np.float32(32))).astype(np.float32)
    invf_bc = np.broadcast_to(invf[None, :], (128, 32)).astype(np.float32)
    q_idx = (np.arange(16)[None, :] * 128 + np.arange(128)[:, None])
    c_end = np.arange(127) * 16 + 31
    nsa_cmask = (c_end[None, None, :] <= q_idx[:, :, None]).astype(np.float32)
    qblk = q_idx // 64
    jj = np.arange(32)[None, None, :]
    valid = jj <= qblk[:, :, None]
    forced = (jj == 0) | (jj == qblk[:, :, None]) | (jj == qblk[:, :, None] - 1)
    nsa_bonus = np.where(valid, 100.0 * forced, -1e30).astype(np.float32)
    nsa_valid = valid.astype(np.float32)
    c_start = np.arange(127) * 16
    j_start = np.arange(32) * 64
    overlap = ((c_start[:, None] < j_start[None, :] + 64) & (c_end[:, None] >= j_start[None, :])).astype(np.float32)
    ov = np.zeros((128, 32), np.float32)
    ov[:127] = overlap
    bf = ml_dtypes.bfloat16
    return {
        "cb": cb.astype(bf), "e8": e8.reshape(128, 1024).astype(bf), "e32": e32.reshape(128, 2048).astype(bf),
        "moba_neg": moba_neg.reshape(128, 512), "moba_notown": moba_notown.reshape(128, 512),
        "invf": invf_bc, "nsa_cmask": nsa_cmask.reshape(128, 16 * 127).astype(bf), "nsa_bonus": nsa_bonus.reshape(128, 512),
        "nsa_valid": nsa_valid.reshape(128, 512), "ov": ov.astype(bf),
    }


WEIGHT_SPECS = [
    ("mod_w", [4, 1024, 6144]), ("mod_b", [4, 6144]), ("ln_g", [4, 2, 1024]), ("ln_b", [4, 2, 1024]),
    ("ffn_w_in", [4, 1024, 5632]), ("ffn_conv_w", [4, 3, 2816]), ("ffn_conv_b", [4, 2816]),
    ("ffn_w_out", [4, 2816, 1024]),
    ("mla_w_in", [1, 1024, 576]), ("mla_q_norm", [1, 256]), ("mla_w_uq", [1, 256, 1536]),
    ("mla_kv_norm", [1, 256]), ("mla_w_ukv", [1, 256, 2048]), ("mla_w_o", [1, 1024, 1024]),
    ("moba_w_in", [1, 1024, 3072]), ("moba_w_o", [1, 1024, 1024]),
    ("nsa_w_in", [1, 1024, 2584]), ("nsa_cmp_pos", [1, 2, 32, 128]), ("nsa_cmp_w1", [1, 2, 4096, 128]),
    ("nsa_cmp_w2", [1, 2, 128, 128]), ("nsa_w_o", [1, 1024, 1024]),
    ("sb_w_in", [1, 1024, 3072]), ("sb_w_o", [1, 1024, 1024]),
]
CONST_SPECS = [
    ("cb", [128, 8, 128], BF16), ("e8", [128, 1024], BF16), ("e32", [128, 2048], BF16),
    ("moba_neg", [128, 512], F32), ("moba_notown", [128, 512], F32), ("invf", [128, 32], F32),
    ("nsa_cmask", [128, 16 * 127], BF16), ("nsa_bonus", [128, 512], F32), ("nsa_valid", [128, 512], F32),
    ("ov", [128, 32], BF16),
]


class Ring:
    def __init__(self, name, n):
        self.vals = list(range(n)) if isinstance(n, int) else list(n)
        self.name, self.n, self.i = name, len(self.vals), 0

    def next(self):
        k = self.vals[self.i % self.n]
        self.i += 1
        return k


class Builder:
    def __init__(self, layers=(0, 1, 2, 3), taps=None):
        from contextlib import ExitStack
        self.layers = layers
        self.nc = nc = bass.Bass("TRN2", target_bir_lowering=False)
        self.es = ExitStack()
        self.P = Prog(nc, self.es)
        self.dr = {}
        self.dr["x"] = nc.dram_tensor("x", [S, D], F32, kind="ExternalInput").ap()
        self.dr["c"] = nc.dram_tensor("c", [1, D], F32, kind="ExternalInput").ap()
        self.dr["positions"] = nc.dram_tensor("positions", [1, S], I32, kind="ExternalInput").ap()
        for name, shp in WEIGHT_SPECS:
            self.dr[name] = nc.dram_tensor(name, shp, F32, kind="ExternalInput").ap()
        for name, shp, dt in CONST_SPECS:
            self.dr[name] = nc.dram_tensor(name, shp, dt, kind="ExternalInput").ap()
        self.dr["out"] = nc.dram_tensor("out", [S, D], F32, kind="ExternalOutput").ap()
        self.uid = 0

    def sb(self, st, name, shape, dt):
        self.uid += 1
        return st.enter_context(self.nc.sbuf_tensor(f"{name}_{self.uid}", shape, dt))

    def A(self, eng, fn, r=(), w=(), dma=None):
        return self.P.add(eng, fn, reads=r, writes=w, dma=dma)

    def psum_setup(self, st):
        self.uid += 1
        self.bank = [st.enter_context(self.nc.psum_tensor(f"bank{i}_{self.uid}", [128, 512], F32)) for i in range(8)]

    def bankbf(self, i):
        return self.bank[i][:].bitcast(BF16)

    def build(self):
        nc, A, dr = self.nc, self.A, self.dr
        from contextlib import ExitStack
        es = self.es
        self.psum_setup(es)
        self.x = self.sb(es, "x", [128, NT, D], F32)
        self.cb = self.sb(es, "cb", [128, 8, 128], BF16)
        self.posq = self.sb(es, "posq", [128, S], F32)
        self.posk = self.sb(es, "posk", [128, NT], F32)
        self.nposk = self.sb(es, "nposk", [128, NT], F32)
        self.crep = self.sb(es, "crep", [128, KC, 128], BF16)
        self.gate = self.sb(es, "gate", [128, D], F32)
        self.small = self.sb(es, "small", [128, 64], F32)
        x, cb = self.x, self.cb
        with ExitStack() as st:
            posi = self.sb(st, "posi", [128, S], I32)
            poski = self.sb(st, "poski", [128, NT], I32)
            cT = self.sb(st, "cT", [128, KC], F32)
            cA = self.sb(st, "cA", [128, KC], F32)
            A(SP, lambda e: e.dma_start(out=cb[:], in_=dr["cb"]), w=["cb"], dma="cb")
            A(SP, lambda e: e.dma_start(out=posi[:], in_=dr["positions"].partition_broadcast(128)), w=["posi"], dma="posi")
            A(SP, lambda e: e.dma_start(out=poski[:], in_=dr["positions"].rearrange("o (t p) -> p (o t)", p=128),
                                        allow_slow_non_contiguous=True), w=["poski"], dma="poski")
            A(SP, lambda e: e.dma_start(out=cT[:], in_=dr["c"].rearrange("o (k p) -> p (o k)", p=128),
                                        allow_slow_non_contiguous=True), w=["cT"], dma="cT")
            for t in range(NT):
                A(SP, lambda e, t=t: e.dma_start(out=x[:, t, :], in_=dr["x"][t * 128:(t + 1) * 128, :]),
                  w=[f"x{t}"], dma=f"x{t % 4}")
            A(DVE, lambda e: e.tensor_copy(out=self.posq[:], in_=posi[:]), r=["posi"], w=["posq"])
            A(DVE, lambda e: e.tensor_copy(out=self.posk[:], in_=poski[:]), r=["poski"], w=["posk"])
            A(DVE, lambda e: e.tensor_scalar(out=self.nposk[:], in0=self.posk[:], scalar1=-1.0, scalar2=None, op0=ALU.mult),
              r=["posk"], w=["posk"])
            A(ACT, lambda e: e.activation(out=cA[:], in_=cT[:], func=AF.Silu), r=["cT"], w=["cA"])
            for k in range(KC):
                A(DVE, lambda e, k=k: e.tensor_scalar(out=self.crep[:, k, :], in0=cb[:, 4, :], scalar1=cA[:, k:k + 1],
                                                      scalar2=None, op0=ALU.mult), r=["cA", "cb"], w=["crep"])
            self.P.flush()
        for li in self.layers:
            self.layer(li)
        for t in range(NT):
            A(SP, lambda e, t=t: e.dma_start(out=dr["out"][t * 128:(t + 1) * 128, :], in_=x[:, t, :]),
              r=[f"x{t}"], w=[f"out{t}"], dma=f"o{t % 4}")
        self.P.flush()
        return nc

    def compute_mod(self, st, li, half):
        A, dr = self.A, self.dr
        self.ms = self.sb(st, "ms", [128, 2, D], F32)
        wsl = [self.sb(st, f"modw{i}", [128, KC, 256], BF16) for i in range(2)]
        bsl = [self.sb(st, f"modb{i}", [128, 256], F32) for i in range(2)]
        for j in range(12):
            s = j % 2
            col0 = half * 3072 + j * 256
            A(POOL, lambda e, s=s, col0=col0: e.dma_start(
                out=wsl[s][:], in_=dr["mod_w"][li, :, col0:col0 + 256].rearrange("(k p) n -> p k n", p=128)),
              w=[f"modw{s}"], dma=f"modw{s}")
            A(SP, lambda e, s=s, col0=col0: e.dma_start(
                out=bsl[s][:], in_=dr["mod_b"][li:li + 1, col0:col0 + 256].partition_broadcast(128)),
              w=[f"modb{s}"], dma=f"modb{s}")
            pb = 6 + s
            for k in range(KC):
                A(PE, lambda e, s=s, k=k, pb=pb: e.matmul(self.bank[pb][:, 0:256], lhsT=self.crep[:, k, :],
                                                          rhs=wsl[s][:, k, :], start=(k == 0), stop=(k == KC - 1)),
                  r=["crep", f"modw{s}"], w=[f"B{pb}"])
            comp, off = (j * 256) // 1024, (j * 256) % 1024
            dst = self.ms[:, comp, off:off + 256] if comp < 2 else self.gate[:, off:off + 256]
            A(DVE, lambda e, s=s, pb=pb, dst=dst: e.tensor_tensor(out=dst, in0=self.bank[pb][:, 0:256], in1=bsl[s][:], op=ALU.add),
              r=[f"B{pb}", f"modb{s}"], w=["modt"])
        A(DVE, lambda e: e.tensor_scalar(out=self.ms[:, 1, :], in0=self.ms[:, 1, :], scalar1=1.0, scalar2=None,
                                         op0=ALU.add), r=["modt"], w=["modt"])

    def build_hT(self, st, hT):
        A, x, modt = self.A, self.x, self.ms
        tmp = [self.sb(st, f"htmp{i}", [128, D], F32) for i in range(2)]
        hb = [self.sb(st, f"hb{i}", [128, D], BF16) for i in range(2)]
        for t in range(NT):
            s = t % 2
            A(DVE, lambda e, t=t, s=s: e.tensor_tensor(out=tmp[s][:], in0=x[:, t, :], in1=modt[:, 1, :], op=ALU.mult),
              r=[f"x{t}", "modt"], w=[f"htmp{s}"])
            A(DVE, lambda e, s=s: e.tensor_tensor(out=hb[s][:], in0=tmp[s][:], in1=modt[:, 0, :], op=ALU.add),
              r=[f"htmp{s}", "modt"], w=[f"hb{s}"])
            pb = 6 + s
            for k in range(KC):
                A(PE, lambda e, s=s, k=k, pb=pb: e.transpose(out=self.bankbf(pb)[:, k * 128:(k + 1) * 128],
                                                             in_=hb[s][:, k * 128:(k + 1) * 128], identity=self.cb[:, 0, :]),
                  r=[f"hb{s}", "cb"], w=[f"B{pb}"])
            A(ACT, lambda e, t=t, pb=pb: e.activation(
                out=hT[:, :, t * 128:(t + 1) * 128], in_=self.bankbf(pb).rearrange("p (k n) -> p k n", k=KC), func=AF.Copy),
              r=[f"B{pb}"], w=[f"hT{t}"])
            A(ACT, lambda e, t=t: e.activation(out=x[:, t, :], in_=x[:, t, :], func=AF.Copy, scale=ALPHA),
              r=[f"x{t}"], w=[f"x{t}"])

    def layer_norm(self, st, li, which):
        A, x, dr = self.A, self.x, self.dr
        self.lng = self.sb(st, "lng", [128, D], F32)
        self.lnb = self.sb(st, "lnb", [128, D], F32)
        A(SP, lambda e: e.dma_start(out=self.lng[:], in_=dr["ln_g"][li, which:which + 1, :].partition_broadcast(128)),
          w=["lng"], dma="lng")
        A(SP, lambda e: e.dma_start(out=self.lnb[:], in_=dr["ln_b"][li, which:which + 1, :].partition_broadcast(128)),
          w=["lnb"], dma="lnb")
        stt = [self.sb(st, f"lnst{i}", [128, 16], F32) for i in range(2)]
        for t in range(NT):
            s = t % 2
            q = stt[s]
            A(DVE, lambda e, t=t, q=q: e.bn_stats(out=q[:, 0:6], in_=x[:, t, 0:512]), r=[f"x{t}"], w=[f"lnst{s}"])
            A(DVE, lambda e, t=t, q=q: e.bn_stats(out=q[:, 6:12], in_=x[:, t, 512:1024]), r=[f"x{t}"], w=[f"lnst{s}"])
            A(DVE, lambda e, q=q: e.bn_aggr(out=q[:, 12:14], in_=q[:, 0:12]), r=[f"lnst{s}"], w=[f"lnst{s}"])
            A(ACT, lambda e, q=q: e.activation(out=q[:, 14:15], in_=q[:, 13:14], func=AF.Ln, bias=1e-5, scale=1.0),
              r=[f"lnst{s}"], w=[f"lnst{s}"])
            A(ACT, lambda e, q=q: e.activation(out=q[:, 14:15], in_=q[:, 14:15], func=AF.Exp, scale=-0.5),
              r=[f"lnst{s}"], w=[f"lnst{s}"])
            A(DVE, lambda e, q=q: e.scalar_tensor_tensor(out=q[:, 15:16], in0=q[:, 12:13], scalar=-1.0, in1=q[:, 14:15],
                                                         op0=ALU.mult, op1=ALU.mult), r=[f"lnst{s}"], w=[f"lnst{s}"])
            A(ACT, lambda e, t=t, q=q: e.activation(out=x[:, t, :], in_=x[:, t, :], func=AF.Identity,
                                                    bias=q[:, 15:16], scale=q[:, 14:15]),
              r=[f"x{t}", f"lnst{s}"], w=[f"x{t}"])
            A(DVE, lambda e, t=t: e.tensor_tensor(out=x[:, t, :], in0=x[:, t, :], in1=self.lng[:], op=ALU.mult),
              r=[f"x{t}", "lng"], w=[f"x{t}"])
            A(DVE, lambda e, t=t: e.tensor_tensor(out=x[:, t, :], in0=x[:, t, :], in1=self.lnb[:], op=ALU.add),
              r=[f"x{t}", "lnb"], w=[f"x{t}"])

    def out_proj(self, wo, wkey, srcT, nj, w_dram_rows, src_keys):
        A, x = self.A, self.x
        A(POOL, lambda e: e.dma_start(out=wo[:, 0:nj, :], in_=w_dram_rows.rearrange("(j p) n -> p j n", p=128)),
          w=[wkey], dma=wkey)
        for j in range(nj):
            A(DVE, lambda e, j=j: e.tensor_tensor(out=wo[:, j, :], in0=wo[:, j, :], in1=self.gate[:], op=ALU.mult),
              r=[wkey, "modt"], w=[wkey])
        for t in range(NT):
            for n in range(2):
                pb = 4 + (2 * t + n) % 2
                for j in range(nj):
                    A(PE, lambda e, t=t, n=n, j=j, pb=pb: e.matmul(
                        self.bank[pb][:], lhsT=srcT[:, j, t * 128:(t + 1) * 128], rhs=wo[:, j, n * 512:(n + 1) * 512],
                        start=(j == 0), stop=(j == nj - 1)), r=[wkey] + src_keys, w=[f"B{pb}"])
                A(DVE, lambda e, t=t, n=n, pb=pb: e.tensor_tensor(
                    out=x[:, t, n * 512:(n + 1) * 512], in0=self.bank[pb][:], in1=x[:, t, n * 512:(n + 1) * 512], op=ALU.add),
                  r=[f"B{pb}", f"x{t}"], w=[f"x{t}"])

    def ffn(self, li):
        from contextlib import ExitStack
        A, dr, x = self.A, self.dr, self.x
        with ExitStack() as st:
            hT = self.sb(st, "hT", [128, KC, S], BF16)
            with ExitStack() as st0:
                self.compute_mod(st0, li, 1)
                self.build_hT(st0, hT)
                self.P.flush()
            hkeys = [f"hT{t}" for t in range(NT)]
            cw = self.sb(st, "cw", [128, 3, NFC], F32)
            cbias = self.sb(st, "cbias", [128, NFC], F32)
            for wi in range(3):
                A(SP, lambda e, wi=wi: e.dma_start(out=cw[:, wi, :], in_=dr["ffn_conv_w"][li, wi:wi + 1, :].rearrange("o (j p) -> p (o j)", p=128),
                                                   allow_slow_non_contiguous=True), w=["cw"], dma="cw")
            A(SP, lambda e: e.dma_start(out=cbias[:], in_=dr["ffn_conv_b"][li:li + 1, :].rearrange("o (j p) -> p (o j)", p=128),
                                        allow_slow_non_contiguous=True), w=["cbias"], dma="cbias")
            wab = [self.sb(st, f"wab{i}", [128, KC, 2, 128], BF16) for i in range(2)]
            aS = [self.sb(st, f"aS{i}", [128, S + 2], F32) for i in range(2)]
            tS = [self.sb(st, f"tS{i}", [128, 512], F32) for i in range(2)]
            gT = [self.sb(st, f"gT{i}", [128, 4, S], BF16) for i in range(2)]
            wof = [self.sb(st, f"wof{i}", [128, 4, D], BF16) for i in range(2)]
            for i in range(2):
                A(POOL, lambda e, i=i: e.memset(aS[i][:, 0:2], 0.0), w=[f"aS{i}"])
            groups = [(0, 4), (4, 4), (8, 4), (12, 4), (16, 4), (20, 2)]
            it = 0
            pending_out = None
            for gi, (f0, nj) in enumerate(groups):
                gs = gi % 2
                for jj in range(nj):
                    j = f0 + jj
                    ws = j % 2
                    for half in range(2):
                        c0 = half * DFF + j * 128
                        A(POOL, lambda e, ws=ws, half=half, c0=c0: e.dma_start(
                            out=wab[ws][:, :, half, :], in_=dr["ffn_w_in"][li, :, c0:c0 + 128].rearrange("(k p) n -> p k n", p=128)),
                          w=[f"wab{ws}"], dma=f"wab{ws}")
                    for c in range(4):
                        s2 = it % 2
                        it += 1
                        pa, pbk = (0, 1, 7)[(it - 1) % 3], (2, 3, 6)[(it - 1) % 3]
                        for half, pbank in ((0, pa), (1, pbk)):
                            for k in range(KC):
                                A(PE, lambda e, ws=ws, half=half, pbank=pbank, k=k, c=c: e.matmul(
                                    self.bank[pbank][:], lhsT=wab[ws][:, k, half, :], rhs=hT[:, k, c * 512:(c + 1) * 512],
                                    start=(k == 0), stop=(k == KC - 1)),
                                  r=[f"wab{ws}"] + hkeys[4 * c:4 * c + 4], w=[f"B{pbank}"])
                        a_ = aS[ws]
                        A(ACT, lambda e, a_=a_, pa=pa, c=c: e.activation(out=a_[:, 2 + c * 512:2 + (c + 1) * 512],
                                                                         in_=self.bank[pa][:], func=AF.Copy),
                          r=[f"B{pa}"], w=[f"aS{ws}"])
                        A(DVE, lambda e, a_=a_, s2=s2, j=j, c=c: e.tensor_scalar(
                            out=tS[s2][:], in0=a_[:, c * 512:(c + 1) * 512], scalar1=cw[:, 0, j:j + 1],
                            scalar2=cbias[:, j:j + 1], op0=ALU.mult, op1=ALU.add),
                          r=[f"aS{ws}", "cw", "cbias"], w=[f"tS{s2}"])
                        A(DVE, lambda e, a_=a_, s2=s2, j=j, c=c: e.scalar_tensor_tensor(
                            out=tS[s2][:], in0=a_[:, 1 + c * 512:1 + (c + 1) * 512], scalar=cw[:, 1, j:j + 1], in1=tS[s2][:],
                            op0=ALU.mult, op1=ALU.add), r=[f"aS{ws}", "cw", f"tS{s2}"], w=[f"tS{s2}"])
                        A(DVE, lambda e, a_=a_, s2=s2, j=j, c=c: e.scalar_tensor_tensor(
                            out=tS[s2][:], in0=a_[:, 2 + c * 512:2 + (c + 1) * 512], scalar=cw[:, 2, j:j + 1], in1=tS[s2][:],
                            op0=ALU.mult, op1=ALU.add), r=[f"aS{ws}", "cw", f"tS{s2}"], w=[f"tS{s2}"])
                        A(ACT, lambda e, s2=s2: e.activation(out=tS[s2][:], in_=tS[s2][:], func=AF.Gelu_apprx_tanh),
                          r=[f"tS{s2}"], w=[f"tS{s2}"])
                        A(DVE, lambda e, s2=s2, gs=gs, jj=jj, c=c, pbk=pbk: e.tensor_tensor(
                            out=gT[gs][:, jj, c * 512:(c + 1) * 512], in0=self.bank[pbk][:], in1=tS[s2][:], op=ALU.mult),
                          r=[f"tS{s2}", f"B{pbk}"], w=[f"gT{gs}"])
                if pending_out is not None:
                    self.out_proj(*pending_out)
                pending_out = (wof[gs], f"wof{gs}", gT[gs], nj, dr["ffn_w_out"][li, f0 * 128:(f0 + nj) * 128, :], [f"gT{gs}"])
            self.out_proj(*pending_out)
            self.layer_norm(st, li, 1)
            self.P.flush()

    def layer(self, li):
        if isinstance(li, tuple):
            self.ffn(li[1])
            return
        kind = li % 4
        if kind == 0:
            self.mla(li)
        elif kind == 1:
            self.moba(li)
        elif kind == 2:
            self.nsa(li)
        else:
            self.sbmix(li)
        self.ffn(li)


_CONSTS = None


def kernel(**inputs):
    return run_kernel(inputs, layers=(0, 1, 2, 3))


def run_kernel(inputs, layers, cores=8, x_override=None, trace=False):
    global _CONSTS
    if _CONSTS is None:
        _CONSTS = host_consts()
    b = Builder(layers=layers)
    nc = b.build()
    in_maps = []
    xs = inputs["x"] if x_override is None else x_override
    for ci in range(cores):
        m = {"x": np.ascontiguousarray(xs[ci], dtype=np.float32),
             "c": np.ascontiguousarray(inputs["c"][ci:ci + 1], dtype=np.float32),
             "positions": np.ascontiguousarray(inputs["positions"][ci:ci + 1], dtype=np.int32)}
        for name, _ in WEIGHT_SPECS:
            m[name] = np.ascontiguousarray(inputs[name], dtype=np.float32)
        for name, _, _ in CONST_SPECS:
            m[name] = _CONSTS[name]
        in_maps.append(m)
    res = run_bass_kernel_spmd(nc, in_maps, core_ids=list(range(cores)), **({'trace': True} if trace else {}))
    if trace:
        print('EXEC_TIME_NS', res.exec_time_ns)
    return np.stack([r["out"] for r in res.results], axis=0).astype(np.float32)


def _mixer_prologue(self, st, li):
    from contextlib import ExitStack
    hT = self.sb(st, "hT", [128, KC, S], BF16)
    with ExitStack() as st0:
        self.compute_mod(st0, li, 0)
        self.build_hT(st0, hT)
        self.P.flush()
    self.sring = Ring("s", 4)
    self.sring_s = None
    return hT, [f"hT{t}" for t in range(NT)]


def _proj_qkv(self, w3, h, wq, wkey, qT, qkey, kT, kkey, V, vkey, hT, hkeys, kmean=None):
    A = self.A
    for which in range(3):
        A(POOL, lambda e, which=which: e.dma_start(out=wq[:, :, which, :], in_=w3[which].rearrange("(k p) n -> p k n", p=128)),
          w=[wkey], dma=wkey)
    for which, (dst, dkey) in enumerate(((qT, qkey), (kT, kkey))):
        for c in range(4):
            pb = self.sring.next()
            for k in range(KC):
                A(PE, lambda e, which=which, k=k, c=c, pb=pb: e.matmul(
                    self.bank[pb][:], lhsT=wq[:, k, which, :], rhs=hT[:, k, c * 512:(c + 1) * 512],
                    start=(k == 0), stop=(k == KC - 1)), r=[wkey] + hkeys[4 * c:4 * c + 4], w=[f"B{pb}"])
            if which == 0:
                A(ACT, lambda e, c=c, pb=pb, dst=dst: e.activation(out=dst[:, c * 512:(c + 1) * 512], in_=self.bank[pb][:], func=AF.Copy),
                  r=[f"B{pb}"], w=[dkey])
            else:
                A(DVE, lambda e, c=c, pb=pb, dst=dst: e.tensor_copy(out=dst[:, c * 512:(c + 1) * 512], in_=self.bank[pb][:]),
                  r=[f"B{pb}"], w=[dkey])
                if kmean is not None:
                    A(DVE, lambda e, c=c, pb=pb: e.tensor_reduce(
                        out=kmean[:, 2 * c:2 * c + 2], in_=self.bank[pb][:].rearrange("p (b n) -> p b n", b=2), axis=AX.X, op=ALU.add),
                      r=[f"B{pb}"], w=["kmean"])
    for tg in range(4):
        pb = self.sring.next()
        for tt in range(4):
            t = tg * 4 + tt
            for k in range(KC):
                A(PE, lambda e, k=k, t=t, tt=tt, pb=pb: e.matmul(
                    self.bank[pb][:, tt * 128:(tt + 1) * 128], lhsT=hT[:, k, t * 128:(t + 1) * 128], rhs=wq[:, k, 2, :],
                    start=(k == 0), stop=(k == KC - 1)), r=[wkey, hkeys[t]], w=[f"B{pb}"])
        eng = ACT if tg % 2 == 0 else DVE
        if eng == ACT:
            A(ACT, lambda e, tg=tg, pb=pb: e.activation(out=V[:, tg * 4:(tg + 1) * 4, 0:128],
                                                        in_=self.bank[pb][:].rearrange("p (a n) -> p a n", a=4), func=AF.Copy),
              r=[f"B{pb}"], w=[vkey])
        else:
            A(DVE, lambda e, tg=tg, pb=pb: e.tensor_copy(out=V[:, tg * 4:(tg + 1) * 4, 0:128],
                                                         in_=self.bank[pb][:].rearrange("p (a n) -> p a n", a=4)),
              r=[f"B{pb}"], w=[vkey])


def _o_to_oT(self, src_ap, src_keys, otok, okey, oT, jj, t, scale_ap=None, scale_keys=()):
    A = self.A
    if scale_ap is None:
        A(ACT, lambda e: e.activation(out=otok[:], in_=src_ap, func=AF.Copy), r=list(src_keys), w=[okey])
    else:
        A(ACT, lambda e: e.activation(out=otok[:], in_=src_ap, func=AF.Copy, scale=scale_ap),
          r=list(src_keys) + list(scale_keys), w=[okey])
    pb = self.sring.next()
    A(PE, lambda e, pb=pb: e.transpose(out=self.bankbf(pb)[:, 0:128], in_=otok[:], identity=self.cb[:, 0, :]),
      r=[okey, "cb"], w=[f"B{pb}"])
    A(ACT, lambda e, pb=pb: e.activation(out=oT[:, jj, t * 128:(t + 1) * 128], in_=self.bankbf(pb)[:, 0:128], func=AF.Copy),
      r=[f"B{pb}"], w=["oT"])


def _sbmix(self, li):
    from contextlib import ExitStack
    A, dr, cb = self.A, self.dr, self.cb
    scale = 128.0 ** -0.5
    with ExitStack() as st:
        hT, hkeys = _mixer_prologue(self, st, li)
        wq1 = self.sb(st, "wq0", [128, KC, 3, 128], BF16)
        wq = [wq1, wq1]
        qT = [self.sb(st, f"qT{i}", [128, S], BF16) for i in range(2)]
        kT = [self.sb(st, f"kT{i}", [128, S], BF16) for i in range(2)]
        V = [self.sb(st, f"V{i}", [128, NT, 128], BF16) for i in range(2)]
        oT = self.sb(st, "oT", [128, 4, S], BF16)
        wo = self.sb(st, "wo", [128, 4, D], BF16)
        SPl = [self.sb(st, f"SPl{i}", [128, 512], F32) for i in range(2)]
        Lb = [self.sb(st, f"Lb{i}", [128, 512], BF16) for i in range(2)]
        Lsum = self.sb(st, "Lsum", [128, 512], BF16)
        T1 = [self.sb(st, f"T1{i}", [128, 512], F32) for i in range(3)]
        AT = [self.sb(st, f"AT{i}", [128, 512], BF16) for i in range(2)]
        otok = [self.sb(st, f"otok{i}", [128, 128], BF16) for i in range(2)]
        w_in = dr["sb_w_in"][0]
        self.atn_it = 0
        self.nls = 0
        self.oi = 0
        def prep(h):
            s = h % 2
            w3 = [w_in[:, which * 1024 + h * 128: which * 1024 + (h + 1) * 128] for which in range(3)]
            _proj_qkv(self, w3, h, wq[s], "wq0", qT[s], f"qT{s}", kT[s], f"kT{s}", V[s], f"V{s}", hT, hkeys)

        prep(0)
        for h in range(8):
            s = h % 2
            stages = []
            for c in range(4):
                for kt in range(4 * c + 3, -1, -1):
                    sd = {}

                    def stage1(sd=sd, c=c, kt=kt, s=s):
                        r_ = kt - 4 * c
                        q0 = max(0, r_) * 128
                        diag = r_ >= 0
                        b = self.atn_it % 2
                        t3 = self.atn_it % 3
                        sd["b"], sd["t3"] = b, t3
                        self.atn_it += 1
                        pz = self.sring.next()
                        A(PE, lambda e: e.matmul(
                            self.bank[pz][:, q0:512], lhsT=kT[s][:, kt * 128:(kt + 1) * 128], rhs=qT[s][:, c * 512 + q0:(c + 1) * 512],
                            start=True, stop=True), r=[f"kT{s}", f"qT{s}"], w=[f"B{pz}"])
                        A(ACT, lambda e: e.activation(out=SPl[b][:, q0:512], in_=self.bank[pz][:, q0:512], func=AF.Exp, scale=scale),
                          r=[f"B{pz}"], w=[f"SPl{b}"])
                        A(ACT, lambda e: e.activation(out=Lb[b][:, q0:512], in_=SPl[b][:, q0:512], func=AF.Ln, bias=1.0, scale=1.0),
                          r=[f"SPl{b}"], w=[f"Lb{b}"])
                        sd["pz"] = pz

                    def stage1d(sd=sd, c=c, kt=kt, s=s):
                        b, t3, pz = sd["b"], sd["t3"], sd["pz"]
                        r_ = kt - 4 * c
                        q0 = max(0, r_) * 128
                        diag = r_ >= 0
                        A(DVE, lambda e: e.scalar_tensor_tensor(
                            out=T1[t3][:, q0:512], in0=self.bank[pz][:, q0:512], scalar=scale, in1=Lb[b][:, q0:512], op0=ALU.mult, op1=ALU.subtract),
                          r=[f"B{pz}", f"Lb{b}"], w=[f"T1{t3}"])
                        if diag:
                            A(DVE, lambda e: e.tensor_tensor(out=Lb[b][:, q0:q0 + 128], in0=Lb[b][:, q0:q0 + 128], in1=cb[:, 6, :], op=ALU.mult),
                              r=[f"Lb{b}", "cb"], w=[f"Lb{b}"])

                    def stage2a(sd=sd, c=c, kt=kt, s=s, h=h):
                        b, t3 = sd["b"], sd["t3"]
                        r_ = kt - 4 * c
                        q0 = max(0, r_) * 128
                        first = (kt == 4 * c + 3)
                        if first:
                            A(DVE, lambda e: e.memset(Lsum[:], 0.0), w=["Lsum"])
                        pt = self.sring.next()
                        A(PE, lambda e: e.matmul(
                            self.bank[pt][:, q0:512], lhsT=cb[:, 3, :], rhs=Lb[b][:, q0:512], start=True, stop=first),
                          r=["cb", f"Lb{b}"], w=[f"B{pt}"])
                        if not first:
                            A(PE, lambda e: e.matmul(
                                self.bank[pt][:, q0:512], lhsT=cb[:, 5, :], rhs=Lsum[:, q0:512], start=False, stop=True),
                              r=["cb", "Lsum"], w=[f"B{pt}"])
                        if kt > 0:
                            A(DVE, lambda e: e.tensor_tensor(out=Lsum[:, q0:512], in0=Lsum[:, q0:512], in1=Lb[b][:, q0:512], op=ALU.add),
                              r=["Lsum", f"Lb{b}"], w=["Lsum"])
                        A(DVE, lambda e: e.tensor_tensor(out=T1[t3][:, q0:512], in0=self.bank[pt][:, q0:512], in1=T1[t3][:, q0:512], op=ALU.add),
                          r=[f"B{pt}", f"T1{t3}"], w=[f"T1{t3}"])

                    def stage2b(sd=sd, c=c, kt=kt, s=s, h=h):
                        b, t3 = sd["b"], sd["t3"]
                        r_ = kt - 4 * c
                        q0 = max(0, r_) * 128
                        diag = r_ >= 0
                        A(ACT, lambda e: e.activation(out=AT[b][:, q0:512], in_=T1[t3][:, q0:512], func=AF.Exp),
                          r=[f"T1{t3}"], w=[f"AT{b}"])
                        if diag:
                            A(DVE, lambda e: e.tensor_tensor(out=AT[b][:, q0:q0 + 128], in0=AT[b][:, q0:q0 + 128], in1=cb[:, 6, :], op=ALU.mult),
                              r=[f"AT{b}", "cb"], w=[f"AT{b}"])

                    def stage2p(sd=sd, c=c, kt=kt, s=s, h=h):
                        b = sd["b"]
                        r_ = kt - 4 * c
                        q0 = max(0, r_) * 128
                        for i in range(q0 // 128, 4):
                            A(PE, lambda e, i=i: e.matmul(
                                self.bank[4 + i][:, 0:128], lhsT=AT[b][:, i * 128:(i + 1) * 128], rhs=V[s][:, kt, :],
                                start=(kt == 4 * c + i), stop=(kt == 0)), r=[f"AT{b}", f"V{s}"], w=[f"B{4 + i}"])
                        if kt == 0:
                            for i in range(4):
                                ob = self.oi % 2
                                self.oi += 1
                                _o_to_oT(self, self.bank[4 + i][:, 0:128], [f"B{4 + i}"], otok[ob], f"otok{ob}", oT, h % 4, 4 * c + i)
                    stages.append((stage2b, stage1, stage2a, stage1d, stage2p))
            nst = len(stages)
            for step in range(-2, nst):
                if step == nst // 2 and h < 7:
                    prep(h + 1)
                for si, off in enumerate((0, 2, 1, 2, 0)):
                    k = step + off
                    if 0 <= k < nst:
                        stages[k][si]()
            if h % 4 == 3:
                hg = h // 4
                self.out_proj(wo, "wo", oT, 4, dr["sb_w_o"][0, hg * 512:(hg + 1) * 512, :], ["oT"])
        self.layer_norm(st, li, 0)
        self.P.flush()


Builder.sbmix = _sbmix


def _attn_softmax(self, c_list, pairs, pkeys, V, vkey, scale, slope, items_of_chunk, maskmm, PT, TMP, DT, epi, far_ok=True, mid_hook=None):
    A, cb = self.A, self.cb
    nb = len(PT)
    stages = []
    for c in c_list:
        items = items_of_chunk(c)
        first_kt, last_kt = {}, {}
        for (kt, q0, q1, diag, far) in items:
            for i in range(q0 // 128, q1 // 128):
                first_kt.setdefault(i, kt)
                last_kt[i] = kt
        for idx, (kt, q0, q1, diag, far) in enumerate(items):
            s1, s2 = [], []

            st_ = {}

            def stage0(st_=st_, c=c, kt=kt, q0=q0, q1=q1):
                b = self.atn_it % nb
                self.atn_it += 1
                st_["b"] = b
                if slope is not None:
                    A(ACT, lambda e: e.activation(
                        out=DT[b][:, q0:q1], in_=self.posq[:, c * 512 + q0:c * 512 + q1], func=AF.Abs, bias=self.nposk[:, kt:kt + 1], scale=1.0),
                      r=["posq", "posk"], w=[f"DT{b}"])

            def stage1a(st_=st_, c=c, kt=kt, q0=q0, q1=q1, diag=diag, far=far):
                b = st_["b"]
                ps = (self.sring_s if (slope is None and self.sring_s is not None) else self.sring).next()
                st_["ps"] = ps
                mms = []
                for (kT_ap, qT_ap) in pairs:
                    mms.append((kT_ap[:, kt * 128:(kt + 1) * 128], qT_ap[:, c * 512 + q0:c * 512 + q1], q0, q1, list(pkeys)))
                if maskmm is not None:
                    lhs_fn, rhs_ap, mkeys = maskmm
                    mms.append((lhs_fn(kt), rhs_ap[:, c * 512 + q0:c * 512 + q1], q0, q1, list(mkeys)))
                if diag:
                    mms.append((cb[:, 0, :], cb[:, 1, :], q0, q0 + 128, ["cb"]))
                if far:
                    mms.append((cb[:, 0, :], cb[:, 2, :], q1 - 128, q1, ["cb"]))
                for mi, (lh, rh, a0, a1, keys) in enumerate(mms):
                    A(PE, lambda e, lh=lh, rh=rh, a0=a0, a1=a1, ps=ps, mi=mi, n=len(mms): e.matmul(
                        self.bank[ps][:, a0:a1], lhsT=lh, rhs=rh, start=(mi == 0), stop=(mi == n - 1)),
                      r=keys, w=[f"B{ps}"])
                if slope is not None:
                    A(DVE, lambda e: e.scalar_tensor_tensor(
                        out=TMP[b][:, q0:q1], in0=DT[b][:, q0:q1], scalar=-slope / scale, in1=self.bank[ps][:, q0:q1],
                        op0=ALU.mult, op1=ALU.add), r=[f"DT{b}", f"B{ps}"], w=[f"TMP{b}"])

            def stage1b(st_=st_, q0=q0, q1=q1):
                b, ps = st_["b"], st_["ps"]
                if slope is not None:
                    A(ACT, lambda e: e.activation(out=PT[b][:, q0:q1], in_=TMP[b][:, q0:q1], func=AF.Exp, scale=scale),
                      r=[f"TMP{b}"], w=[f"PT{b}"])
                else:
                    A(ACT, lambda e: e.activation(out=PT[b][:, q0:q1], in_=self.bank[ps][:, q0:q1], func=AF.Exp, scale=scale),
                      r=[f"B{ps}"], w=[f"PT{b}"])

            def stage2(st_=st_, c=c, kt=kt, q0=q0, q1=q1, last=(idx == len(items) - 1), first_kt=first_kt, last_kt=last_kt):
                b = st_["b"]
                for i in range(q0 // 128, q1 // 128):
                    A(PE, lambda e, i=i, s_=(kt == first_kt[i]), p_=(kt == last_kt[i]): e.matmul(
                        self.bank[4 + i][:, 0:129], lhsT=PT[b][:, i * 128:(i + 1) * 128], rhs=V[:, kt, 0:129], start=s_, stop=p_),
                      r=[f"PT{b}", vkey], w=[f"B{4 + i}"])
                if last:
                    for i in range(4):
                        if i in first_kt:
                            epi(c, i)
            stages.append((stage0, stage1a, stage1b, stage2))
    n = len(stages)
    offs = (3, 2, 1, 0)
    for step in range(-3, n):
        if mid_hook is not None and step == n // 2:
            mid_hook()
        for si, off in enumerate(offs):
            k = step + off
            if 0 <= k < n:
                stages[k][si]()


def _causal_items(c):
    out = []
    for kt in range(0, 4 * c + 4):
        r_ = kt - 4 * c
        out.append((kt, max(0, r_) * 128, 512, r_ >= 0, False))
    return out


def _moba(self, li):
    from contextlib import ExitStack
    A, dr, cb = self.A, self.dr, self.cb
    scale = 128.0 ** -0.5
    self.atn_it = 0
    with ExitStack() as st:
        hT, hkeys = _mixer_prologue(self, st, li)
        wq = self.sb(st, "wq0", [128, KC, 3, 128], BF16)
        qT = [self.sb(st, f"qT{i}", [128, S], BF16) for i in range(2)]
        kT = [self.sb(st, f"kT{i}", [128, S], BF16) for i in range(2)]
        V = [self.sb(st, f"V{i}", [128, NT, 129], BF16) for i in range(2)]
        oT = self.sb(st, "oT", [128, 4, S], BF16)
        wo = self.sb(st, "wo", [128, 4, D], BF16)
        PT = [self.sb(st, f"PT{i}", [128, 512], BF16) for i in range(2)]
        TMP = [self.sb(st, f"TMP{i}", [128, 512], F32) for i in range(2)]
        DT = [self.sb(st, f"DT{i}", [128, 512], F32) for i in range(2)]
        otok = [self.sb(st, f"otok{i}", [128, 128], BF16) for i in range(2)]
        rden = [self.sb(st, f"rden{i}", [128, 1], F32) for i in range(2)]
        e8t = self.sb(st, "e8t", [128, 1024], BF16)
        mneg = self.sb(st, "mneg", [128, 512], F32)
        mnot = self.sb(st, "mnot", [128, 512], F32)
        kmean = self.sb(st, "kmean", [128, 32], F32)
        kmh = self.sb(st, "kmh", [128, 32], BF16)
        kml = self.sb(st, "kml", [128, 32], BF16)
        kmr = self.sb(st, "kmr", [128, 32], F32)
        gm = self.sb(st, "gm", [128, 512], F32)
        m8 = self.sb(st, "m8", [128, 128], F32)
        sel = self.sb(st, "sel", [128, 512], F32)
        nbb = self.sb(st, "nbb", [128, 512], BF16)
        selbT = [self.sb(st, f"selbT{i}", [128, S], BF16) for i in range(2)]
        A(SP, lambda e: e.dma_start(out=e8t[:], in_=dr["e8"]), w=["e8t"], dma="e8t")
        A(SP, lambda e: e.dma_start(out=mneg[:], in_=dr["moba_neg"]), w=["mneg"], dma="mneg")
        A(SP, lambda e: e.dma_start(out=mnot[:], in_=dr["moba_notown"]), w=["mnot"], dma="mnot")
        for s in range(2):
            A(POOL, lambda e, s=s: e.memset(V[s][:, :, 128:129], 1.0), w=[f"V{s}"])
        A(POOL, lambda e: e.memset(kmean[:], 0.0), w=["kmean"])
        for i in range(2):
            A(DVE, lambda e, i=i: e.memset(selbT[i][:], 0.0), w=[f"selbT{i}"])
        w_in = dr["moba_w_in"][0]
        oi = [0]
        def prep(h):
            s = h % 2
            w3 = [w_in[:, which * 1024 + h * 128: which * 1024 + (h + 1) * 128] for which in range(3)]
            _proj_qkv(self, w3, h, wq, "wq0", qT[s], f"qT{s}", kT[s], f"kT{s}", V[s], f"V{s}", hT, hkeys, kmean=kmean)
            A(DVE, lambda e: e.tensor_copy(out=kmh[:], in_=kmean[:]), r=["kmean"], w=["kmh"])
            A(DVE, lambda e: e.tensor_tensor(out=kmr[:], in0=kmean[:], in1=kmh[:], op=ALU.subtract), r=["kmean", "kmh"], w=["kmr"])
            A(DVE, lambda e: e.tensor_copy(out=kml[:], in_=kmr[:]), r=["kmr"], w=["kml"])
            pg = self.sring.next()
            for t in range(NT):
                A(PE, lambda e, t=t, s=s, pg=pg: e.matmul(self.bank[pg][:, t * 32:(t + 1) * 32], lhsT=qT[s][:, t * 128:(t + 1) * 128],
                                                          rhs=kmh[:], start=True, stop=False), r=[f"qT{s}", "kmh"], w=[f"B{pg}"])
                A(PE, lambda e, t=t, s=s, pg=pg: e.matmul(self.bank[pg][:, t * 32:(t + 1) * 32], lhsT=qT[s][:, t * 128:(t + 1) * 128],
                                                          rhs=kml[:], start=False, stop=True), r=[f"qT{s}", "kml"], w=[f"B{pg}"])
            A(DVE, lambda e, pg=pg: e.tensor_tensor(out=gm[:], in0=self.bank[pg][:], in1=mneg[:], op=ALU.add),
              r=[f"B{pg}", "mneg"], w=["gm"])
            for t in range(NT):
                A(DVE, lambda e, t=t: e.max(out=m8[:, t * 8:(t + 1) * 8], in_=gm[:, t * 32:(t + 1) * 32]), r=["gm"], w=["m8"])
            for t in range(NT):
                A(DVE, lambda e, t=t: e.tensor_scalar(out=sel[:, t * 32:(t + 1) * 32], in0=gm[:, t * 32:(t + 1) * 32],
                                                      scalar1=m8[:, t * 8 + 2:t * 8 + 3], scalar2=None, op0=ALU.is_ge),
                  r=["gm", "m8"], w=["sel"])
            A(DVE, lambda e: e.tensor_scalar(out=sel[:], in0=sel[:], scalar1=-NEGB, scalar2=NEGB, op0=ALU.mult, op1=ALU.add),
              r=["sel"], w=["sel"])
            A(DVE, lambda e: e.tensor_tensor(out=nbb[:], in0=sel[:], in1=mnot[:], op=ALU.mult), r=["sel", "mnot"], w=["nbb"])
            for half in range(2):
                pb = self.sring.next()
                for tt in range(8):
                    t = half * 8 + tt
                    A(PE, lambda e, t=t, tt=tt, pb=pb: e.transpose(out=self.bankbf(pb)[0:32, tt * 128:(tt + 1) * 128],
                                                                  in_=nbb[:, t * 32:(t + 1) * 32], identity=cb[:, 0, :]),
                      r=["nbb", "cb"], w=[f"B{pb}"])
                A(ACT, lambda e, half=half, pb=pb: e.activation(out=selbT[s][0:32, half * 1024:(half + 1) * 1024],
                                                                in_=self.bankbf(pb)[0:32, :], func=AF.Copy),
                  r=[f"B{pb}"], w=[f"selbT{s}"])


        prep(0)
        for h in range(8):
            s = h % 2
            slope = 2.0 ** (-(h + 1))
            def epi(c, i, h=h, s=s):
                ob = oi[0] % 2
                oi[0] += 1
                A(DVE, lambda e, ob=ob, i=i: e.reciprocal(out=rden[ob][:], in_=self.bank[4 + i][:, 128:129]),
                  r=[f"B{4 + i}"], w=[f"rden{ob}"])
                _o_to_oT(self, self.bank[4 + i][:, 0:128], [f"B{4 + i}"], otok[ob], f"otok{ob}", oT, h % 4, 4 * c + i,
                         scale_ap=rden[ob][:, 0:1], scale_keys=[f"rden{ob}"])

            _attn_softmax(self, range(4), [(kT[s], qT[s])], [f"kT{s}", f"qT{s}"], V[s], f"V{s}", scale, slope, _causal_items,
                          (lambda kt: e8t[:, (kt // 2) * 128:(kt // 2 + 1) * 128], selbT[s], ["e8t", f"selbT{s}"]), PT, TMP, DT, epi,
                          mid_hook=((lambda h=h: prep(h + 1)) if h < 7 else None))
            if h % 4 == 3:
                hg = h // 4
                self.out_proj(wo, "wo", oT, 4, dr["moba_w_o"][0, hg * 512:(hg + 1) * 512, :], ["oT"])
        self.layer_norm(st, li, 0)
        self.P.flush()


Builder.moba = _moba


def _rope_ops(self, x1, x2, cos, sin, o1, o2, R, rkeys, in_keys, okey):
    A = self.A
    A(DVE, lambda e: e.tensor_tensor(out=R[0], in0=x1, in1=cos, op=ALU.mult), r=in_keys + ["rope"], w=[rkeys[0]])
    A(DVE, lambda e: e.tensor_tensor(out=R[1], in0=x2, in1=sin, op=ALU.mult), r=in_keys + ["rope"], w=[rkeys[1]])
    A(DVE, lambda e: e.tensor_tensor(out=o1, in0=R[0], in1=R[1], op=ALU.subtract), r=[rkeys[0], rkeys[1]], w=[okey])
    A(DVE, lambda e: e.tensor_tensor(out=R[2], in0=x2, in1=cos, op=ALU.mult), r=in_keys + ["rope"], w=[rkeys[2]])
    A(DVE, lambda e: e.tensor_tensor(out=R[3], in0=x1, in1=sin, op=ALU.mult), r=in_keys + ["rope"], w=[rkeys[3]])
    A(DVE, lambda e: e.tensor_tensor(out=o2, in0=R[2], in1=R[3], op=ALU.add), r=[rkeys[2], rkeys[3]], w=[okey])


def _mla(self, li):
    from contextlib import ExitStack
    A, dr, cb = self.A, self.dr, self.cb
    scale = 192.0 ** -0.5
    self.atn_it = 0
    with ExitStack() as st:
        wuq = self.sb(st, "wuq", [128, 2, 1536], BF16)
        wukv = self.sb(st, "wukv", [128, 2, 2048], BF16)
        cos_t = self.sb(st, "cos_t", [128, NT, 32], F32)
        sin_t = self.sb(st, "sin_t", [128, NT, 32], F32)
        c_qT = self.sb(st, "c_qT", [128, 2, S], BF16)
        c_kvT = self.sb(st, "c_kvT", [128, 2, S], BF16)
        krT = self.sb(st, "krT", [128, S], BF16)
        gains = self.sb(st, "gains", [128, 4], F32)
        Rt = self.sb(st, "Rt", [128, 4, 128], F32)
        with ExitStack() as sth:
            hT, hkeys = _mixer_prologue(self, sth, li)
            w_in = self.sb(sth, "mlawin", [128, KC, 576], BF16)
            ang = self.sb(sth, "ang", [128, NT, 32], F32)
            invf = self.sb(sth, "invf", [128, 32], F32)
            junk = self.sb(sth, "junk", [128, 256], F32)
            ss = [self.sb(sth, f"ss{i}", [128, 4], F32) for i in range(2)]
            cn = [self.sb(sth, f"cn{i}", [128, 512], BF16) for i in range(2)]
            kr = [self.sb(sth, f"kr{i}", [128, 64], BF16) for i in range(2)]
            A(POOL, lambda e: e.dma_start(out=w_in[:], in_=dr["mla_w_in"][0].rearrange("(k p) n -> p k n", p=128)), w=["mlawin"], dma="mlawin")
            A(POOL, lambda e: e.dma_start(out=wuq[:], in_=dr["mla_w_uq"][0].rearrange("(k p) n -> p k n", p=128)), w=["wuq"], dma="wuq")
            for hf in range(2):
                A(POOL, lambda e, hf=hf: e.dma_start(out=wukv[:, :, hf * 1024:(hf + 1) * 1024],
                                                     in_=dr["mla_w_ukv"][0][:, hf * 1024:(hf + 1) * 1024].rearrange("(k p) n -> p k n", p=128)),
                  w=["wukv"], dma="wukv")
            A(SP, lambda e: e.dma_start(out=invf[:], in_=dr["invf"]), w=["invf"], dma="invf")
            A(SP, lambda e: e.dma_start(out=gains[:, 0:2], in_=dr["mla_q_norm"][0:1, :].rearrange("o (j p) -> p (o j)", p=128),
                                        allow_slow_non_contiguous=True), w=["gains"], dma="gains")
            A(SP, lambda e: e.dma_start(out=gains[:, 2:4], in_=dr["mla_kv_norm"][0:1, :].rearrange("o (j p) -> p (o j)", p=128),
                                        allow_slow_non_contiguous=True), w=["gains"], dma="gains")
            for t in range(NT):
                A(DVE, lambda e, t=t: e.tensor_scalar(out=ang[:, t, :], in0=invf[:], scalar1=self.posk[:, t:t + 1], scalar2=None, op0=ALU.mult),
                  r=["invf", "posk"], w=["ang"])
            ki = self.sb(sth, "ki", [128, NT, 32], I32)
            kf = self.sb(sth, "kf", [128, NT, 32], F32)
            mk = self.sb(sth, "mk", [128, NT, 32], F32)
            C1, C2 = 6.28125, 2 * np.pi - 6.28125
            for dst, shift in ((sin_t, 0.0), (cos_t, 0.5 * PI)):
                A(DVE, lambda e, dst=dst, shift=shift: e.tensor_scalar(out=dst[:], in0=ang[:], scalar1=shift, scalar2=None, op0=ALU.add), r=["ang"], w=["rope"])
                A(DVE, lambda e, dst=dst: e.tensor_scalar(out=kf[:], in0=dst[:], scalar1=float(1.0 / (2 * np.pi)), scalar2=None, op0=ALU.mult), r=["rope"], w=["kf"])
                A(DVE, lambda e: e.tensor_copy(out=ki[:], in_=kf[:]), r=["kf"], w=["ki"])
                A(DVE, lambda e: e.tensor_copy(out=kf[:], in_=ki[:]), r=["ki"], w=["kf"])
                A(DVE, lambda e, dst=dst: e.scalar_tensor_tensor(out=dst[:], in0=kf[:], scalar=-C1, in1=dst[:], op0=ALU.mult, op1=ALU.add), r=["kf", "rope"], w=["rope"])
                A(DVE, lambda e, dst=dst: e.scalar_tensor_tensor(out=dst[:], in0=kf[:], scalar=-C2, in1=dst[:], op0=ALU.mult, op1=ALU.add), r=["kf", "rope"], w=["rope"])
                A(DVE, lambda e, dst=dst: e.tensor_scalar(out=mk[:], in0=dst[:], scalar1=PI, scalar2=None, op0=ALU.is_gt), r=["rope"], w=["mk"])
                A(DVE, lambda e, dst=dst: e.scalar_tensor_tensor(out=dst[:], in0=mk[:], scalar=-2 * PI, in1=dst[:], op0=ALU.mult, op1=ALU.add), r=["mk", "rope"], w=["rope"])
                A(DVE, lambda e, dst=dst: e.tensor_scalar(out=mk[:], in0=dst[:], scalar1=-PI, scalar2=None, op0=ALU.is_lt), r=["rope"], w=["mk"])
                A(DVE, lambda e, dst=dst: e.scalar_tensor_tensor(out=dst[:], in0=mk[:], scalar=2 * PI, in1=dst[:], op0=ALU.mult, op1=ALU.add), r=["mk", "rope"], w=["rope"])
                A(DVE, lambda e, dst=dst: e.tensor_scalar(out=dst[:], in0=dst[:], scalar1=-3.1415925, scalar2=3.1415925, op0=ALU.max, op1=ALU.min), r=["rope"], w=["rope"])
            A(ACT, lambda e: e.activation(out=sin_t[:], in_=sin_t[:], func=AF.Sin), r=["rope"], w=["rope"])
            A(ACT, lambda e: e.activation(out=cos_t[:], in_=cos_t[:], func=AF.Sin), r=["rope"], w=["rope"])
            for t in range(NT):
                b = t % 2
                pa, pb2 = self.sring.next(), self.sring.next()
                for k in range(KC):
                    A(PE, lambda e, k=k, t=t, pa=pa: e.matmul(self.bank[pa][:], lhsT=hT[:, k, t * 128:(t + 1) * 128], rhs=w_in[:, k, 0:512],
                                                              start=(k == 0), stop=(k == KC - 1)), r=[hkeys[t], "mlawin"], w=[f"B{pa}"])
                for k in range(KC):
                    A(PE, lambda e, k=k, t=t, pb2=pb2: e.matmul(self.bank[pb2][:, 0:64], lhsT=hT[:, k, t * 128:(t + 1) * 128], rhs=w_in[:, k, 512:576],
                                                                start=(k == 0), stop=(k == KC - 1)), r=[hkeys[t], "mlawin"], w=[f"B{pb2}"])
                for j in range(2):
                    A(ACT, lambda e, j=j, pa=pa, b=b: e.activation(out=junk[:], in_=self.bank[pa][:, j * 256:(j + 1) * 256], func=AF.Square,
                                                                   accum_out=ss[b][:, j:j + 1]), r=[f"B{pa}"], w=["junk", f"ss{b}"])
                A(ACT, lambda e, b=b: e.activation(out=ss[b][:, 2:4], in_=ss[b][:, 0:2], func=AF.Ln, bias=1e-6, scale=1.0 / 256.0), r=[f"ss{b}"], w=[f"ss{b}"])
                A(ACT, lambda e, b=b: e.activation(out=ss[b][:, 2:4], in_=ss[b][:, 2:4], func=AF.Exp, scale=-0.5), r=[f"ss{b}"], w=[f"ss{b}"])
                for j in range(2):
                    A(DVE, lambda e, j=j, pa=pa, b=b: e.tensor_scalar(out=cn[b][:, j * 256:(j + 1) * 256], in0=self.bank[pa][:, j * 256:(j + 1) * 256],
                                                                      scalar1=ss[b][:, 2 + j:3 + j], scalar2=None, op0=ALU.mult),
                      r=[f"B{pa}", f"ss{b}"], w=[f"cn{b}"])
                pt = self.sring.next()
                for j in range(4):
                    A(PE, lambda e, j=j, b=b, pt=pt: e.transpose(out=self.bankbf(pt)[:, j * 128:(j + 1) * 128], in_=cn[b][:, j * 128:(j + 1) * 128],
                                                                 identity=cb[:, 0, :]), r=[f"cn{b}", "cb"], w=[f"B{pt}"])
                for j in range(4):
                    dst = c_qT if j < 2 else c_kvT
                    A(DVE, lambda e, j=j, t=t, pt=pt, dst=dst: e.tensor_scalar(
                        out=dst[:, j % 2, t * 128:(t + 1) * 128], in0=self.bankbf(pt)[:, j * 128:(j + 1) * 128], scalar1=gains[:, j:j + 1],
                        scalar2=None, op0=ALU.mult), r=[f"B{pt}", "gains"], w=["c_qT" if j < 2 else "c_kvT"])
                _rope_ops(self, self.bank[pb2][:, 0:32], self.bank[pb2][:, 32:64], cos_t[:, t, :], sin_t[:, t, :],
                          kr[b][:, 0:32], kr[b][:, 32:64], [Rt[:, i, 0:32] for i in range(4)], [f"Rt{i}" for i in range(4)], [f"B{pb2}"], f"kr{b}")
                pk = self.sring.next()
                A(PE, lambda e, b=b, pk=pk: e.transpose(out=self.bankbf(pk)[0:64, 0:128], in_=kr[b][:], identity=cb[:, 0, :]), r=[f"kr{b}", "cb"], w=[f"B{pk}"])
                A(ACT, lambda e, t=t, pk=pk: e.activation(out=krT[0:64, t * 128:(t + 1) * 128], in_=self.bankbf(pk)[0:64, 0:128], func=AF.Copy),
                  r=[f"B{pk}"], w=["krT"])
            self.P.flush()
        self.sring = Ring("m", [2, 3])
        self.sring_s = Ring("sc", [0, 1])
        qnT = [self.sb(st, f"qnT{i}", [128, S], BF16) for i in range(2)]
        qrT = [self.sb(st, f"qrT{i}", [128, S], BF16) for i in range(2)]
        knT = [self.sb(st, f"knT{i}", [128, S], BF16) for i in range(2)]
        V = [self.sb(st, f"V{i}", [128, NT, 129], BF16) for i in range(2)]
        oT = self.sb(st, "oT", [128, 4, S], BF16)
        wo = self.sb(st, "wo", [128, 4, D], BF16)
        PT = [self.sb(st, f"PT{i}", [128, 512], BF16) for i in range(2)]
        otok = [self.sb(st, f"otok{i}", [128, 128], BF16) for i in range(2)]
        rden = [self.sb(st, f"rden{i}", [128, 1], F32) for i in range(2)]
        qr = [self.sb(st, f"qr{i}", [128, 4, 64], BF16) for i in range(2)]
        for s in range(2):
            A(POOL, lambda e, s=s: e.memset(V[s][:, :, 128:129], 1.0), w=[f"V{s}"])
            A(DVE, lambda e, s=s: e.memset(qrT[s][64:128, :], 0.0), w=[f"qrT{s}"])
        A(DVE, lambda e: e.memset(krT[64:128, :], 0.0), w=["krT"])
        oi = [0]
        qi = 0
        def prep(h):
            nonlocal qi
            s = h % 2
            for c in range(4):
                pb = self.sring.next()
                for k in range(2):
                    A(PE, lambda e, k=k, c=c, pb=pb, h=h: e.matmul(self.bank[pb][:], lhsT=wuq[:, k, h * 192:h * 192 + 128], rhs=c_qT[:, k, c * 512:(c + 1) * 512],
                                                                   start=(k == 0), stop=(k == 1)), r=["wuq", "c_qT"], w=[f"B{pb}"])
                A(ACT, lambda e, c=c, pb=pb, s=s: e.activation(out=qnT[s][:, c * 512:(c + 1) * 512], in_=self.bank[pb][:], func=AF.Copy), r=[f"B{pb}"], w=[f"qnT{s}"])
                pb = self.sring.next()
                for k in range(2):
                    A(PE, lambda e, k=k, c=c, pb=pb, h=h: e.matmul(self.bank[pb][:], lhsT=wukv[:, k, h * 256:h * 256 + 128], rhs=c_kvT[:, k, c * 512:(c + 1) * 512],
                                                                   start=(k == 0), stop=(k == 1)), r=["wukv", "c_kvT"], w=[f"B{pb}"])
                A(DVE, lambda e, c=c, pb=pb, s=s: e.tensor_copy(out=knT[s][:, c * 512:(c + 1) * 512], in_=self.bank[pb][:]), r=[f"B{pb}"], w=[f"knT{s}"])
            for tg in range(4):
                pb = self.sring.next()
                for tt in range(4):
                    t = tg * 4 + tt
                    for k in range(2):
                        A(PE, lambda e, k=k, t=t, tt=tt, pb=pb, h=h: e.matmul(
                            self.bank[pb][:, tt * 128:(tt + 1) * 128], lhsT=c_kvT[:, k, t * 128:(t + 1) * 128], rhs=wukv[:, k, h * 256 + 128:h * 256 + 256],
                            start=(k == 0), stop=(k == 1)), r=["wukv", "c_kvT"], w=[f"B{pb}"])
                A(ACT, lambda e, tg=tg, pb=pb, s=s: e.activation(out=V[s][:, tg * 4:(tg + 1) * 4, 0:128],
                                                                 in_=self.bank[pb][:].rearrange("p (a n) -> p a n", a=4), func=AF.Copy), r=[f"B{pb}"], w=[f"V{s}"])
                pq = self.sring.next()
                for tt in range(4):
                    t = tg * 4 + tt
                    for k in range(2):
                        A(PE, lambda e, k=k, t=t, tt=tt, pq=pq, h=h: e.matmul(
                            self.bank[pq][:, tt * 64:(tt + 1) * 64], lhsT=c_qT[:, k, t * 128:(t + 1) * 128], rhs=wuq[:, k, h * 192 + 128:h * 192 + 192],
                            start=(k == 0), stop=(k == 1)), r=["wuq", "c_qT"], w=[f"B{pq}"])
                qb = qi % 2
                qi += 1
                xv = self.bank[pq][:, 0:256].rearrange("p (a n) -> p a n", a=4)
                _rope_ops(self, xv[:, :, 0:32], xv[:, :, 32:64], cos_t[:, tg * 4:(tg + 1) * 4, :], sin_t[:, tg * 4:(tg + 1) * 4, :],
                          qr[qb][:, :, 0:32], qr[qb][:, :, 32:64], [Rt[:, i, :].rearrange("p (a n) -> p a n", a=4) for i in range(4)],
                          [f"Rt{i}" for i in range(4)], [f"B{pq}"], f"qr{qb}")
                pt = self.sring.next()
                for tt in range(4):
                    A(PE, lambda e, tt=tt, qb=qb, pt=pt: e.transpose(out=self.bankbf(pt)[0:64, tt * 128:(tt + 1) * 128], in_=qr[qb][:, tt, :],
                                                                    identity=cb[:, 0, :]), r=[f"qr{qb}", "cb"], w=[f"B{pt}"])
                A(ACT, lambda e, tg=tg, pt=pt, s=s: e.activation(out=qrT[s][0:64, tg * 512:(tg + 1) * 512], in_=self.bankbf(pt)[0:64, 0:512], func=AF.Copy),
                  r=[f"B{pt}"], w=[f"qrT{s}"])


        prep(0)
        for h in range(8):
            s = h % 2
            def epi(c, i, h=h):
                ob = oi[0] % 2
                oi[0] += 1
                A(DVE, lambda e, ob=ob, i=i: e.reciprocal(out=rden[ob][:], in_=self.bank[4 + i][:, 128:129]), r=[f"B{4 + i}"], w=[f"rden{ob}"])
                _o_to_oT(self, self.bank[4 + i][:, 0:128], [f"B{4 + i}"], otok[ob], f"otok{ob}", oT, h % 4, 4 * c + i,
                         scale_ap=rden[ob][:, 0:1], scale_keys=[f"rden{ob}"])

            _attn_softmax(self, range(4), [(knT[s], qnT[s]), (krT, qrT[s])], [f"knT{s}", f"qnT{s}", "krT", f"qrT{s}"], V[s], f"V{s}",
                          scale, None, _causal_items, None, PT, None, None, epi,
                          mid_hook=((lambda h=h: prep(h + 1)) if h < 7 else None))
            if h % 4 == 3:
                hg = h // 4
                self.out_proj(wo, "wo", oT, 4, dr["mla_w_o"][0, hg * 512:(hg + 1) * 512, :], ["oT"])
        self.layer_norm(st, li, 0)
        self.P.flush()


Builder.mla = _mla


def _proj_fm(self, wcols, wsl, wring, dst, dkey, hT, hkeys, use_act):
    A = self.A
    ws = wring.next()
    A(POOL, lambda e: e.dma_start(out=wsl[ws][:], in_=wcols.rearrange("(k p) n -> p k n", p=128)), w=[f"wsl{ws}"], dma=f"wsl{ws}")
    for c in range(4):
        pb = self.sring.next()
        for k in range(KC):
            A(PE, lambda e, k=k, c=c, pb=pb: e.matmul(self.bank[pb][:], lhsT=wsl[ws][:, k, :], rhs=hT[:, k, c * 512:(c + 1) * 512],
                                                      start=(k == 0), stop=(k == KC - 1)), r=[f"wsl{ws}"] + hkeys[4 * c:4 * c + 4], w=[f"B{pb}"])
        if use_act:
            A(ACT, lambda e, c=c, pb=pb: e.activation(out=dst[:, c * 512:(c + 1) * 512], in_=self.bank[pb][:], func=AF.Copy), r=[f"B{pb}"], w=[dkey])
        else:
            A(DVE, lambda e, c=c, pb=pb: e.tensor_copy(out=dst[:, c * 512:(c + 1) * 512], in_=self.bank[pb][:]), r=[f"B{pb}"], w=[dkey])


def _proj_tm(self, wcols, wsl, wring, V, vkey, hT, hkeys):
    A = self.A
    ws = wring.next()
    A(POOL, lambda e: e.dma_start(out=wsl[ws][:], in_=wcols.rearrange("(k p) n -> p k n", p=128)), w=[f"wsl{ws}"], dma=f"wsl{ws}")
    for tg in range(4):
        pb = self.sring.next()
        for tt in range(4):
            t = tg * 4 + tt
            for k in range(KC):
                A(PE, lambda e, k=k, t=t, tt=tt, pb=pb: e.matmul(self.bank[pb][:, tt * 128:(tt + 1) * 128], lhsT=hT[:, k, t * 128:(t + 1) * 128],
                                                                rhs=wsl[ws][:, k, :], start=(k == 0), stop=(k == KC - 1)),
                  r=[f"wsl{ws}", hkeys[t]], w=[f"B{pb}"])
        if tg % 2 == 0:
            A(ACT, lambda e, tg=tg, pb=pb: e.activation(out=V[:, tg * 4:(tg + 1) * 4, 0:128], in_=self.bank[pb][:].rearrange("p (a n) -> p a n", a=4),
                                                        func=AF.Copy), r=[f"B{pb}"], w=[vkey])
        else:
            A(DVE, lambda e, tg=tg, pb=pb: e.tensor_copy(out=V[:, tg * 4:(tg + 1) * 4, 0:128], in_=self.bank[pb][:].rearrange("p (a n) -> p a n", a=4)),
              r=[f"B{pb}"], w=[vkey])


def _win_items(c):
    out = []
    for kt in range(max(0, 4 * c - 4), 4 * c + 4):
        r_ = kt - 4 * c
        i_lo, i_hi = max(0, r_), min(3, r_ + 4)
        out.append((kt, i_lo * 128, (i_hi + 1) * 128, r_ >= 0, r_ + 4 <= 3))
    return out


def _nsa(self, li):
    from contextlib import ExitStack
    A, dr, cb = self.A, self.dr, self.cb
    scale = 128.0 ** -0.5
    self.atn_it = 0
    w_in = dr["nsa_w_in"][0]
    with ExitStack() as st:
        hT, hkeys = _mixer_prologue(self, st, li)
        oT = self.sb(st, "oT", [128, 4, S], BF16)
        gates = self.sb(st, "gates", [128, NT * 24], F32)
        wsl = [self.sb(st, f"wsl{i}", [128, KC, 128], BF16) for i in range(2)]
        wring = Ring("w", 2)
        with ExitStack() as sg:
            wg = self.sb(sg, "wg", [128, KC, 24], BF16)
            A(POOL, lambda e: e.dma_start(out=wg[:], in_=w_in[:, 2560:2584].rearrange("(k p) n -> p k n", p=128)), w=["wg"], dma="wg")
            pb = self.sring.next()
            for t in range(NT):
                for k in range(KC):
                    A(PE, lambda e, k=k, t=t, pb=pb: e.matmul(self.bank[pb][:, t * 24:(t + 1) * 24], lhsT=hT[:, k, t * 128:(t + 1) * 128], rhs=wg[:, k, :],
                                                              start=(k == 0), stop=(k == KC - 1)), r=["wg", hkeys[t]], w=[f"B{pb}"])
            A(ACT, lambda e, pb=pb: e.activation(out=gates[:], in_=self.bank[pb][:, 0:NT * 24], func=AF.Sigmoid), r=[f"B{pb}"], w=["gates"])
            self.P.flush()
        for g in range(2):
            with ExitStack() as sgp:
                qT = [self.sb(sgp, f"qT{i}", [128, S], BF16) for i in range(4)]
                kslT = self.sb(sgp, "kslT", [128, S], BF16)
                kwnT = self.sb(sgp, "kwnT", [128, S], BF16)
                vsl = self.sb(sgp, "vsl", [128, NT, 129], BF16)
                vwn = self.sb(sgp, "vwn", [128, NT, 129], BF16)
                ocs = [self.sb(sgp, f"ocs{i}", [128, NT, 128], BF16) for i in range(4)]
                nbT = self.sb(sgp, "nbT", [128, S], BF16)
                A(DVE, lambda e: e.memset(nbT[:], 0.0), w=["nbT"])
                A(POOL, lambda e: e.memset(vsl[:, :, 128:129], 1.0), w=["vsl"])
                A(POOL, lambda e: e.memset(vwn[:, :, 128:129], 1.0), w=["vwn"])
                for r in range(4):
                    hh = g * 4 + r
                    _proj_fm(self, w_in[:, hh * 128:(hh + 1) * 128], wsl, wring, qT[r], f"qT{r}", hT, hkeys, r % 2 == 0)
                kvc = lambda which: w_in[:, 1024 + which * 256 + g * 128: 1024 + which * 256 + (g + 1) * 128]
                _proj_fm(self, kvc(2), wsl, wring, kslT, "kslT", hT, hkeys, True)
                _proj_tm(self, kvc(3), wsl, wring, vsl, "vsl", hT, hkeys)
                _proj_fm(self, kvc(4), wsl, wring, kwnT, "kwnT", hT, hkeys, False)
                _proj_tm(self, kvc(5), wsl, wring, vwn, "vwn", hT, hkeys)
                with ExitStack() as sc:
                    kcT = self.sb(sc, "kcT", [128, 128], BF16)
                    vc = self.sb(sc, "vc", [128, 128], BF16)
                    with ExitStack() as sca:
                        rawT = [self.sb(sca, f"rawT{i}", [128, S], BF16) for i in range(2)]
                        w1b = self.sb(sca, "w1b", [128, 32, 128], BF16)
                        w2b = self.sb(sca, "w2b", [128, 128], BF16)
                        peT = self.sb(sca, "peT", [128, 32], F32)
                        peTb = self.sb(sca, "peTb", [128, 32], BF16)
                        b1 = self.sb(sca, "b1", [128, 1], F32)
                        g1 = self.sb(sca, "g1", [128, 128], BF16)
                        _proj_fm(self, kvc(0), wsl, wring, rawT[0], "rawT0", hT, hkeys, True)
                        _proj_fm(self, kvc(1), wsl, wring, rawT[1], "rawT1", hT, hkeys, False)
                        for which in range(2):
                            A(POOL, lambda e, which=which: e.dma_start(out=w1b[:], in_=dr["nsa_cmp_w1"][0, which].rearrange("(l d) f -> d l f", d=128)),
                              w=["w1b"], dma="w1b")
                            A(POOL, lambda e, which=which: e.dma_start(out=w2b[:], in_=dr["nsa_cmp_w2"][0, which]), w=["w2b"], dma="w2b")
                            A(SP, lambda e, which=which: e.dma_start(out=peT[:], in_=dr["nsa_cmp_pos"][0, which].rearrange("l d -> d l"),
                                                                     allow_slow_non_contiguous=True), w=["peT"], dma="peT")
                            A(DVE, lambda e: e.tensor_copy(out=peTb[:], in_=peT[:]), r=["peT"], w=["peTb"])
                            pz = self.sring.next()
                            for l in range(32):
                                A(PE, lambda e, l=l, pz=pz: e.matmul(self.bank[pz][:, 0:1], lhsT=w1b[:, l, :], rhs=peTb[:, l:l + 1], start=(l == 0), stop=(l == 31)),
                                  r=["w1b", "peTb"], w=[f"B{pz}"])
                            A(DVE, lambda e, pz=pz: e.tensor_copy(out=b1[:], in_=self.bank[pz][:, 0:1]), r=[f"B{pz}"], w=["b1"])
                            ph = self.sring.next()
                            for l in range(32):
                                A(PE, lambda e, l=l, ph=ph, which=which: e.matmul(self.bank[ph][:, 0:127], lhsT=w1b[:, l, :], rhs=rawT[which][:, l:l + 2017:16],
                                                                                 start=(l == 0), stop=(l == 31)), r=["w1b", f"rawT{which}"], w=[f"B{ph}"])
                            A(ACT, lambda e, ph=ph: e.activation(out=g1[:, 0:127], in_=self.bank[ph][:, 0:127], func=AF.Gelu_apprx_tanh, bias=b1[:, 0:1], scale=1.0),
                              r=[f"B{ph}", "b1"], w=["g1"])
                            po = self.sring.next()
                            if which == 0:
                                A(PE, lambda e, po=po: e.matmul(self.bank[po][:, 0:127], lhsT=w2b[:], rhs=g1[:, 0:127], start=True, stop=True), r=["w2b", "g1"], w=[f"B{po}"])
                                A(DVE, lambda e, po=po: e.tensor_copy(out=kcT[:, 0:127], in_=self.bank[po][:, 0:127]), r=[f"B{po}"], w=["kcT"])
                            else:
                                A(PE, lambda e, po=po: e.matmul(self.bank[po][0:127, 0:128], lhsT=g1[:, 0:127], rhs=w2b[:], start=True, stop=True), r=["w2b", "g1"], w=[f"B{po}"])
                                A(DVE, lambda e, po=po: e.tensor_copy(out=vc[0:127, :], in_=self.bank[po][0:127, 0:128]), r=[f"B{po}"], w=["vc"])
                        self.P.flush()
                    cmask = self.sb(sc, "cmask", [128, NT * 127], BF16)
                    ovt = self.sb(sc, "ovt", [128, 32], BF16)
                    zer = self.sb(sc, "zer", [128, 512], BF16)
                    Dc = [self.sb(sc, f"Dc{i}", [128, 127], F32) for i in range(2)]
                    tc_ = [self.sb(sc, f"tc{i}", [128, 127], F32) for i in range(2)]
                    pc = [self.sb(sc, f"pc{i}", [128, 127], F32) for i in range(2)]
                    pnb = [self.sb(sc, f"pnb{i}", [128, 127], BF16) for i in range(2)]
                    pnT = [self.sb(sc, f"pnT{i}", [128, 128], BF16) for i in range(2)]
                    sm = [self.sb(sc, f"sm{i}", [128, 8], F32) for i in range(2)]
                    impm = self.sb(sc, "impm", [128, 512], F32)
                    work = self.sb(sc, "work", [128, 512], F32)
                    nval = self.sb(sc, "nval", [128, 512], F32)
                    nbon = self.sb(sc, "nbon", [128, 512], F32)
                    m8a = self.sb(sc, "m8a", [128, 128], F32)
                    m8b = self.sb(sc, "m8b", [128, 128], F32)
                    selt = self.sb(sc, "selt", [128, 512], F32)
                    nbb = self.sb(sc, "nbb", [128, 512], BF16)
                    A(SP, lambda e: e.dma_start(out=cmask[:], in_=dr["nsa_cmask"]), w=["cmask"], dma="cmask")
                    A(SP, lambda e: e.dma_start(out=ovt[:], in_=dr["ov"]), w=["ovt"], dma="ovt")
                    A(SP, lambda e: e.dma_start(out=nval[:], in_=dr["nsa_valid"]), w=["nval"], dma="nval")
                    A(SP, lambda e: e.dma_start(out=nbon[:], in_=dr["nsa_bonus"]), w=["nbon"], dma="nbon")
                    A(POOL, lambda e: e.memset(zer[:], 0.0), w=["zer"])
                    A(PE, lambda e: e.matmul(self.bank[4][:], lhsT=cb[:, 4, :], rhs=zer[:], start=True, stop=False, skip_group_check=True), r=["cb", "zer"], w=["B4"])
                    ci = 0
                    for r in range(4):
                        hh = g * 4 + r
                        slope = 2.0 ** (-(hh + 1))
                        for t in range(NT):
                            b = ci % 2
                            ci += 1
                            pS = self.sring.next()
                            A(PE, lambda e, t=t, r=r, pS=pS: e.matmul(self.bank[pS][:, 0:127], lhsT=qT[r][:, t * 128:(t + 1) * 128], rhs=kcT[:, 0:127], start=True, stop=True),
                              r=[f"qT{r}", "kcT"], w=[f"B{pS}"])
                            A(ACT, lambda e, t=t, b=b: e.activation(out=Dc[b][:], in_=self.posq[:, 31:2048:16], func=AF.Abs, bias=self.nposk[:, t:t + 1], scale=1.0),
                              r=["posq", "posk"], w=[f"Dc{b}"])
                            A(DVE, lambda e, b=b, pS=pS, slope=slope: e.scalar_tensor_tensor(out=tc_[b][:], in0=Dc[b][:], scalar=-slope / scale, in1=self.bank[pS][:, 0:127],
                                                                                             op0=ALU.mult, op1=ALU.add), r=[f"Dc{b}", f"B{pS}"], w=[f"tc{b}"])
                            A(DVE, lambda e, b=b: e.reduce_max(out=sm[b][:, 0:1], in_=tc_[b][:], axis=AX.X), r=[f"tc{b}"], w=[f"sm{b}"])
                            A(DVE, lambda e, b=b: e.tensor_scalar(out=sm[b][:, 1:2], in0=sm[b][:, 0:1], scalar1=-scale, scalar2=None, op0=ALU.mult), r=[f"sm{b}"], w=[f"sm{b}"])
                            A(ACT, lambda e, b=b: e.activation(out=pc[b][:], in_=tc_[b][:], func=AF.Exp, bias=sm[b][:, 1:2], scale=scale), r=[f"tc{b}", f"sm{b}"], w=[f"pc{b}"])
                            A(DVE, lambda e, b=b, t=t: e.tensor_tensor(out=pc[b][:], in0=pc[b][:], in1=cmask[:, t * 127:(t + 1) * 127], op=ALU.mult), r=[f"pc{b}", "cmask"], w=[f"pc{b}"])
                            A(DVE, lambda e, b=b: e.reduce_sum(out=sm[b][:, 2:3], in_=pc[b][:], axis=AX.X), r=[f"pc{b}"], w=[f"sm{b}"])
                            A(DVE, lambda e, b=b: e.tensor_scalar(out=sm[b][:, 2:3], in0=sm[b][:, 2:3], scalar1=1e-30, scalar2=None, op0=ALU.max), r=[f"sm{b}"], w=[f"sm{b}"])
                            A(DVE, lambda e, b=b: e.reciprocal(out=sm[b][:, 3:4], in_=sm[b][:, 2:3]), r=[f"sm{b}"], w=[f"sm{b}"])
                            A(DVE, lambda e, b=b: e.tensor_scalar(out=pnb[b][:], in0=pc[b][:], scalar1=sm[b][:, 3:4], scalar2=None, op0=ALU.mult), r=[f"pc{b}", f"sm{b}"], w=[f"pnb{b}"])
                            pt = self.sring.next()
                            A(PE, lambda e, b=b, pt=pt: e.transpose(out=self.bankbf(pt)[0:127, 0:128], in_=pnb[b][:], identity=cb[:, 0, :]), r=[f"pnb{b}", "cb"], w=[f"B{pt}"])
                            A(ACT, lambda e, b=b, pt=pt: e.activation(out=pnT[b][0:127, :], in_=self.bankbf(pt)[0:127, 0:128], func=AF.Copy), r=[f"B{pt}"], w=[f"pnT{b}"])
                            po = self.sring.next()
                            A(PE, lambda e, b=b, po=po: e.matmul(self.bank[po][:, 0:128], lhsT=pnT[b][0:127, :], rhs=vc[0:127, :], start=True, stop=True), r=[f"pnT{b}", "vc"], w=[f"B{po}"])
                            A(PE, lambda e, b=b, t=t: e.matmul(self.bank[4][:, t * 32:(t + 1) * 32], lhsT=pnT[b][0:127, :], rhs=ovt[0:127, :], start=False, stop=False,
                                                               skip_group_check=True), r=[f"pnT{b}", "ovt"], w=["B4"])
                            A(ACT, lambda e, r=r, t=t, po=po, hh=hh: e.activation(out=ocs[r][:, t, :], in_=self.bank[po][:, 0:128], func=AF.Copy,
                                                                                  scale=gates[:, t * 24 + hh:t * 24 + hh + 1]), r=[f"B{po}", "gates"], w=[f"ocs{r}"])
                    A(DVE, lambda e: e.tensor_tensor(out=impm[:], in0=self.bank[4][:], in1=nval[:], op=ALU.mult), r=["B4", "nval"], w=["impm"])
                    A(DVE, lambda e: e.tensor_tensor(out=impm[:], in0=impm[:], in1=nbon[:], op=ALU.add), r=["impm", "nbon"], w=["impm"])
                    for t in range(NT):
                        sl = slice(t * 32, (t + 1) * 32)
                        s8 = slice(t * 8, (t + 1) * 8)
                        A(DVE, lambda e, sl=sl, s8=s8: e.max(out=m8a[:, s8], in_=impm[:, sl]), r=["impm"], w=["m8a"])
                        A(DVE, lambda e, sl=sl, s8=s8: e.match_replace(out=work[:, sl], in_to_replace=m8a[:, s8], in_values=impm[:, sl], imm_value=-3.0e38),
                          r=["impm", "m8a"], w=["work"])
                        A(DVE, lambda e, sl=sl, s8=s8: e.max(out=m8b[:, s8], in_=work[:, sl]), r=["work"], w=["m8b"])
                        A(DVE, lambda e, sl=sl, t=t: e.tensor_scalar(out=selt[:, sl], in0=impm[:, sl], scalar1=m8b[:, t * 8 + 7:t * 8 + 8], scalar2=None, op0=ALU.is_ge),
                          r=["impm", "m8b"], w=["selt"])
                    A(DVE, lambda e: e.tensor_tensor(out=selt[:], in0=selt[:], in1=nval[:], op=ALU.mult), r=["selt", "nval"], w=["selt"])
                    A(DVE, lambda e: e.tensor_scalar(out=nbb[:], in0=selt[:], scalar1=-NEGB, scalar2=NEGB, op0=ALU.mult, op1=ALU.add), r=["selt"], w=["nbb"])
                    for q4 in range(2):
                        pb = self.sring.next()
                        for tt in range(8):
                            t = q4 * 8 + tt
                            A(PE, lambda e, t=t, tt=tt, pb=pb: e.transpose(out=self.bankbf(pb)[0:32, tt * 128:(tt + 1) * 128], in_=nbb[:, t * 32:(t + 1) * 32],
                                                                          identity=cb[:, 0, :]), r=["nbb", "cb"], w=[f"B{pb}"])
                        A(ACT, lambda e, q4=q4, pb=pb: e.activation(out=nbT[0:32, q4 * 1024:(q4 + 1) * 1024], in_=self.bankbf(pb)[0:32, :], func=AF.Copy),
                          r=[f"B{pb}"], w=["nbT"])
                    self.P.flush()
                with ExitStack() as sw:
                    e32t = self.sb(sw, "e32t", [128, 2048], BF16)
                    PT = [self.sb(sw, f"PT{i}", [128, 512], BF16) for i in range(2)]
                    TMP = [self.sb(sw, f"TMP{i}", [128, 512], F32) for i in range(2)]
                    DT = [self.sb(sw, f"DT{i}", [128, 512], F32) for i in range(2)]
                    otok = [self.sb(sw, f"otok{i}", [128, 128], BF16) for i in range(2)]
                    rd = [self.sb(sw, f"rd{i}", [128, 2], F32) for i in range(2)]
                    A(SP, lambda e: e.dma_start(out=e32t[:], in_=dr["e32"]), w=["e32t"], dma="e32t")
                    oi = [0]
                    for r in range(4):
                        hh = g * 4 + r
                        slope = 2.0 ** (-(hh + 1))

                        def epi_sel(c, i, r=r, hh=hh):
                            ob = oi[0] % 2
                            oi[0] += 1
                            t = 4 * c + i
                            A(DVE, lambda e: e.reciprocal(out=rd[ob][:, 0:1], in_=self.bank[4 + i][:, 128:129]), r=[f"B{4 + i}"], w=[f"rd{ob}"])
                            A(DVE, lambda e: e.tensor_tensor(out=rd[ob][:, 1:2], in0=rd[ob][:, 0:1], in1=gates[:, t * 24 + 8 + hh:t * 24 + 9 + hh], op=ALU.mult),
                              r=[f"rd{ob}", "gates"], w=[f"rd{ob}"])
                            A(DVE, lambda e: e.scalar_tensor_tensor(out=ocs[r][:, t, :], in0=self.bank[4 + i][:, 0:128], scalar=rd[ob][:, 1:2], in1=ocs[r][:, t, :],
                                                                    op0=ALU.mult, op1=ALU.add), r=[f"B{4 + i}", f"rd{ob}", f"ocs{r}"], w=[f"ocs{r}"])

                        def epi_win(c, i, r=r, hh=hh):
                            ob = oi[0] % 2
                            oi[0] += 1
                            t = 4 * c + i
                            A(DVE, lambda e: e.reciprocal(out=rd[ob][:, 0:1], in_=self.bank[4 + i][:, 128:129]), r=[f"B{4 + i}"], w=[f"rd{ob}"])
                            A(DVE, lambda e: e.tensor_tensor(out=rd[ob][:, 1:2], in0=rd[ob][:, 0:1], in1=gates[:, t * 24 + 16 + hh:t * 24 + 17 + hh], op=ALU.mult),
                              r=[f"rd{ob}", "gates"], w=[f"rd{ob}"])
                            A(DVE, lambda e: e.scalar_tensor_tensor(out=otok[ob][:], in0=self.bank[4 + i][:, 0:128], scalar=rd[ob][:, 1:2], in1=ocs[r][:, t, :],
                                                                    op0=ALU.mult, op1=ALU.add), r=[f"B{4 + i}", f"rd{ob}", f"ocs{r}"], w=[f"otok{ob}"])
                            pb = self.sring.next()
                            A(PE, lambda e, pb=pb: e.transpose(out=self.bankbf(pb)[:, 0:128], in_=otok[ob][:], identity=cb[:, 0, :]), r=[f"otok{ob}", "cb"], w=[f"B{pb}"])
                            A(ACT, lambda e, pb=pb: e.activation(out=oT[:, r, t * 128:(t + 1) * 128], in_=self.bankbf(pb)[:, 0:128], func=AF.Copy), r=[f"B{pb}"], w=["oT"])

                        _attn_softmax(self, range(4), [(kslT, qT[r])], ["kslT", f"qT{r}"], vsl, "vsl", scale, slope, _causal_items,
                                      (lambda kt: e32t[:, kt * 128:(kt + 1) * 128], nbT, ["e32t", "nbT"]), PT, TMP, DT, epi_sel)
                        _attn_softmax(self, range(4), [(kwnT, qT[r])], ["kwnT", f"qT{r}"], vwn, "vwn", scale, slope, _win_items,
                                      None, PT, TMP, DT, epi_win)
                    self.P.flush()
            with ExitStack() as so:
                wo = self.sb(so, "wo", [128, 4, D], BF16)
                self.out_proj(wo, "wo", oT, 4, dr["nsa_w_o"][0, g * 512:(g + 1) * 512, :], ["oT"])
                self.P.flush()
        self.layer_norm(st, li, 0)
        self.P.flush()


Builder.nsa = _nsa
```

```python
import numpy as np
import ml_dtypes
import concourse.bass as bass
import concourse.mybir as mybir
from concourse.bass_utils import run_bass_kernel_spmd

F32 = mybir.dt.float32
BF16 = mybir.dt.bfloat16
I32 = mybir.dt.int32
AF = mybir.ActivationFunctionType
ALU = mybir.AluOpType
AX = mybir.AxisListType

PE, ACT, DVE, POOL, SP = "tensor", "scalar", "vector", "gpsimd", "sync"
ENGINES = (PE, ACT, DVE, POOL, SP)


class Prog:
    def __init__(self, nc, es):
        self.nc = nc
        self.es = es
        self.sems = {}
        for e in ENGINES:
            self.sems[("eng", e)] = es.enter_context(nc.semaphore("s_" + e))
        self.counters = {e: 0 for e in ENGINES}
        self.chan_count = {}
        self.water = {e: {} for e in ENGINES}
        self.n_total = 0
        self._reset()

    def _reset(self):
        self.ops = []
        self.writers = {}
        self.readers = {}

    def add(self, eng, fn, reads=(), writes=(), dma=None):
        idx = len(self.ops)
        deps = set()
        src = eng if dma is None else ("dma", dma)
        for b in reads:
            deps.update(self.writers.get(b, {}).values())
        for b in writes:
            deps.update(self.writers.get(b, {}).values())
            deps.update(self.readers.get(b, {}).values())
        op = dict(eng=eng, fn=fn, deps=deps, dma=dma, ticket=None)
        if dma is not None:
            self.chan_count[dma] = self.chan_count.get(dma, 0) + 1
            op["dma_val"] = 16 * self.chan_count[dma]
        self.ops.append(op)
        for b in writes:
            self.writers.setdefault(b, {})[src] = idx
        for b in reads:
            self.readers.setdefault(b, {})[src] = idx
        return idx

    def flush(self):
        nc = self.nc
        ops = self.ops
        needed = set()
        last = {}
        for i, op in enumerate(ops):
            if op["dma"] is None:
                last[op["eng"]] = i
        needed.update(last.values())
        for op in ops:
            for d in op["deps"]:
                dop = ops[d]
                if dop["dma"] is None and not (dop["eng"] == PE and op["eng"] == PE and op["dma"] is None):
                    needed.add(d)
        for i, op in enumerate(ops):
            if op["dma"] is None and i in needed:
                self.counters[op["eng"]] += 1
                op["ticket"] = self.counters[op["eng"]]
        for c in self.chan_count:
            if ("dma", c) not in self.sems:
                self.sems[("dma", c)] = self.es.enter_context(nc.semaphore("d_" + str(c)))
        streams = {e: [] for e in ENGINES}
        for i, op in enumerate(ops):
            e = op["eng"]
            waits = {}
            for d in op["deps"]:
                dop = ops[d]
                if dop["dma"] is not None:
                    key, val = ("dma", dop["dma"]), dop["dma_val"]
                else:
                    if dop["eng"] == PE and e == PE and op["dma"] is None:
                        continue
                    key, val = ("eng", dop["eng"]), dop["ticket"]
                if val > waits.get(key, 0):
                    waits[key] = val
            wl = []
            for key, val in waits.items():
                if self.water[e].get(key, 0) >= val:
                    continue
                self.water[e][key] = val
                wl.append((key, val))
            streams[e].append((op, wl))
        final = [(("eng", e), self.counters[e]) for e in ENGINES] + \
                [(("dma", c), 16 * n) for c, n in self.chan_count.items()]
        sems = self.sems
        with nc.Block() as block:
            def mk(ename):
                def body(eng):
                    for op, wl in streams[ename]:
                        for key, val in wl:
                            eng.wait_ge(sems[key], val)
                        ins = op["fn"](eng)
                        if op["dma"] is not None:
                            ins.then_inc(sems[("dma", op["dma"])], 16)
                        elif op["ticket"] is not None:
                            ins.then_inc(sems[("eng", ename)], 1)
                    for key, val in final:
                        if val == 0 or self.water[ename].get(key, 0) >= val:
                            continue
                        eng.wait_ge(sems[key], val)
                        self.water[ename][key] = val
                return body
            block.tensor(mk(PE))
            block.scalar(mk(ACT))
            block.vector(mk(DVE))
            block.gpsimd(mk(POOL))
            block.sync(mk(SP))
        self.n_total += len(ops)
        self._reset()


S, D, NT, KC, DFF, NFC = 2048, 1024, 16, 8, 2816, 22
ALPHA = 8.0 ** 0.25
NEGB = -30000.0
PI = float(np.pi)


def host_consts():
    kp = np.arange(128)[:, None]
    qf = np.arange(128)[None, :]
    cb = np.zeros((128, 8, 128), np.float32)
    cb[:, 0] = np.eye(128)
    cb[:, 1] = np.where(kp <= qf, 0.0, NEGB)
    cb[:, 2] = np.where(kp > qf, 0.0, NEGB)
    cb[:, 3] = -(kp > qf).astype(np.float32)
    cb[:, 4] = 1.0
    cb[:, 5] = -1.0
    cb[:, 6] = (kp < qf).astype(np.float32)
    e8 = np.zeros((128, 8, 128), np.float32)
    for n in range(8):
        e8[n, n, :] = 1.0
    e32 = np.zeros((128, 16, 128), np.float32)
    for kt in range(16):
        for k in range(128):
            e32[2 * kt + k // 64, kt, k] = 1.0
    t_idx = np.arange(16)[None, :, None]
    n_idx = np.arange(32)[None, None, :]
    own = t_idx // 2
    moba_neg = np.broadcast_to(np.where(n_idx < own, 0.0, -1e30), (128, 16, 32)).astype(np.float32)
    moba_notown = np.broadcast_to((n_idx != own).astype(np.float32), (128, 16, 32)).astype(np.float32)
    invf = (np.float32(10000.0) ** (-np.arange(32, dtype=np.float32) / np.float32(32))).astype(np.float32)
    invf_bc = np.broadcast_to(invf[None, :], (128, 32)).astype(np.float32)
    q_idx = (np.arange(16)[None, :] * 128 + np.arange(128)[:, None])
    c_end = np.arange(127) * 16 + 31
    nsa_cmask = (c_end[None, None, :] <= q_idx[:, :, None]).astype(np.float32)
    qblk = q_idx // 64
    jj = np.arange(32)[None, None, :]
    valid = jj <= qblk[:, :, None]
    forced = (jj == 0) | (jj == qblk[:, :, None]) | (jj == qblk[:, :, None] - 1)
    nsa_bonus = np.where(valid, 100.0 * forced, -1e30).astype(np.float32)
    nsa_valid = valid.astype(np.float32)
    c_start = np.arange(127) * 16
    j_start = np.arange(32) * 64
    overlap = ((c_start[:, None] < j_start[None, :] + 64) & (c_end[:, None] >= j_start[None, :])).astype(np.float32)
    ov = np.zeros((128, 32), np.float32)
    ov[:127] = overlap
    bf = ml_dtypes.bfloat16
    return {
        "cb": cb.astype(bf), "e8": e8.reshape(128, 1024).astype(bf), "e32": e32.reshape(128, 2048).astype(bf),
        "moba_neg": moba_neg.reshape(128, 512), "moba_notown": moba_notown.reshape(128, 512),
        "invf": invf_bc, "nsa_cmask": nsa_cmask.reshape(128, 16 * 127).astype(bf), "nsa_bonus": nsa_bonus.reshape(128, 512),
        "nsa_valid": nsa_valid.reshape(128, 512), "ov": ov.astype(bf),
    }


WEIGHT_SPECS = [
    ("mod_w", [4, 1024, 6144]), ("mod_b", [4, 6144]), ("ln_g", [4, 2, 1024]), ("ln_b", [4, 2, 1024]),
    ("ffn_w_in", [4, 1024, 5632]), ("ffn_conv_w", [4, 3, 2816]), ("ffn_conv_b", [4, 2816]),
    ("ffn_w_out", [4, 2816, 1024]),
    ("mla_w_in", [1, 1024, 576]), ("mla_q_norm", [1, 256]), ("mla_w_uq", [1, 256, 1536]),
    ("mla_kv_norm", [1, 256]), ("mla_w_ukv", [1, 256, 2048]), ("mla_w_o", [1, 1024, 1024]),
    ("moba_w_in", [1, 1024, 3072]), ("moba_w_o", [1, 1024, 1024]),
    ("nsa_w_in", [1, 1024, 2584]), ("nsa_cmp_pos", [1, 2, 32, 128]), ("nsa_cmp_w1", [1, 2, 4096, 128]),
    ("nsa_cmp_w2", [1, 2, 128, 128]), ("nsa_w_o", [1, 1024, 1024]),
    ("sb_w_in", [1, 1024, 3072]), ("sb_w_o", [1, 1024, 1024]),
]
CONST_SPECS = [
    ("cb", [128, 8, 128], BF16), ("e8", [128, 1024], BF16), ("e32", [128, 2048], BF16),
    ("moba_neg", [128, 512], F32), ("moba_notown", [128, 512], F32), ("invf", [128, 32], F32),
    ("nsa_cmask", [128, 16 * 127], BF16), ("nsa_bonus", [128, 512], F32), ("nsa_valid", [128, 512], F32),
    ("ov", [128, 32], BF16),
]


class Ring:
    def __init__(self, name, n):
        self.vals = list(range(n)) if isinstance(n, int) else list(n)
        self.name, self.n, self.i = name, len(self.vals), 0

    def next(self):
        k = self.vals[self.i % self.n]
        self.i += 1
        return k


class Builder:
    def __init__(self, layers=(0, 1, 2, 3), taps=None):
        from contextlib import ExitStack
        self.layers = layers
        self.nc = nc = bass.Bass("TRN2", target_bir_lowering=False)
        self.es = ExitStack()
        self.P = Prog(nc, self.es)
        self.dr = {}
        self.dr["x"] = nc.dram_tensor("x", [S, D], F32, kind="ExternalInput").ap()
        self.dr["c"] = nc.dram_tensor("c", [1, D], F32, kind="ExternalInput").ap()
        self.dr["positions"] = nc.dram_tensor("positions", [1, S], I32, kind="ExternalInput").ap()
        for name, shp in WEIGHT_SPECS:
            self.dr[name] = nc.dram_tensor(name, shp, F32, kind="ExternalInput").ap()
        for name, shp, dt in CONST_SPECS:
            self.dr[name] = nc.dram_tensor(name, shp, dt, kind="ExternalInput").ap()
        self.dr["out"] = nc.dram_tensor("out", [S, D], F32, kind="ExternalOutput").ap()
        self.uid = 0

    def sb(self, st, name, shape, dt):
        self.uid += 1
        return st.enter_context(self.nc.sbuf_tensor(f"{name}_{self.uid}", shape, dt))

    def A(self, eng, fn, r=(), w=(), dma=None):
        return self.P.add(eng, fn, reads=r, writes=w, dma=dma)

    def psum_setup(self, st):
        self.uid += 1
        self.bank = [st.enter_context(self.nc.psum_tensor(f"bank{i}_{self.uid}", [128, 512], F32)) for i in range(8)]

    def bankbf(self, i):
        return self.bank[i][:].bitcast(BF16)

    def build(self):
        nc, A, dr = self.nc, self.A, self.dr
        from contextlib import ExitStack
        es = self.es
        self.psum_setup(es)
        self.x = self.sb(es, "x", [128, NT, D], F32)
        self.cb = self.sb(es, "cb", [128, 8, 128], BF16)
        self.posq = self.sb(es, "posq", [128, S], F32)
        self.posk = self.sb(es, "posk", [128, NT], F32)
        self.nposk = self.sb(es, "nposk", [128, NT], F32)
        self.crep = self.sb(es, "crep", [128, KC, 128], BF16)
        self.gate = self.sb(es, "gate", [128, D], F32)
        self.ms = self.sb(es, "ms", [128, 2, D], F32)
        self.small = self.sb(es, "small", [128, 64], F32)
        x, cb = self.x, self.cb
        with ExitStack() as st:
            posi = self.sb(st, "posi", [128, S], I32)
            poski = self.sb(st, "poski", [128, NT], I32)
            cT = self.sb(st, "cT", [128, KC], F32)
            cA = self.sb(st, "cA", [128, KC], F32)
            A(SP, lambda e: e.dma_start(out=cb[:], in_=dr["cb"]), w=["cb"], dma="cb")
            A(SP, lambda e: e.dma_start(out=posi[:], in_=dr["positions"].partition_broadcast(128)), w=["posi"], dma="posi")
            A(SP, lambda e: e.dma_start(out=poski[:], in_=dr["positions"].rearrange("o (t p) -> p (o t)", p=128),
                                        allow_slow_non_contiguous=True), w=["poski"], dma="poski")
            A(SP, lambda e: e.dma_start(out=cT[:], in_=dr["c"].rearrange("o (k p) -> p (o k)", p=128),
                                        allow_slow_non_contiguous=True), w=["cT"], dma="cT")
            for t in range(NT):
                A(SP, lambda e, t=t: e.dma_start(out=x[:, t, :], in_=dr["x"][t * 128:(t + 1) * 128, :]),
                  w=[f"x{t}"], dma=f"x{t % 4}")
            A(DVE, lambda e: e.tensor_copy(out=self.posq[:], in_=posi[:]), r=["posi"], w=["posq"])
            A(DVE, lambda e: e.tensor_copy(out=self.posk[:], in_=poski[:]), r=["poski"], w=["posk"])
            A(DVE, lambda e: e.tensor_scalar(out=self.nposk[:], in0=self.posk[:], scalar1=-1.0, scalar2=None, op0=ALU.mult),
              r=["posk"], w=["posk"])
            A(ACT, lambda e: e.activation(out=cA[:], in_=cT[:], func=AF.Silu), r=["cT"], w=["cA"])
            for k in range(KC):
                A(DVE, lambda e, k=k: e.tensor_scalar(out=self.crep[:, k, :], in0=cb[:, 4, :], scalar1=cA[:, k:k + 1],
                                                      scalar2=None, op0=ALU.mult), r=["cA", "cb"], w=["crep"])
            self.subs = []
            for li in self.layers:
                if isinstance(li, tuple):
                    self.subs.append((li[1], 1))
                else:
                    self.subs += [(li, 0), (li, 1)]
            self.sub_i = 0
            for th in self.mod_slabs(st, self.subs[0][0], self.subs[0][1]):
                th()
            self.P.flush()
        for li in self.layers:
            self.layer(li)
        for t in range(NT):
            A(SP, lambda e, t=t: e.dma_start(out=dr["out"][t * 128:(t + 1) * 128, :], in_=x[:, t, :]),
              r=[f"x{t}"], w=[f"out{t}"], dma=f"o{t % 4}")
        self.P.flush()
        return nc

    def mod_slabs(self, st, li, half):
        A, dr = self.A, self.dr
        wsl = [self.sb(st, f"modw{i}", [128, KC, 256], BF16) for i in range(2)]
        bsl = [self.sb(st, f"modb{i}", [128, 256], F32) for i in range(2)]
        thunks = []
        for j in range(12):
            def slab(j=j):
                s = j % 2
                col0 = half * 3072 + j * 256
                A(POOL, lambda e: e.dma_start(
                    out=wsl[s][:], in_=dr["mod_w"][li, :, col0:col0 + 256].rearrange("(k p) n -> p k n", p=128)),
                  w=[f"modw{s}"], dma=f"modw{s}")
                A(SP, lambda e: e.dma_start(
                    out=bsl[s][:], in_=dr["mod_b"][li:li + 1, col0:col0 + 256].partition_broadcast(128)),
                  w=[f"modb{s}"], dma=f"modb{s}")
                pb = 6 + s
                for k in range(KC):
                    A(PE, lambda e, k=k: e.matmul(self.bank[pb][:, 0:256], lhsT=self.crep[:, k, :],
                                                  rhs=wsl[s][:, k, :], start=(k == 0), stop=(k == KC - 1)),
                      r=["crep", f"modw{s}"], w=[f"B{pb}"])
                comp, off = (j * 256) // 1024, (j * 256) % 1024
                dst = self.ms[:, comp, off:off + 256] if comp < 2 else self.gate[:, off:off + 256]
                A(DVE, lambda e: e.tensor_tensor(out=dst, in0=self.bank[pb][:, 0:256], in1=bsl[s][:], op=ALU.add),
                  r=[f"B{pb}", f"modb{s}"], w=["modt"])
            thunks.append(slab)

        def fin():
            A(DVE, lambda e: e.tensor_scalar(out=self.ms[:, 1, :], in0=self.ms[:, 1, :], scalar1=1.0, scalar2=None,
                                             op0=ALU.add), r=["modt"], w=["modt"])
        thunks.append(fin)
        return thunks

    def build_hT(self, st, hT):
        A, x, modt = self.A, self.x, self.ms
        tmp = [self.sb(st, f"htmp{i}", [128, D], F32) for i in range(2)]
        hb = [self.sb(st, f"hb{i}", [128, D], BF16) for i in range(2)]
        for t in range(NT):
            s = t % 2
            A(DVE, lambda e, t=t, s=s: e.tensor_tensor(out=tmp[s][:], in0=x[:, t, :], in1=modt[:, 1, :], op=ALU.mult),
              r=[f"x{t}", "modt"], w=[f"htmp{s}"])
            A(DVE, lambda e, s=s: e.tensor_tensor(out=hb[s][:], in0=tmp[s][:], in1=modt[:, 0, :], op=ALU.add),
              r=[f"htmp{s}", "modt"], w=[f"hb{s}"])
            pb = 6 + s
            for k in range(KC):
                A(PE, lambda e, s=s, k=k, pb=pb: e.transpose(out=self.bankbf(pb)[:, k * 128:(k + 1) * 128],
                                                             in_=hb[s][:, k * 128:(k + 1) * 128], identity=self.cb[:, 0, :]),
                  r=[f"hb{s}", "cb"], w=[f"B{pb}"])
            A(ACT, lambda e, t=t, pb=pb: e.activation(
                out=hT[:, :, t * 128:(t + 1) * 128], in_=self.bankbf(pb).rearrange("p (k n) -> p k n", k=KC), func=AF.Copy),
              r=[f"B{pb}"], w=[f"hT{t}"])
            A(ACT, lambda e, t=t: e.activation(out=x[:, t, :], in_=x[:, t, :], func=AF.Copy, scale=ALPHA),
              r=[f"x{t}"], w=[f"x{t}"])

    def layer_norm(self, st, li, which):
        A, x, dr = self.A, self.x, self.dr
        self.sub_i += 1
        nxt = self.subs[self.sub_i] if self.sub_i < len(self.subs) else None
        slabs = self.mod_slabs(st, nxt[0], nxt[1]) if nxt is not None else []
        self.lng = self.sb(st, "lng", [128, D], F32)
        self.lnb = self.sb(st, "lnb", [128, D], F32)
        A(SP, lambda e: e.dma_start(out=self.lng[:], in_=dr["ln_g"][li, which:which + 1, :].partition_broadcast(128)),
          w=["lng"], dma="lng")
        A(SP, lambda e: e.dma_start(out=self.lnb[:], in_=dr["ln_b"][li, which:which + 1, :].partition_broadcast(128)),
          w=["lnb"], dma="lnb")
        stt = [self.sb(st, f"lnst{i}", [128, 16], F32) for i in range(2)]
        for t in range(NT):
            s = t % 2
            q = stt[s]
            A(DVE, lambda e, t=t, q=q: e.bn_stats(out=q[:, 0:6], in_=x[:, t, 0:512]), r=[f"x{t}"], w=[f"lnst{s}"])
            A(DVE, lambda e, t=t, q=q: e.bn_stats(out=q[:, 6:12], in_=x[:, t, 512:1024]), r=[f"x{t}"], w=[f"lnst{s}"])
            A(DVE, lambda e, q=q: e.bn_aggr(out=q[:, 12:14], in_=q[:, 0:12]), r=[f"lnst{s}"], w=[f"lnst{s}"])
            A(ACT, lambda e, q=q: e.activation(out=q[:, 14:15], in_=q[:, 13:14], func=AF.Ln, bias=1e-5, scale=1.0),
              r=[f"lnst{s}"], w=[f"lnst{s}"])
            A(ACT, lambda e, q=q: e.activation(out=q[:, 14:15], in_=q[:, 14:15], func=AF.Exp, scale=-0.5),
              r=[f"lnst{s}"], w=[f"lnst{s}"])
            A(DVE, lambda e, q=q: e.scalar_tensor_tensor(out=q[:, 15:16], in0=q[:, 12:13], scalar=-1.0, in1=q[:, 14:15],
                                                         op0=ALU.mult, op1=ALU.mult), r=[f"lnst{s}"], w=[f"lnst{s}"])
            A(ACT, lambda e, t=t, q=q: e.activation(out=x[:, t, :], in_=x[:, t, :], func=AF.Identity,
                                                    bias=q[:, 15:16], scale=q[:, 14:15]),
              r=[f"x{t}", f"lnst{s}"], w=[f"x{t}"])
            A(DVE, lambda e, t=t: e.tensor_tensor(out=x[:, t, :], in0=x[:, t, :], in1=self.lng[:], op=ALU.mult),
              r=[f"x{t}", "lng"], w=[f"x{t}"])
            A(DVE, lambda e, t=t: e.tensor_tensor(out=x[:, t, :], in0=x[:, t, :], in1=self.lnb[:], op=ALU.add),
              r=[f"x{t}", "lnb"], w=[f"x{t}"])
            if t < len(slabs):
                slabs[t]()
        for th in slabs[NT:]:
            th()

    def out_proj(self, wo, wkey, srcT, nj, w_dram_rows, src_keys):
        A, x = self.A, self.x
        A(POOL, lambda e: e.dma_start(out=wo[:, 0:nj, :], in_=w_dram_rows.rearrange("(j p) n -> p j n", p=128)),
          w=[wkey], dma=wkey)
        for j in range(nj):
            A(DVE, lambda e, j=j: e.tensor_tensor(out=wo[:, j, :], in0=wo[:, j, :], in1=self.gate[:], op=ALU.mult),
              r=[wkey, "modt"], w=[wkey])
        for t in range(NT):
            for n in range(2):
                pb = 4 + (2 * t + n) % 2
                for j in range(nj):
                    A(PE, lambda e, t=t, n=n, j=j, pb=pb: e.matmul(
                        self.bank[pb][:], lhsT=srcT[:, j, t * 128:(t + 1) * 128], rhs=wo[:, j, n * 512:(n + 1) * 512],
                        start=(j == 0), stop=(j == nj - 1)), r=[wkey] + src_keys, w=[f"B{pb}"])
                A(DVE, lambda e, t=t, n=n, pb=pb: e.tensor_tensor(
                    out=x[:, t, n * 512:(n + 1) * 512], in0=self.bank[pb][:], in1=x[:, t, n * 512:(n + 1) * 512], op=ALU.add),
                  r=[f"B{pb}", f"x{t}"], w=[f"x{t}"])

    def ffn(self, li):
        from contextlib import ExitStack
        A, dr, x = self.A, self.dr, self.x
        with ExitStack() as st:
            hT = self.sb(st, "hT", [128, KC, S], BF16)
            with ExitStack() as st0:
                self.build_hT(st0, hT)
                self.P.flush()
            hkeys = [f"hT{t}" for t in range(NT)]
            cw = self.sb(st, "cw", [128, 3, NFC], F32)
            cbias = self.sb(st, "cbias", [128, NFC], F32)
            for wi in range(3):
                A(SP, lambda e, wi=wi: e.dma_start(out=cw[:, wi, :], in_=dr["ffn_conv_w"][li, wi:wi + 1, :].rearrange("o (j p) -> p (o j)", p=128),
                                                   allow_slow_non_contiguous=True), w=["cw"], dma="cw")
            A(SP, lambda e: e.dma_start(out=cbias[:], in_=dr["ffn_conv_b"][li:li + 1, :].rearrange("o (j p) -> p (o j)", p=128),
                                        allow_slow_non_contiguous=True), w=["cbias"], dma="cbias")
            wab = [self.sb(st, f"wab{i}", [128, KC, 2, 128], BF16) for i in range(2)]
            aS = [self.sb(st, f"aS{i}", [128, S + 2], F32) for i in range(2)]
            tS = [self.sb(st, f"tS{i}", [128, 512], F32) for i in range(2)]
            gT = [self.sb(st, f"gT{i}", [128, 4, S], BF16) for i in range(2)]
            wof = [self.sb(st, f"wof{i}", [128, 4, D], BF16) for i in range(2)]
            for i in range(2):
                A(POOL, lambda e, i=i: e.memset(aS[i][:, 0:2], 0.0), w=[f"aS{i}"])
            groups = [(0, 4), (4, 4), (8, 4), (12, 4), (16, 4), (20, 2)]
            it = 0
            pending_out = None
            for gi, (f0, nj) in enumerate(groups):
                gs = gi % 2
                for jj in range(nj):
                    j = f0 + jj
                    ws = j % 2
                    for half in range(2):
                        c0 = half * DFF + j * 128
                        A(POOL, lambda e, ws=ws, half=half, c0=c0: e.dma_start(
                            out=wab[ws][:, :, half, :], in_=dr["ffn_w_in"][li, :, c0:c0 + 128].rearrange("(k p) n -> p k n", p=128)),
                          w=[f"wab{ws}"], dma=f"wab{ws}")
                    for c in range(4):
                        s2 = it % 2
                        it += 1
                        pa, pbk = (0, 1, 7)[(it - 1) % 3], (2, 3, 6)[(it - 1) % 3]
                        for half, pbank in ((0, pa), (1, pbk)):
                            for k in range(KC):
                                A(PE, lambda e, ws=ws, half=half, pbank=pbank, k=k, c=c: e.matmul(
                                    self.bank[pbank][:], lhsT=wab[ws][:, k, half, :], rhs=hT[:, k, c * 512:(c + 1) * 512],
                                    start=(k == 0), stop=(k == KC - 1)),
                                  r=[f"wab{ws}"] + hkeys[4 * c:4 * c + 4], w=[f"B{pbank}"])
                        a_ = aS[ws]
                        A(ACT, lambda e, a_=a_, pa=pa, c=c: e.activation(out=a_[:, 2 + c * 512:2 + (c + 1) * 512],
                                                                         in_=self.bank[pa][:], func=AF.Copy),
                          r=[f"B{pa}"], w=[f"aS{ws}"])
                        A(DVE, lambda e, a_=a_, s2=s2, j=j, c=c: e.tensor_scalar(
                            out=tS[s2][:], in0=a_[:, c * 512:(c + 1) * 512], scalar1=cw[:, 0, j:j + 1],
                            scalar2=cbias[:, j:j + 1], op0=ALU.mult, op1=ALU.add),
                          r=[f"aS{ws}", "cw", "cbias"], w=[f"tS{s2}"])
                        A(DVE, lambda e, a_=a_, s2=s2, j=j, c=c: e.scalar_tensor_tensor(
                            out=tS[s2][:], in0=a_[:, 1 + c * 512:1 + (c + 1) * 512], scalar=cw[:, 1, j:j + 1], in1=tS[s2][:],
                            op0=ALU.mult, op1=ALU.add), r=[f"aS{ws}", "cw", f"tS{s2}"], w=[f"tS{s2}"])
                        A(DVE, lambda e, a_=a_, s2=s2, j=j, c=c: e.scalar_tensor_tensor(
                            out=tS[s2][:], in0=a_[:, 2 + c * 512:2 + (c + 1) * 512], scalar=cw[:, 2, j:j + 1], in1=tS[s2][:],
                            op0=ALU.mult, op1=ALU.add), r=[f"aS{ws}", "cw", f"tS{s2}"], w=[f"tS{s2}"])
                        A(ACT, lambda e, s2=s2: e.activation(out=tS[s2][:], in_=tS[s2][:], func=AF.Gelu_apprx_tanh),
                          r=[f"tS{s2}"], w=[f"tS{s2}"])
                        A(DVE, lambda e, s2=s2, gs=gs, jj=jj, c=c, pbk=pbk: e.tensor_tensor(
                            out=gT[gs][:, jj, c * 512:(c + 1) * 512], in0=self.bank[pbk][:], in1=tS[s2][:], op=ALU.mult),
                          r=[f"tS{s2}", f"B{pbk}"], w=[f"gT{gs}"])
                if pending_out is not None:
                    self.out_proj(*pending_out)
                pending_out = (wof[gs], f"wof{gs}", gT[gs], nj, dr["ffn_w_out"][li, f0 * 128:(f0 + nj) * 128, :], [f"gT{gs}"])
            self.out_proj(*pending_out)
            self.P.flush()
        with ExitStack() as st_ln:
            self.layer_norm(st_ln, li, 1)
            self.P.flush()

    def layer(self, li):
        if isinstance(li, tuple):
            self.ffn(li[1])
            return
        kind = li % 4
        if kind == 0:
            self.mla(li)
        elif kind == 1:
            self.moba(li)
        elif kind == 2:
            self.nsa(li)
        else:
            self.sbmix(li)
        self.ffn(li)


_CONSTS = None


def kernel(**inputs):
    return run_kernel(inputs, layers=(0, 1, 2, 3))


def run_kernel(inputs, layers, cores=8, x_override=None, trace=False):
    global _CONSTS
    if _CONSTS is None:
        _CONSTS = host_consts()
    b = Builder(layers=layers)
    nc = b.build()
    in_maps = []
    xs = inputs["x"] if x_override is None else x_override
    for ci in range(cores):
        m = {"x": np.ascontiguousarray(xs[ci], dtype=np.float32),
             "c": np.ascontiguousarray(inputs["c"][ci:ci + 1], dtype=np.float32),
             "positions": np.ascontiguousarray(inputs["positions"][ci:ci + 1], dtype=np.int32)}
        for name, _ in WEIGHT_SPECS:
            m[name] = np.ascontiguousarray(inputs[name], dtype=np.float32)
        for name, _, _ in CONST_SPECS:
            m[name] = _CONSTS[name]
        in_maps.append(m)
    res = run_bass_kernel_spmd(nc, in_maps, core_ids=list(range(cores)), **({'trace': True} if trace else {}))
    if trace:
        print('EXEC_TIME_NS', res.exec_time_ns)
    return np.stack([r["out"] for r in res.results], axis=0).astype(np.float32)


def _mixer_prologue(self, st, li):
    from contextlib import ExitStack
    hT = self.sb(st, "hT", [128, KC, S], BF16)
    with ExitStack() as st0:
        self.build_hT(st0, hT)
        self.P.flush()
    self.sring = Ring("s", 4)
    self.sring_s = None
    return hT, [f"hT{t}" for t in range(NT)]


def _proj_qkv(self, w3, h, wq, wkey, qT, qkey, kT, kkey, V, vkey, hT, hkeys, kmean=None):
    A = self.A
    for which in range(3):
        A(POOL, lambda e, which=which: e.dma_start(out=wq[:, :, which, :], in_=w3[which].rearrange("(k p) n -> p k n", p=128)),
          w=[wkey], dma=wkey)
    for which, (dst, dkey) in enumerate(((qT, qkey), (kT, kkey))):
        for c in range(4):
            pb = self.sring.next()
            for k in range(KC):
                A(PE, lambda e, which=which, k=k, c=c, pb=pb: e.matmul(
                    self.bank[pb][:], lhsT=wq[:, k, which, :], rhs=hT[:, k, c * 512:(c + 1) * 512],
                    start=(k == 0), stop=(k == KC - 1)), r=[wkey] + hkeys[4 * c:4 * c + 4], w=[f"B{pb}"])
            if which == 0:
                A(ACT, lambda e, c=c, pb=pb, dst=dst: e.activation(out=dst[:, c * 512:(c + 1) * 512], in_=self.bank[pb][:], func=AF.Copy),
                  r=[f"B{pb}"], w=[dkey])
            else:
                A(DVE, lambda e, c=c, pb=pb, dst=dst: e.tensor_copy(out=dst[:, c * 512:(c + 1) * 512], in_=self.bank[pb][:]),
                  r=[f"B{pb}"], w=[dkey])
                if kmean is not None:
                    A(DVE, lambda e, c=c, pb=pb: e.tensor_reduce(
                        out=kmean[:, 2 * c:2 * c + 2], in_=self.bank[pb][:].rearrange("p (b n) -> p b n", b=2), axis=AX.X, op=ALU.add),
                      r=[f"B{pb}"], w=["kmean"])
    for tg in range(4):
        pb = self.sring.next()
        for tt in range(4):
            t = tg * 4 + tt
            for k in range(KC):
                A(PE, lambda e, k=k, t=t, tt=tt, pb=pb: e.matmul(
                    self.bank[pb][:, tt * 128:(tt + 1) * 128], lhsT=hT[:, k, t * 128:(t + 1) * 128], rhs=wq[:, k, 2, :],
                    start=(k == 0), stop=(k == KC - 1)), r=[wkey, hkeys[t]], w=[f"B{pb}"])
        eng = ACT if tg % 2 == 0 else DVE
        if eng == ACT:
            A(ACT, lambda e, tg=tg, pb=pb: e.activation(out=V[:, tg * 4:(tg + 1) * 4, 0:128],
                                                        in_=self.bank[pb][:].rearrange("p (a n) -> p a n", a=4), func=AF.Copy),
              r=[f"B{pb}"], w=[vkey])
        else:
            A(DVE, lambda e, tg=tg, pb=pb: e.tensor_copy(out=V[:, tg * 4:(tg + 1) * 4, 0:128],
                                                         in_=self.bank[pb][:].rearrange("p (a n) -> p a n", a=4)),
              r=[f"B{pb}"], w=[vkey])


def _o_to_oT(self, src_ap, src_keys, otok, okey, oT, jj, t, scale_ap=None, scale_keys=()):
    A = self.A
    if scale_ap is None:
        A(ACT, lambda e: e.activation(out=otok[:], in_=src_ap, func=AF.Copy), r=list(src_keys), w=[okey])
    else:
        A(ACT, lambda e: e.activation(out=otok[:], in_=src_ap, func=AF.Copy, scale=scale_ap),
          r=list(src_keys) + list(scale_keys), w=[okey])
    pb = self.sring.next()
    A(PE, lambda e, pb=pb: e.transpose(out=self.bankbf(pb)[:, 0:128], in_=otok[:], identity=self.cb[:, 0, :]),
      r=[okey, "cb"], w=[f"B{pb}"])
    A(ACT, lambda e, pb=pb: e.activation(out=oT[:, jj, t * 128:(t + 1) * 128], in_=self.bankbf(pb)[:, 0:128], func=AF.Copy),
      r=[f"B{pb}"], w=["oT"])


def _sbmix(self, li):
    from contextlib import ExitStack
    A, dr, cb = self.A, self.dr, self.cb
    scale = 128.0 ** -0.5
    with ExitStack() as st:
        hT, hkeys = _mixer_prologue(self, st, li)
        wq1 = self.sb(st, "wq0", [128, KC, 3, 128], BF16)
        wq = [wq1, wq1]
        qT = [self.sb(st, f"qT{i}", [128, S], BF16) for i in range(2)]
        kT = [self.sb(st, f"kT{i}", [128, S], BF16) for i in range(2)]
        V = [self.sb(st, f"V{i}", [128, NT, 128], BF16) for i in range(2)]
        oT = self.sb(st, "oT", [128, 4, S], BF16)
        wo = self.sb(st, "wo", [128, 4, D], BF16)
        SPl = [self.sb(st, f"SPl{i}", [128, 512], F32) for i in range(2)]
        Lb = [self.sb(st, f"Lb{i}", [128, 512], BF16) for i in range(2)]
        Lsum = self.sb(st, "Lsum", [128, 512], BF16)
        T1 = [self.sb(st, f"T1{i}", [128, 512], F32) for i in range(3)]
        AT = [self.sb(st, f"AT{i}", [128, 512], BF16) for i in range(2)]
        otok = [self.sb(st, f"otok{i}", [128, 128], BF16) for i in range(2)]
        w_in = dr["sb_w_in"][0]
        self.atn_it = 0
        self.nls = 0
        self.oi = 0
        def prep(h):
            s = h % 2
            w3 = [w_in[:, which * 1024 + h * 128: which * 1024 + (h + 1) * 128] for which in range(3)]
            _proj_qkv(self, w3, h, wq[s], "wq0", qT[s], f"qT{s}", kT[s], f"kT{s}", V[s], f"V{s}", hT, hkeys)

        prep(0)
        for h in range(8):
            s = h % 2
            stages = []
            for c in range(4):
                for kt in range(4 * c + 3, -1, -1):
                    sd = {}

                    def stage1(sd=sd, c=c, kt=kt, s=s):
                        r_ = kt - 4 * c
                        q0 = max(0, r_) * 128
                        diag = r_ >= 0
                        b = self.atn_it % 2
                        t3 = self.atn_it % 3
                        sd["b"], sd["t3"] = b, t3
                        self.atn_it += 1
                        pz = self.sring.next()
                        A(PE, lambda e: e.matmul(
                            self.bank[pz][:, q0:512], lhsT=kT[s][:, kt * 128:(kt + 1) * 128], rhs=qT[s][:, c * 512 + q0:(c + 1) * 512],
                            start=True, stop=True), r=[f"kT{s}", f"qT{s}"], w=[f"B{pz}"])
                        A(ACT, lambda e: e.activation(out=SPl[b][:, q0:512], in_=self.bank[pz][:, q0:512], func=AF.Exp, scale=scale),
                          r=[f"B{pz}"], w=[f"SPl{b}"])
                        A(ACT, lambda e: e.activation(out=Lb[b][:, q0:512], in_=SPl[b][:, q0:512], func=AF.Ln, bias=1.0, scale=1.0),
                          r=[f"SPl{b}"], w=[f"Lb{b}"])
                        sd["pz"] = pz

                    def stage1d(sd=sd, c=c, kt=kt, s=s):
                        b, t3, pz = sd["b"], sd["t3"], sd["pz"]
                        r_ = kt - 4 * c
                        q0 = max(0, r_) * 128
                        diag = r_ >= 0
                        A(DVE, lambda e: e.scalar_tensor_tensor(
                            out=T1[t3][:, q0:512], in0=self.bank[pz][:, q0:512], scalar=scale, in1=Lb[b][:, q0:512], op0=ALU.mult, op1=ALU.subtract),
                          r=[f"B{pz}", f"Lb{b}"], w=[f"T1{t3}"])
                        if diag:
                            A(DVE, lambda e: e.tensor_tensor(out=Lb[b][:, q0:q0 + 128], in0=Lb[b][:, q0:q0 + 128], in1=cb[:, 6, :], op=ALU.mult),
                              r=[f"Lb{b}", "cb"], w=[f"Lb{b}"])

                    def stage2a(sd=sd, c=c, kt=kt, s=s, h=h):
                        b, t3 = sd["b"], sd["t3"]
                        r_ = kt - 4 * c
                        q0 = max(0, r_) * 128
                        first = (kt == 4 * c + 3)
                        if first:
                            A(DVE, lambda e: e.memset(Lsum[:], 0.0), w=["Lsum"])
                        pt = self.sring.next()
                        A(PE, lambda e: e.matmul(
                            self.bank[pt][:, q0:512], lhsT=cb[:, 3, :], rhs=Lb[b][:, q0:512], start=True, stop=first),
                          r=["cb", f"Lb{b}"], w=[f"B{pt}"])
                        if not first:
                            A(PE, lambda e: e.matmul(
                                self.bank[pt][:, q0:512], lhsT=cb[:, 5, :], rhs=Lsum[:, q0:512], start=False, stop=True),
                              r=["cb", "Lsum"], w=[f"B{pt}"])
                        if kt > 0:
                            A(DVE, lambda e: e.tensor_tensor(out=Lsum[:, q0:512], in0=Lsum[:, q0:512], in1=Lb[b][:, q0:512], op=ALU.add),
                              r=["Lsum", f"Lb{b}"], w=["Lsum"])
                        A(DVE, lambda e: e.tensor_tensor(out=T1[t3][:, q0:512], in0=self.bank[pt][:, q0:512], in1=T1[t3][:, q0:512], op=ALU.add),
                          r=[f"B{pt}", f"T1{t3}"], w=[f"T1{t3}"])

                    def stage2b(sd=sd, c=c, kt=kt, s=s, h=h):
                        b, t3 = sd["b"], sd["t3"]
                        r_ = kt - 4 * c
                        q0 = max(0, r_) * 128
                        diag = r_ >= 0
                        A(ACT, lambda e: e.activation(out=AT[b][:, q0:512], in_=T1[t3][:, q0:512], func=AF.Exp),
                          r=[f"T1{t3}"], w=[f"AT{b}"])
                        if diag:
                            A(DVE, lambda e: e.tensor_tensor(out=AT[b][:, q0:q0 + 128], in0=AT[b][:, q0:q0 + 128], in1=cb[:, 6, :], op=ALU.mult),
                              r=[f"AT{b}", "cb"], w=[f"AT{b}"])

                    def stage2p(sd=sd, c=c, kt=kt, s=s, h=h):
                        b = sd["b"]
                        r_ = kt - 4 * c
                        q0 = max(0, r_) * 128
                        for i in range(q0 // 128, 4):
                            A(PE, lambda e, i=i: e.matmul(
                                self.bank[4 + i][:, 0:128], lhsT=AT[b][:, i * 128:(i + 1) * 128], rhs=V[s][:, kt, :],
                                start=(kt == 4 * c + i), stop=(kt == 0)), r=[f"AT{b}", f"V{s}"], w=[f"B{4 + i}"])
                        if kt == 0:
                            for i in range(4):
                                ob = self.oi % 2
                                self.oi += 1
                                _o_to_oT(self, self.bank[4 + i][:, 0:128], [f"B{4 + i}"], otok[ob], f"otok{ob}", oT, h % 4, 4 * c + i)
                    stages.append((stage2b, stage1, stage2a, stage1d, stage2p))
            nst = len(stages)
            for step in range(-2, nst):
                if step == nst // 2 and h < 7:
                    prep(h + 1)
                for si, off in enumerate((0, 2, 1, 2, 0)):
                    k = step + off
                    if 0 <= k < nst:
                        stages[k][si]()
            if h % 4 == 3:
                hg = h // 4
                self.out_proj(wo, "wo", oT, 4, dr["sb_w_o"][0, hg * 512:(hg + 1) * 512, :], ["oT"])
        self.P.flush()
    with ExitStack() as st_ln:
        self.layer_norm(st_ln, li, 0)
        self.P.flush()


Builder.sbmix = _sbmix


def _attn_softmax(self, c_list, pairs, pkeys, V, vkey, scale, slope, items_of_chunk, maskmm, PT, TMP, DT, epi, far_ok=True, mid_hook=None):
    A, cb = self.A, self.cb
    nb = len(PT)
    stages = []
    for c in c_list:
        items = items_of_chunk(c)
        first_kt, last_kt = {}, {}
        for (kt, q0, q1, diag, far) in items:
            for i in range(q0 // 128, q1 // 128):
                first_kt.setdefault(i, kt)
                last_kt[i] = kt
        for idx, (kt, q0, q1, diag, far) in enumerate(items):
            s1, s2 = [], []

            st_ = {}

            def stage0(st_=st_, c=c, kt=kt, q0=q0, q1=q1):
                b = self.atn_it % nb
                self.atn_it += 1
                st_["b"] = b
                if slope is not None:
                    A(ACT, lambda e: e.activation(
                        out=DT[b][:, q0:q1], in_=self.posq[:, c * 512 + q0:c * 512 + q1], func=AF.Abs, bias=self.nposk[:, kt:kt + 1], scale=1.0),
                      r=["posq", "posk"], w=[f"DT{b}"])

            def stage1a(st_=st_, c=c, kt=kt, q0=q0, q1=q1, diag=diag, far=far):
                b = st_["b"]
                ps = (self.sring_s if (slope is None and self.sring_s is not None) else self.sring).next()
                st_["ps"] = ps
                mms = []
                for (kT_ap, qT_ap) in pairs:
                    mms.append((kT_ap[:, kt * 128:(kt + 1) * 128], qT_ap[:, c * 512 + q0:c * 512 + q1], q0, q1, list(pkeys)))
                if maskmm is not None:
                    lhs_fn, rhs_ap, mkeys = maskmm
                    mms.append((lhs_fn(kt), rhs_ap[:, c * 512 + q0:c * 512 + q1], q0, q1, list(mkeys)))
                if diag:
                    mms.append((cb[:, 0, :], cb[:, 1, :], q0, q0 + 128, ["cb"]))
                if far:
                    mms.append((cb[:, 0, :], cb[:, 2, :], q1 - 128, q1, ["cb"]))
                for mi, (lh, rh, a0, a1, keys) in enumerate(mms):
                    A(PE, lambda e, lh=lh, rh=rh, a0=a0, a1=a1, ps=ps, mi=mi, n=len(mms): e.matmul(
                        self.bank[ps][:, a0:a1], lhsT=lh, rhs=rh, start=(mi == 0), stop=(mi == n - 1)),
                      r=keys, w=[f"B{ps}"])
                if slope is not None:
                    A(DVE, lambda e: e.scalar_tensor_tensor(
                        out=TMP[b][:, q0:q1], in0=DT[b][:, q0:q1], scalar=-slope / scale, in1=self.bank[ps][:, q0:q1],
                        op0=ALU.mult, op1=ALU.add), r=[f"DT{b}", f"B{ps}"], w=[f"TMP{b}"])

            def stage1b(st_=st_, q0=q0, q1=q1):
                b, ps = st_["b"], st_["ps"]
                if slope is not None:
                    A(ACT, lambda e: e.activation(out=PT[b][:, q0:q1], in_=TMP[b][:, q0:q1], func=AF.Exp, scale=scale),
                      r=[f"TMP{b}"], w=[f"PT{b}"])
                else:
                    A(ACT, lambda e: e.activation(out=PT[b][:, q0:q1], in_=self.bank[ps][:, q0:q1], func=AF.Exp, scale=scale),
                      r=[f"B{ps}"], w=[f"PT{b}"])

            def stage2(st_=st_, c=c, kt=kt, q0=q0, q1=q1, last=(idx == len(items) - 1), first_kt=first_kt, last_kt=last_kt):
                b = st_["b"]
                for i in range(q0 // 128, q1 // 128):
                    A(PE, lambda e, i=i, s_=(kt == first_kt[i]), p_=(kt == last_kt[i]): e.matmul(
                        self.bank[4 + i][:, 0:129], lhsT=PT[b][:, i * 128:(i + 1) * 128], rhs=V[:, kt, 0:129], start=s_, stop=p_),
                      r=[f"PT{b}", vkey], w=[f"B{4 + i}"])
                if last:
                    for i in range(4):
                        if i in first_kt:
                            epi(c, i)
            stages.append((stage0, stage1a, stage1b, stage2))
    n = len(stages)
    offs = (3, 2, 1, 0)
    for step in range(-3, n):
        if mid_hook is not None and step == n // 2:
            mid_hook()
        for si, off in enumerate(offs):
            k = step + off
            if 0 <= k < n:
                stages[k][si]()


def _causal_items(c):
    out = []
    for kt in range(0, 4 * c + 4):
        r_ = kt - 4 * c
        out.append((kt, max(0, r_) * 128, 512, r_ >= 0, False))
    return out


def _moba(self, li):
    from contextlib import ExitStack
    A, dr, cb = self.A, self.dr, self.cb
    scale = 128.0 ** -0.5
    self.atn_it = 0
    with ExitStack() as st:
        hT, hkeys = _mixer_prologue(self, st, li)
        wq = self.sb(st, "wq0", [128, KC, 3, 128], BF16)
        qT = [self.sb(st, f"qT{i}", [128, S], BF16) for i in range(2)]
        kT = [self.sb(st, f"kT{i}", [128, S], BF16) for i in range(2)]
        V = [self.sb(st, f"V{i}", [128, NT, 129], BF16) for i in range(2)]
        oT = self.sb(st, "oT", [128, 4, S], BF16)
        wo = self.sb(st, "wo", [128, 4, D], BF16)
        PT = [self.sb(st, f"PT{i}", [128, 512], BF16) for i in range(2)]
        TMP = [self.sb(st, f"TMP{i}", [128, 512], F32) for i in range(2)]
        DT = [self.sb(st, f"DT{i}", [128, 512], F32) for i in range(2)]
        otok = [self.sb(st, f"otok{i}", [128, 128], BF16) for i in range(2)]
        rden = [self.sb(st, f"rden{i}", [128, 1], F32) for i in range(2)]
        e8t = self.sb(st, "e8t", [128, 1024], BF16)
        mneg = self.sb(st, "mneg", [128, 512], F32)
        mnot = self.sb(st, "mnot", [128, 512], F32)
        kmean = self.sb(st, "kmean", [128, 32], F32)
        kmh = self.sb(st, "kmh", [128, 32], BF16)
        kml = self.sb(st, "kml", [128, 32], BF16)
        kmr = self.sb(st, "kmr", [128, 32], F32)
        gm = self.sb(st, "gm", [128, 512], F32)
        m8 = self.sb(st, "m8", [128, 128], F32)
        sel = self.sb(st, "sel", [128, 512], F32)
        nbb = self.sb(st, "nbb", [128, 512], BF16)
        selbT = [self.sb(st, f"selbT{i}", [128, S], BF16) for i in range(2)]
        A(SP, lambda e: e.dma_start(out=e8t[:], in_=dr["e8"]), w=["e8t"], dma="e8t")
        A(SP, lambda e: e.dma_start(out=mneg[:], in_=dr["moba_neg"]), w=["mneg"], dma="mneg")
        A(SP, lambda e: e.dma_start(out=mnot[:], in_=dr["moba_notown"]), w=["mnot"], dma="mnot")
        for s in range(2):
            A(POOL, lambda e, s=s: e.memset(V[s][:, :, 128:129], 1.0), w=[f"V{s}"])
        A(POOL, lambda e: e.memset(kmean[:], 0.0), w=["kmean"])
        for i in range(2):
            A(DVE, lambda e, i=i: e.memset(selbT[i][:], 0.0), w=[f"selbT{i}"])
        w_in = dr["moba_w_in"][0]
        oi = [0]
        def prep(h):
            s = h % 2
            w3 = [w_in[:, which * 1024 + h * 128: which * 1024 + (h + 1) * 128] for which in range(3)]
            _proj_qkv(self, w3, h, wq, "wq0", qT[s], f"qT{s}", kT[s], f"kT{s}", V[s], f"V{s}", hT, hkeys, kmean=kmean)
            A(DVE, lambda e: e.tensor_copy(out=kmh[:], in_=kmean[:]), r=["kmean"], w=["kmh"])
            A(DVE, lambda e: e.tensor_tensor(out=kmr[:], in0=kmean[:], in1=kmh[:], op=ALU.subtract), r=["kmean", "kmh"], w=["kmr"])
            A(DVE, lambda e: e.tensor_copy(out=kml[:], in_=kmr[:]), r=["kmr"], w=["kml"])
            pg = self.sring.next()
            for t in range(NT):
                A(PE, lambda e, t=t, s=s, pg=pg: e.matmul(self.bank[pg][:, t * 32:(t + 1) * 32], lhsT=qT[s][:, t * 128:(t + 1) * 128],
                                                          rhs=kmh[:], start=True, stop=False), r=[f"qT{s}", "kmh"], w=[f"B{pg}"])
                A(PE, lambda e, t=t, s=s, pg=pg: e.matmul(self.bank[pg][:, t * 32:(t + 1) * 32], lhsT=qT[s][:, t * 128:(t + 1) * 128],
                                                          rhs=kml[:], start=False, stop=True), r=[f"qT{s}", "kml"], w=[f"B{pg}"])
            A(DVE, lambda e, pg=pg: e.tensor_tensor(out=gm[:], in0=self.bank[pg][:], in1=mneg[:], op=ALU.add),
              r=[f"B{pg}", "mneg"], w=["gm"])
            for t in range(NT):
                A(DVE, lambda e, t=t: e.max(out=m8[:, t * 8:(t + 1) * 8], in_=gm[:, t * 32:(t + 1) * 32]), r=["gm"], w=["m8"])
            for t in range(NT):
                A(DVE, lambda e, t=t: e.tensor_scalar(out=sel[:, t * 32:(t + 1) * 32], in0=gm[:, t * 32:(t + 1) * 32],
                                                      scalar1=m8[:, t * 8 + 2:t * 8 + 3], scalar2=None, op0=ALU.is_ge),
                  r=["gm", "m8"], w=["sel"])
            A(DVE, lambda e: e.tensor_scalar(out=sel[:], in0=sel[:], scalar1=-NEGB, scalar2=NEGB, op0=ALU.mult, op1=ALU.add),
              r=["sel"], w=["sel"])
            A(DVE, lambda e: e.tensor_tensor(out=nbb[:], in0=sel[:], in1=mnot[:], op=ALU.mult), r=["sel", "mnot"], w=["nbb"])
            for half in range(2):
                pb = self.sring.next()
                for tt in range(8):
                    t = half * 8 + tt
                    A(PE, lambda e, t=t, tt=tt, pb=pb: e.transpose(out=self.bankbf(pb)[0:32, tt * 128:(tt + 1) * 128],
                                                                  in_=nbb[:, t * 32:(t + 1) * 32], identity=cb[:, 0, :]),
                      r=["nbb", "cb"], w=[f"B{pb}"])
                A(ACT, lambda e, half=half, pb=pb: e.activation(out=selbT[s][0:32, half * 1024:(half + 1) * 1024],
                                                                in_=self.bankbf(pb)[0:32, :], func=AF.Copy),
                  r=[f"B{pb}"], w=[f"selbT{s}"])


        prep(0)
        for h in range(8):
            s = h % 2
            slope = 2.0 ** (-(h + 1))
            def epi(c, i, h=h, s=s):
                ob = oi[0] % 2
                oi[0] += 1
                A(DVE, lambda e, ob=ob, i=i: e.reciprocal(out=rden[ob][:], in_=self.bank[4 + i][:, 128:129]),
                  r=[f"B{4 + i}"], w=[f"rden{ob}"])
                _o_to_oT(self, self.bank[4 + i][:, 0:128], [f"B{4 + i}"], otok[ob], f"otok{ob}", oT, h % 4, 4 * c + i,
                         scale_ap=rden[ob][:, 0:1], scale_keys=[f"rden{ob}"])

            _attn_softmax(self, range(4), [(kT[s], qT[s])], [f"kT{s}", f"qT{s}"], V[s], f"V{s}", scale, slope, _causal_items,
                          (lambda kt: e8t[:, (kt // 2) * 128:(kt // 2 + 1) * 128], selbT[s], ["e8t", f"selbT{s}"]), PT, TMP, DT, epi,
                          mid_hook=((lambda h=h: prep(h + 1)) if h < 7 else None))
            if h % 4 == 3:
                hg = h // 4
                self.out_proj(wo, "wo", oT, 4, dr["moba_w_o"][0, hg * 512:(hg + 1) * 512, :], ["oT"])
        self.P.flush()
    with ExitStack() as st_ln:
        self.layer_norm(st_ln, li, 0)
        self.P.flush()


Builder.moba = _moba


def _rope_ops(self, x1, x2, cos, sin, o1, o2, R, rkeys, in_keys, okey):
    A = self.A
    A(DVE, lambda e: e.tensor_tensor(out=R[0], in0=x1, in1=cos, op=ALU.mult), r=in_keys + ["rope"], w=[rkeys[0]])
    A(DVE, lambda e: e.tensor_tensor(out=R[1], in0=x2, in1=sin, op=ALU.mult), r=in_keys + ["rope"], w=[rkeys[1]])
    A(DVE, lambda e: e.tensor_tensor(out=o1, in0=R[0], in1=R[1], op=ALU.subtract), r=[rkeys[0], rkeys[1]], w=[okey])
    A(DVE, lambda e: e.tensor_tensor(out=R[2], in0=x2, in1=cos, op=ALU.mult), r=in_keys + ["rope"], w=[rkeys[2]])
    A(DVE, lambda e: e.tensor_tensor(out=R[3], in0=x1, in1=sin, op=ALU.mult), r=in_keys + ["rope"], w=[rkeys[3]])
    A(DVE, lambda e: e.tensor_tensor(out=o2, in0=R[2], in1=R[3], op=ALU.add), r=[rkeys[2], rkeys[3]], w=[okey])


def _mla(self, li):
    from contextlib import ExitStack
    A, dr, cb = self.A, self.dr, self.cb
    scale = 192.0 ** -0.5
    self.atn_it = 0
    with ExitStack() as st:
        wuq = self.sb(st, "wuq", [128, 2, 1536], BF16)
        wukv = self.sb(st, "wukv", [128, 2, 2048], BF16)
        cos_t = self.sb(st, "cos_t", [128, NT, 32], F32)
        sin_t = self.sb(st, "sin_t", [128, NT, 32], F32)
        c_qT = self.sb(st, "c_qT", [128, 2, S], BF16)
        c_kvT = self.sb(st, "c_kvT", [128, 2, S], BF16)
        krT = self.sb(st, "krT", [128, S], BF16)
        gains = self.sb(st, "gains", [128, 4], F32)
        Rt = self.sb(st, "Rt", [128, 4, 128], F32)
        with ExitStack() as sth:
            hT, hkeys = _mixer_prologue(self, sth, li)
            w_in = self.sb(sth, "mlawin", [128, KC, 576], BF16)
            ang = self.sb(sth, "ang", [128, NT, 32], F32)
            invf = self.sb(sth, "invf", [128, 32], F32)
            junk = self.sb(sth, "junk", [128, 256], F32)
            ss = [self.sb(sth, f"ss{i}", [128, 4], F32) for i in range(2)]
            cn = [self.sb(sth, f"cn{i}", [128, 512], BF16) for i in range(2)]
            kr = [self.sb(sth, f"kr{i}", [128, 64], BF16) for i in range(2)]
            A(POOL, lambda e: e.dma_start(out=w_in[:], in_=dr["mla_w_in"][0].rearrange("(k p) n -> p k n", p=128)), w=["mlawin"], dma="mlawin")
            A(POOL, lambda e: e.dma_start(out=wuq[:], in_=dr["mla_w_uq"][0].rearrange("(k p) n -> p k n", p=128)), w=["wuq"], dma="wuq")
            for hf in range(2):
                A(POOL, lambda e, hf=hf: e.dma_start(out=wukv[:, :, hf * 1024:(hf + 1) * 1024],
                                                     in_=dr["mla_w_ukv"][0][:, hf * 1024:(hf + 1) * 1024].rearrange("(k p) n -> p k n", p=128)),
                  w=["wukv"], dma="wukv")
            A(SP, lambda e: e.dma_start(out=invf[:], in_=dr["invf"]), w=["invf"], dma="invf")
            A(SP, lambda e: e.dma_start(out=gains[:, 0:2], in_=dr["mla_q_norm"][0:1, :].rearrange("o (j p) -> p (o j)", p=128),
                                        allow_slow_non_contiguous=True), w=["gains"], dma="gains")
            A(SP, lambda e: e.dma_start(out=gains[:, 2:4], in_=dr["mla_kv_norm"][0:1, :].rearrange("o (j p) -> p (o j)", p=128),
                                        allow_slow_non_contiguous=True), w=["gains"], dma="gains")
            for t in range(NT):
                A(DVE, lambda e, t=t: e.tensor_scalar(out=ang[:, t, :], in0=invf[:], scalar1=self.posk[:, t:t + 1], scalar2=None, op0=ALU.mult),
                  r=["invf", "posk"], w=["ang"])
            ki = self.sb(sth, "ki", [128, NT, 32], I32)
            kf = self.sb(sth, "kf", [128, NT, 32], F32)
            mk = self.sb(sth, "mk", [128, NT, 32], F32)
            C1, C2 = 6.28125, 2 * np.pi - 6.28125
            for dst, shift in ((sin_t, 0.0), (cos_t, 0.5 * PI)):
                A(DVE, lambda e, dst=dst, shift=shift: e.tensor_scalar(out=dst[:], in0=ang[:], scalar1=shift, scalar2=None, op0=ALU.add), r=["ang"], w=["rope"])
                A(DVE, lambda e, dst=dst: e.tensor_scalar(out=kf[:], in0=dst[:], scalar1=float(1.0 / (2 * np.pi)), scalar2=None, op0=ALU.mult), r=["rope"], w=["kf"])
                A(DVE, lambda e: e.tensor_copy(out=ki[:], in_=kf[:]), r=["kf"], w=["ki"])
                A(DVE, lambda e: e.tensor_copy(out=kf[:], in_=ki[:]), r=["ki"], w=["kf"])
                A(DVE, lambda e, dst=dst: e.scalar_tensor_tensor(out=dst[:], in0=kf[:], scalar=-C1, in1=dst[:], op0=ALU.mult, op1=ALU.add), r=["kf", "rope"], w=["rope"])
                A(DVE, lambda e, dst=dst: e.scalar_tensor_tensor(out=dst[:], in0=kf[:], scalar=-C2, in1=dst[:], op0=ALU.mult, op1=ALU.add), r=["kf", "rope"], w=["rope"])
                A(DVE, lambda e, dst=dst: e.tensor_scalar(out=mk[:], in0=dst[:], scalar1=PI, scalar2=None, op0=ALU.is_gt), r=["rope"], w=["mk"])
                A(DVE, lambda e, dst=dst: e.scalar_tensor_tensor(out=dst[:], in0=mk[:], scalar=-2 * PI, in1=dst[:], op0=ALU.mult, op1=ALU.add), r=["mk", "rope"], w=["rope"])
                A(DVE, lambda e, dst=dst: e.tensor_scalar(out=mk[:], in0=dst[:], scalar1=-PI, scalar2=None, op0=ALU.is_lt), r=["rope"], w=["mk"])
                A(DVE, lambda e, dst=dst: e.scalar_tensor_tensor(out=dst[:], in0=mk[:], scalar=2 * PI, in1=dst[:], op0=ALU.mult, op1=ALU.add), r=["mk", "rope"], w=["rope"])
                A(DVE, lambda e, dst=dst: e.tensor_scalar(out=dst[:], in0=dst[:], scalar1=-3.1415925, scalar2=3.1415925, op0=ALU.max, op1=ALU.min), r=["rope"], w=["rope"])
            A(ACT, lambda e: e.activation(out=sin_t[:], in_=sin_t[:], func=AF.Sin), r=["rope"], w=["rope"])
            A(ACT, lambda e: e.activation(out=cos_t[:], in_=cos_t[:], func=AF.Sin), r=["rope"], w=["rope"])
            for t in range(NT):
                b = t % 2
                pa, pb2 = self.sring.next(), self.sring.next()
                for k in range(KC):
                    A(PE, lambda e, k=k, t=t, pa=pa: e.matmul(self.bank[pa][:], lhsT=hT[:, k, t * 128:(t + 1) * 128], rhs=w_in[:, k, 0:512],
                                                              start=(k == 0), stop=(k == KC - 1)), r=[hkeys[t], "mlawin"], w=[f"B{pa}"])
                for k in range(KC):
                    A(PE, lambda e, k=k, t=t, pb2=pb2: e.matmul(self.bank[pb2][:, 0:64], lhsT=hT[:, k, t * 128:(t + 1) * 128], rhs=w_in[:, k, 512:576],
                                                                start=(k == 0), stop=(k == KC - 1)), r=[hkeys[t], "mlawin"], w=[f"B{pb2}"])
                for j in range(2):
                    A(ACT, lambda e, j=j, pa=pa, b=b: e.activation(out=junk[:], in_=self.bank[pa][:, j * 256:(j + 1) * 256], func=AF.Square,
                                                                   accum_out=ss[b][:, j:j + 1]), r=[f"B{pa}"], w=["junk", f"ss{b}"])
                A(ACT, lambda e, b=b: e.activation(out=ss[b][:, 2:4], in_=ss[b][:, 0:2], func=AF.Ln, bias=1e-6, scale=1.0 / 256.0), r=[f"ss{b}"], w=[f"ss{b}"])
                A(ACT, lambda e, b=b: e.activation(out=ss[b][:, 2:4], in_=ss[b][:, 2:4], func=AF.Exp, scale=-0.5), r=[f"ss{b}"], w=[f"ss{b}"])
                for j in range(2):
                    A(DVE, lambda e, j=j, pa=pa, b=b: e.tensor_scalar(out=cn[b][:, j * 256:(j + 1) * 256], in0=self.bank[pa][:, j * 256:(j + 1) * 256],
                                                                      scalar1=ss[b][:, 2 + j:3 + j], scalar2=None, op0=ALU.mult),
                      r=[f"B{pa}", f"ss{b}"], w=[f"cn{b}"])
                pt = self.sring.next()
                for j in range(4):
                    A(PE, lambda e, j=j, b=b, pt=pt: e.transpose(out=self.bankbf(pt)[:, j * 128:(j + 1) * 128], in_=cn[b][:, j * 128:(j + 1) * 128],
                                                                 identity=cb[:, 0, :]), r=[f"cn{b}", "cb"], w=[f"B{pt}"])
                for j in range(4):
                    dst = c_qT if j < 2 else c_kvT
                    A(DVE, lambda e, j=j, t=t, pt=pt, dst=dst: e.tensor_scalar(
                        out=dst[:, j % 2, t * 128:(t + 1) * 128], in0=self.bankbf(pt)[:, j * 128:(j + 1) * 128], scalar1=gains[:, j:j + 1],
                        scalar2=None, op0=ALU.mult), r=[f"B{pt}", "gains"], w=["c_qT" if j < 2 else "c_kvT"])
                _rope_ops(self, self.bank[pb2][:, 0:32], self.bank[pb2][:, 32:64], cos_t[:, t, :], sin_t[:, t, :],
                          kr[b][:, 0:32], kr[b][:, 32:64], [Rt[:, i, 0:32] for i in range(4)], [f"Rt{i}" for i in range(4)], [f"B{pb2}"], f"kr{b}")
                pk = self.sring.next()
                A(PE, lambda e, b=b, pk=pk: e.transpose(out=self.bankbf(pk)[0:64, 0:128], in_=kr[b][:], identity=cb[:, 0, :]), r=[f"kr{b}", "cb"], w=[f"B{pk}"])
                A(ACT, lambda e, t=t, pk=pk: e.activation(out=krT[0:64, t * 128:(t + 1) * 128], in_=self.bankbf(pk)[0:64, 0:128], func=AF.Copy),
                  r=[f"B{pk}"], w=["krT"])
            self.P.flush()
        self.sring = Ring("m", [2, 3])
        self.sring_s = Ring("sc", [0, 1])
        qnT = [self.sb(st, f"qnT{i}", [128, S], BF16) for i in range(2)]
        qrT = [self.sb(st, f"qrT{i}", [128, S], BF16) for i in range(2)]
        knT = [self.sb(st, f"knT{i}", [128, S], BF16) for i in range(2)]
        V = [self.sb(st, f"V{i}", [128, NT, 129], BF16) for i in range(2)]
        oT = self.sb(st, "oT", [128, 4, S], BF16)
        wo = self.sb(st, "wo", [128, 4, D], BF16)
        PT = [self.sb(st, f"PT{i}", [128, 512], BF16) for i in range(2)]
        otok = [self.sb(st, f"otok{i}", [128, 128], BF16) for i in range(2)]
        rden = [self.sb(st, f"rden{i}", [128, 1], F32) for i in range(2)]
        qr = [self.sb(st, f"qr{i}", [128, 4, 64], BF16) for i in range(2)]
        for s in range(2):
            A(POOL, lambda e, s=s: e.memset(V[s][:, :, 128:129], 1.0), w=[f"V{s}"])
            A(DVE, lambda e, s=s: e.memset(qrT[s][64:128, :], 0.0), w=[f"qrT{s}"])
        A(DVE, lambda e: e.memset(krT[64:128, :], 0.0), w=["krT"])
        oi = [0]
        qi = 0
        def prep(h):
            nonlocal qi
            s = h % 2
            for c in range(4):
                pb = self.sring.next()
                for k in range(2):
                    A(PE, lambda e, k=k, c=c, pb=pb, h=h: e.matmul(self.bank[pb][:], lhsT=wuq[:, k, h * 192:h * 192 + 128], rhs=c_qT[:, k, c * 512:(c + 1) * 512],
                                                                   start=(k == 0), stop=(k == 1)), r=["wuq", "c_qT"], w=[f"B{pb}"])
                A(ACT, lambda e, c=c, pb=pb, s=s: e.activation(out=qnT[s][:, c * 512:(c + 1) * 512], in_=self.bank[pb][:], func=AF.Copy), r=[f"B{pb}"], w=[f"qnT{s}"])
                pb = self.sring.next()
                for k in range(2):
                    A(PE, lambda e, k=k, c=c, pb=pb, h=h: e.matmul(self.bank[pb][:], lhsT=wukv[:, k, h * 256:h * 256 + 128], rhs=c_kvT[:, k, c * 512:(c + 1) * 512],
                                                                   start=(k == 0), stop=(k == 1)), r=["wukv", "c_kvT"], w=[f"B{pb}"])
                A(DVE, lambda e, c=c, pb=pb, s=s: e.tensor_copy(out=knT[s][:, c * 512:(c + 1) * 512], in_=self.bank[pb][:]), r=[f"B{pb}"], w=[f"knT{s}"])
            for tg in range(4):
                pb = self.sring.next()
                for tt in range(4):
                    t = tg * 4 + tt
                    for k in range(2):
                        A(PE, lambda e, k=k, t=t, tt=tt, pb=pb, h=h: e.matmul(
                            self.bank[pb][:, tt * 128:(tt + 1) * 128], lhsT=c_kvT[:, k, t * 128:(t + 1) * 128], rhs=wukv[:, k, h * 256 + 128:h * 256 + 256],
                            start=(k == 0), stop=(k == 1)), r=["wukv", "c_kvT"], w=[f"B{pb}"])
                A(ACT, lambda e, tg=tg, pb=pb, s=s: e.activation(out=V[s][:, tg * 4:(tg + 1) * 4, 0:128],
                                                                 in_=self.bank[pb][:].rearrange("p (a n) -> p a n", a=4), func=AF.Copy), r=[f"B{pb}"], w=[f"V{s}"])
                pq = self.sring.next()
                for tt in range(4):
                    t = tg * 4 + tt
                    for k in range(2):
                        A(PE, lambda e, k=k, t=t, tt=tt, pq=pq, h=h: e.matmul(
                            self.bank[pq][:, tt * 64:(tt + 1) * 64], lhsT=c_qT[:, k, t * 128:(t + 1) * 128], rhs=wuq[:, k, h * 192 + 128:h * 192 + 192],
                            start=(k == 0), stop=(k == 1)), r=["wuq", "c_qT"], w=[f"B{pq}"])
                qb = qi % 2
                qi += 1
                xv = self.bank[pq][:, 0:256].rearrange("p (a n) -> p a n", a=4)
                _rope_ops(self, xv[:, :, 0:32], xv[:, :, 32:64], cos_t[:, tg * 4:(tg + 1) * 4, :], sin_t[:, tg * 4:(tg + 1) * 4, :],
                          qr[qb][:, :, 0:32], qr[qb][:, :, 32:64], [Rt[:, i, :].rearrange("p (a n) -> p a n", a=4) for i in range(4)],
                          [f"Rt{i}" for i in range(4)], [f"B{pq}"], f"qr{qb}")
                pt = self.sring.next()
                for tt in range(4):
                    A(PE, lambda e, tt=tt, qb=qb, pt=pt: e.transpose(out=self.bankbf(pt)[0:64, tt * 128:(tt + 1) * 128], in_=qr[qb][:, tt, :],
                                                                    identity=cb[:, 0, :]), r=[f"qr{qb}", "cb"], w=[f"B{pt}"])
                A(ACT, lambda e, tg=tg, pt=pt, s=s: e.activation(out=qrT[s][0:64, tg * 512:(tg + 1) * 512], in_=self.bankbf(pt)[0:64, 0:512], func=AF.Copy),
                  r=[f"B{pt}"], w=[f"qrT{s}"])


        prep(0)
        for h in range(8):
            s = h % 2
            def epi(c, i, h=h):
                ob = oi[0] % 2
                oi[0] += 1
                A(DVE, lambda e, ob=ob, i=i: e.reciprocal(out=rden[ob][:], in_=self.bank[4 + i][:, 128:129]), r=[f"B{4 + i}"], w=[f"rden{ob}"])
                _o_to_oT(self, self.bank[4 + i][:, 0:128], [f"B{4 + i}"], otok[ob], f"otok{ob}", oT, h % 4, 4 * c + i,
                         scale_ap=rden[ob][:, 0:1], scale_keys=[f"rden{ob}"])

            _attn_softmax(self, range(4), [(knT[s], qnT[s]), (krT, qrT[s])], [f"knT{s}", f"qnT{s}", "krT", f"qrT{s}"], V[s], f"V{s}",
                          scale, None, _causal_items, None, PT, None, None, epi,
                          mid_hook=((lambda h=h: prep(h + 1)) if h < 7 else None))
            if h % 4 == 3:
                hg = h // 4
                self.out_proj(wo, "wo", oT, 4, dr["mla_w_o"][0, hg * 512:(hg + 1) * 512, :], ["oT"])
        self.P.flush()
    with ExitStack() as st_ln:
        self.layer_norm(st_ln, li, 0)
        self.P.flush()


Builder.mla = _mla


def _proj_fm(self, wcols, wsl, wring, dst, dkey, hT, hkeys, use_act):
    A = self.A
    ws = wring.next()
    A(POOL, lambda e: e.dma_start(out=wsl[ws][:], in_=wcols.rearrange("(k p) n -> p k n", p=128)), w=[f"wsl{ws}"], dma=f"wsl{ws}")
    for c in range(4):
        pb = self.sring.next()
        for k in range(KC):
            A(PE, lambda e, k=k, c=c, pb=pb: e.matmul(self.bank[pb][:], lhsT=wsl[ws][:, k, :], rhs=hT[:, k, c * 512:(c + 1) * 512],
                                                      start=(k == 0), stop=(k == KC - 1)), r=[f"wsl{ws}"] + hkeys[4 * c:4 * c + 4], w=[f"B{pb}"])
        if use_act:
            A(ACT, lambda e, c=c, pb=pb: e.activation(out=dst[:, c * 512:(c + 1) * 512], in_=self.bank[pb][:], func=AF.Copy), r=[f"B{pb}"], w=[dkey])
        else:
            A(DVE, lambda e, c=c, pb=pb: e.tensor_copy(out=dst[:, c * 512:(c + 1) * 512], in_=self.bank[pb][:]), r=[f"B{pb}"], w=[dkey])


def _proj_tm(self, wcols, wsl, wring, V, vkey, hT, hkeys):
    A = self.A
    ws = wring.next()
    A(POOL, lambda e: e.dma_start(out=wsl[ws][:], in_=wcols.rearrange("(k p) n -> p k n", p=128)), w=[f"wsl{ws}"], dma=f"wsl{ws}")
    for tg in range(4):
        pb = self.sring.next()
        for tt in range(4):
            t = tg * 4 + tt
            for k in range(KC):
                A(PE, lambda e, k=k, t=t, tt=tt, pb=pb: e.matmul(self.bank[pb][:, tt * 128:(tt + 1) * 128], lhsT=hT[:, k, t * 128:(t + 1) * 128],
                                                                rhs=wsl[ws][:, k, :], start=(k == 0), stop=(k == KC - 1)),
                  r=[f"wsl{ws}", hkeys[t]], w=[f"B{pb}"])
        if tg % 2 == 0:
            A(ACT, lambda e, tg=tg, pb=pb: e.activation(out=V[:, tg * 4:(tg + 1) * 4, 0:128], in_=self.bank[pb][:].rearrange("p (a n) -> p a n", a=4),
                                                        func=AF.Copy), r=[f"B{pb}"], w=[vkey])
        else:
            A(DVE, lambda e, tg=tg, pb=pb: e.tensor_copy(out=V[:, tg * 4:(tg + 1) * 4, 0:128], in_=self.bank[pb][:].rearrange("p (a n) -> p a n", a=4)),
              r=[f"B{pb}"], w=[vkey])


def _win_items(c):
    out = []
    for kt in range(max(0, 4 * c - 4), 4 * c + 4):
        r_ = kt - 4 * c
        i_lo, i_hi = max(0, r_), min(3, r_ + 4)
        out.append((kt, i_lo * 128, (i_hi + 1) * 128, r_ >= 0, r_ + 4 <= 3))
    return out


def _nsa(self, li):
    from contextlib import ExitStack
    A, dr, cb = self.A, self.dr, self.cb
    scale = 128.0 ** -0.5
    self.atn_it = 0
    w_in = dr["nsa_w_in"][0]
    with ExitStack() as st:
        hT, hkeys = _mixer_prologue(self, st, li)
        oT = self.sb(st, "oT", [128, 4, S], BF16)
        gates = self.sb(st, "gates", [128, NT * 24], F32)
        wring = Ring("w", 2)
        with ExitStack() as sg:
            wg = self.sb(sg, "wg", [128, KC, 24], BF16)
            A(POOL, lambda e: e.dma_start(out=wg[:], in_=w_in[:, 2560:2584].rearrange("(k p) n -> p k n", p=128)), w=["wg"], dma="wg")
            pb = self.sring.next()
            for t in range(NT):
                for k in range(KC):
                    A(PE, lambda e, k=k, t=t, pb=pb: e.matmul(self.bank[pb][:, t * 24:(t + 1) * 24], lhsT=hT[:, k, t * 128:(t + 1) * 128], rhs=wg[:, k, :],
                                                              start=(k == 0), stop=(k == KC - 1)), r=["wg", hkeys[t]], w=[f"B{pb}"])
            A(ACT, lambda e, pb=pb: e.activation(out=gates[:], in_=self.bank[pb][:, 0:NT * 24], func=AF.Sigmoid), r=[f"B{pb}"], w=["gates"])
            self.P.flush()
        for g in range(2):
            with ExitStack() as sgp:
                qT = [self.sb(sgp, f"qT{i}", [128, S], BF16) for i in range(4)]
                kslT = self.sb(sgp, "kslT", [128, S], BF16)
                kwnT = self.sb(sgp, "kwnT", [128, S], BF16)
                vsl = self.sb(sgp, "vsl", [128, NT, 129], BF16)
                vwn = self.sb(sgp, "vwn", [128, NT, 129], BF16)
                ocs = [self.sb(sgp, f"ocs{i}", [128, NT, 128], BF16) for i in range(4)]
                nbT = self.sb(sgp, "nbT", [128, S], BF16)
                A(DVE, lambda e: e.memset(nbT[:], 0.0), w=["nbT"])
                A(POOL, lambda e: e.memset(vsl[:, :, 128:129], 1.0), w=["vsl"])
                A(POOL, lambda e: e.memset(vwn[:, :, 128:129], 1.0), w=["vwn"])
                kvc = lambda which: w_in[:, 1024 + which * 256 + g * 128: 1024 + which * 256 + (g + 1) * 128]
                with ExitStack() as spj:
                    wsl = [self.sb(spj, f"wsl{i}", [128, KC, 128], BF16) for i in range(2)]
                    for r in range(4):
                        hh = g * 4 + r
                        _proj_fm(self, w_in[:, hh * 128:(hh + 1) * 128], wsl, wring, qT[r], f"qT{r}", hT, hkeys, r % 2 == 0)
                    _proj_fm(self, kvc(2), wsl, wring, kslT, "kslT", hT, hkeys, True)
                    _proj_tm(self, kvc(3), wsl, wring, vsl, "vsl", hT, hkeys)
                    _proj_fm(self, kvc(4), wsl, wring, kwnT, "kwnT", hT, hkeys, False)
                    _proj_tm(self, kvc(5), wsl, wring, vwn, "vwn", hT, hkeys)
                    self.P.flush()
                with ExitStack() as sc:
                    kcT = self.sb(sc, "kcT", [128, 128], BF16)
                    vc = self.sb(sc, "vc", [128, 128], BF16)
                    with ExitStack() as sca:
                        wsl = [self.sb(sca, f"wsl{i}", [128, KC, 128], BF16) for i in range(2)]
                        rawT1 = self.sb(sca, "rawT0", [128, S], BF16)
                        rawT = [rawT1, rawT1]
                        w1b = self.sb(sca, "w1b", [128, 32, 128], BF16)
                        w2b = self.sb(sca, "w2b", [128, 128], BF16)
                        peT = self.sb(sca, "peT", [128, 32], F32)
                        peTb = self.sb(sca, "peTb", [128, 32], BF16)
                        b1 = self.sb(sca, "b1", [128, 1], F32)
                        g1 = self.sb(sca, "g1", [128, 128], BF16)
                        for which in range(2):
                            _proj_fm(self, kvc(which), wsl, wring, rawT[which], "rawT0", hT, hkeys, which == 0)
                            A(POOL, lambda e, which=which: e.dma_start(out=w1b[:], in_=dr["nsa_cmp_w1"][0, which].rearrange("(l d) f -> d l f", d=128)),
                              w=["w1b"], dma="w1b")
                            A(POOL, lambda e, which=which: e.dma_start(out=w2b[:], in_=dr["nsa_cmp_w2"][0, which]), w=["w2b"], dma="w2b")
                            A(SP, lambda e, which=which: e.dma_start(out=peT[:], in_=dr["nsa_cmp_pos"][0, which].rearrange("l d -> d l"),
                                                                     allow_slow_non_contiguous=True), w=["peT"], dma="peT")
                            A(DVE, lambda e: e.tensor_copy(out=peTb[:], in_=peT[:]), r=["peT"], w=["peTb"])
                            pz = self.sring.next()
                            for l in range(32):
                                A(PE, lambda e, l=l, pz=pz: e.matmul(self.bank[pz][:, 0:1], lhsT=w1b[:, l, :], rhs=peTb[:, l:l + 1], start=(l == 0), stop=(l == 31)),
                                  r=["w1b", "peTb"], w=[f"B{pz}"])
                            A(DVE, lambda e, pz=pz: e.tensor_copy(out=b1[:], in_=self.bank[pz][:, 0:1]), r=[f"B{pz}"], w=["b1"])
                            ph = self.sring.next()
                            for l in range(32):
                                A(PE, lambda e, l=l, ph=ph, which=which: e.matmul(self.bank[ph][:, 0:127], lhsT=w1b[:, l, :], rhs=rawT[which][:, l:l + 2017:16],
                                                                                 start=(l == 0), stop=(l == 31)), r=["w1b", "rawT0"], w=[f"B{ph}"])
                            A(ACT, lambda e, ph=ph: e.activation(out=g1[:, 0:127], in_=self.bank[ph][:, 0:127], func=AF.Gelu_apprx_tanh, bias=b1[:, 0:1], scale=1.0),
                              r=[f"B{ph}", "b1"], w=["g1"])
                            po = self.sring.next()
                            if which == 0:
                                A(PE, lambda e, po=po: e.matmul(self.bank[po][:, 0:127], lhsT=w2b[:], rhs=g1[:, 0:127], start=True, stop=True), r=["w2b", "g1"], w=[f"B{po}"])
                                A(DVE, lambda e, po=po: e.tensor_copy(out=kcT[:, 0:127], in_=self.bank[po][:, 0:127]), r=[f"B{po}"], w=["kcT"])
                            else:
                                A(PE, lambda e, po=po: e.matmul(self.bank[po][0:127, 0:128], lhsT=g1[:, 0:127], rhs=w2b[:], start=True, stop=True), r=["w2b", "g1"], w=[f"B{po}"])
                                A(DVE, lambda e, po=po: e.tensor_copy(out=vc[0:127, :], in_=self.bank[po][0:127, 0:128]), r=[f"B{po}"], w=["vc"])
                        self.P.flush()
                    with ExitStack() as scl:
                        cmask = self.sb(scl, "cmask", [128, NT * 127], BF16)
                        ovt = self.sb(scl, "ovt", [128, 32], BF16)
                        zer = self.sb(scl, "zer", [128, 512], BF16)
                        Dc = [self.sb(scl, f"Dc{i}", [128, 127], F32) for i in range(2)]
                        tc_ = [self.sb(scl, f"tc{i}", [128, 127], F32) for i in range(2)]
                        pc = [self.sb(scl, f"pc{i}", [128, 127], F32) for i in range(2)]
                        pnb = [self.sb(scl, f"pnb{i}", [128, 127], BF16) for i in range(2)]
                        pnT = [self.sb(scl, f"pnT{i}", [128, 128], BF16) for i in range(2)]
                        sm = [self.sb(scl, f"sm{i}", [128, 8], F32) for i in range(2)]
                        A(SP, lambda e: e.dma_start(out=cmask[:], in_=dr["nsa_cmask"]), w=["cmask"], dma="cmask")
                        A(SP, lambda e: e.dma_start(out=ovt[:], in_=dr["ov"]), w=["ovt"], dma="ovt")
                        A(POOL, lambda e: e.memset(zer[:], 0.0), w=["zer"])
                        A(PE, lambda e: e.matmul(self.bank[4][:], lhsT=cb[:, 4, :], rhs=zer[:], start=True, stop=False, skip_group_check=True), r=["cb", "zer"], w=["B4"])
                        cstages = []
                        ci = 0
                        for r in range(4):
                            hh = g * 4 + r
                            slope = 2.0 ** (-(hh + 1))
                            for t in range(NT):
                                b = ci % 2
                                ci += 1
                                cd = {}

                                def ca1(cd=cd, t=t, r=r, b=b):
                                    pS = self.sring.next()
                                    cd["pS"] = pS
                                    A(PE, lambda e: e.matmul(self.bank[pS][:, 0:127], lhsT=qT[r][:, t * 128:(t + 1) * 128], rhs=kcT[:, 0:127], start=True, stop=True),
                                      r=[f"qT{r}", "kcT"], w=[f"B{pS}"])
                                    A(ACT, lambda e: e.activation(out=Dc[b][:], in_=self.posq[:, 31:2048:16], func=AF.Abs, bias=self.nposk[:, t:t + 1], scale=1.0),
                                      r=["posq", "posk"], w=[f"Dc{b}"])

                                def ca2(cd=cd, t=t, r=r, b=b, slope=slope):
                                    pS = cd["pS"]
                                    A(DVE, lambda e: e.scalar_tensor_tensor(out=tc_[b][:], in0=Dc[b][:], scalar=-slope / scale, in1=self.bank[pS][:, 0:127],
                                                                            op0=ALU.mult, op1=ALU.add), r=[f"Dc{b}", f"B{pS}"], w=[f"tc{b}"])
                                    A(DVE, lambda e: e.reduce_max(out=sm[b][:, 0:1], in_=tc_[b][:], axis=AX.X), r=[f"tc{b}"], w=[f"sm{b}"])
                                    A(DVE, lambda e: e.tensor_scalar(out=sm[b][:, 1:2], in0=sm[b][:, 0:1], scalar1=-scale, scalar2=None, op0=ALU.mult), r=[f"sm{b}"], w=[f"sm{b}"])
                                    A(ACT, lambda e: e.activation(out=pc[b][:], in_=tc_[b][:], func=AF.Exp, bias=sm[b][:, 1:2], scale=scale), r=[f"tc{b}", f"sm{b}"], w=[f"pc{b}"])

                                def cb_(cd=cd, t=t, r=r, b=b, hh=hh):
                                    A(DVE, lambda e: e.tensor_tensor(out=pc[b][:], in0=pc[b][:], in1=cmask[:, t * 127:(t + 1) * 127], op=ALU.mult), r=[f"pc{b}", "cmask"], w=[f"pc{b}"])
                                    A(DVE, lambda e: e.reduce_sum(out=sm[b][:, 2:3], in_=pc[b][:], axis=AX.X), r=[f"pc{b}"], w=[f"sm{b}"])
                                    A(DVE, lambda e: e.tensor_scalar(out=sm[b][:, 2:3], in0=sm[b][:, 2:3], scalar1=1e-30, scalar2=None, op0=ALU.max), r=[f"sm{b}"], w=[f"sm{b}"])
                                    A(DVE, lambda e: e.reciprocal(out=sm[b][:, 3:4], in_=sm[b][:, 2:3]), r=[f"sm{b}"], w=[f"sm{b}"])
                                    A(DVE, lambda e: e.tensor_scalar(out=pnb[b][:], in0=pc[b][:], scalar1=sm[b][:, 3:4], scalar2=None, op0=ALU.mult), r=[f"pc{b}", f"sm{b}"], w=[f"pnb{b}"])
                                    pt = self.sring.next()
                                    A(PE, lambda e: e.transpose(out=self.bankbf(pt)[0:127, 0:128], in_=pnb[b][:], identity=cb[:, 0, :]), r=[f"pnb{b}", "cb"], w=[f"B{pt}"])
                                    A(ACT, lambda e: e.activation(out=pnT[b][0:127, :], in_=self.bankbf(pt)[0:127, 0:128], func=AF.Copy), r=[f"B{pt}"], w=[f"pnT{b}"])
                                    po = self.sring.next()
                                    A(PE, lambda e: e.matmul(self.bank[po][:, 0:128], lhsT=pnT[b][0:127, :], rhs=vc[0:127, :], start=True, stop=True), r=[f"pnT{b}", "vc"], w=[f"B{po}"])
                                    A(PE, lambda e: e.matmul(self.bank[4][:, t * 32:(t + 1) * 32], lhsT=pnT[b][0:127, :], rhs=ovt[0:127, :], start=False, stop=False,
                                                             skip_group_check=True), r=[f"pnT{b}", "ovt"], w=["B4"])
                                    A(ACT, lambda e: e.activation(out=ocs[r][:, t, :], in_=self.bank[po][:, 0:128], func=AF.Copy,
                                                                  scale=gates[:, t * 24 + hh:t * 24 + hh + 1]), r=[f"B{po}", "gates"], w=[f"ocs{r}"])
                                cstages.append((ca1, ca2, cb_))
                        ncs = len(cstages)
                        for step in range(-2, ncs):
                            for si, off in enumerate((2, 1, 0)):
                                k = step + off
                                if 0 <= k < ncs:
                                    cstages[k][si]()
                        self.P.flush()
                    impm = self.sb(sc, "impm", [128, 512], F32)
                    work = self.sb(sc, "work", [128, 512], F32)
                    nval = self.sb(sc, "nval", [128, 512], F32)
                    nbon = self.sb(sc, "nbon", [128, 512], F32)
                    m8a = self.sb(sc, "m8a", [128, 128], F32)
                    m8b = self.sb(sc, "m8b", [128, 128], F32)
                    selt = self.sb(sc, "selt", [128, 512], F32)
                    nbb = self.sb(sc, "nbb", [128, 512], BF16)
                    A(SP, lambda e: e.dma_start(out=nval[:], in_=dr["nsa_valid"]), w=["nval"], dma="nval")
                    A(SP, lambda e: e.dma_start(out=nbon[:], in_=dr["nsa_bonus"]), w=["nbon"], dma="nbon")
                    A(DVE, lambda e: e.tensor_tensor(out=impm[:], in0=self.bank[4][:], in1=nval[:], op=ALU.mult), r=["B4", "nval"], w=["impm"])
                    A(DVE, lambda e: e.tensor_tensor(out=impm[:], in0=impm[:], in1=nbon[:], op=ALU.add), r=["impm", "nbon"], w=["impm"])
                    for t in range(NT):
                        sl = slice(t * 32, (t + 1) * 32)
                        s8 = slice(t * 8, (t + 1) * 8)
                        A(DVE, lambda e, sl=sl, s8=s8: e.max(out=m8a[:, s8], in_=impm[:, sl]), r=["impm"], w=["m8a"])
                        A(DVE, lambda e, sl=sl, s8=s8: e.match_replace(out=work[:, sl], in_to_replace=m8a[:, s8], in_values=impm[:, sl], imm_value=-3.0e38),
                          r=["impm", "m8a"], w=["work"])
                        A(DVE, lambda e, sl=sl, s8=s8: e.max(out=m8b[:, s8], in_=work[:, sl]), r=["work"], w=["m8b"])
                        A(DVE, lambda e, sl=sl, t=t: e.tensor_scalar(out=selt[:, sl], in0=impm[:, sl], scalar1=m8b[:, t * 8 + 7:t * 8 + 8], scalar2=None, op0=ALU.is_ge),
                          r=["impm", "m8b"], w=["selt"])
                    A(DVE, lambda e: e.tensor_tensor(out=selt[:], in0=selt[:], in1=nval[:], op=ALU.mult), r=["selt", "nval"], w=["selt"])
                    A(DVE, lambda e: e.tensor_scalar(out=nbb[:], in0=selt[:], scalar1=-NEGB, scalar2=NEGB, op0=ALU.mult, op1=ALU.add), r=["selt"], w=["nbb"])
                    for q4 in range(2):
                        pb = self.sring.next()
                        for tt in range(8):
                            t = q4 * 8 + tt
                            A(PE, lambda e, t=t, tt=tt, pb=pb: e.transpose(out=self.bankbf(pb)[0:32, tt * 128:(tt + 1) * 128], in_=nbb[:, t * 32:(t + 1) * 32],
                                                                          identity=cb[:, 0, :]), r=["nbb", "cb"], w=[f"B{pb}"])
                        A(ACT, lambda e, q4=q4, pb=pb: e.activation(out=nbT[0:32, q4 * 1024:(q4 + 1) * 1024], in_=self.bankbf(pb)[0:32, :], func=AF.Copy),
                          r=[f"B{pb}"], w=["nbT"])
                    self.P.flush()
                with ExitStack() as sw:
                    e32t = self.sb(sw, "e32t", [128, 2048], BF16)
                    PT = [self.sb(sw, f"PT{i}", [128, 512], BF16) for i in range(2)]
                    TMP = [self.sb(sw, f"TMP{i}", [128, 512], F32) for i in range(2)]
                    DT = [self.sb(sw, f"DT{i}", [128, 512], F32) for i in range(2)]
                    otok = [self.sb(sw, f"otok{i}", [128, 128], BF16) for i in range(2)]
                    rd = [self.sb(sw, f"rd{i}", [128, 2], F32) for i in range(2)]
                    A(SP, lambda e: e.dma_start(out=e32t[:], in_=dr["e32"]), w=["e32t"], dma="e32t")
                    oi = [0]
                    for r in range(4):
                        hh = g * 4 + r
                        slope = 2.0 ** (-(hh + 1))

                        def epi_sel(c, i, r=r, hh=hh):
                            ob = oi[0] % 2
                            oi[0] += 1
                            t = 4 * c + i
                            A(DVE, lambda e: e.reciprocal(out=rd[ob][:, 0:1], in_=self.bank[4 + i][:, 128:129]), r=[f"B{4 + i}"], w=[f"rd{ob}"])
                            A(DVE, lambda e: e.tensor_tensor(out=rd[ob][:, 1:2], in0=rd[ob][:, 0:1], in1=gates[:, t * 24 + 8 + hh:t * 24 + 9 + hh], op=ALU.mult),
                              r=[f"rd{ob}", "gates"], w=[f"rd{ob}"])
                            A(DVE, lambda e: e.scalar_tensor_tensor(out=ocs[r][:, t, :], in0=self.bank[4 + i][:, 0:128], scalar=rd[ob][:, 1:2], in1=ocs[r][:, t, :],
                                                                    op0=ALU.mult, op1=ALU.add), r=[f"B{4 + i}", f"rd{ob}", f"ocs{r}"], w=[f"ocs{r}"])

                        def epi_win(c, i, r=r, hh=hh):
                            ob = oi[0] % 2
                            oi[0] += 1
                            t = 4 * c + i
                            A(DVE, lambda e: e.reciprocal(out=rd[ob][:, 0:1], in_=self.bank[4 + i][:, 128:129]), r=[f"B{4 + i}"], w=[f"rd{ob}"])
                            A(DVE, lambda e: e.tensor_tensor(out=rd[ob][:, 1:2], in0=rd[ob][:, 0:1], in1=gates[:, t * 24 + 16 + hh:t * 24 + 17 + hh], op=ALU.mult),
                              r=[f"rd{ob}", "gates"], w=[f"rd{ob}"])
                            A(DVE, lambda e: e.scalar_tensor_tensor(out=otok[ob][:], in0=self.bank[4 + i][:, 0:128], scalar=rd[ob][:, 1:2], in1=ocs[r][:, t, :],
                                                                    op0=ALU.mult, op1=ALU.add), r=[f"B{4 + i}", f"rd{ob}", f"ocs{r}"], w=[f"otok{ob}"])
                            pb = self.sring.next()
                            A(PE, lambda e, pb=pb: e.transpose(out=self.bankbf(pb)[:, 0:128], in_=otok[ob][:], identity=cb[:, 0, :]), r=[f"otok{ob}", "cb"], w=[f"B{pb}"])
                            A(ACT, lambda e, pb=pb: e.activation(out=oT[:, r, t * 128:(t + 1) * 128], in_=self.bankbf(pb)[:, 0:128], func=AF.Copy), r=[f"B{pb}"], w=["oT"])

                        _attn_softmax(self, range(4), [(kslT, qT[r])], ["kslT", f"qT{r}"], vsl, "vsl", scale, slope, _causal_items,
                                      (lambda kt: e32t[:, kt * 128:(kt + 1) * 128], nbT, ["e32t", "nbT"]), PT, TMP, DT, epi_sel)
                        _attn_softmax(self, range(4), [(kwnT, qT[r])], ["kwnT", f"qT{r}"], vwn, "vwn", scale, slope, _win_items,
                                      None, PT, TMP, DT, epi_win)
                    self.P.flush()
            with ExitStack() as so:
                wo = self.sb(so, "wo", [128, 4, D], BF16)
                self.out_proj(wo, "wo", oT, 4, dr["nsa_w_o"][0, g * 512:(g + 1) * 512, :], ["oT"])
                self.P.flush()
        self.P.flush()
    with ExitStack() as st_ln:
        self.layer_norm(st_ln, li, 0)
        self.P.flush()


Builder.nsa = _nsa
```

```python
import numpy as np
import ml_dtypes
import concourse.bass as bass
import concourse.mybir as mybir
from concourse.bass_utils import run_bass_kernel_spmd

F32 = mybir.dt.float32
BF16 = mybir.dt.bfloat16
I32 = mybir.dt.int32
AF = mybir.ActivationFunctionType
ALU = mybir.AluOpType
AX = mybir.AxisListType

PE, ACT, DVE, POOL, SP = "tensor", "scalar", "vector", "gpsimd", "sync"
ENGINES = (PE, ACT, DVE, POOL, SP)


class Prog:
    def __init__(self, nc, es):
        self.nc = nc
        self.es = es
        self.sems = {}
        for e in ENGINES:
            self.sems[("eng", e)] = es.enter_context(nc.semaphore("s_" + e))
        self.counters = {e: 0 for e in ENGINES}
        self.chan_count = {}
        self.water = {e: {} for e in ENGINES}
        self.n_total = 0
        self._reset()

    def _reset(self):
        self.ops = []
        self.writers = {}
        self.readers = {}

    def add(self, eng, fn, reads=(), writes=(), dma=None):
        idx = len(self.ops)
        deps = set()
        src = eng if dma is None else ("dma", dma)
        for b in reads:
            deps.update(self.writers.get(b, {}).values())
        for b in writes:
            deps.update(self.writers.get(b, {}).values())
            deps.update(self.readers.get(b, {}).values())
        op = dict(eng=eng, fn=fn, deps=deps, dma=dma, ticket=None)
        if dma is not None:
            self.chan_count[dma] = self.chan_count.get(dma, 0) + 1
            op["dma_val"] = 16 * self.chan_count[dma]
        self.ops.append(op)
        for b in writes:
            self.writers.setdefault(b, {})[src] = idx
        for b in reads:
            self.readers.setdefault(b, {})[src] = idx
        return idx

    def flush(self):
        nc = self.nc
        ops = self.ops
        needed = set()
        last = {}
        for i, op in enumerate(ops):
            if op["dma"] is None:
                last[op["eng"]] = i
        needed.update(last.values())
        for op in ops:
            for d in op["deps"]:
                dop = ops[d]
                if dop["dma"] is None and not (dop["eng"] == PE and op["eng"] == PE and op["dma"] is None):
                    needed.add(d)
        for i, op in enumerate(ops):
            if op["dma"] is None and i in needed:
                self.counters[op["eng"]] += 1
                op["ticket"] = self.counters[op["eng"]]
        for c in self.chan_count:
            if ("dma", c) not in self.sems:
                self.sems[("dma", c)] = self.es.enter_context(nc.semaphore("d_" + str(c)))
        streams = {e: [] for e in ENGINES}
        for i, op in enumerate(ops):
            e = op["eng"]
            waits = {}
            for d in op["deps"]:
                dop = ops[d]
                if dop["dma"] is not None:
                    key, val = ("dma", dop["dma"]), dop["dma_val"]
                else:
                    if dop["eng"] == PE and e == PE and op["dma"] is None:
                        continue
                    key, val = ("eng", dop["eng"]), dop["ticket"]
                if val > waits.get(key, 0):
                    waits[key] = val
            wl = []
            for key, val in waits.items():
                if self.water[e].get(key, 0) >= val:
                    continue
                self.water[e][key] = val
                wl.append((key, val))
            streams[e].append((op, wl))
        final = [(("eng", e), self.counters[e]) for e in ENGINES] + \
                [(("dma", c), 16 * n) for c, n in self.chan_count.items()]
        sems = self.sems
        with nc.Block() as block:
            def mk(ename):
                def body(eng):
                    for op, wl in streams[ename]:
                        for key, val in wl:
                            eng.wait_ge(sems[key], val)
                        ins = op["fn"](eng)
                        if op["dma"] is not None:
                            ins.then_inc(sems[("dma", op["dma"])], 16)
                        elif op["ticket"] is not None:
                            ins.then_inc(sems[("eng", ename)], 1)
                    for key, val in final:
                        if val == 0 or self.water[ename].get(key, 0) >= val:
                            continue
                        eng.wait_ge(sems[key], val)
                        self.water[ename][key] = val
                return body
            block.tensor(mk(PE))
            block.scalar(mk(ACT))
            block.vector(mk(DVE))
            block.gpsimd(mk(POOL))
            block.sync(mk(SP))
        self.n_total += len(ops)
        self._reset()


S, D, NT, KC, DFF, NFC = 2048, 1024, 16, 8, 2816, 22
ALPHA = 8.0 ** 0.25
NEGB = -30000.0
PI = float(np.pi)


def host_consts():
    kp = np.arange(128)[:, None]
    qf = np.arange(128)[None, :]
    cb = np.zeros((128, 8, 128), np.float32)
    cb[:, 0] = np.eye(128)
    cb[:, 1] = np.where(kp <= qf, 0.0, NEGB)
    cb[:, 2] = np.where(kp > qf, 0.0, NEGB)
    cb[:, 3] = -(kp > qf).astype(np.float32)
    cb[:, 4] = 1.0
    cb[:, 5] = -1.0
    cb[:, 6] = (kp < qf).astype(np.float32)
    e8 = np.zeros((128, 8, 128), np.float32)
    for n in range(8):
        e8[n, n, :] = 1.0
    e32 = np.zeros((128, 16, 128), np.float32)
    for kt in range(16):
        for k in range(128):
            e32[2 * kt + k // 64, kt, k] = 1.0
    t_idx = np.arange(16)[None, :, None]
    n_idx = np.arange(32)[None, None, :]
    own = t_idx // 2
    moba_neg = np.broadcast_to(np.where(n_idx < own, 0.0, -1e30), (128, 16, 32)).astype(np.float32)
    moba_notown = np.broadcast_to((n_idx != own).astype(np.float32), (128, 16, 32)).astype(np.float32)
    invf = (np.float32(10000.0) ** (-np.arange(32, dtype=np.float32) / np.float32(32))).astype(np.float32)
    invf_bc = np.broadcast_to(invf[None, :], (128, 32)).astype(np.float32)
    q_idx = (np.arange(16)[None, :] * 128 + np.arange(128)[:, None])
    c_end = np.arange(127) * 16 + 31
    nsa_cmask = (c_end[None, None, :] <= q_idx[:, :, None]).astype(np.float32)
    qblk = q_idx // 64
    jj = np.arange(32)[None, None, :]
    valid = jj <= qblk[:, :, None]
    forced = (jj == 0) | (jj == qblk[:, :, None]) | (jj == qblk[:, :, None] - 1)
    nsa_bonus = np.where(valid, 100.0 * forced, -1e30).astype(np.float32)
    nsa_valid = valid.astype(np.float32)
    c_start = np.arange(127) * 16
    j_start = np.arange(32) * 64
    overlap = ((c_start[:, None] < j_start[None, :] + 64) & (c_end[:, None] >= j_start[None, :])).astype(np.float32)
    ov = np.zeros((128, 32), np.float32)
    ov[:127] = overlap
    bf = ml_dtypes.bfloat16
    return {
        "cb": cb.astype(bf), "e8": e8.reshape(128, 1024).astype(bf), "e32": e32.reshape(128, 2048).astype(bf),
        "moba_neg": moba_neg.reshape(128, 512), "moba_notown": moba_notown.reshape(128, 512),
        "invf": invf_bc, "nsa_cmask": nsa_cmask.reshape(128, 16 * 127).astype(bf), "nsa_bonus": nsa_bonus.reshape(128, 512),
        "nsa_valid": nsa_valid.reshape(128, 512), "ov": ov.astype(bf),
    }


WEIGHT_SPECS = [
    ("mod_w", [4, 1024, 6144]), ("mod_b", [4, 6144]), ("ln_g", [4, 2, 1024]), ("ln_b", [4, 2, 1024]),
    ("ffn_w_in", [4, 1024, 5632]), ("ffn_conv_w", [4, 3, 2816]), ("ffn_conv_b", [4, 2816]),
    ("ffn_w_out", [4, 2816, 1024]),
    ("mla_w_in", [1, 1024, 576]), ("mla_q_norm", [1, 256]), ("mla_w_uq", [1, 256, 1536]),
    ("mla_kv_norm", [1, 256]), ("mla_w_ukv", [1, 256, 2048]), ("mla_w_o", [1, 1024, 1024]),
    ("moba_w_in", [1, 1024, 3072]), ("moba_w_o", [1, 1024, 1024]),
    ("nsa_w_in", [1, 1024, 2584]), ("nsa_cmp_pos", [1, 2, 32, 128]), ("nsa_cmp_w1", [1, 2, 4096, 128]),
    ("nsa_cmp_w2", [1, 2, 128, 128]), ("nsa_w_o", [1, 1024, 1024]),
    ("sb_w_in", [1, 1024, 3072]), ("sb_w_o", [1, 1024, 1024]),
]
CONST_SPECS = [
    ("cb", [128, 8, 128], BF16), ("e8", [128, 1024], BF16), ("e32", [128, 2048], BF16),
    ("moba_neg", [128, 512], F32), ("moba_notown", [128, 512], F32), ("invf", [128, 32], F32),
    ("nsa_cmask", [128, 16 * 127], BF16), ("nsa_bonus", [128, 512], F32), ("nsa_valid", [128, 512], F32),
    ("ov", [128, 32], BF16),
]


class Ring:
    def __init__(self, name, n):
        self.vals = list(range(n)) if isinstance(n, int) else list(n)
        self.name, self.n, self.i = name, len(self.vals), 0

    def next(self):
        k = self.vals[self.i % self.n]
        self.i += 1
        return k


class Builder:
    def __init__(self, layers=(0, 1, 2, 3), taps=None):
        from contextlib import ExitStack
        self.layers = layers
        self.nc = nc = bass.Bass("TRN2", target_bir_lowering=False)
        self.es = ExitStack()
        self.P = Prog(nc, self.es)
        self.dr = {}
        self.dr["x"] = nc.dram_tensor("x", [S, D], F32, kind="ExternalInput").ap()
        self.dr["c"] = nc.dram_tensor("c", [1, D], F32, kind="ExternalInput").ap()
        self.dr["positions"] = nc.dram_tensor("positions", [1, S], I32, kind="ExternalInput").ap()
        for name, shp in WEIGHT_SPECS:
            self.dr[name] = nc.dram_tensor(name, shp, F32, kind="ExternalInput").ap()
        for name, shp, dt in CONST_SPECS:
            self.dr[name] = nc.dram_tensor(name, shp, dt, kind="ExternalInput").ap()
        self.dr["out"] = nc.dram_tensor("out", [S, D], F32, kind="ExternalOutput").ap()
        self.uid = 0

    def sb(self, st, name, shape, dt):
        self.uid += 1
        return st.enter_context(self.nc.sbuf_tensor(f"{name}_{self.uid}", shape, dt))

    def A(self, eng, fn, r=(), w=(), dma=None):
        return self.P.add(eng, fn, reads=r, writes=w, dma=dma)

    def psum_setup(self, st):
        self.uid += 1
        self.bank = [st.enter_context(self.nc.psum_tensor(f"bank{i}_{self.uid}", [128, 512], F32)) for i in range(8)]

    def bankbf(self, i):
        return self.bank[i][:].bitcast(BF16)

    def build(self):
        nc, A, dr = self.nc, self.A, self.dr
        from contextlib import ExitStack
        es = self.es
        self.psum_setup(es)
        self.x = self.sb(es, "x", [128, NT, D], F32)
        self.cb = self.sb(es, "cb", [128, 8, 128], BF16)
        self.posq = self.sb(es, "posq", [128, S], F32)
        self.posk = self.sb(es, "posk", [128, NT], F32)
        self.nposk = self.sb(es, "nposk", [128, NT], F32)
        self.crep = self.sb(es, "crep", [128, KC, 128], BF16)
        self.gate = self.sb(es, "gate", [128, D], F32)
        self.ms = self.sb(es, "ms", [128, 2, D], F32)
        self.small = self.sb(es, "small", [128, 64], F32)
        x, cb = self.x, self.cb
        with ExitStack() as st:
            posi = self.sb(st, "posi", [128, S], I32)
            poski = self.sb(st, "poski", [128, NT], I32)
            cT = self.sb(st, "cT", [128, KC], F32)
            cA = self.sb(st, "cA", [128, KC], F32)
            A(SP, lambda e: e.dma_start(out=cb[:], in_=dr["cb"]), w=["cb"], dma="cb")
            A(SP, lambda e: e.dma_start(out=posi[:], in_=dr["positions"].partition_broadcast(128)), w=["posi"], dma="posi")
            A(SP, lambda e: e.dma_start(out=poski[:], in_=dr["positions"].rearrange("o (t p) -> p (o t)", p=128),
                                        allow_slow_non_contiguous=True), w=["poski"], dma="poski")
            A(SP, lambda e: e.dma_start(out=cT[:], in_=dr["c"].rearrange("o (k p) -> p (o k)", p=128),
                                        allow_slow_non_contiguous=True), w=["cT"], dma="cT")
            for t in range(NT):
                A(SP, lambda e, t=t: e.dma_start(out=x[:, t, :], in_=dr["x"][t * 128:(t + 1) * 128, :]),
                  w=[f"x{t}"], dma=f"x{t % 4}")
            A(DVE, lambda e: e.tensor_copy(out=self.posq[:], in_=posi[:]), r=["posi"], w=["posq"])
            A(DVE, lambda e: e.tensor_copy(out=self.posk[:], in_=poski[:]), r=["poski"], w=["posk"])
            A(DVE, lambda e: e.tensor_scalar(out=self.nposk[:], in0=self.posk[:], scalar1=-1.0, scalar2=None, op0=ALU.mult),
              r=["posk"], w=["posk"])
            A(ACT, lambda e: e.activation(out=cA[:], in_=cT[:], func=AF.Silu), r=["cT"], w=["cA"])
            for k in range(KC):
                A(DVE, lambda e, k=k: e.tensor_scalar(out=self.crep[:, k, :], in0=cb[:, 4, :], scalar1=cA[:, k:k + 1],
                                                      scalar2=None, op0=ALU.mult), r=["cA", "cb"], w=["crep"])
            self.subs = []
            for li in self.layers:
                if isinstance(li, tuple):
                    self.subs.append((li[1], 1))
                else:
                    self.subs += [(li, 0), (li, 1)]
            self.sub_i = 0
            for th in self.mod_slabs(st, self.subs[0][0], self.subs[0][1]):
                th()
            self.P.flush()
        for li in self.layers:
            self.layer(li)
        for t in range(NT):
            A(SP, lambda e, t=t: e.dma_start(out=dr["out"][t * 128:(t + 1) * 128, :], in_=x[:, t, :]),
              r=[f"x{t}"], w=[f"out{t}"], dma=f"o{t % 4}")
        self.P.flush()
        return nc

    def mod_slabs(self, st, li, half):
        A, dr = self.A, self.dr
        wsl = [self.sb(st, f"modw{i}", [128, KC, 256], BF16) for i in range(2)]
        bsl = [self.sb(st, f"modb{i}", [128, 256], F32) for i in range(2)]
        thunks = []
        for j in range(12):
            def slab(j=j):
                s = j % 2
                col0 = half * 3072 + j * 256
                A(POOL, lambda e: e.dma_start(
                    out=wsl[s][:], in_=dr["mod_w"][li, :, col0:col0 + 256].rearrange("(k p) n -> p k n", p=128)),
                  w=[f"modw{s}"], dma=f"modw{s}")
                A(SP, lambda e: e.dma_start(
                    out=bsl[s][:], in_=dr["mod_b"][li:li + 1, col0:col0 + 256].partition_broadcast(128)),
                  w=[f"modb{s}"], dma=f"modb{s}")
                pb = 6 + s
                for k in range(KC):
                    A(PE, lambda e, k=k: e.matmul(self.bank[pb][:, 0:256], lhsT=self.crep[:, k, :],
                                                  rhs=wsl[s][:, k, :], start=(k == 0), stop=(k == KC - 1)),
                      r=["crep", f"modw{s}"], w=[f"B{pb}"])
                comp, off = (j * 256) // 1024, (j * 256) % 1024
                dst = self.ms[:, comp, off:off + 256] if comp < 2 else self.gate[:, off:off + 256]
                A(DVE, lambda e: e.tensor_tensor(out=dst, in0=self.bank[pb][:, 0:256], in1=bsl[s][:], op=ALU.add),
                  r=[f"B{pb}", f"modb{s}"], w=["modt"])
            thunks.append(slab)

        def fin():
            A(DVE, lambda e: e.tensor_scalar(out=self.ms[:, 1, :], in0=self.ms[:, 1, :], scalar1=1.0, scalar2=None,
                                             op0=ALU.add), r=["modt"], w=["modt"])
        thunks.append(fin)
        return thunks

    def build_hT(self, st, hT):
        A, x, modt = self.A, self.x, self.ms
        tmp = [self.sb(st, f"htmp{i}", [128, D], F32) for i in range(2)]
        hb = [self.sb(st, f"hb{i}", [128, D], BF16) for i in range(2)]
        for t in range(NT):
            s = t % 2
            A(DVE, lambda e, t=t, s=s: e.tensor_tensor(out=tmp[s][:], in0=x[:, t, :], in1=modt[:, 1, :], op=ALU.mult),
              r=[f"x{t}", "modt"], w=[f"htmp{s}"])
            A(DVE, lambda e, s=s: e.tensor_tensor(out=hb[s][:], in0=tmp[s][:], in1=modt[:, 0, :], op=ALU.add),
              r=[f"htmp{s}", "modt"], w=[f"hb{s}"])
            pb = 6 + s
            for k in range(KC):
                A(PE, lambda e, s=s, k=k, pb=pb: e.transpose(out=self.bankbf(pb)[:, k * 128:(k + 1) * 128],
                                                             in_=hb[s][:, k * 128:(k + 1) * 128], identity=self.cb[:, 0, :]),
                  r=[f"hb{s}", "cb"], w=[f"B{pb}"])
            A(ACT, lambda e, t=t, pb=pb: e.activation(
                out=hT[:, :, t * 128:(t + 1) * 128], in_=self.bankbf(pb).rearrange("p (k n) -> p k n", k=KC), func=AF.Copy),
              r=[f"B{pb}"], w=[f"hT{t}"])
            A(ACT, lambda e, t=t: e.activation(out=x[:, t, :], in_=x[:, t, :], func=AF.Copy, scale=ALPHA),
              r=[f"x{t}"], w=[f"x{t}"])

    def layer_norm(self, st, li, which):
        A, x, dr = self.A, self.x, self.dr
        self.sub_i += 1
        nxt = self.subs[self.sub_i] if self.sub_i < len(self.subs) else None
        slabs = self.mod_slabs(st, nxt[0], nxt[1]) if nxt is not None else []
        self.lng = self.sb(st, "lng", [128, D], F32)
        self.lnb = self.sb(st, "lnb", [128, D], F32)
        A(SP, lambda e: e.dma_start(out=self.lng[:], in_=dr["ln_g"][li, which:which + 1, :].partition_broadcast(128)),
          w=["lng"], dma="lng")
        A(SP, lambda e: e.dma_start(out=self.lnb[:], in_=dr["ln_b"][li, which:which + 1, :].partition_broadcast(128)),
          w=["lnb"], dma="lnb")
        stt = [self.sb(st, f"lnst{i}", [128, 16], F32) for i in range(3)]
        stages = []
        for t in range(NT):
            s = t % 3
            q = stt[s]

            def b_act(t=t, q=q, s=s):
                A(ACT, lambda e: e.activation(out=x[:, t, :], in_=x[:, t, :], func=AF.Identity, bias=q[:, 15:16], scale=q[:, 14:15]),
                  r=[f"x{t}", f"lnst{s}"], w=[f"x{t}"])

            def a1(t=t, q=q, s=s):
                A(DVE, lambda e: e.bn_stats(out=q[:, 0:6], in_=x[:, t, 0:512]), r=[f"x{t}"], w=[f"lnst{s}"])
                A(DVE, lambda e: e.bn_stats(out=q[:, 6:12], in_=x[:, t, 512:1024]), r=[f"x{t}"], w=[f"lnst{s}"])
                A(DVE, lambda e: e.bn_aggr(out=q[:, 12:14], in_=q[:, 0:12]), r=[f"lnst{s}"], w=[f"lnst{s}"])
                A(ACT, lambda e: e.activation(out=q[:, 14:15], in_=q[:, 13:14], func=AF.Ln, bias=1e-5, scale=1.0), r=[f"lnst{s}"], w=[f"lnst{s}"])
                A(ACT, lambda e: e.activation(out=q[:, 14:15], in_=q[:, 14:15], func=AF.Exp, scale=-0.5), r=[f"lnst{s}"], w=[f"lnst{s}"])

            def a2(t=t, q=q, s=s):
                A(DVE, lambda e: e.scalar_tensor_tensor(out=q[:, 15:16], in0=q[:, 12:13], scalar=-1.0, in1=q[:, 14:15],
                                                        op0=ALU.mult, op1=ALU.mult), r=[f"lnst{s}"], w=[f"lnst{s}"])

            def b_dve(t=t):
                A(DVE, lambda e: e.tensor_tensor(out=x[:, t, :], in0=x[:, t, :], in1=self.lng[:], op=ALU.mult), r=[f"x{t}", "lng"], w=[f"x{t}"])
                A(DVE, lambda e: e.tensor_tensor(out=x[:, t, :], in0=x[:, t, :], in1=self.lnb[:], op=ALU.add), r=[f"x{t}", "lnb"], w=[f"x{t}"])
                if t < len(slabs):
                    slabs[t]()
            stages.append((b_act, a1, a2, b_dve))
        for step in range(-2, NT):
            for si, off in enumerate((0, 2, 1, 0)):
                k = step + off
                if 0 <= k < NT:
                    stages[k][si]()
        for th in slabs[NT:]:
            th()

    def out_proj(self, wo, wkey, srcT, nj, w_dram_rows, src_keys):
        A, x = self.A, self.x
        A(POOL, lambda e: e.dma_start(out=wo[:, 0:nj, :], in_=w_dram_rows.rearrange("(j p) n -> p j n", p=128)),
          w=[wkey], dma=wkey)
        for j in range(nj):
            A(DVE, lambda e, j=j: e.tensor_tensor(out=wo[:, j, :], in0=wo[:, j, :], in1=self.gate[:], op=ALU.mult),
              r=[wkey, "modt"], w=[wkey])
        for t in range(NT):
            for n in range(2):
                pb = 4 + (2 * t + n) % 2
                for j in range(nj):
                    A(PE, lambda e, t=t, n=n, j=j, pb=pb: e.matmul(
                        self.bank[pb][:], lhsT=srcT[:, j, t * 128:(t + 1) * 128], rhs=wo[:, j, n * 512:(n + 1) * 512],
                        start=(j == 0), stop=(j == nj - 1)), r=[wkey] + src_keys, w=[f"B{pb}"])
                A(DVE, lambda e, t=t, n=n, pb=pb: e.tensor_tensor(
                    out=x[:, t, n * 512:(n + 1) * 512], in0=self.bank[pb][:], in1=x[:, t, n * 512:(n + 1) * 512], op=ALU.add),
                  r=[f"B{pb}", f"x{t}"], w=[f"x{t}"])

    def ffn(self, li):
        from contextlib import ExitStack
        A, dr, x = self.A, self.dr, self.x
        with ExitStack() as st:
            hT = self.sb(st, "hT", [128, KC, S], BF16)
            with ExitStack() as st0:
                self.build_hT(st0, hT)
                self.P.flush()
            hkeys = [f"hT{t}" for t in range(NT)]
            cw = self.sb(st, "cw", [128, 3, NFC], F32)
            cbias = self.sb(st, "cbias", [128, NFC], F32)
            for wi in range(3):
                A(SP, lambda e, wi=wi: e.dma_start(out=cw[:, wi, :], in_=dr["ffn_conv_w"][li, wi:wi + 1, :].rearrange("o (j p) -> p (o j)", p=128),
                                                   allow_slow_non_contiguous=True), w=["cw"], dma="cw")
            A(SP, lambda e: e.dma_start(out=cbias[:], in_=dr["ffn_conv_b"][li:li + 1, :].rearrange("o (j p) -> p (o j)", p=128),
                                        allow_slow_non_contiguous=True), w=["cbias"], dma="cbias")
            wab = [self.sb(st, f"wab{i}", [128, KC, 2, 128], BF16) for i in range(2)]
            aS = [self.sb(st, f"aS{i}", [128, S + 2], F32) for i in range(2)]
            tS = [self.sb(st, f"tS{i}", [128, 512], F32) for i in range(2)]
            gT = [self.sb(st, f"gT{i}", [128, 4, S], BF16) for i in range(2)]
            wof = [self.sb(st, f"wof{i}", [128, 4, D], BF16) for i in range(2)]
            for i in range(2):
                A(POOL, lambda e, i=i: e.memset(aS[i][:, 0:2], 0.0), w=[f"aS{i}"])
            groups = [(0, 4), (4, 4), (8, 4), (12, 4), (16, 4), (20, 2)]
            it = 0
            pending_out = None
            for gi, (f0, nj) in enumerate(groups):
                gs = gi % 2
                for jj in range(nj):
                    j = f0 + jj
                    ws = j % 2
                    for half in range(2):
                        c0 = half * DFF + j * 128
                        A(POOL, lambda e, ws=ws, half=half, c0=c0: e.dma_start(
                            out=wab[ws][:, :, half, :], in_=dr["ffn_w_in"][li, :, c0:c0 + 128].rearrange("(k p) n -> p k n", p=128)),
                          w=[f"wab{ws}"], dma=f"wab{ws}")
                    for c in range(4):
                        s2 = it % 2
                        it += 1
                        pa, pbk = (0, 1, 7)[(it - 1) % 3], (2, 3, 6)[(it - 1) % 3]
                        for half, pbank in ((0, pa), (1, pbk)):
                            for k in range(KC):
                                A(PE, lambda e, ws=ws, half=half, pbank=pbank, k=k, c=c: e.matmul(
                                    self.bank[pbank][:], lhsT=wab[ws][:, k, half, :], rhs=hT[:, k, c * 512:(c + 1) * 512],
                                    start=(k == 0), stop=(k == KC - 1)),
                                  r=[f"wab{ws}"] + hkeys[4 * c:4 * c + 4], w=[f"B{pbank}"])
                        a_ = aS[ws]
                        A(ACT, lambda e, a_=a_, pa=pa, c=c: e.activation(out=a_[:, 2 + c * 512:2 + (c + 1) * 512],
                                                                         in_=self.bank[pa][:], func=AF.Copy),
                          r=[f"B{pa}"], w=[f"aS{ws}"])
                        A(DVE, lambda e, a_=a_, s2=s2, j=j, c=c: e.tensor_scalar(
                            out=tS[s2][:], in0=a_[:, c * 512:(c + 1) * 512], scalar1=cw[:, 0, j:j + 1],
                            scalar2=cbias[:, j:j + 1], op0=ALU.mult, op1=ALU.add),
                          r=[f"aS{ws}", "cw", "cbias"], w=[f"tS{s2}"])
                        A(DVE, lambda e, a_=a_, s2=s2, j=j, c=c: e.scalar_tensor_tensor(
                            out=tS[s2][:], in0=a_[:, 1 + c * 512:1 + (c + 1) * 512], scalar=cw[:, 1, j:j + 1], in1=tS[s2][:],
                            op0=ALU.mult, op1=ALU.add), r=[f"aS{ws}", "cw", f"tS{s2}"], w=[f"tS{s2}"])
                        A(DVE, lambda e, a_=a_, s2=s2, j=j, c=c: e.scalar_tensor_tensor(
                            out=tS[s2][:], in0=a_[:, 2 + c * 512:2 + (c + 1) * 512], scalar=cw[:, 2, j:j + 1], in1=tS[s2][:],
                            op0=ALU.mult, op1=ALU.add), r=[f"aS{ws}", "cw", f"tS{s2}"], w=[f"tS{s2}"])
                        A(ACT, lambda e, s2=s2: e.activation(out=tS[s2][:], in_=tS[s2][:], func=AF.Gelu_apprx_tanh),
                          r=[f"tS{s2}"], w=[f"tS{s2}"])
                        A(DVE, lambda e, s2=s2, gs=gs, jj=jj, c=c, pbk=pbk: e.tensor_tensor(
                            out=gT[gs][:, jj, c * 512:(c + 1) * 512], in0=self.bank[pbk][:], in1=tS[s2][:], op=ALU.mult),
                          r=[f"tS{s2}", f"B{pbk}"], w=[f"gT{gs}"])
                if pending_out is not None:
                    self.out_proj(*pending_out)
                pending_out = (wof[gs], f"wof{gs}", gT[gs], nj, dr["ffn_w_out"][li, f0 * 128:(f0 + nj) * 128, :], [f"gT{gs}"])
            self.out_proj(*pending_out)
            self.P.flush()
        with ExitStack() as st_ln:
            self.layer_norm(st_ln, li, 1)
            self.P.flush()

    def layer(self, li):
        if isinstance(li, tuple):
            self.ffn(li[1])
            return
        kind = li % 4
        if kind == 0:
            self.mla(li)
        elif kind == 1:
            self.moba(li)
        elif kind == 2:
            self.nsa(li)
        else:
            self.sbmix(li)
        self.ffn(li)


_CONSTS = None


def kernel(**inputs):
    return run_kernel(inputs, layers=(0, 1, 2, 3))


def run_kernel(inputs, layers, cores=8, x_override=None, trace=False):
    global _CONSTS
    if _CONSTS is None:
        _CONSTS = host_consts()
    b = Builder(layers=layers)
    nc = b.build()
    in_maps = []
    xs = inputs["x"] if x_override is None else x_override
    for ci in range(cores):
        m = {"x": np.ascontiguousarray(xs[ci], dtype=np.float32),
             "c": np.ascontiguousarray(inputs["c"][ci:ci + 1], dtype=np.float32),
             "positions": np.ascontiguousarray(inputs["positions"][ci:ci + 1], dtype=np.int32)}
        for name, _ in WEIGHT_SPECS:
            m[name] = np.ascontiguousarray(inputs[name], dtype=np.float32)
        for name, _, _ in CONST_SPECS:
            m[name] = _CONSTS[name]
        in_maps.append(m)
    res = run_bass_kernel_spmd(nc, in_maps, core_ids=list(range(cores)), **({'trace': True} if trace else {}))
    if trace:
        print('EXEC_TIME_NS', res.exec_time_ns)
    return np.stack([r["out"] for r in res.results], axis=0).astype(np.float32)


def _mixer_prologue(self, st, li):
    from contextlib import ExitStack
    hT = self.sb(st, "hT", [128, KC, S], BF16)
    with ExitStack() as st0:
        self.build_hT(st0, hT)
        self.P.flush()
    self.sring = Ring("s", 4)
    self.sring_s = None
    return hT, [f"hT{t}" for t in range(NT)]


def _proj_qkv(self, w3, h, wq, wkey, qT, qkey, kT, kkey, V, vkey, hT, hkeys, kmean=None):
    A = self.A
    for which in range(3):
        A(POOL, lambda e, which=which: e.dma_start(out=wq[:, :, which, :], in_=w3[which].rearrange("(k p) n -> p k n", p=128)),
          w=[wkey], dma=wkey)
    for which, (dst, dkey) in enumerate(((qT, qkey), (kT, kkey))):
        for c in range(4):
            pb = self.sring.next()
            for k in range(KC):
                A(PE, lambda e, which=which, k=k, c=c, pb=pb: e.matmul(
                    self.bank[pb][:], lhsT=wq[:, k, which, :], rhs=hT[:, k, c * 512:(c + 1) * 512],
                    start=(k == 0), stop=(k == KC - 1)), r=[wkey] + hkeys[4 * c:4 * c + 4], w=[f"B{pb}"])
            if which == 0:
                A(ACT, lambda e, c=c, pb=pb, dst=dst: e.activation(out=dst[:, c * 512:(c + 1) * 512], in_=self.bank[pb][:], func=AF.Copy),
                  r=[f"B{pb}"], w=[dkey])
            else:
                A(DVE, lambda e, c=c, pb=pb, dst=dst: e.tensor_copy(out=dst[:, c * 512:(c + 1) * 512], in_=self.bank[pb][:]),
                  r=[f"B{pb}"], w=[dkey])
                if kmean is not None:
                    A(DVE, lambda e, c=c, pb=pb: e.tensor_reduce(
                        out=kmean[:, 2 * c:2 * c + 2], in_=self.bank[pb][:].rearrange("p (b n) -> p b n", b=2), axis=AX.X, op=ALU.add),
                      r=[f"B{pb}"], w=["kmean"])
    for tg in range(4):
        pb = self.sring.next()
        for tt in range(4):
            t = tg * 4 + tt
            for k in range(KC):
                A(PE, lambda e, k=k, t=t, tt=tt, pb=pb: e.matmul(
                    self.bank[pb][:, tt * 128:(tt + 1) * 128], lhsT=hT[:, k, t * 128:(t + 1) * 128], rhs=wq[:, k, 2, :],
                    start=(k == 0), stop=(k == KC - 1)), r=[wkey, hkeys[t]], w=[f"B{pb}"])
        eng = ACT if tg % 2 == 0 else DVE
        if eng == ACT:
            A(ACT, lambda e, tg=tg, pb=pb: e.activation(out=V[:, tg * 4:(tg + 1) * 4, 0:128],
                                                        in_=self.bank[pb][:].rearrange("p (a n) -> p a n", a=4), func=AF.Copy),
              r=[f"B{pb}"], w=[vkey])
        else:
            A(DVE, lambda e, tg=tg, pb=pb: e.tensor_copy(out=V[:, tg * 4:(tg + 1) * 4, 0:128],
                                                         in_=self.bank[pb][:].rearrange("p (a n) -> p a n", a=4)),
              r=[f"B{pb}"], w=[vkey])


def _o_to_oT(self, src_ap, src_keys, otok, okey, oT, jj, t, scale_ap=None, scale_keys=()):
    A = self.A
    if scale_ap is None:
        A(ACT, lambda e: e.activation(out=otok[:], in_=src_ap, func=AF.Copy), r=list(src_keys), w=[okey])
    else:
        A(ACT, lambda e: e.activation(out=otok[:], in_=src_ap, func=AF.Copy, scale=scale_ap),
          r=list(src_keys) + list(scale_keys), w=[okey])
    pb = self.sring.next()
    A(PE, lambda e, pb=pb: e.transpose(out=self.bankbf(pb)[:, 0:128], in_=otok[:], identity=self.cb[:, 0, :]),
      r=[okey, "cb"], w=[f"B{pb}"])
    A(ACT, lambda e, pb=pb: e.activation(out=oT[:, jj, t * 128:(t + 1) * 128], in_=self.bankbf(pb)[:, 0:128], func=AF.Copy),
      r=[f"B{pb}"], w=["oT"])


def _sbmix(self, li):
    from contextlib import ExitStack
    A, dr, cb = self.A, self.dr, self.cb
    scale = 128.0 ** -0.5
    with ExitStack() as st:
        hT, hkeys = _mixer_prologue(self, st, li)
        wq1 = self.sb(st, "wq0", [128, KC, 3, 128], BF16)
        wq = [wq1, wq1]
        qT = [self.sb(st, f"qT{i}", [128, S], BF16) for i in range(2)]
        kT = [self.sb(st, f"kT{i}", [128, S], BF16) for i in range(2)]
        V = [self.sb(st, f"V{i}", [128, NT, 128], BF16) for i in range(2)]
        oT = self.sb(st, "oT", [128, 4, S], BF16)
        wo = self.sb(st, "wo", [128, 4, D], BF16)
        SPl = [self.sb(st, f"SPl{i}", [128, 512], F32) for i in range(2)]
        Lb = [self.sb(st, f"Lb{i}", [128, 512], BF16) for i in range(2)]
        Lsum = self.sb(st, "Lsum", [128, 512], BF16)
        T1 = [self.sb(st, f"T1{i}", [128, 512], F32) for i in range(3)]
        AT = [self.sb(st, f"AT{i}", [128, 512], BF16) for i in range(2)]
        otok = [self.sb(st, f"otok{i}", [128, 128], BF16) for i in range(2)]
        w_in = dr["sb_w_in"][0]
        self.atn_it = 0
        self.nls = 0
        self.oi = 0
        def prep(h):
            s = h % 2
            w3 = [w_in[:, which * 1024 + h * 128: which * 1024 + (h + 1) * 128] for which in range(3)]
            _proj_qkv(self, w3, h, wq[s], "wq0", qT[s], f"qT{s}", kT[s], f"kT{s}", V[s], f"V{s}", hT, hkeys)

        prep(0)
        for h in range(8):
            s = h % 2
            stages = []
            for c in range(4):
                for kt in range(4 * c + 3, -1, -1):
                    sd = {}

                    def stage1(sd=sd, c=c, kt=kt, s=s):
                        r_ = kt - 4 * c
                        q0 = max(0, r_) * 128
                        diag = r_ >= 0
                        b = self.atn_it % 2
                        t3 = self.atn_it % 3
                        sd["b"], sd["t3"] = b, t3
                        self.atn_it += 1
                        pz = self.sring.next()
                        A(PE, lambda e: e.matmul(
                            self.bank[pz][:, q0:512], lhsT=kT[s][:, kt * 128:(kt + 1) * 128], rhs=qT[s][:, c * 512 + q0:(c + 1) * 512],
                            start=True, stop=True), r=[f"kT{s}", f"qT{s}"], w=[f"B{pz}"])
                        A(ACT, lambda e: e.activation(out=SPl[b][:, q0:512], in_=self.bank[pz][:, q0:512], func=AF.Exp, scale=scale),
                          r=[f"B{pz}"], w=[f"SPl{b}"])
                        A(ACT, lambda e: e.activation(out=Lb[b][:, q0:512], in_=SPl[b][:, q0:512], func=AF.Ln, bias=1.0, scale=1.0),
                          r=[f"SPl{b}"], w=[f"Lb{b}"])
                        sd["pz"] = pz

                    def stage1d(sd=sd, c=c, kt=kt, s=s):
                        b, t3, pz = sd["b"], sd["t3"], sd["pz"]
                        r_ = kt - 4 * c
                        q0 = max(0, r_) * 128
                        diag = r_ >= 0
                        A(DVE, lambda e: e.scalar_tensor_tensor(
                            out=T1[t3][:, q0:512], in0=self.bank[pz][:, q0:512], scalar=scale, in1=Lb[b][:, q0:512], op0=ALU.mult, op1=ALU.subtract),
                          r=[f"B{pz}", f"Lb{b}"], w=[f"T1{t3}"])
                        if diag:
                            A(DVE, lambda e: e.tensor_tensor(out=Lb[b][:, q0:q0 + 128], in0=Lb[b][:, q0:q0 + 128], in1=cb[:, 6, :], op=ALU.mult),
                              r=[f"Lb{b}", "cb"], w=[f"Lb{b}"])

                    def stage2a(sd=sd, c=c, kt=kt, s=s, h=h):
                        b, t3 = sd["b"], sd["t3"]
                        r_ = kt - 4 * c
                        q0 = max(0, r_) * 128
                        first = (kt == 4 * c + 3)
                        if first:
                            A(DVE, lambda e: e.memset(Lsum[:], 0.0), w=["Lsum"])
                        pt = self.sring.next()
                        A(PE, lambda e: e.matmul(
                            self.bank[pt][:, q0:512], lhsT=cb[:, 3, :], rhs=Lb[b][:, q0:512], start=True, stop=first),
                          r=["cb", f"Lb{b}"], w=[f"B{pt}"])
                        if not first:
                            A(PE, lambda e: e.matmul(
                                self.bank[pt][:, q0:512], lhsT=cb[:, 5, :], rhs=Lsum[:, q0:512], start=False, stop=True),
                              r=["cb", "Lsum"], w=[f"B{pt}"])
                        if kt > 0:
                            A(DVE, lambda e: e.tensor_tensor(out=Lsum[:, q0:512], in0=Lsum[:, q0:512], in1=Lb[b][:, q0:512], op=ALU.add),
                              r=["Lsum", f"Lb{b}"], w=["Lsum"])
                        A(DVE, lambda e: e.tensor_tensor(out=T1[t3][:, q0:512], in0=self.bank[pt][:, q0:512], in1=T1[t3][:, q0:512], op=ALU.add),
                          r=[f"B{pt}", f"T1{t3}"], w=[f"T1{t3}"])

                    def stage2b(sd=sd, c=c, kt=kt, s=s, h=h):
                        b, t3 = sd["b"], sd["t3"]
                        r_ = kt - 4 * c
                        q0 = max(0, r_) * 128
                        diag = r_ >= 0
                        A(ACT, lambda e: e.activation(out=AT[b][:, q0:512], in_=T1[t3][:, q0:512], func=AF.Exp),
                          r=[f"T1{t3}"], w=[f"AT{b}"])
                        if diag:
                            A(DVE, lambda e: e.tensor_tensor(out=AT[b][:, q0:q0 + 128], in0=AT[b][:, q0:q0 + 128], in1=cb[:, 6, :], op=ALU.mult),
                              r=[f"AT{b}", "cb"], w=[f"AT{b}"])

                    def stage2p(sd=sd, c=c, kt=kt, s=s, h=h):
                        b = sd["b"]
                        r_ = kt - 4 * c
                        q0 = max(0, r_) * 128
                        for i in range(q0 // 128, 4):
                            A(PE, lambda e, i=i: e.matmul(
                                self.bank[4 + i][:, 0:128], lhsT=AT[b][:, i * 128:(i + 1) * 128], rhs=V[s][:, kt, :],
                                start=(kt == 4 * c + i), stop=(kt == 0)), r=[f"AT{b}", f"V{s}"], w=[f"B{4 + i}"])
                        if kt == 0:
                            for i in range(4):
                                ob = self.oi % 2
                                self.oi += 1
                                _o_to_oT(self, self.bank[4 + i][:, 0:128], [f"B{4 + i}"], otok[ob], f"otok{ob}", oT, h % 4, 4 * c + i)
                    stages.append((stage2b, stage1, stage2a, stage1d, stage2p))
            nst = len(stages)
            for step in range(-2, nst):
                if step == nst // 2 and h < 7:
                    prep(h + 1)
                for si, off in enumerate((0, 2, 1, 2, 0)):
                    k = step + off
                    if 0 <= k < nst:
                        stages[k][si]()
            if h % 4 == 3:
                hg = h // 4
                self.out_proj(wo, "wo", oT, 4, dr["sb_w_o"][0, hg * 512:(hg + 1) * 512, :], ["oT"])
        self.P.flush()
    with ExitStack() as st_ln:
        self.layer_norm(st_ln, li, 0)
        self.P.flush()


Builder.sbmix = _sbmix


def _attn_softmax(self, c_list, pairs, pkeys, V, vkey, scale, slope, items_of_chunk, maskmm, PT, TMP, DT, epi, far_ok=True, mid_hook=None):
    A, cb = self.A, self.cb
    nb = len(PT)
    stages = []
    for c in c_list:
        items = items_of_chunk(c)
        first_kt, last_kt = {}, {}
        for (kt, q0, q1, diag, far) in items:
            for i in range(q0 // 128, q1 // 128):
                first_kt.setdefault(i, kt)
                last_kt[i] = kt
        for idx, (kt, q0, q1, diag, far) in enumerate(items):
            s1, s2 = [], []

            st_ = {}

            def stage0(st_=st_, c=c, kt=kt, q0=q0, q1=q1):
                b = self.atn_it % nb
                self.atn_it += 1
                st_["b"] = b
                if slope is not None:
                    A(ACT, lambda e: e.activation(
                        out=DT[b][:, q0:q1], in_=self.posq[:, c * 512 + q0:c * 512 + q1], func=AF.Abs, bias=self.nposk[:, kt:kt + 1], scale=1.0),
                      r=["posq", "posk"], w=[f"DT{b}"])

            def stage1a(st_=st_, c=c, kt=kt, q0=q0, q1=q1, diag=diag, far=far):
                b = st_["b"]
                ps = (self.sring_s if (slope is None and self.sring_s is not None) else self.sring).next()
                st_["ps"] = ps
                mms = []
                for (kT_ap, qT_ap) in pairs:
                    mms.append((kT_ap[:, kt * 128:(kt + 1) * 128], qT_ap[:, c * 512 + q0:c * 512 + q1], q0, q1, list(pkeys)))
                if maskmm is not None:
                    lhs_fn, rhs_ap, mkeys = maskmm
                    mms.append((lhs_fn(kt), rhs_ap[:, c * 512 + q0:c * 512 + q1], q0, q1, list(mkeys)))
                if diag:
                    mms.append((cb[:, 0, :], cb[:, 1, :], q0, q0 + 128, ["cb"]))
                if far:
                    mms.append((cb[:, 0, :], cb[:, 2, :], q1 - 128, q1, ["cb"]))
                for mi, (lh, rh, a0, a1, keys) in enumerate(mms):
                    A(PE, lambda e, lh=lh, rh=rh, a0=a0, a1=a1, ps=ps, mi=mi, n=len(mms): e.matmul(
                        self.bank[ps][:, a0:a1], lhsT=lh, rhs=rh, start=(mi == 0), stop=(mi == n - 1)),
                      r=keys, w=[f"B{ps}"])
                if slope is not None:
                    A(DVE, lambda e: e.scalar_tensor_tensor(
                        out=TMP[b][:, q0:q1], in0=DT[b][:, q0:q1], scalar=-slope / scale, in1=self.bank[ps][:, q0:q1],
                        op0=ALU.mult, op1=ALU.add), r=[f"DT{b}", f"B{ps}"], w=[f"TMP{b}"])

            def stage1b(st_=st_, q0=q0, q1=q1):
                b, ps = st_["b"], st_["ps"]
                if slope is not None:
                    A(ACT, lambda e: e.activation(out=PT[b][:, q0:q1], in_=TMP[b][:, q0:q1], func=AF.Exp, scale=scale),
                      r=[f"TMP{b}"], w=[f"PT{b}"])
                else:
                    A(ACT, lambda e: e.activation(out=PT[b][:, q0:q1], in_=self.bank[ps][:, q0:q1], func=AF.Exp, scale=scale),
                      r=[f"B{ps}"], w=[f"PT{b}"])

            def stage2(st_=st_, c=c, kt=kt, q0=q0, q1=q1, last=(idx == len(items) - 1), first_kt=first_kt, last_kt=last_kt):
                b = st_["b"]
                for i in range(q0 // 128, q1 // 128):
                    A(PE, lambda e, i=i, s_=(kt == first_kt[i]), p_=(kt == last_kt[i]): e.matmul(
                        self.bank[4 + i][:, 0:129], lhsT=PT[b][:, i * 128:(i + 1) * 128], rhs=V[:, kt, 0:129], start=s_, stop=p_),
                      r=[f"PT{b}", vkey], w=[f"B{4 + i}"])
                if last:
                    for i in range(4):
                        if i in first_kt:
                            epi(c, i)
            stages.append((stage0, stage1a, stage1b, stage2))
    n = len(stages)
    offs = (3, 2, 1, 0)
    for step in range(-3, n):
        if mid_hook is not None and step == n // 2:
            mid_hook()
        for si, off in enumerate(offs):
            k = step + off
            if 0 <= k < n:
                stages[k][si]()


def _causal_items(c):
    out = []
    for kt in range(0, 4 * c + 4):
        r_ = kt - 4 * c
        out.append((kt, max(0, r_) * 128, 512, r_ >= 0, False))
    return out


def _moba(self, li):
    from contextlib import ExitStack
    A, dr, cb = self.A, self.dr, self.cb
    scale = 128.0 ** -0.5
    self.atn_it = 0
    with ExitStack() as st:
        hT, hkeys = _mixer_prologue(self, st, li)
        wq = self.sb(st, "wq0", [128, KC, 3, 128], BF16)
        qT = [self.sb(st, f"qT{i}", [128, S], BF16) for i in range(2)]
        kT = [self.sb(st, f"kT{i}", [128, S], BF16) for i in range(2)]
        V = [self.sb(st, f"V{i}", [128, NT, 129], BF16) for i in range(2)]
        oT = self.sb(st, "oT", [128, 4, S], BF16)
        wo = self.sb(st, "wo", [128, 4, D], BF16)
        PT = [self.sb(st, f"PT{i}", [128, 512], BF16) for i in range(2)]
        TMP = [self.sb(st, f"TMP{i}", [128, 512], F32) for i in range(2)]
        DT = [self.sb(st, f"DT{i}", [128, 512], F32) for i in range(2)]
        otok = [self.sb(st, f"otok{i}", [128, 128], BF16) for i in range(2)]
        rden = [self.sb(st, f"rden{i}", [128, 1], F32) for i in range(2)]
        e8t = self.sb(st, "e8t", [128, 1024], BF16)
        mneg = self.sb(st, "mneg", [128, 512], F32)
        mnot = self.sb(st, "mnot", [128, 512], F32)
        kmean = self.sb(st, "kmean", [128, 32], F32)
        kmh = self.sb(st, "kmh", [128, 32], BF16)
        kml = self.sb(st, "kml", [128, 32], BF16)
        kmr = self.sb(st, "kmr", [128, 32], F32)
        gm = self.sb(st, "gm", [128, 512], F32)
        m8 = self.sb(st, "m8", [128, 128], F32)
        sel = self.sb(st, "sel", [128, 512], F32)
        nbb = self.sb(st, "nbb", [128, 512], BF16)
        selbT = [self.sb(st, f"selbT{i}", [128, S], BF16) for i in range(2)]
        A(SP, lambda e: e.dma_start(out=e8t[:], in_=dr["e8"]), w=["e8t"], dma="e8t")
        A(SP, lambda e: e.dma_start(out=mneg[:], in_=dr["moba_neg"]), w=["mneg"], dma="mneg")
        A(SP, lambda e: e.dma_start(out=mnot[:], in_=dr["moba_notown"]), w=["mnot"], dma="mnot")
        for s in range(2):
            A(POOL, lambda e, s=s: e.memset(V[s][:, :, 128:129], 1.0), w=[f"V{s}"])
        A(POOL, lambda e: e.memset(kmean[:], 0.0), w=["kmean"])
        for i in range(2):
            A(DVE, lambda e, i=i: e.memset(selbT[i][:], 0.0), w=[f"selbT{i}"])
        w_in = dr["moba_w_in"][0]
        oi = [0]
        def prep(h):
            s = h % 2
            w3 = [w_in[:, which * 1024 + h * 128: which * 1024 + (h + 1) * 128] for which in range(3)]
            _proj_qkv(self, w3, h, wq, "wq0", qT[s], f"qT{s}", kT[s], f"kT{s}", V[s], f"V{s}", hT, hkeys, kmean=kmean)
            A(DVE, lambda e: e.tensor_copy(out=kmh[:], in_=kmean[:]), r=["kmean"], w=["kmh"])
            A(DVE, lambda e: e.tensor_tensor(out=kmr[:], in0=kmean[:], in1=kmh[:], op=ALU.subtract), r=["kmean", "kmh"], w=["kmr"])
            A(DVE, lambda e: e.tensor_copy(out=kml[:], in_=kmr[:]), r=["kmr"], w=["kml"])
            pg = self.sring.next()
            for t in range(NT):
                A(PE, lambda e, t=t, s=s, pg=pg: e.matmul(self.bank[pg][:, t * 32:(t + 1) * 32], lhsT=qT[s][:, t * 128:(t + 1) * 128],
                                                          rhs=kmh[:], start=True, stop=False), r=[f"qT{s}", "kmh"], w=[f"B{pg}"])
                A(PE, lambda e, t=t, s=s, pg=pg: e.matmul(self.bank[pg][:, t * 32:(t + 1) * 32], lhsT=qT[s][:, t * 128:(t + 1) * 128],
                                                          rhs=kml[:], start=False, stop=True), r=[f"qT{s}", "kml"], w=[f"B{pg}"])
            A(DVE, lambda e, pg=pg: e.tensor_tensor(out=gm[:], in0=self.bank[pg][:], in1=mneg[:], op=ALU.add),
              r=[f"B{pg}", "mneg"], w=["gm"])
            for t in range(NT):
                A(DVE, lambda e, t=t: e.max(out=m8[:, t * 8:(t + 1) * 8], in_=gm[:, t * 32:(t + 1) * 32]), r=["gm"], w=["m8"])
            for t in range(NT):
                A(DVE, lambda e, t=t: e.tensor_scalar(out=sel[:, t * 32:(t + 1) * 32], in0=gm[:, t * 32:(t + 1) * 32],
                                                      scalar1=m8[:, t * 8 + 2:t * 8 + 3], scalar2=None, op0=ALU.is_ge),
                  r=["gm", "m8"], w=["sel"])
            A(DVE, lambda e: e.tensor_scalar(out=sel[:], in0=sel[:], scalar1=-NEGB, scalar2=NEGB, op0=ALU.mult, op1=ALU.add),
              r=["sel"], w=["sel"])
            A(DVE, lambda e: e.tensor_tensor(out=nbb[:], in0=sel[:], in1=mnot[:], op=ALU.mult), r=["sel", "mnot"], w=["nbb"])
            for half in range(2):
                pb = self.sring.next()
                for tt in range(8):
                    t = half * 8 + tt
                    A(PE, lambda e, t=t, tt=tt, pb=pb: e.transpose(out=self.bankbf(pb)[0:32, tt * 128:(tt + 1) * 128],
                                                                  in_=nbb[:, t * 32:(t + 1) * 32], identity=cb[:, 0, :]),
                      r=["nbb", "cb"], w=[f"B{pb}"])
                A(ACT, lambda e, half=half, pb=pb: e.activation(out=selbT[s][0:32, half * 1024:(half + 1) * 1024],
                                                                in_=self.bankbf(pb)[0:32, :], func=AF.Copy),
                  r=[f"B{pb}"], w=[f"selbT{s}"])


        prep(0)
        for h in range(8):
            s = h % 2
            slope = 2.0 ** (-(h + 1))
            def epi(c, i, h=h, s=s):
                ob = oi[0] % 2
                oi[0] += 1
                A(DVE, lambda e, ob=ob, i=i: e.reciprocal(out=rden[ob][:], in_=self.bank[4 + i][:, 128:129]),
                  r=[f"B{4 + i}"], w=[f"rden{ob}"])
                _o_to_oT(self, self.bank[4 + i][:, 0:128], [f"B{4 + i}"], otok[ob], f"otok{ob}", oT, h % 4, 4 * c + i,
                         scale_ap=rden[ob][:, 0:1], scale_keys=[f"rden{ob}"])

            _attn_softmax(self, range(4), [(kT[s], qT[s])], [f"kT{s}", f"qT{s}"], V[s], f"V{s}", scale, slope, _causal_items,
                          (lambda kt: e8t[:, (kt // 2) * 128:(kt // 2 + 1) * 128], selbT[s], ["e8t", f"selbT{s}"]), PT, TMP, DT, epi,
                          mid_hook=((lambda h=h: prep(h + 1)) if h < 7 else None))
            if h % 4 == 3:
                hg = h // 4
                self.out_proj(wo, "wo", oT, 4, dr["moba_w_o"][0, hg * 512:(hg + 1) * 512, :], ["oT"])
        self.P.flush()
    with ExitStack() as st_ln:
        self.layer_norm(st_ln, li, 0)
        self.P.flush()


Builder.moba = _moba


def _rope_ops(self, x1, x2, cos, sin, o1, o2, R, rkeys, in_keys, okey):
    A = self.A
    A(DVE, lambda e: e.tensor_tensor(out=R[0], in0=x1, in1=cos, op=ALU.mult), r=in_keys + ["rope"], w=[rkeys[0]])
    A(DVE, lambda e: e.tensor_tensor(out=R[1], in0=x2, in1=sin, op=ALU.mult), r=in_keys + ["rope"], w=[rkeys[1]])
    A(DVE, lambda e: e.tensor_tensor(out=o1, in0=R[0], in1=R[1], op=ALU.subtract), r=[rkeys[0], rkeys[1]], w=[okey])
    A(DVE, lambda e: e.tensor_tensor(out=R[2], in0=x2, in1=cos, op=ALU.mult), r=in_keys + ["rope"], w=[rkeys[2]])
    A(DVE, lambda e: e.tensor_tensor(out=R[3], in0=x1, in1=sin, op=ALU.mult), r=in_keys + ["rope"], w=[rkeys[3]])
    A(DVE, lambda e: e.tensor_tensor(out=o2, in0=R[2], in1=R[3], op=ALU.add), r=[rkeys[2], rkeys[3]], w=[okey])


def _mla(self, li):
    from contextlib import ExitStack
    A, dr, cb = self.A, self.dr, self.cb
    scale = 192.0 ** -0.5
    self.atn_it = 0
    with ExitStack() as st:
        wuq = self.sb(st, "wuq", [128, 2, 1536], BF16)
        wukv = self.sb(st, "wukv", [128, 2, 2048], BF16)
        cos_t = self.sb(st, "cos_t", [128, NT, 32], F32)
        sin_t = self.sb(st, "sin_t", [128, NT, 32], F32)
        c_qT = self.sb(st, "c_qT", [128, 2, S], BF16)
        c_kvT = self.sb(st, "c_kvT", [128, 2, S], BF16)
        krT = self.sb(st, "krT", [128, S], BF16)
        gains = self.sb(st, "gains", [128, 4], F32)
        Rt = self.sb(st, "Rt", [128, 4, 128], F32)
        with ExitStack() as sth:
            hT, hkeys = _mixer_prologue(self, sth, li)
            w_in = self.sb(sth, "mlawin", [128, KC, 576], BF16)
            ang = self.sb(sth, "ang", [128, NT, 32], F32)
            invf = self.sb(sth, "invf", [128, 32], F32)
            junk = self.sb(sth, "junk", [128, 256], F32)
            ss = [self.sb(sth, f"ss{i}", [128, 4], F32) for i in range(2)]
            cn = [self.sb(sth, f"cn{i}", [128, 512], BF16) for i in range(2)]
            kr = [self.sb(sth, f"kr{i}", [128, 64], BF16) for i in range(2)]
            A(POOL, lambda e: e.dma_start(out=w_in[:], in_=dr["mla_w_in"][0].rearrange("(k p) n -> p k n", p=128)), w=["mlawin"], dma="mlawin")
            A(POOL, lambda e: e.dma_start(out=wuq[:], in_=dr["mla_w_uq"][0].rearrange("(k p) n -> p k n", p=128)), w=["wuq"], dma="wuq")
            for hf in range(2):
                A(POOL, lambda e, hf=hf: e.dma_start(out=wukv[:, :, hf * 1024:(hf + 1) * 1024],
                                                     in_=dr["mla_w_ukv"][0][:, hf * 1024:(hf + 1) * 1024].rearrange("(k p) n -> p k n", p=128)),
                  w=["wukv"], dma="wukv")
            A(SP, lambda e: e.dma_start(out=invf[:], in_=dr["invf"]), w=["invf"], dma="invf")
            A(SP, lambda e: e.dma_start(out=gains[:, 0:2], in_=dr["mla_q_norm"][0:1, :].rearrange("o (j p) -> p (o j)", p=128),
                                        allow_slow_non_contiguous=True), w=["gains"], dma="gains")
            A(SP, lambda e: e.dma_start(out=gains[:, 2:4], in_=dr["mla_kv_norm"][0:1, :].rearrange("o (j p) -> p (o j)", p=128),
                                        allow_slow_non_contiguous=True), w=["gains"], dma="gains")
            for t in range(NT):
                A(DVE, lambda e, t=t: e.tensor_scalar(out=ang[:, t, :], in0=invf[:], scalar1=self.posk[:, t:t + 1], scalar2=None, op0=ALU.mult),
                  r=["invf", "posk"], w=["ang"])
            ki = self.sb(sth, "ki", [128, NT, 32], I32)
            kf = self.sb(sth, "kf", [128, NT, 32], F32)
            mk = self.sb(sth, "mk", [128, NT, 32], F32)
            C1, C2 = 6.28125, 2 * np.pi - 6.28125
            for dst, shift in ((sin_t, 0.0), (cos_t, 0.5 * PI)):
                A(DVE, lambda e, dst=dst, shift=shift: e.tensor_scalar(out=dst[:], in0=ang[:], scalar1=shift, scalar2=None, op0=ALU.add), r=["ang"], w=["rope"])
                A(DVE, lambda e, dst=dst: e.tensor_scalar(out=kf[:], in0=dst[:], scalar1=float(1.0 / (2 * np.pi)), scalar2=None, op0=ALU.mult), r=["rope"], w=["kf"])
                A(DVE, lambda e: e.tensor_copy(out=ki[:], in_=kf[:]), r=["kf"], w=["ki"])
                A(DVE, lambda e: e.tensor_copy(out=kf[:], in_=ki[:]), r=["ki"], w=["kf"])
                A(DVE, lambda e, dst=dst: e.scalar_tensor_tensor(out=dst[:], in0=kf[:], scalar=-C1, in1=dst[:], op0=ALU.mult, op1=ALU.add), r=["kf", "rope"], w=["rope"])
                A(DVE, lambda e, dst=dst: e.scalar_tensor_tensor(out=dst[:], in0=kf[:], scalar=-C2, in1=dst[:], op0=ALU.mult, op1=ALU.add), r=["kf", "rope"], w=["rope"])
                A(DVE, lambda e, dst=dst: e.tensor_scalar(out=mk[:], in0=dst[:], scalar1=PI, scalar2=None, op0=ALU.is_gt), r=["rope"], w=["mk"])
                A(DVE, lambda e, dst=dst: e.scalar_tensor_tensor(out=dst[:], in0=mk[:], scalar=-2 * PI, in1=dst[:], op0=ALU.mult, op1=ALU.add), r=["mk", "rope"], w=["rope"])
                A(DVE, lambda e, dst=dst: e.tensor_scalar(out=mk[:], in0=dst[:], scalar1=-PI, scalar2=None, op0=ALU.is_lt), r=["rope"], w=["mk"])
                A(DVE, lambda e, dst=dst: e.scalar_tensor_tensor(out=dst[:], in0=mk[:], scalar=2 * PI, in1=dst[:], op0=ALU.mult, op1=ALU.add), r=["mk", "rope"], w=["rope"])
                A(DVE, lambda e, dst=dst: e.tensor_scalar(out=dst[:], in0=dst[:], scalar1=-3.1415925, scalar2=3.1415925, op0=ALU.max, op1=ALU.min), r=["rope"], w=["rope"])
            A(ACT, lambda e: e.activation(out=sin_t[:], in_=sin_t[:], func=AF.Sin), r=["rope"], w=["rope"])
            A(ACT, lambda e: e.activation(out=cos_t[:], in_=cos_t[:], func=AF.Sin), r=["rope"], w=["rope"])
            for t in range(NT):
                b = t % 2
                pa, pb2 = self.sring.next(), self.sring.next()
                for k in range(KC):
                    A(PE, lambda e, k=k, t=t, pa=pa: e.matmul(self.bank[pa][:], lhsT=hT[:, k, t * 128:(t + 1) * 128], rhs=w_in[:, k, 0:512],
                                                              start=(k == 0), stop=(k == KC - 1)), r=[hkeys[t], "mlawin"], w=[f"B{pa}"])
                for k in range(KC):
                    A(PE, lambda e, k=k, t=t, pb2=pb2: e.matmul(self.bank[pb2][:, 0:64], lhsT=hT[:, k, t * 128:(t + 1) * 128], rhs=w_in[:, k, 512:576],
                                                                start=(k == 0), stop=(k == KC - 1)), r=[hkeys[t], "mlawin"], w=[f"B{pb2}"])
                for j in range(2):
                    A(ACT, lambda e, j=j, pa=pa, b=b: e.activation(out=junk[:], in_=self.bank[pa][:, j * 256:(j + 1) * 256], func=AF.Square,
                                                                   accum_out=ss[b][:, j:j + 1]), r=[f"B{pa}"], w=["junk", f"ss{b}"])
                A(ACT, lambda e, b=b: e.activation(out=ss[b][:, 2:4], in_=ss[b][:, 0:2], func=AF.Ln, bias=1e-6, scale=1.0 / 256.0), r=[f"ss{b}"], w=[f"ss{b}"])
                A(ACT, lambda e, b=b: e.activation(out=ss[b][:, 2:4], in_=ss[b][:, 2:4], func=AF.Exp, scale=-0.5), r=[f"ss{b}"], w=[f"ss{b}"])
                for j in range(2):
                    A(DVE, lambda e, j=j, pa=pa, b=b: e.tensor_scalar(out=cn[b][:, j * 256:(j + 1) * 256], in0=self.bank[pa][:, j * 256:(j + 1) * 256],
                                                                      scalar1=ss[b][:, 2 + j:3 + j], scalar2=None, op0=ALU.mult),
                      r=[f"B{pa}", f"ss{b}"], w=[f"cn{b}"])
                pt = self.sring.next()
                for j in range(4):
                    A(PE, lambda e, j=j, b=b, pt=pt: e.transpose(out=self.bankbf(pt)[:, j * 128:(j + 1) * 128], in_=cn[b][:, j * 128:(j + 1) * 128],
                                                                 identity=cb[:, 0, :]), r=[f"cn{b}", "cb"], w=[f"B{pt}"])
                for j in range(4):
                    dst = c_qT if j < 2 else c_kvT
                    A(DVE, lambda e, j=j, t=t, pt=pt, dst=dst: e.tensor_scalar(
                        out=dst[:, j % 2, t * 128:(t + 1) * 128], in0=self.bankbf(pt)[:, j * 128:(j + 1) * 128], scalar1=gains[:, j:j + 1],
                        scalar2=None, op0=ALU.mult), r=[f"B{pt}", "gains"], w=["c_qT" if j < 2 else "c_kvT"])
                _rope_ops(self, self.bank[pb2][:, 0:32], self.bank[pb2][:, 32:64], cos_t[:, t, :], sin_t[:, t, :],
                          kr[b][:, 0:32], kr[b][:, 32:64], [Rt[:, i, 0:32] for i in range(4)], [f"Rt{i}" for i in range(4)], [f"B{pb2}"], f"kr{b}")
                pk = self.sring.next()
                A(PE, lambda e, b=b, pk=pk: e.transpose(out=self.bankbf(pk)[0:64, 0:128], in_=kr[b][:], identity=cb[:, 0, :]), r=[f"kr{b}", "cb"], w=[f"B{pk}"])
                A(ACT, lambda e, t=t, pk=pk: e.activation(out=krT[0:64, t * 128:(t + 1) * 128], in_=self.bankbf(pk)[0:64, 0:128], func=AF.Copy),
                  r=[f"B{pk}"], w=["krT"])
            self.P.flush()
        self.sring = Ring("m", [2, 3])
        self.sring_s = Ring("sc", [0, 1])
        qnT = [self.sb(st, f"qnT{i}", [128, S], BF16) for i in range(2)]
        qrT = [self.sb(st, f"qrT{i}", [128, S], BF16) for i in range(2)]
        knT = [self.sb(st, f"knT{i}", [128, S], BF16) for i in range(2)]
        V = [self.sb(st, f"V{i}", [128, NT, 129], BF16) for i in range(2)]
        oT = self.sb(st, "oT", [128, 4, S], BF16)
        wo = self.sb(st, "wo", [128, 4, D], BF16)
        PT = [self.sb(st, f"PT{i}", [128, 512], BF16) for i in range(2)]
        otok = [self.sb(st, f"otok{i}", [128, 128], BF16) for i in range(2)]
        rden = [self.sb(st, f"rden{i}", [128, 1], F32) for i in range(2)]
        qr = [self.sb(st, f"qr{i}", [128, 4, 64], BF16) for i in range(2)]
        for s in range(2):
            A(POOL, lambda e, s=s: e.memset(V[s][:, :, 128:129], 1.0), w=[f"V{s}"])
            A(DVE, lambda e, s=s: e.memset(qrT[s][64:128, :], 0.0), w=[f"qrT{s}"])
        A(DVE, lambda e: e.memset(krT[64:128, :], 0.0), w=["krT"])
        oi = [0]
        qi = 0
        def prep(h):
            nonlocal qi
            s = h % 2
            for c in range(4):
                pb = self.sring.next()
                for k in range(2):
                    A(PE, lambda e, k=k, c=c, pb=pb, h=h: e.matmul(self.bank[pb][:], lhsT=wuq[:, k, h * 192:h * 192 + 128], rhs=c_qT[:, k, c * 512:(c + 1) * 512],
                                                                   start=(k == 0), stop=(k == 1)), r=["wuq", "c_qT"], w=[f"B{pb}"])
                A(ACT, lambda e, c=c, pb=pb, s=s: e.activation(out=qnT[s][:, c * 512:(c + 1) * 512], in_=self.bank[pb][:], func=AF.Copy), r=[f"B{pb}"], w=[f"qnT{s}"])
                pb = self.sring.next()
                for k in range(2):
                    A(PE, lambda e, k=k, c=c, pb=pb, h=h: e.matmul(self.bank[pb][:], lhsT=wukv[:, k, h * 256:h * 256 + 128], rhs=c_kvT[:, k, c * 512:(c + 1) * 512],
                                                                   start=(k == 0), stop=(k == 1)), r=["wukv", "c_kvT"], w=[f"B{pb}"])
                A(DVE, lambda e, c=c, pb=pb, s=s: e.tensor_copy(out=knT[s][:, c * 512:(c + 1) * 512], in_=self.bank[pb][:]), r=[f"B{pb}"], w=[f"knT{s}"])
            for tg in range(4):
                pb = self.sring.next()
                for tt in range(4):
                    t = tg * 4 + tt
                    for k in range(2):
                        A(PE, lambda e, k=k, t=t, tt=tt, pb=pb, h=h: e.matmul(
                            self.bank[pb][:, tt * 128:(tt + 1) * 128], lhsT=c_kvT[:, k, t * 128:(t + 1) * 128], rhs=wukv[:, k, h * 256 + 128:h * 256 + 256],
                            start=(k == 0), stop=(k == 1)), r=["wukv", "c_kvT"], w=[f"B{pb}"])
                A(ACT, lambda e, tg=tg, pb=pb, s=s: e.activation(out=V[s][:, tg * 4:(tg + 1) * 4, 0:128],
                                                                 in_=self.bank[pb][:].rearrange("p (a n) -> p a n", a=4), func=AF.Copy), r=[f"B{pb}"], w=[f"V{s}"])
                pq = self.sring.next()
                for tt in range(4):
                    t = tg * 4 + tt
                    for k in range(2):
                        A(PE, lambda e, k=k, t=t, tt=tt, pq=pq, h=h: e.matmul(
                            self.bank[pq][:, tt * 64:(tt + 1) * 64], lhsT=c_qT[:, k, t * 128:(t + 1) * 128], rhs=wuq[:, k, h * 192 + 128:h * 192 + 192],
                            start=(k == 0), stop=(k == 1)), r=["wuq", "c_qT"], w=[f"B{pq}"])
                qb = qi % 2
                qi += 1
                xv = self.bank[pq][:, 0:256].rearrange("p (a n) -> p a n", a=4)
                _rope_ops(self, xv[:, :, 0:32], xv[:, :, 32:64], cos_t[:, tg * 4:(tg + 1) * 4, :], sin_t[:, tg * 4:(tg + 1) * 4, :],
                          qr[qb][:, :, 0:32], qr[qb][:, :, 32:64], [Rt[:, i, :].rearrange("p (a n) -> p a n", a=4) for i in range(4)],
                          [f"Rt{i}" for i in range(4)], [f"B{pq}"], f"qr{qb}")
                pt = self.sring.next()
                for tt in range(4):
                    A(PE, lambda e, tt=tt, qb=qb, pt=pt: e.transpose(out=self.bankbf(pt)[0:64, tt * 128:(tt + 1) * 128], in_=qr[qb][:, tt, :],
                                                                    identity=cb[:, 0, :]), r=[f"qr{qb}", "cb"], w=[f"B{pt}"])
                A(ACT, lambda e, tg=tg, pt=pt, s=s: e.activation(out=qrT[s][0:64, tg * 512:(tg + 1) * 512], in_=self.bankbf(pt)[0:64, 0:512], func=AF.Copy),
                  r=[f"B{pt}"], w=[f"qrT{s}"])


        prep(0)
        for h in range(8):
            s = h % 2
            def epi(c, i, h=h):
                ob = oi[0] % 2
                oi[0] += 1
                A(DVE, lambda e, ob=ob, i=i: e.reciprocal(out=rden[ob][:], in_=self.bank[4 + i][:, 128:129]), r=[f"B{4 + i}"], w=[f"rden{ob}"])
                _o_to_oT(self, self.bank[4 + i][:, 0:128], [f"B{4 + i}"], otok[ob], f"otok{ob}", oT, h % 4, 4 * c + i,
                         scale_ap=rden[ob][:, 0:1], scale_keys=[f"rden{ob}"])

            _attn_softmax(self, range(4), [(knT[s], qnT[s]), (krT, qrT[s])], [f"knT{s}", f"qnT{s}", "krT", f"qrT{s}"], V[s], f"V{s}",
                          scale, None, _causal_items, None, PT, None, None, epi,
                          mid_hook=((lambda h=h: prep(h + 1)) if h < 7 else None))
            if h % 4 == 3:
                hg = h // 4
                self.out_proj(wo, "wo", oT, 4, dr["mla_w_o"][0, hg * 512:(hg + 1) * 512, :], ["oT"])
        self.P.flush()
    with ExitStack() as st_ln:
        self.layer_norm(st_ln, li, 0)
        self.P.flush()


Builder.mla = _mla


def _proj_fm(self, wcols, wsl, wring, dst, dkey, hT, hkeys, use_act):
    A = self.A
    ws = wring.next()
    A(POOL, lambda e: e.dma_start(out=wsl[ws][:], in_=wcols.rearrange("(k p) n -> p k n", p=128)), w=[f"wsl{ws}"], dma=f"wsl{ws}")
    for c in range(4):
        pb = self.sring.next()
        for k in range(KC):
            A(PE, lambda e, k=k, c=c, pb=pb: e.matmul(self.bank[pb][:], lhsT=wsl[ws][:, k, :], rhs=hT[:, k, c * 512:(c + 1) * 512],
                                                      start=(k == 0), stop=(k == KC - 1)), r=[f"wsl{ws}"] + hkeys[4 * c:4 * c + 4], w=[f"B{pb}"])
        if use_act:
            A(ACT, lambda e, c=c, pb=pb: e.activation(out=dst[:, c * 512:(c + 1) * 512], in_=self.bank[pb][:], func=AF.Copy), r=[f"B{pb}"], w=[dkey])
        else:
            A(DVE, lambda e, c=c, pb=pb: e.tensor_copy(out=dst[:, c * 512:(c + 1) * 512], in_=self.bank[pb][:]), r=[f"B{pb}"], w=[dkey])


def _proj_tm(self, wcols, wsl, wring, V, vkey, hT, hkeys):
    A = self.A
    ws = wring.next()
    A(POOL, lambda e: e.dma_start(out=wsl[ws][:], in_=wcols.rearrange("(k p) n -> p k n", p=128)), w=[f"wsl{ws}"], dma=f"wsl{ws}")
    for tg in range(4):
        pb = self.sring.next()
        for tt in range(4):
            t = tg * 4 + tt
            for k in range(KC):
                A(PE, lambda e, k=k, t=t, tt=tt, pb=pb: e.matmul(self.bank[pb][:, tt * 128:(tt + 1) * 128], lhsT=hT[:, k, t * 128:(t + 1) * 128],
                                                                rhs=wsl[ws][:, k, :], start=(k == 0), stop=(k == KC - 1)),
                  r=[f"wsl{ws}", hkeys[t]], w=[f"B{pb}"])
        if tg % 2 == 0:
            A(ACT, lambda e, tg=tg, pb=pb: e.activation(out=V[:, tg * 4:(tg + 1) * 4, 0:128], in_=self.bank[pb][:].rearrange("p (a n) -> p a n", a=4),
                                                        func=AF.Copy), r=[f"B{pb}"], w=[vkey])
        else:
            A(DVE, lambda e, tg=tg, pb=pb: e.tensor_copy(out=V[:, tg * 4:(tg + 1) * 4, 0:128], in_=self.bank[pb][:].rearrange("p (a n) -> p a n", a=4)),
              r=[f"B{pb}"], w=[vkey])


def _win_items(c):
    out = []
    for kt in range(max(0, 4 * c - 4), 4 * c + 4):
        r_ = kt - 4 * c
        i_lo, i_hi = max(0, r_), min(3, r_ + 4)
        out.append((kt, i_lo * 128, (i_hi + 1) * 128, r_ >= 0, r_ + 4 <= 3))
    return out


def _nsa(self, li):
    from contextlib import ExitStack
    A, dr, cb = self.A, self.dr, self.cb
    scale = 128.0 ** -0.5
    self.atn_it = 0
    w_in = dr["nsa_w_in"][0]
    with ExitStack() as st:
        hT, hkeys = _mixer_prologue(self, st, li)
        oT = self.sb(st, "oT", [128, 4, S], BF16)
        gates = self.sb(st, "gates", [128, NT * 24], F32)
        wring = Ring("w", 2)
        with ExitStack() as sg:
            wg = self.sb(sg, "wg", [128, KC, 24], BF16)
            A(POOL, lambda e: e.dma_start(out=wg[:], in_=w_in[:, 2560:2584].rearrange("(k p) n -> p k n", p=128)), w=["wg"], dma="wg")
            pb = self.sring.next()
            for t in range(NT):
                for k in range(KC):
                    A(PE, lambda e, k=k, t=t, pb=pb: e.matmul(self.bank[pb][:, t * 24:(t + 1) * 24], lhsT=hT[:, k, t * 128:(t + 1) * 128], rhs=wg[:, k, :],
                                                              start=(k == 0), stop=(k == KC - 1)), r=["wg", hkeys[t]], w=[f"B{pb}"])
            A(ACT, lambda e, pb=pb: e.activation(out=gates[:], in_=self.bank[pb][:, 0:NT * 24], func=AF.Sigmoid), r=[f"B{pb}"], w=["gates"])
            self.P.flush()
        for g in range(2):
            with ExitStack() as sgp:
                qT = [self.sb(sgp, f"qT{i}", [128, S], BF16) for i in range(4)]
                kslT = self.sb(sgp, "kslT", [128, S], BF16)
                kwnT = self.sb(sgp, "kwnT", [128, S], BF16)
                vsl = self.sb(sgp, "vsl", [128, NT, 129], BF16)
                vwn = self.sb(sgp, "vwn", [128, NT, 129], BF16)
                ocs = [self.sb(sgp, f"ocs{i}", [128, NT, 128], BF16) for i in range(4)]
                nbT = self.sb(sgp, "nbT", [128, S], BF16)
                A(DVE, lambda e: e.memset(nbT[:], 0.0), w=["nbT"])
                A(POOL, lambda e: e.memset(vsl[:, :, 128:129], 1.0), w=["vsl"])
                A(POOL, lambda e: e.memset(vwn[:, :, 128:129], 1.0), w=["vwn"])
                kvc = lambda which: w_in[:, 1024 + which * 256 + g * 128: 1024 + which * 256 + (g + 1) * 128]
                with ExitStack() as spj:
                    wsl = [self.sb(spj, f"wsl{i}", [128, KC, 128], BF16) for i in range(2)]
                    for r in range(4):
                        hh = g * 4 + r
                        _proj_fm(self, w_in[:, hh * 128:(hh + 1) * 128], wsl, wring, qT[r], f"qT{r}", hT, hkeys, r % 2 == 0)
                    _proj_fm(self, kvc(2), wsl, wring, kslT, "kslT", hT, hkeys, True)
                    _proj_tm(self, kvc(3), wsl, wring, vsl, "vsl", hT, hkeys)
                    _proj_fm(self, kvc(4), wsl, wring, kwnT, "kwnT", hT, hkeys, False)
                    _proj_tm(self, kvc(5), wsl, wring, vwn, "vwn", hT, hkeys)
                    self.P.flush()
                with ExitStack() as sc:
                    kcT = self.sb(sc, "kcT", [128, 128], BF16)
                    vc = self.sb(sc, "vc", [128, 128], BF16)
                    with ExitStack() as sca:
                        wsl = [self.sb(sca, f"wsl{i}", [128, KC, 128], BF16) for i in range(2)]
                        rawT1 = self.sb(sca, "rawT0", [128, S], BF16)
                        rawT = [rawT1, rawT1]
                        w1b = self.sb(sca, "w1b", [128, 32, 128], BF16)
                        w2b = self.sb(sca, "w2b", [128, 128], BF16)
                        peT = self.sb(sca, "peT", [128, 32], F32)
                        peTb = self.sb(sca, "peTb", [128, 32], BF16)
                        b1 = self.sb(sca, "b1", [128, 1], F32)
                        g1 = self.sb(sca, "g1", [128, 128], BF16)
                        for which in range(2):
                            _proj_fm(self, kvc(which), wsl, wring, rawT[which], "rawT0", hT, hkeys, which == 0)
                            A(POOL, lambda e, which=which: e.dma_start(out=w1b[:], in_=dr["nsa_cmp_w1"][0, which].rearrange("(l d) f -> d l f", d=128)),
                              w=["w1b"], dma="w1b")
                            A(POOL, lambda e, which=which: e.dma_start(out=w2b[:], in_=dr["nsa_cmp_w2"][0, which]), w=["w2b"], dma="w2b")
                            A(SP, lambda e, which=which: e.dma_start(out=peT[:], in_=dr["nsa_cmp_pos"][0, which].rearrange("l d -> d l"),
                                                                     allow_slow_non_contiguous=True), w=["peT"], dma="peT")
                            A(DVE, lambda e: e.tensor_copy(out=peTb[:], in_=peT[:]), r=["peT"], w=["peTb"])
                            pz = self.sring.next()
                            for l in range(32):
                                A(PE, lambda e, l=l, pz=pz: e.matmul(self.bank[pz][:, 0:1], lhsT=w1b[:, l, :], rhs=peTb[:, l:l + 1], start=(l == 0), stop=(l == 31)),
                                  r=["w1b", "peTb"], w=[f"B{pz}"])
                            A(DVE, lambda e, pz=pz: e.tensor_copy(out=b1[:], in_=self.bank[pz][:, 0:1]), r=[f"B{pz}"], w=["b1"])
                            ph = self.sring.next()
                            for l in range(32):
                                A(PE, lambda e, l=l, ph=ph, which=which: e.matmul(self.bank[ph][:, 0:127], lhsT=w1b[:, l, :], rhs=rawT[which][:, l:l + 2017:16],
                                                                                 start=(l == 0), stop=(l == 31)), r=["w1b", "rawT0"], w=[f"B{ph}"])
                            A(ACT, lambda e, ph=ph: e.activation(out=g1[:, 0:127], in_=self.bank[ph][:, 0:127], func=AF.Gelu_apprx_tanh, bias=b1[:, 0:1], scale=1.0),
                              r=[f"B{ph}", "b1"], w=["g1"])
                            po = self.sring.next()
                            if which == 0:
                                A(PE, lambda e, po=po: e.matmul(self.bank[po][:, 0:127], lhsT=w2b[:], rhs=g1[:, 0:127], start=True, stop=True), r=["w2b", "g1"], w=[f"B{po}"])
                                A(DVE, lambda e, po=po: e.tensor_copy(out=kcT[:, 0:127], in_=self.bank[po][:, 0:127]), r=[f"B{po}"], w=["kcT"])
                            else:
                                A(PE, lambda e, po=po: e.matmul(self.bank[po][0:127, 0:128], lhsT=g1[:, 0:127], rhs=w2b[:], start=True, stop=True), r=["w2b", "g1"], w=[f"B{po}"])
                                A(DVE, lambda e, po=po: e.tensor_copy(out=vc[0:127, :], in_=self.bank[po][0:127, 0:128]), r=[f"B{po}"], w=["vc"])
                        self.P.flush()
                    with ExitStack() as scl:
                        cmask = self.sb(scl, "cmask", [128, NT * 127], BF16)
                        ovt = self.sb(scl, "ovt", [128, 32], BF16)
                        zer = self.sb(scl, "zer", [128, 512], BF16)
                        Dc = [self.sb(scl, f"Dc{i}", [128, 127], F32) for i in range(2)]
                        tc_ = [self.sb(scl, f"tc{i}", [128, 127], F32) for i in range(2)]
                        pc = [self.sb(scl, f"pc{i}", [128, 127], F32) for i in range(2)]
                        pnb = [self.sb(scl, f"pnb{i}", [128, 127], BF16) for i in range(2)]
                        pnT = [self.sb(scl, f"pnT{i}", [128, 128], BF16) for i in range(2)]
                        sm = [self.sb(scl, f"sm{i}", [128, 8], F32) for i in range(2)]
                        A(SP, lambda e: e.dma_start(out=cmask[:], in_=dr["nsa_cmask"]), w=["cmask"], dma="cmask")
                        A(SP, lambda e: e.dma_start(out=ovt[:], in_=dr["ov"]), w=["ovt"], dma="ovt")
                        A(POOL, lambda e: e.memset(zer[:], 0.0), w=["zer"])
                        A(PE, lambda e: e.matmul(self.bank[4][:], lhsT=cb[:, 4, :], rhs=zer[:], start=True, stop=False, skip_group_check=True), r=["cb", "zer"], w=["B4"])
                        cstages = []
                        ci = 0
                        for r in range(4):
                            hh = g * 4 + r
                            slope = 2.0 ** (-(hh + 1))
                            for t in range(NT):
                                b = ci % 2
                                ci += 1
                                cd = {}

                                def ca1(cd=cd, t=t, r=r, b=b):
                                    pS = self.sring.next()
                                    cd["pS"] = pS
                                    A(PE, lambda e: e.matmul(self.bank[pS][:, 0:127], lhsT=qT[r][:, t * 128:(t + 1) * 128], rhs=kcT[:, 0:127], start=True, stop=True),
                                      r=[f"qT{r}", "kcT"], w=[f"B{pS}"])
                                    A(ACT, lambda e: e.activation(out=Dc[b][:], in_=self.posq[:, 31:2048:16], func=AF.Abs, bias=self.nposk[:, t:t + 1], scale=1.0),
                                      r=["posq", "posk"], w=[f"Dc{b}"])

                                def ca2(cd=cd, t=t, r=r, b=b, slope=slope):
                                    pS = cd["pS"]
                                    A(DVE, lambda e: e.scalar_tensor_tensor(out=tc_[b][:], in0=Dc[b][:], scalar=-slope / scale, in1=self.bank[pS][:, 0:127],
                                                                            op0=ALU.mult, op1=ALU.add), r=[f"Dc{b}", f"B{pS}"], w=[f"tc{b}"])
                                    A(DVE, lambda e: e.reduce_max(out=sm[b][:, 0:1], in_=tc_[b][:], axis=AX.X), r=[f"tc{b}"], w=[f"sm{b}"])
                                    A(DVE, lambda e: e.tensor_scalar(out=sm[b][:, 1:2], in0=sm[b][:, 0:1], scalar1=-scale, scalar2=None, op0=ALU.mult), r=[f"sm{b}"], w=[f"sm{b}"])
                                    A(ACT, lambda e: e.activation(out=pc[b][:], in_=tc_[b][:], func=AF.Exp, bias=sm[b][:, 1:2], scale=scale), r=[f"tc{b}", f"sm{b}"], w=[f"pc{b}"])

                                def cb_(cd=cd, t=t, r=r, b=b, hh=hh):
                                    A(DVE, lambda e: e.tensor_tensor(out=pc[b][:], in0=pc[b][:], in1=cmask[:, t * 127:(t + 1) * 127], op=ALU.mult), r=[f"pc{b}", "cmask"], w=[f"pc{b}"])
                                    A(DVE, lambda e: e.reduce_sum(out=sm[b][:, 2:3], in_=pc[b][:], axis=AX.X), r=[f"pc{b}"], w=[f"sm{b}"])
                                    A(DVE, lambda e: e.tensor_scalar(out=sm[b][:, 2:3], in0=sm[b][:, 2:3], scalar1=1e-30, scalar2=None, op0=ALU.max), r=[f"sm{b}"], w=[f"sm{b}"])
                                    A(DVE, lambda e: e.reciprocal(out=sm[b][:, 3:4], in_=sm[b][:, 2:3]), r=[f"sm{b}"], w=[f"sm{b}"])
                                    A(DVE, lambda e: e.tensor_scalar(out=pnb[b][:], in0=pc[b][:], scalar1=sm[b][:, 3:4], scalar2=None, op0=ALU.mult), r=[f"pc{b}", f"sm{b}"], w=[f"pnb{b}"])
                                    pt = self.sring.next()
                                    A(PE, lambda e: e.transpose(out=self.bankbf(pt)[0:127, 0:128], in_=pnb[b][:], identity=cb[:, 0, :]), r=[f"pnb{b}", "cb"], w=[f"B{pt}"])
                                    A(ACT, lambda e: e.activation(out=pnT[b][0:127, :], in_=self.bankbf(pt)[0:127, 0:128], func=AF.Copy), r=[f"B{pt}"], w=[f"pnT{b}"])
                                    po = self.sring.next()
                                    A(PE, lambda e: e.matmul(self.bank[po][:, 0:128], lhsT=pnT[b][0:127, :], rhs=vc[0:127, :], start=True, stop=True), r=[f"pnT{b}", "vc"], w=[f"B{po}"])
                                    A(PE, lambda e: e.matmul(self.bank[4][:, t * 32:(t + 1) * 32], lhsT=pnT[b][0:127, :], rhs=ovt[0:127, :], start=False, stop=False,
                                                             skip_group_check=True), r=[f"pnT{b}", "ovt"], w=["B4"])
                                    A(ACT, lambda e: e.activation(out=ocs[r][:, t, :], in_=self.bank[po][:, 0:128], func=AF.Copy,
                                                                  scale=gates[:, t * 24 + hh:t * 24 + hh + 1]), r=[f"B{po}", "gates"], w=[f"ocs{r}"])
                                cstages.append((ca1, ca2, cb_))
                        ncs = len(cstages)
                        for step in range(-2, ncs):
                            for si, off in enumerate((2, 1, 0)):
                                k = step + off
                                if 0 <= k < ncs:
                                    cstages[k][si]()
                        self.P.flush()
                    impm = self.sb(sc, "impm", [128, 512], F32)
                    work = self.sb(sc, "work", [128, 512], F32)
                    nval = self.sb(sc, "nval", [128, 512], F32)
                    nbon = self.sb(sc, "nbon", [128, 512], F32)
                    m8a = self.sb(sc, "m8a", [128, 128], F32)
                    m8b = self.sb(sc, "m8b", [128, 128], F32)
                    selt = self.sb(sc, "selt", [128, 512], F32)
                    nbb = self.sb(sc, "nbb", [128, 512], BF16)
                    A(SP, lambda e: e.dma_start(out=nval[:], in_=dr["nsa_valid"]), w=["nval"], dma="nval")
                    A(SP, lambda e: e.dma_start(out=nbon[:], in_=dr["nsa_bonus"]), w=["nbon"], dma="nbon")
                    A(DVE, lambda e: e.tensor_tensor(out=impm[:], in0=self.bank[4][:], in1=nval[:], op=ALU.mult), r=["B4", "nval"], w=["impm"])
                    A(DVE, lambda e: e.tensor_tensor(out=impm[:], in0=impm[:], in1=nbon[:], op=ALU.add), r=["impm", "nbon"], w=["impm"])
                    for t in range(NT):
                        sl = slice(t * 32, (t + 1) * 32)
                        s8 = slice(t * 8, (t + 1) * 8)
                        A(DVE, lambda e, sl=sl, s8=s8: e.max(out=m8a[:, s8], in_=impm[:, sl]), r=["impm"], w=["m8a"])
                        A(DVE, lambda e, sl=sl, s8=s8: e.match_replace(out=work[:, sl], in_to_replace=m8a[:, s8], in_values=impm[:, sl], imm_value=-3.0e38),
                          r=["impm", "m8a"], w=["work"])
                        A(DVE, lambda e, sl=sl, s8=s8: e.max(out=m8b[:, s8], in_=work[:, sl]), r=["work"], w=["m8b"])
                        A(DVE, lambda e, sl=sl, t=t: e.tensor_scalar(out=selt[:, sl], in0=impm[:, sl], scalar1=m8b[:, t * 8 + 7:t * 8 + 8], scalar2=None, op0=ALU.is_ge),
                          r=["impm", "m8b"], w=["selt"])
                    A(DVE, lambda e: e.tensor_tensor(out=selt[:], in0=selt[:], in1=nval[:], op=ALU.mult), r=["selt", "nval"], w=["selt"])
                    A(DVE, lambda e: e.tensor_scalar(out=nbb[:], in0=selt[:], scalar1=-NEGB, scalar2=NEGB, op0=ALU.mult, op1=ALU.add), r=["selt"], w=["nbb"])
                    for q4 in range(2):
                        pb = self.sring.next()
                        for tt in range(8):
                            t = q4 * 8 + tt
                            A(PE, lambda e, t=t, tt=tt, pb=pb: e.transpose(out=self.bankbf(pb)[0:32, tt * 128:(tt + 1) * 128], in_=nbb[:, t * 32:(t + 1) * 32],
                                                                          identity=cb[:, 0, :]), r=["nbb", "cb"], w=[f"B{pb}"])
                        A(ACT, lambda e, q4=q4, pb=pb: e.activation(out=nbT[0:32, q4 * 1024:(q4 + 1) * 1024], in_=self.bankbf(pb)[0:32, :], func=AF.Copy),
                          r=[f"B{pb}"], w=["nbT"])
                    self.P.flush()
                with ExitStack() as sw:
                    e32t = self.sb(sw, "e32t", [128, 2048], BF16)
                    PT = [self.sb(sw, f"PT{i}", [128, 512], BF16) for i in range(2)]
                    TMP = [self.sb(sw, f"TMP{i}", [128, 512], F32) for i in range(2)]
                    DT = [self.sb(sw, f"DT{i}", [128, 512], F32) for i in range(2)]
                    otok = [self.sb(sw, f"otok{i}", [128, 128], BF16) for i in range(2)]
                    rd = [self.sb(sw, f"rd{i}", [128, 2], F32) for i in range(2)]
                    A(SP, lambda e: e.dma_start(out=e32t[:], in_=dr["e32"]), w=["e32t"], dma="e32t")
                    oi = [0]
                    for r in range(4):
                        hh = g * 4 + r
                        slope = 2.0 ** (-(hh + 1))

                        def epi_sel(c, i, r=r, hh=hh):
                            ob = oi[0] % 2
                            oi[0] += 1
                            t = 4 * c + i
                            A(DVE, lambda e: e.reciprocal(out=rd[ob][:, 0:1], in_=self.bank[4 + i][:, 128:129]), r=[f"B{4 + i}"], w=[f"rd{ob}"])
                            A(DVE, lambda e: e.tensor_tensor(out=rd[ob][:, 1:2], in0=rd[ob][:, 0:1], in1=gates[:, t * 24 + 8 + hh:t * 24 + 9 + hh], op=ALU.mult),
                              r=[f"rd{ob}", "gates"], w=[f"rd{ob}"])
                            A(DVE, lambda e: e.scalar_tensor_tensor(out=ocs[r][:, t, :], in0=self.bank[4 + i][:, 0:128], scalar=rd[ob][:, 1:2], in1=ocs[r][:, t, :],
                                                                    op0=ALU.mult, op1=ALU.add), r=[f"B{4 + i}", f"rd{ob}", f"ocs{r}"], w=[f"ocs{r}"])

                        def epi_win(c, i, r=r, hh=hh):
                            ob = oi[0] % 2
                            oi[0] += 1
                            t = 4 * c + i
                            A(DVE, lambda e: e.reciprocal(out=rd[ob][:, 0:1], in_=self.bank[4 + i][:, 128:129]), r=[f"B{4 + i}"], w=[f"rd{ob}"])
                            A(DVE, lambda e: e.tensor_tensor(out=rd[ob][:, 1:2], in0=rd[ob][:, 0:1], in1=gates[:, t * 24 + 16 + hh:t * 24 + 17 + hh], op=ALU.mult),
                              r=[f"rd{ob}", "gates"], w=[f"rd{ob}"])
                            A(DVE, lambda e: e.scalar_tensor_tensor(out=otok[ob][:], in0=self.bank[4 + i][:, 0:128], scalar=rd[ob][:, 1:2], in1=ocs[r][:, t, :],
                                                                    op0=ALU.mult, op1=ALU.add), r=[f"B{4 + i}", f"rd{ob}", f"ocs{r}"], w=[f"otok{ob}"])
                            pb = self.sring.next()
                            A(PE, lambda e, pb=pb: e.transpose(out=self.bankbf(pb)[:, 0:128], in_=otok[ob][:], identity=cb[:, 0, :]), r=[f"otok{ob}", "cb"], w=[f"B{pb}"])
                            A(ACT, lambda e, pb=pb: e.activation(out=oT[:, r, t * 128:(t + 1) * 128], in_=self.bankbf(pb)[:, 0:128], func=AF.Copy), r=[f"B{pb}"], w=["oT"])

                        _attn_softmax(self, range(4), [(kslT, qT[r])], ["kslT", f"qT{r}"], vsl, "vsl", scale, slope, _causal_items,
                                      (lambda kt: e32t[:, kt * 128:(kt + 1) * 128], nbT, ["e32t", "nbT"]), PT, TMP, DT, epi_sel)
                        _attn_softmax(self, range(4), [(kwnT, qT[r])], ["kwnT", f"qT{r}"], vwn, "vwn", scale, slope, _win_items,
                                      None, PT, TMP, DT, epi_win)
                    self.P.flush()
            with ExitStack() as so:
                wo = self.sb(so, "wo", [128, 4, D], BF16)
                self.out_proj(wo, "wo", oT, 4, dr["nsa_w_o"][0, g * 512:(g + 1) * 512, :], ["oT"])
                self.P.flush()
        self.P.flush()
    with ExitStack() as st_ln:
        self.layer_norm(st_ln, li, 0)
        self.P.flush()


Builder.nsa = _nsa
```

```python
import numpy as np
import ml_dtypes
import concourse.bass as bass
import concourse.mybir as mybir
from concourse.bass_utils import run_bass_kernel_spmd

F32 = mybir.dt.float32
BF16 = mybir.dt.bfloat16
I32 = mybir.dt.int32
AF = mybir.ActivationFunctionType
ALU = mybir.AluOpType
AX = mybir.AxisListType

PE, ACT, DVE, POOL, SP = "tensor", "scalar", "vector", "gpsimd", "sync"
ENGINES = (PE, ACT, DVE, POOL, SP)


class Prog:
    def __init__(self, nc, es):
        self.nc = nc
        self.es = es
        self.sems = {}
        for e in ENGINES:
            self.sems[("eng", e)] = es.enter_context(nc.semaphore("s_" + e))
        self.counters = {e: 0 for e in ENGINES}
        self.chan_count = {}
        self.water = {e: {} for e in ENGINES}
        self.n_total = 0
        self._reset()

    def _reset(self):
        self.ops = []
        self.writers = {}
        self.readers = {}

    def add(self, eng, fn, reads=(), writes=(), dma=None):
        idx = len(self.ops)
        deps = set()
        src = eng if dma is None else ("dma", dma)
        for b in reads:
            deps.update(self.writers.get(b, {}).values())
        for b in writes:
            deps.update(self.writers.get(b, {}).values())
            deps.update(self.readers.get(b, {}).values())
        op = dict(eng=eng, fn=fn, deps=deps, dma=dma, ticket=None)
        if dma is not None:
            self.chan_count[dma] = self.chan_count.get(dma, 0) + 1
            op["dma_val"] = 16 * self.chan_count[dma]
        self.ops.append(op)
        for b in writes:
            self.writers.setdefault(b, {})[src] = idx
        for b in reads:
            self.readers.setdefault(b, {})[src] = idx
        return idx

    def flush(self):
        nc = self.nc
        ops = self.ops
        needed = set()
        last = {}
        for i, op in enumerate(ops):
            if op["dma"] is None:
                last[op["eng"]] = i
        needed.update(last.values())
        for op in ops:
            for d in op["deps"]:
                dop = ops[d]
                if dop["dma"] is None and not (dop["eng"] == PE and op["eng"] == PE and op["dma"] is None):
                    needed.add(d)
        for i, op in enumerate(ops):
            if op["dma"] is None and i in needed:
                self.counters[op["eng"]] += 1
                op["ticket"] = self.counters[op["eng"]]
        for c in self.chan_count:
            if ("dma", c) not in self.sems:
                self.sems[("dma", c)] = self.es.enter_context(nc.semaphore("d_" + str(c)))
        streams = {e: [] for e in ENGINES}
        for i, op in enumerate(ops):
            e = op["eng"]
            waits = {}
            for d in op["deps"]:
                dop = ops[d]
                if dop["dma"] is not None:
                    key, val = ("dma", dop["dma"]), dop["dma_val"]
                else:
                    if dop["eng"] == PE and e == PE and op["dma"] is None:
                        continue
                    key, val = ("eng", dop["eng"]), dop["ticket"]
                if val > waits.get(key, 0):
                    waits[key] = val
            wl = []
            for key, val in waits.items():
                if self.water[e].get(key, 0) >= val:
                    continue
                self.water[e][key] = val
                wl.append((key, val))
            streams[e].append((op, wl))
        final = [(("eng", e), self.counters[e]) for e in ENGINES] + \
                [(("dma", c), 16 * n) for c, n in self.chan_count.items()]
        sems = self.sems
        with nc.Block() as block:
            def mk(ename):
                def body(eng):
                    for op, wl in streams[ename]:
                        for key, val in wl:
                            eng.wait_ge(sems[key], val)
                        ins = op["fn"](eng)
                        if op["dma"] is not None:
                            ins.then_inc(sems[("dma", op["dma"])], 16)
                        elif op["ticket"] is not None:
                            ins.then_inc(sems[("eng", ename)], 1)
                    for key, val in final:
                        if val == 0 or self.water[ename].get(key, 0) >= val:
                            continue
                        eng.wait_ge(sems[key], val)
                        self.water[ename][key] = val
                return body
            block.tensor(mk(PE))
            block.scalar(mk(ACT))
            block.vector(mk(DVE))
            block.gpsimd(mk(POOL))
            block.sync(mk(SP))
        self.n_total += len(ops)
        self._reset()


S, D, NT, KC, DFF, NFC = 2048, 1024, 16, 8, 2816, 22
ALPHA = 8.0 ** 0.25
NEGB = -30000.0
PI = float(np.pi)


def host_consts():
    kp = np.arange(128)[:, None]
    qf = np.arange(128)[None, :]
    cb = np.zeros((128, 8, 128), np.float32)
    cb[:, 0] = np.eye(128)
    cb[:, 1] = np.where(kp <= qf, 0.0, NEGB)
    cb[:, 2] = np.where(kp > qf, 0.0, NEGB)
    cb[:, 3] = -(kp > qf).astype(np.float32)
    cb[:, 4] = 1.0
    cb[:, 5] = -1.0
    cb[:, 6] = (kp < qf).astype(np.float32)
    e8 = np.zeros((128, 8, 128), np.float32)
    for n in range(8):
        e8[n, n, :] = 1.0
    e32 = np.zeros((128, 16, 128), np.float32)
    for kt in range(16):
        for k in range(128):
            e32[2 * kt + k // 64, kt, k] = 1.0
    t_idx = np.arange(16)[None, :, None]
    n_idx = np.arange(32)[None, None, :]
    own = t_idx // 2
    moba_neg = np.broadcast_to(np.where(n_idx < own, 0.0, -1e30), (128, 16, 32)).astype(np.float32)
    moba_notown = np.broadcast_to((n_idx != own).astype(np.float32), (128, 16, 32)).astype(np.float32)
    invf = (np.float32(10000.0) ** (-np.arange(32, dtype=np.float32) / np.float32(32))).astype(np.float32)
    invf_bc = np.broadcast_to(invf[None, :], (128, 32)).astype(np.float32)
    q_idx = (np.arange(16)[None, :] * 128 + np.arange(128)[:, None])
    c_end = np.arange(127) * 16 + 31
    nsa_cmask = (c_end[None, None, :] <= q_idx[:, :, None]).astype(np.float32)
    qblk = q_idx // 64
    jj = np.arange(32)[None, None, :]
    valid = jj <= qblk[:, :, None]
    forced = (jj == 0) | (jj == qblk[:, :, None]) | (jj == qblk[:, :, None] - 1)
    nsa_bonus = np.where(valid, 100.0 * forced, -1e30).astype(np.float32)
    nsa_valid = valid.astype(np.float32)
    c_start = np.arange(127) * 16
    j_start = np.arange(32) * 64
    overlap = ((c_start[:, None] < j_start[None, :] + 64) & (c_end[:, None] >= j_start[None, :])).astype(np.float32)
    ov = np.zeros((128, 32), np.float32)
    ov[:127] = overlap
    bf = ml_dtypes.bfloat16
    return {
        "cb": cb.astype(bf), "e8": e8.reshape(128, 1024).astype(bf), "e32": e32.reshape(128, 2048).astype(bf),
        "moba_neg": moba_neg.reshape(128, 512), "moba_notown": moba_notown.reshape(128, 512),
        "invf": invf_bc, "nsa_cmask": nsa_cmask.reshape(128, 16 * 127).astype(bf), "nsa_bonus": nsa_bonus.reshape(128, 512),
        "nsa_valid": nsa_valid.reshape(128, 512), "ov": ov.astype(bf),
    }


WEIGHT_SPECS = [
    ("mod_w", [4, 1024, 6144]), ("mod_b", [4, 6144]), ("ln_g", [4, 2, 1024]), ("ln_b", [4, 2, 1024]),
    ("ffn_w_in", [4, 1024, 5632]), ("ffn_conv_w", [4, 3, 2816]), ("ffn_conv_b", [4, 2816]),
    ("ffn_w_out", [4, 2816, 1024]),
    ("mla_w_in", [1, 1024, 576]), ("mla_q_norm", [1, 256]), ("mla_w_uq", [1, 256, 1536]),
    ("mla_kv_norm", [1, 256]), ("mla_w_ukv", [1, 256, 2048]), ("mla_w_o", [1, 1024, 1024]),
    ("moba_w_in", [1, 1024, 3072]), ("moba_w_o", [1, 1024, 1024]),
    ("nsa_w_in", [1, 1024, 2584]), ("nsa_cmp_pos", [1, 2, 32, 128]), ("nsa_cmp_w1", [1, 2, 4096, 128]),
    ("nsa_cmp_w2", [1, 2, 128, 128]), ("nsa_w_o", [1, 1024, 1024]),
    ("sb_w_in", [1, 1024, 3072]), ("sb_w_o", [1, 1024, 1024]),
]
CONST_SPECS = [
    ("cb", [128, 8, 128], BF16), ("e8", [128, 1024], BF16), ("e32", [128, 2048], BF16),
    ("moba_neg", [128, 512], F32), ("moba_notown", [128, 512], F32), ("invf", [128, 32], F32),
    ("nsa_cmask", [128, 16 * 127], BF16), ("nsa_bonus", [128, 512], F32), ("nsa_valid", [128, 512], F32),
    ("ov", [128, 32], BF16),
]


class Ring:
    def __init__(self, name, n):
        self.vals = list(range(n)) if isinstance(n, int) else list(n)
        self.name, self.n, self.i = name, len(self.vals), 0

    def next(self):
        k = self.vals[self.i % self.n]
        self.i += 1
        return k


class Builder:
    def __init__(self, layers=(0, 1, 2, 3), taps=None):
        from contextlib import ExitStack
        self.layers = layers
        self.nc = nc = bass.Bass("TRN2", target_bir_lowering=False)
        self.es = ExitStack()
        self.P = Prog(nc, self.es)
        self.dr = {}
        self.dr["x"] = nc.dram_tensor("x", [S, D], F32, kind="ExternalInput").ap()
        self.dr["c"] = nc.dram_tensor("c", [1, D], F32, kind="ExternalInput").ap()
        self.dr["positions"] = nc.dram_tensor("positions", [1, S], I32, kind="ExternalInput").ap()
        for name, shp in WEIGHT_SPECS:
            self.dr[name] = nc.dram_tensor(name, shp, F32, kind="ExternalInput").ap()
        for name, shp, dt in CONST_SPECS:
            self.dr[name] = nc.dram_tensor(name, shp, dt, kind="ExternalInput").ap()
        self.dr["out"] = nc.dram_tensor("out", [S, D], F32, kind="ExternalOutput").ap()
        self.uid = 0

    def sb(self, st, name, shape, dt):
        self.uid += 1
        return st.enter_context(self.nc.sbuf_tensor(f"{name}_{self.uid}", shape, dt))

    def A(self, eng, fn, r=(), w=(), dma=None):
        return self.P.add(eng, fn, reads=r, writes=w, dma=dma)

    def psum_setup(self, st):
        self.uid += 1
        self.bank = [st.enter_context(self.nc.psum_tensor(f"bank{i}_{self.uid}", [128, 512], F32)) for i in range(8)]

    def bankbf(self, i):
        return self.bank[i][:].bitcast(BF16)

    def build(self):
        nc, A, dr = self.nc, self.A, self.dr
        from contextlib import ExitStack
        es = self.es
        self.psum_setup(es)
        self.x = self.sb(es, "x", [128, NT, D], F32)
        self.cb = self.sb(es, "cb", [128, 8, 128], BF16)
        self.posq = self.sb(es, "posq", [128, S], F32)
        self.posk = self.sb(es, "posk", [128, NT], F32)
        self.nposk = self.sb(es, "nposk", [128, NT], F32)
        self.crep = self.sb(es, "crep", [128, KC, 128], BF16)
        self.gate = self.sb(es, "gate", [128, D], F32)
        self.ms = self.sb(es, "ms", [128, 2, D], F32)
        self.small = self.sb(es, "small", [128, 64], F32)
        x, cb = self.x, self.cb
        with ExitStack() as st:
            posi = self.sb(st, "posi", [128, S], I32)
            poski = self.sb(st, "poski", [128, NT], I32)
            cT = self.sb(st, "cT", [128, KC], F32)
            cA = self.sb(st, "cA", [128, KC], F32)
            A(SP, lambda e: e.dma_start(out=cb[:], in_=dr["cb"]), w=["cb"], dma="cb")
            A(SP, lambda e: e.dma_start(out=posi[:], in_=dr["positions"].partition_broadcast(128)), w=["posi"], dma="posi")
            A(SP, lambda e: e.dma_start(out=poski[:], in_=dr["positions"].rearrange("o (t p) -> p (o t)", p=128),
                                        allow_slow_non_contiguous=True), w=["poski"], dma="poski")
            A(SP, lambda e: e.dma_start(out=cT[:], in_=dr["c"].rearrange("o (k p) -> p (o k)", p=128),
                                        allow_slow_non_contiguous=True), w=["cT"], dma="cT")
            for t in range(NT):
                A(SP, lambda e, t=t: e.dma_start(out=x[:, t, :], in_=dr["x"][t * 128:(t + 1) * 128, :]),
                  w=[f"x{t}"], dma=f"x{t % 4}")
            A(DVE, lambda e: e.tensor_copy(out=self.posq[:], in_=posi[:]), r=["posi"], w=["posq"])
            A(DVE, lambda e: e.tensor_copy(out=self.posk[:], in_=poski[:]), r=["poski"], w=["posk"])
            A(DVE, lambda e: e.tensor_scalar(out=self.nposk[:], in0=self.posk[:], scalar1=-1.0, scalar2=None, op0=ALU.mult),
              r=["posk"], w=["posk"])
            A(ACT, lambda e: e.activation(out=cA[:], in_=cT[:], func=AF.Silu), r=["cT"], w=["cA"])
            for k in range(KC):
                A(DVE, lambda e, k=k: e.tensor_scalar(out=self.crep[:, k, :], in0=cb[:, 4, :], scalar1=cA[:, k:k + 1],
                                                      scalar2=None, op0=ALU.mult), r=["cA", "cb"], w=["crep"])
            self.subs = []
            for li in self.layers:
                if isinstance(li, tuple):
                    self.subs.append((li[1], 1))
                else:
                    self.subs += [(li, 0), (li, 1)]
            self.sub_i = 0
            for th in self.mod_slabs(st, self.subs[0][0], self.subs[0][1], nbuf=6):
                th()
            self.P.flush()
        for li in self.layers:
            self.layer(li)
        self.P.flush()
        return nc

    def mod_slabs(self, st, li, half, nbuf=2):
        A, dr = self.A, self.dr
        wsl = [self.sb(st, f"modw{i}", [128, KC, 256], BF16) for i in range(nbuf)]
        bsl = [self.sb(st, f"modb{i}", [128, 256], F32) for i in range(nbuf)]
        thunks = []
        for j in range(12):
            def slab(j=j):
                s = j % nbuf
                col0 = half * 3072 + j * 256
                A(POOL, lambda e: e.dma_start(
                    out=wsl[s][:], in_=dr["mod_w"][li, :, col0:col0 + 256].rearrange("(k p) n -> p k n", p=128)),
                  w=[f"modw{s}"], dma=f"modw{s}")
                A(SP, lambda e: e.dma_start(
                    out=bsl[s][:], in_=dr["mod_b"][li:li + 1, col0:col0 + 256].partition_broadcast(128)),
                  w=[f"modb{s}"], dma=f"modb{s}")
                pb = 6 + j % 2
                for k in range(KC):
                    A(PE, lambda e, k=k: e.matmul(self.bank[pb][:, 0:256], lhsT=self.crep[:, k, :],
                                                  rhs=wsl[s][:, k, :], start=(k == 0), stop=(k == KC - 1)),
                      r=["crep", f"modw{s}"], w=[f"B{pb}"])
                comp, off = (j * 256) // 1024, (j * 256) % 1024
                dst = self.ms[:, comp, off:off + 256] if comp < 2 else self.gate[:, off:off + 256]
                A(DVE, lambda e: e.tensor_tensor(out=dst, in0=self.bank[pb][:, 0:256], in1=bsl[s][:], op=ALU.add),
                  r=[f"B{pb}", f"modb{s}"], w=["modt"])
            thunks.append(slab)

        def fin():
            A(DVE, lambda e: e.tensor_scalar(out=self.ms[:, 1, :], in0=self.ms[:, 1, :], scalar1=1.0, scalar2=None,
                                             op0=ALU.add), r=["modt"], w=["modt"])
        thunks.append(fin)
        return thunks

    def build_hT(self, st, hT):
        A, x, modt = self.A, self.x, self.ms
        tmp = [self.sb(st, f"htmp{i}", [128, D], F32) for i in range(2)]
        hb = [self.sb(st, f"hb{i}", [128, D], BF16) for i in range(2)]
        for t in range(NT):
            s = t % 2
            A(DVE, lambda e, t=t, s=s: e.tensor_tensor(out=tmp[s][:], in0=x[:, t, :], in1=modt[:, 1, :], op=ALU.mult),
              r=[f"x{t}", "modt"], w=[f"htmp{s}"])
            A(DVE, lambda e, s=s: e.tensor_tensor(out=hb[s][:], in0=tmp[s][:], in1=modt[:, 0, :], op=ALU.add),
              r=[f"htmp{s}", "modt"], w=[f"hb{s}"])
            pb = 6 + s
            for k in range(KC):
                A(PE, lambda e, s=s, k=k, pb=pb: e.transpose(out=self.bankbf(pb)[:, k * 128:(k + 1) * 128],
                                                             in_=hb[s][:, k * 128:(k + 1) * 128], identity=self.cb[:, 0, :]),
                  r=[f"hb{s}", "cb"], w=[f"B{pb}"])
            A(ACT, lambda e, t=t, pb=pb: e.activation(
                out=hT[:, :, t * 128:(t + 1) * 128], in_=self.bankbf(pb).rearrange("p (k n) -> p k n", k=KC), func=AF.Copy),
              r=[f"B{pb}"], w=[f"hT{t}"])
            A(ACT, lambda e, t=t: e.activation(out=x[:, t, :], in_=x[:, t, :], func=AF.Copy, scale=ALPHA),
              r=[f"x{t}"], w=[f"x{t}"])

    def layer_norm(self, st, li, which):
        A, x, dr = self.A, self.x, self.dr
        self.sub_i += 1
        nxt = self.subs[self.sub_i] if self.sub_i < len(self.subs) else None
        slabs = self.mod_slabs(st, nxt[0], nxt[1]) if nxt is not None else []
        self.lng = self.sb(st, "lng", [128, D], F32)
        self.lnb = self.sb(st, "lnb", [128, D], F32)
        A(SP, lambda e: e.dma_start(out=self.lng[:], in_=dr["ln_g"][li, which:which + 1, :].partition_broadcast(128)),
          w=["lng"], dma="lng")
        A(SP, lambda e: e.dma_start(out=self.lnb[:], in_=dr["ln_b"][li, which:which + 1, :].partition_broadcast(128)),
          w=["lnb"], dma="lnb")
        stt = [self.sb(st, f"lnst{i}", [128, 16], F32) for i in range(3)]
        stages = []
        for t in range(NT):
            s = t % 3
            q = stt[s]

            def b_act(t=t, q=q, s=s):
                A(ACT, lambda e: e.activation(out=x[:, t, :], in_=x[:, t, :], func=AF.Identity, bias=q[:, 15:16], scale=q[:, 14:15]),
                  r=[f"x{t}", f"lnst{s}"], w=[f"x{t}"])

            def a1(t=t, q=q, s=s):
                A(DVE, lambda e: e.bn_stats(out=q[:, 0:6], in_=x[:, t, 0:512]), r=[f"x{t}"], w=[f"lnst{s}"])
                A(DVE, lambda e: e.bn_stats(out=q[:, 6:12], in_=x[:, t, 512:1024]), r=[f"x{t}"], w=[f"lnst{s}"])
                A(DVE, lambda e: e.bn_aggr(out=q[:, 12:14], in_=q[:, 0:12]), r=[f"lnst{s}"], w=[f"lnst{s}"])
                A(ACT, lambda e: e.activation(out=q[:, 14:15], in_=q[:, 13:14], func=AF.Ln, bias=1e-5, scale=1.0), r=[f"lnst{s}"], w=[f"lnst{s}"])
                A(ACT, lambda e: e.activation(out=q[:, 14:15], in_=q[:, 14:15], func=AF.Exp, scale=-0.5), r=[f"lnst{s}"], w=[f"lnst{s}"])

            def a2(t=t, q=q, s=s):
                A(DVE, lambda e: e.scalar_tensor_tensor(out=q[:, 15:16], in0=q[:, 12:13], scalar=-1.0, in1=q[:, 14:15],
                                                        op0=ALU.mult, op1=ALU.mult), r=[f"lnst{s}"], w=[f"lnst{s}"])

            def b_dve(t=t):
                A(DVE, lambda e: e.tensor_tensor(out=x[:, t, :], in0=x[:, t, :], in1=self.lng[:], op=ALU.mult), r=[f"x{t}", "lng"], w=[f"x{t}"])
                A(DVE, lambda e: e.tensor_tensor(out=x[:, t, :], in0=x[:, t, :], in1=self.lnb[:], op=ALU.add), r=[f"x{t}", "lnb"], w=[f"x{t}"])
                if t < len(slabs):
                    slabs[t]()
                if nxt is None:
                    A(SP, lambda e: e.dma_start(out=dr["out"][t * 128:(t + 1) * 128, :], in_=x[:, t, :]),
                      r=[f"x{t}"], w=[f"out{t}"], dma=f"o{t % 4}")
            stages.append((b_act, a1, a2, b_dve))
        for step in range(-2, NT):
            for si, off in enumerate((0, 2, 1, 0)):
                k = step + off
                if 0 <= k < NT:
                    stages[k][si]()
        for th in slabs[NT:]:
            th()

    def out_proj(self, wo, wkey, srcT, nj, w_dram_rows, src_keys):
        A, x = self.A, self.x
        A(POOL, lambda e: e.dma_start(out=wo[:, 0:nj, :], in_=w_dram_rows.rearrange("(j p) n -> p j n", p=128)),
          w=[wkey], dma=wkey)
        for j in range(nj):
            A(DVE, lambda e, j=j: e.tensor_tensor(out=wo[:, j, :], in0=wo[:, j, :], in1=self.gate[:], op=ALU.mult),
              r=[wkey, "modt"], w=[wkey])
        for t in range(NT):
            for n in range(2):
                pb = 4 + (2 * t + n) % 2
                for j in range(nj):
                    A(PE, lambda e, t=t, n=n, j=j, pb=pb: e.matmul(
                        self.bank[pb][:], lhsT=srcT[:, j, t * 128:(t + 1) * 128], rhs=wo[:, j, n * 512:(n + 1) * 512],
                        start=(j == 0), stop=(j == nj - 1)), r=[wkey] + src_keys, w=[f"B{pb}"])
                A(DVE, lambda e, t=t, n=n, pb=pb: e.tensor_tensor(
                    out=x[:, t, n * 512:(n + 1) * 512], in0=self.bank[pb][:], in1=x[:, t, n * 512:(n + 1) * 512], op=ALU.add),
                  r=[f"B{pb}", f"x{t}"], w=[f"x{t}"])

    def ffn(self, li):
        from contextlib import ExitStack
        A, dr, x = self.A, self.dr, self.x
        with ExitStack() as st:
            hT = self.sb(st, "hT", [128, KC, S], BF16)
            with ExitStack() as st0:
                self.build_hT(st0, hT)
                self.P.flush()
            hkeys = [f"hT{t}" for t in range(NT)]
            cw = self.sb(st, "cw", [128, 3, NFC], F32)
            cbias = self.sb(st, "cbias", [128, NFC], F32)
            for wi in range(3):
                A(SP, lambda e, wi=wi: e.dma_start(out=cw[:, wi, :], in_=dr["ffn_conv_w"][li, wi:wi + 1, :].rearrange("o (j p) -> p (o j)", p=128),
                                                   allow_slow_non_contiguous=True), w=["cw"], dma="cw")
            A(SP, lambda e: e.dma_start(out=cbias[:], in_=dr["ffn_conv_b"][li:li + 1, :].rearrange("o (j p) -> p (o j)", p=128),
                                        allow_slow_non_contiguous=True), w=["cbias"], dma="cbias")
            wab = [self.sb(st, f"wab{i}", [128, KC, 2, 128], BF16) for i in range(2)]
            aS = [self.sb(st, f"aS{i}", [128, S + 2], F32) for i in range(2)]
            tS = [self.sb(st, f"tS{i}", [128, 512], F32) for i in range(2)]
            gT = [self.sb(st, f"gT{i}", [128, 4, S], BF16) for i in range(2)]
            wof = [self.sb(st, f"wof{i}", [128, 4, D], BF16) for i in range(2)]
            for i in range(2):
                A(POOL, lambda e, i=i: e.memset(aS[i][:, 0:2], 0.0), w=[f"aS{i}"])
            groups = [(0, 4), (4, 4), (8, 4), (12, 4), (16, 4), (20, 2)]
            it = 0
            pending_out = None
            for gi, (f0, nj) in enumerate(groups):
                gs = gi % 2
                for jj in range(nj):
                    j = f0 + jj
                    ws = j % 2
                    for half in range(2):
                        c0 = half * DFF + j * 128
                        A(POOL, lambda e, ws=ws, half=half, c0=c0: e.dma_start(
                            out=wab[ws][:, :, half, :], in_=dr["ffn_w_in"][li, :, c0:c0 + 128].rearrange("(k p) n -> p k n", p=128)),
                          w=[f"wab{ws}"], dma=f"wab{ws}")
                    for c in range(4):
                        s2 = it % 2
                        it += 1
                        pa, pbk = (0, 1, 7)[(it - 1) % 3], (2, 3, 6)[(it - 1) % 3]
                        for half, pbank in ((0, pa), (1, pbk)):
                            for k in range(KC):
                                A(PE, lambda e, ws=ws, half=half, pbank=pbank, k=k, c=c: e.matmul(
                                    self.bank[pbank][:], lhsT=wab[ws][:, k, half, :], rhs=hT[:, k, c * 512:(c + 1) * 512],
                                    start=(k == 0), stop=(k == KC - 1)),
                                  r=[f"wab{ws}"] + hkeys[4 * c:4 * c + 4], w=[f"B{pbank}"])
                        a_ = aS[ws]
                        A(ACT, lambda e, a_=a_, pa=pa, c=c: e.activation(out=a_[:, 2 + c * 512:2 + (c + 1) * 512],
                                                                         in_=self.bank[pa][:], func=AF.Copy),
                          r=[f"B{pa}"], w=[f"aS{ws}"])
                        A(DVE, lambda e, a_=a_, s2=s2, j=j, c=c: e.tensor_scalar(
                            out=tS[s2][:], in0=a_[:, c * 512:(c + 1) * 512], scalar1=cw[:, 0, j:j + 1],
                            scalar2=cbias[:, j:j + 1], op0=ALU.mult, op1=ALU.add),
                          r=[f"aS{ws}", "cw", "cbias"], w=[f"tS{s2}"])
                        A(DVE, lambda e, a_=a_, s2=s2, j=j, c=c: e.scalar_tensor_tensor(
                            out=tS[s2][:], in0=a_[:, 1 + c * 512:1 + (c + 1) * 512], scalar=cw[:, 1, j:j + 1], in1=tS[s2][:],
                            op0=ALU.mult, op1=ALU.add), r=[f"aS{ws}", "cw", f"tS{s2}"], w=[f"tS{s2}"])
                        A(DVE, lambda e, a_=a_, s2=s2, j=j, c=c: e.scalar_tensor_tensor(
                            out=tS[s2][:], in0=a_[:, 2 + c * 512:2 + (c + 1) * 512], scalar=cw[:, 2, j:j + 1], in1=tS[s2][:],
                            op0=ALU.mult, op1=ALU.add), r=[f"aS{ws}", "cw", f"tS{s2}"], w=[f"tS{s2}"])
                        A(ACT, lambda e, s2=s2: e.activation(out=tS[s2][:], in_=tS[s2][:], func=AF.Gelu_apprx_tanh),
                          r=[f"tS{s2}"], w=[f"tS{s2}"])
                        A(DVE, lambda e, s2=s2, gs=gs, jj=jj, c=c, pbk=pbk: e.tensor_tensor(
                            out=gT[gs][:, jj, c * 512:(c + 1) * 512], in0=self.bank[pbk][:], in1=tS[s2][:], op=ALU.mult),
                          r=[f"tS{s2}", f"B{pbk}"], w=[f"gT{gs}"])
                if pending_out is not None:
                    self.out_proj(*pending_out)
                pending_out = (wof[gs], f"wof{gs}", gT[gs], nj, dr["ffn_w_out"][li, f0 * 128:(f0 + nj) * 128, :], [f"gT{gs}"])
            self.out_proj(*pending_out)
            self.P.flush()
        with ExitStack() as st_ln:
            self.layer_norm(st_ln, li, 1)
            self.P.flush()

    def layer(self, li):
        if isinstance(li, tuple):
            self.ffn(li[1])
            return
        kind = li % 4
        if kind == 0:
            self.mla(li)
        elif kind == 1:
            self.moba(li)
        elif kind == 2:
            self.nsa(li)
        else:
            self.sbmix(li)
        self.ffn(li)


_CONSTS = None


def kernel(**inputs):
    return run_kernel(inputs, layers=(0, 1, 2, 3))


def run_kernel(inputs, layers, cores=8, x_override=None, trace=False):
    global _CONSTS
    if _CONSTS is None:
        _CONSTS = host_consts()
    b = Builder(layers=layers)
    nc = b.build()
    in_maps = []
    xs = inputs["x"] if x_override is None else x_override
    for ci in range(cores):
        m = {"x": np.ascontiguousarray(xs[ci], dtype=np.float32),
             "c": np.ascontiguousarray(inputs["c"][ci:ci + 1], dtype=np.float32),
             "positions": np.ascontiguousarray(inputs["positions"][ci:ci + 1], dtype=np.int32)}
        for name, _ in WEIGHT_SPECS:
            m[name] = np.ascontiguousarray(inputs[name], dtype=np.float32)
        for name, _, _ in CONST_SPECS:
            m[name] = _CONSTS[name]
        in_maps.append(m)
    res = run_bass_kernel_spmd(nc, in_maps, core_ids=list(range(cores)), **({'trace': True} if trace else {}))
    if trace:
        print('EXEC_TIME_NS', res.exec_time_ns)
    return np.stack([r["out"] for r in res.results], axis=0).astype(np.float32)


def _mixer_prologue(self, st, li):
    from contextlib import ExitStack
    hT = self.sb(st, "hT", [128, KC, S], BF16)
    with ExitStack() as st0:
        self.build_hT(st0, hT)
        self.P.flush()
    self.sring = Ring("s", 4)
    self.sring_s = None
    return hT, [f"hT{t}" for t in range(NT)]


def _proj_qkv(self, w3, h, wq, wkey, qT, qkey, kT, kkey, V, vkey, hT, hkeys, kmean=None):
    A = self.A
    for which in range(3):
        A(POOL, lambda e, which=which: e.dma_start(out=wq[:, :, which, :], in_=w3[which].rearrange("(k p) n -> p k n", p=128)),
          w=[wkey], dma=wkey)
    for which, (dst, dkey) in enumerate(((qT, qkey), (kT, kkey))):
        for c in range(4):
            pb = self.sring.next()
            for k in range(KC):
                A(PE, lambda e, which=which, k=k, c=c, pb=pb: e.matmul(
                    self.bank[pb][:], lhsT=wq[:, k, which, :], rhs=hT[:, k, c * 512:(c + 1) * 512],
                    start=(k == 0), stop=(k == KC - 1)), r=[wkey] + hkeys[4 * c:4 * c + 4], w=[f"B{pb}"])
            if which == 0:
                A(ACT, lambda e, c=c, pb=pb, dst=dst: e.activation(out=dst[:, c * 512:(c + 1) * 512], in_=self.bank[pb][:], func=AF.Copy),
                  r=[f"B{pb}"], w=[dkey])
            else:
                A(DVE, lambda e, c=c, pb=pb, dst=dst: e.tensor_copy(out=dst[:, c * 512:(c + 1) * 512], in_=self.bank[pb][:]),
                  r=[f"B{pb}"], w=[dkey])
                if kmean is not None:
                    A(DVE, lambda e, c=c, pb=pb: e.tensor_reduce(
                        out=kmean[:, 2 * c:2 * c + 2], in_=self.bank[pb][:].rearrange("p (b n) -> p b n", b=2), axis=AX.X, op=ALU.add),
                      r=[f"B{pb}"], w=["kmean"])
    for tg in range(4):
        pb = self.sring.next()
        for tt in range(4):
            t = tg * 4 + tt
            for k in range(KC):
                A(PE, lambda e, k=k, t=t, tt=tt, pb=pb: e.matmul(
                    self.bank[pb][:, tt * 128:(tt + 1) * 128], lhsT=hT[:, k, t * 128:(t + 1) * 128], rhs=wq[:, k, 2, :],
                    start=(k == 0), stop=(k == KC - 1)), r=[wkey, hkeys[t]], w=[f"B{pb}"])
        eng = ACT if tg % 2 == 0 else DVE
        if eng == ACT:
            A(ACT, lambda e, tg=tg, pb=pb: e.activation(out=V[:, tg * 4:(tg + 1) * 4, 0:128],
                                                        in_=self.bank[pb][:].rearrange("p (a n) -> p a n", a=4), func=AF.Copy),
              r=[f"B{pb}"], w=[vkey])
        else:
            A(DVE, lambda e, tg=tg, pb=pb: e.tensor_copy(out=V[:, tg * 4:(tg + 1) * 4, 0:128],
                                                         in_=self.bank[pb][:].rearrange("p (a n) -> p a n", a=4)),
              r=[f"B{pb}"], w=[vkey])


def _o_to_oT(self, src_ap, src_keys, otok, okey, oT, jj, t, scale_ap=None, scale_keys=()):
    A = self.A
    if scale_ap is None:
        A(ACT, lambda e: e.activation(out=otok[:], in_=src_ap, func=AF.Copy), r=list(src_keys), w=[okey])
    else:
        A(ACT, lambda e: e.activation(out=otok[:], in_=src_ap, func=AF.Copy, scale=scale_ap),
          r=list(src_keys) + list(scale_keys), w=[okey])
    pb = self.sring.next()
    A(PE, lambda e, pb=pb: e.transpose(out=self.bankbf(pb)[:, 0:128], in_=otok[:], identity=self.cb[:, 0, :]),
      r=[okey, "cb"], w=[f"B{pb}"])
    A(ACT, lambda e, pb=pb: e.activation(out=oT[:, jj, t * 128:(t + 1) * 128], in_=self.bankbf(pb)[:, 0:128], func=AF.Copy),
      r=[f"B{pb}"], w=["oT"])


def _sbmix(self, li):
    from contextlib import ExitStack
    A, dr, cb = self.A, self.dr, self.cb
    scale = 128.0 ** -0.5
    with ExitStack() as st:
        hT, hkeys = _mixer_prologue(self, st, li)
        wq1 = self.sb(st, "wq0", [128, KC, 3, 128], BF16)
        wq = [wq1, wq1]
        qT = [self.sb(st, f"qT{i}", [128, S], BF16) for i in range(2)]
        kT = [self.sb(st, f"kT{i}", [128, S], BF16) for i in range(2)]
        V = [self.sb(st, f"V{i}", [128, NT, 128], BF16) for i in range(2)]
        oT = self.sb(st, "oT", [128, 4, S], BF16)
        wo = self.sb(st, "wo", [128, 4, D], BF16)
        SPl = [self.sb(st, f"SPl{i}", [128, 512], F32) for i in range(2)]
        Lb = [self.sb(st, f"Lb{i}", [128, 512], BF16) for i in range(2)]
        Lsum = self.sb(st, "Lsum", [128, 512], BF16)
        T1 = [self.sb(st, f"T1{i}", [128, 512], F32) for i in range(3)]
        AT = [self.sb(st, f"AT{i}", [128, 512], BF16) for i in range(2)]
        otok = [self.sb(st, f"otok{i}", [128, 128], BF16) for i in range(2)]
        w_in = dr["sb_w_in"][0]
        self.atn_it = 0
        self.nls = 0
        self.oi = 0
        def prep(h):
            s = h % 2
            w3 = [w_in[:, which * 1024 + h * 128: which * 1024 + (h + 1) * 128] for which in range(3)]
            _proj_qkv(self, w3, h, wq[s], "wq0", qT[s], f"qT{s}", kT[s], f"kT{s}", V[s], f"V{s}", hT, hkeys)

        prep(0)
        for h in range(8):
            s = h % 2
            stages = []
            for c in range(4):
                for kt in range(4 * c + 3, -1, -1):
                    sd = {}

                    def stage1(sd=sd, c=c, kt=kt, s=s):
                        r_ = kt - 4 * c
                        q0 = max(0, r_) * 128
                        diag = r_ >= 0
                        b = self.atn_it % 2
                        t3 = self.atn_it % 3
                        sd["b"], sd["t3"] = b, t3
                        self.atn_it += 1
                        pz = self.sring.next()
                        A(PE, lambda e: e.matmul(
                            self.bank[pz][:, q0:512], lhsT=kT[s][:, kt * 128:(kt + 1) * 128], rhs=qT[s][:, c * 512 + q0:(c + 1) * 512],
                            start=True, stop=True), r=[f"kT{s}", f"qT{s}"], w=[f"B{pz}"])
                        A(ACT, lambda e: e.activation(out=SPl[b][:, q0:512], in_=self.bank[pz][:, q0:512], func=AF.Exp, scale=scale),
                          r=[f"B{pz}"], w=[f"SPl{b}"])
                        A(ACT, lambda e: e.activation(out=Lb[b][:, q0:512], in_=SPl[b][:, q0:512], func=AF.Ln, bias=1.0, scale=1.0),
                          r=[f"SPl{b}"], w=[f"Lb{b}"])
                        sd["pz"] = pz

                    def stage1d(sd=sd, c=c, kt=kt, s=s):
                        b, t3, pz = sd["b"], sd["t3"], sd["pz"]
                        r_ = kt - 4 * c
                        q0 = max(0, r_) * 128
                        diag = r_ >= 0
                        A(DVE, lambda e: e.scalar_tensor_tensor(
                            out=T1[t3][:, q0:512], in0=self.bank[pz][:, q0:512], scalar=scale, in1=Lb[b][:, q0:512], op0=ALU.mult, op1=ALU.subtract),
                          r=[f"B{pz}", f"Lb{b}"], w=[f"T1{t3}"])
                        if diag:
                            A(DVE, lambda e: e.tensor_tensor(out=Lb[b][:, q0:q0 + 128], in0=Lb[b][:, q0:q0 + 128], in1=cb[:, 6, :], op=ALU.mult),
                              r=[f"Lb{b}", "cb"], w=[f"Lb{b}"])

                    def stage2a(sd=sd, c=c, kt=kt, s=s, h=h):
                        b, t3 = sd["b"], sd["t3"]
                        r_ = kt - 4 * c
                        q0 = max(0, r_) * 128
                        first = (kt == 4 * c + 3)
                        if first:
                            A(DVE, lambda e: e.memset(Lsum[:], 0.0), w=["Lsum"])
                        pt = self.sring.next()
                        A(PE, lambda e: e.matmul(
                            self.bank[pt][:, q0:512], lhsT=cb[:, 3, :], rhs=Lb[b][:, q0:512], start=True, stop=first),
                          r=["cb", f"Lb{b}"], w=[f"B{pt}"])
                        if not first:
                            A(PE, lambda e: e.matmul(
                                self.bank[pt][:, q0:512], lhsT=cb[:, 5, :], rhs=Lsum[:, q0:512], start=False, stop=True),
                              r=["cb", "Lsum"], w=[f"B{pt}"])
                        if kt > 0:
                            A(DVE, lambda e: e.tensor_tensor(out=Lsum[:, q0:512], in0=Lsum[:, q0:512], in1=Lb[b][:, q0:512], op=ALU.add),
                              r=["Lsum", f"Lb{b}"], w=["Lsum"])
                        A(DVE, lambda e: e.tensor_tensor(out=T1[t3][:, q0:512], in0=self.bank[pt][:, q0:512], in1=T1[t3][:, q0:512], op=ALU.add),
                          r=[f"B{pt}", f"T1{t3}"], w=[f"T1{t3}"])

                    def stage2b(sd=sd, c=c, kt=kt, s=s, h=h):
                        b, t3 = sd["b"], sd["t3"]
                        r_ = kt - 4 * c
                        q0 = max(0, r_) * 128
                        diag = r_ >= 0
                        A(ACT, lambda e: e.activation(out=AT[b][:, q0:512], in_=T1[t3][:, q0:512], func=AF.Exp),
                          r=[f"T1{t3}"], w=[f"AT{b}"])
                        if diag:
                            A(DVE, lambda e: e.tensor_tensor(out=AT[b][:, q0:q0 + 128], in0=AT[b][:, q0:q0 + 128], in1=cb[:, 6, :], op=ALU.mult),
                              r=[f"AT{b}", "cb"], w=[f"AT{b}"])

                    def stage2p(sd=sd, c=c, kt=kt, s=s, h=h):
                        b = sd["b"]
                        r_ = kt - 4 * c
                        q0 = max(0, r_) * 128
                        for i in range(q0 // 128, 4):
                            A(PE, lambda e, i=i: e.matmul(
                                self.bank[4 + i][:, 0:128], lhsT=AT[b][:, i * 128:(i + 1) * 128], rhs=V[s][:, kt, :],
                                start=(kt == 4 * c + i), stop=(kt == 0)), r=[f"AT{b}", f"V{s}"], w=[f"B{4 + i}"])
                        if kt == 0:
                            for i in range(4):
                                ob = self.oi % 2
                                self.oi += 1
                                _o_to_oT(self, self.bank[4 + i][:, 0:128], [f"B{4 + i}"], otok[ob], f"otok{ob}", oT, h % 4, 4 * c + i)
                    stages.append((stage2b, stage1, stage2a, stage1d, stage2p))
            nst = len(stages)
            for step in range(-2, nst):
                if step == nst // 2 and h < 7:
                    prep(h + 1)
                for si, off in enumerate((0, 2, 1, 2, 0)):
                    k = step + off
                    if 0 <= k < nst:
                        stages[k][si]()
            if h % 4 == 3:
                hg = h // 4
                self.out_proj(wo, "wo", oT, 4, dr["sb_w_o"][0, hg * 512:(hg + 1) * 512, :], ["oT"])
        self.P.flush()
    with ExitStack() as st_ln:
        self.layer_norm(st_ln, li, 0)
        self.P.flush()


Builder.sbmix = _sbmix


def _attn_softmax(self, c_list, pairs, pkeys, V, vkey, scale, slope, items_of_chunk, maskmm, PT, TMP, DT, epi, far_ok=True, mid_hook=None):
    A, cb = self.A, self.cb
    nb = len(PT)
    stages = []
    for c in c_list:
        items = items_of_chunk(c)
        first_kt, last_kt = {}, {}
        for (kt, q0, q1, diag, far) in items:
            for i in range(q0 // 128, q1 // 128):
                first_kt.setdefault(i, kt)
                last_kt[i] = kt
        for idx, (kt, q0, q1, diag, far) in enumerate(items):
            s1, s2 = [], []

            st_ = {}

            def stage0(st_=st_, c=c, kt=kt, q0=q0, q1=q1):
                b = self.atn_it % nb
                self.atn_it += 1
                st_["b"] = b
                if slope is not None:
                    A(ACT, lambda e: e.activation(
                        out=DT[b][:, q0:q1], in_=self.posq[:, c * 512 + q0:c * 512 + q1], func=AF.Abs, bias=self.nposk[:, kt:kt + 1], scale=1.0),
                      r=["posq", "posk"], w=[f"DT{b}"])

            def stage1a(st_=st_, c=c, kt=kt, q0=q0, q1=q1, diag=diag, far=far):
                b = st_["b"]
                ps = (self.sring_s if (slope is None and self.sring_s is not None) else self.sring).next()
                st_["ps"] = ps
                mms = []
                for (kT_ap, qT_ap) in pairs:
                    mms.append((kT_ap[:, kt * 128:(kt + 1) * 128], qT_ap[:, c * 512 + q0:c * 512 + q1], q0, q1, list(pkeys)))
                if maskmm is not None:
                    lhs_fn, rhs_ap, mkeys = maskmm
                    mms.append((lhs_fn(kt), rhs_ap[:, c * 512 + q0:c * 512 + q1], q0, q1, list(mkeys)))
                if diag:
                    mms.append((cb[:, 0, :], cb[:, 1, :], q0, q0 + 128, ["cb"]))
                if far:
                    mms.append((cb[:, 0, :], cb[:, 2, :], q1 - 128, q1, ["cb"]))
                for mi, (lh, rh, a0, a1, keys) in enumerate(mms):
                    A(PE, lambda e, lh=lh, rh=rh, a0=a0, a1=a1, ps=ps, mi=mi, n=len(mms): e.matmul(
                        self.bank[ps][:, a0:a1], lhsT=lh, rhs=rh, start=(mi == 0), stop=(mi == n - 1)),
                      r=keys, w=[f"B{ps}"])
                if slope is not None:
                    A(DVE, lambda e: e.scalar_tensor_tensor(
                        out=TMP[b][:, q0:q1], in0=DT[b][:, q0:q1], scalar=-slope / scale, in1=self.bank[ps][:, q0:q1],
                        op0=ALU.mult, op1=ALU.add), r=[f"DT{b}", f"B{ps}"], w=[f"TMP{b}"])

            def stage1b(st_=st_, q0=q0, q1=q1):
                b, ps = st_["b"], st_["ps"]
                if slope is not None:
                    A(ACT, lambda e: e.activation(out=PT[b][:, q0:q1], in_=TMP[b][:, q0:q1], func=AF.Exp, scale=scale),
                      r=[f"TMP{b}"], w=[f"PT{b}"])
                else:
                    A(ACT, lambda e: e.activation(out=PT[b][:, q0:q1], in_=self.bank[ps][:, q0:q1], func=AF.Exp, scale=scale),
                      r=[f"B{ps}"], w=[f"PT{b}"])

            def stage2(st_=st_, c=c, kt=kt, q0=q0, q1=q1, last=(idx == len(items) - 1), first_kt=first_kt, last_kt=last_kt):
                b = st_["b"]
                for i in range(q0 // 128, q1 // 128):
                    A(PE, lambda e, i=i, s_=(kt == first_kt[i]), p_=(kt == last_kt[i]): e.matmul(
                        self.bank[4 + i][:, 0:129], lhsT=PT[b][:, i * 128:(i + 1) * 128], rhs=V[:, kt, 0:129], start=s_, stop=p_),
                      r=[f"PT{b}", vkey], w=[f"B{4 + i}"])
                if last:
                    for i in range(4):
                        if i in first_kt:
                            epi(c, i)
            stages.append((stage0, stage1a, stage1b, stage2))
    n = len(stages)
    offs = (3, 2, 1, 0)
    for step in range(-3, n):
        if mid_hook is not None and step == n // 2:
            mid_hook()
        for si, off in enumerate(offs):
            k = step + off
            if 0 <= k < n:
                stages[k][si]()


def _causal_items(c):
    out = []
    for kt in range(0, 4 * c + 4):
        r_ = kt - 4 * c
        out.append((kt, max(0, r_) * 128, 512, r_ >= 0, False))
    return out


def _moba(self, li):
    from contextlib import ExitStack
    A, dr, cb = self.A, self.dr, self.cb
    scale = 128.0 ** -0.5
    self.atn_it = 0
    with ExitStack() as st:
        hT, hkeys = _mixer_prologue(self, st, li)
        wq = self.sb(st, "wq0", [128, KC, 3, 128], BF16)
        qT = [self.sb(st, f"qT{i}", [128, S], BF16) for i in range(2)]
        kT = [self.sb(st, f"kT{i}", [128, S], BF16) for i in range(2)]
        V = [self.sb(st, f"V{i}", [128, NT, 129], BF16) for i in range(2)]
        oT = self.sb(st, "oT", [128, 4, S], BF16)
        wo = self.sb(st, "wo", [128, 4, D], BF16)
        PT = [self.sb(st, f"PT{i}", [128, 512], BF16) for i in range(2)]
        TMP = [self.sb(st, f"TMP{i}", [128, 512], F32) for i in range(2)]
        DT = [self.sb(st, f"DT{i}", [128, 512], F32) for i in range(2)]
        otok = [self.sb(st, f"otok{i}", [128, 128], BF16) for i in range(2)]
        rden = [self.sb(st, f"rden{i}", [128, 1], F32) for i in range(2)]
        e8t = self.sb(st, "e8t", [128, 1024], BF16)
        mneg = self.sb(st, "mneg", [128, 512], F32)
        mnot = self.sb(st, "mnot", [128, 512], F32)
        kmean = self.sb(st, "kmean", [128, 32], F32)
        kmh = self.sb(st, "kmh", [128, 32], BF16)
        kml = self.sb(st, "kml", [128, 32], BF16)
        kmr = self.sb(st, "kmr", [128, 32], F32)
        gm = self.sb(st, "gm", [128, 512], F32)
        m8 = self.sb(st, "m8", [128, 128], F32)
        sel = self.sb(st, "sel", [128, 512], F32)
        nbb = self.sb(st, "nbb", [128, 512], BF16)
        selbT = [self.sb(st, f"selbT{i}", [128, S], BF16) for i in range(2)]
        A(SP, lambda e: e.dma_start(out=e8t[:], in_=dr["e8"]), w=["e8t"], dma="e8t")
        A(SP, lambda e: e.dma_start(out=mneg[:], in_=dr["moba_neg"]), w=["mneg"], dma="mneg")
        A(SP, lambda e: e.dma_start(out=mnot[:], in_=dr["moba_notown"]), w=["mnot"], dma="mnot")
        for s in range(2):
            A(POOL, lambda e, s=s: e.memset(V[s][:, :, 128:129], 1.0), w=[f"V{s}"])
        A(POOL, lambda e: e.memset(kmean[:], 0.0), w=["kmean"])
        for i in range(2):
            A(DVE, lambda e, i=i: e.memset(selbT[i][:], 0.0), w=[f"selbT{i}"])
        w_in = dr["moba_w_in"][0]
        oi = [0]
        def prep(h):
            s = h % 2
            w3 = [w_in[:, which * 1024 + h * 128: which * 1024 + (h + 1) * 128] for which in range(3)]
            _proj_qkv(self, w3, h, wq, "wq0", qT[s], f"qT{s}", kT[s], f"kT{s}", V[s], f"V{s}", hT, hkeys, kmean=kmean)
            A(DVE, lambda e: e.tensor_copy(out=kmh[:], in_=kmean[:]), r=["kmean"], w=["kmh"])
            A(DVE, lambda e: e.tensor_tensor(out=kmr[:], in0=kmean[:], in1=kmh[:], op=ALU.subtract), r=["kmean", "kmh"], w=["kmr"])
            A(DVE, lambda e: e.tensor_copy(out=kml[:], in_=kmr[:]), r=["kmr"], w=["kml"])
            pg = self.sring.next()
            for t in range(NT):
                A(PE, lambda e, t=t, s=s, pg=pg: e.matmul(self.bank[pg][:, t * 32:(t + 1) * 32], lhsT=qT[s][:, t * 128:(t + 1) * 128],
                                                          rhs=kmh[:], start=True, stop=False), r=[f"qT{s}", "kmh"], w=[f"B{pg}"])
                A(PE, lambda e, t=t, s=s, pg=pg: e.matmul(self.bank[pg][:, t * 32:(t + 1) * 32], lhsT=qT[s][:, t * 128:(t + 1) * 128],
                                                          rhs=kml[:], start=False, stop=True), r=[f"qT{s}", "kml"], w=[f"B{pg}"])
            A(DVE, lambda e, pg=pg: e.tensor_tensor(out=gm[:], in0=self.bank[pg][:], in1=mneg[:], op=ALU.add),
              r=[f"B{pg}", "mneg"], w=["gm"])
            for t in range(NT):
                A(DVE, lambda e, t=t: e.max(out=m8[:, t * 8:(t + 1) * 8], in_=gm[:, t * 32:(t + 1) * 32]), r=["gm"], w=["m8"])
            for t in range(NT):
                A(DVE, lambda e, t=t: e.tensor_scalar(out=sel[:, t * 32:(t + 1) * 32], in0=gm[:, t * 32:(t + 1) * 32],
                                                      scalar1=m8[:, t * 8 + 2:t * 8 + 3], scalar2=None, op0=ALU.is_ge),
                  r=["gm", "m8"], w=["sel"])
            A(DVE, lambda e: e.tensor_scalar(out=sel[:], in0=sel[:], scalar1=-NEGB, scalar2=NEGB, op0=ALU.mult, op1=ALU.add),
              r=["sel"], w=["sel"])
            A(DVE, lambda e: e.tensor_tensor(out=nbb[:], in0=sel[:], in1=mnot[:], op=ALU.mult), r=["sel", "mnot"], w=["nbb"])
            for half in range(2):
                pb = self.sring.next()
                for tt in range(8):
                    t = half * 8 + tt
                    A(PE, lambda e, t=t, tt=tt, pb=pb: e.transpose(out=self.bankbf(pb)[0:32, tt * 128:(tt + 1) * 128],
                                                                  in_=nbb[:, t * 32:(t + 1) * 32], identity=cb[:, 0, :]),
                      r=["nbb", "cb"], w=[f"B{pb}"])
                A(ACT, lambda e, half=half, pb=pb: e.activation(out=selbT[s][0:32, half * 1024:(half + 1) * 1024],
                                                                in_=self.bankbf(pb)[0:32, :], func=AF.Copy),
                  r=[f"B{pb}"], w=[f"selbT{s}"])


        prep(0)
        for h in range(8):
            s = h % 2
            slope = 2.0 ** (-(h + 1))
            def epi(c, i, h=h, s=s):
                ob = oi[0] % 2
                oi[0] += 1
                A(DVE, lambda e, ob=ob, i=i: e.reciprocal(out=rden[ob][:], in_=self.bank[4 + i][:, 128:129]),
                  r=[f"B{4 + i}"], w=[f"rden{ob}"])
                _o_to_oT(self, self.bank[4 + i][:, 0:128], [f"B{4 + i}"], otok[ob], f"otok{ob}", oT, h % 4, 4 * c + i,
                         scale_ap=rden[ob][:, 0:1], scale_keys=[f"rden{ob}"])

            _attn_softmax(self, range(4), [(kT[s], qT[s])], [f"kT{s}", f"qT{s}"], V[s], f"V{s}", scale, slope, _causal_items,
                          (lambda kt: e8t[:, (kt // 2) * 128:(kt // 2 + 1) * 128], selbT[s], ["e8t", f"selbT{s}"]), PT, TMP, DT, epi,
                          mid_hook=((lambda h=h: prep(h + 1)) if h < 7 else None))
            if h % 4 == 3:
                hg = h // 4
                self.out_proj(wo, "wo", oT, 4, dr["moba_w_o"][0, hg * 512:(hg + 1) * 512, :], ["oT"])
        self.P.flush()
    with ExitStack() as st_ln:
        self.layer_norm(st_ln, li, 0)
        self.P.flush()


Builder.moba = _moba


def _rope_ops(self, x1, x2, cos, sin, o1, o2, R, rkeys, in_keys, okey):
    A = self.A
    A(DVE, lambda e: e.tensor_tensor(out=R[0], in0=x1, in1=cos, op=ALU.mult), r=in_keys + ["rope"], w=[rkeys[0]])
    A(DVE, lambda e: e.tensor_tensor(out=R[1], in0=x2, in1=sin, op=ALU.mult), r=in_keys + ["rope"], w=[rkeys[1]])
    A(DVE, lambda e: e.tensor_tensor(out=o1, in0=R[0], in1=R[1], op=ALU.subtract), r=[rkeys[0], rkeys[1]], w=[okey])
    A(DVE, lambda e: e.tensor_tensor(out=R[2], in0=x2, in1=cos, op=ALU.mult), r=in_keys + ["rope"], w=[rkeys[2]])
    A(DVE, lambda e: e.tensor_tensor(out=R[3], in0=x1, in1=sin, op=ALU.mult), r=in_keys + ["rope"], w=[rkeys[3]])
    A(DVE, lambda e: e.tensor_tensor(out=o2, in0=R[2], in1=R[3], op=ALU.add), r=[rkeys[2], rkeys[3]], w=[okey])


def _mla(self, li):
    from contextlib import ExitStack
    A, dr, cb = self.A, self.dr, self.cb
    scale = 192.0 ** -0.5
    self.atn_it = 0
    with ExitStack() as st:
        wuq = self.sb(st, "wuq", [128, 2, 1536], BF16)
        wukv = self.sb(st, "wukv", [128, 2, 2048], BF16)
        cos_t = self.sb(st, "cos_t", [128, NT, 32], F32)
        sin_t = self.sb(st, "sin_t", [128, NT, 32], F32)
        c_qT = self.sb(st, "c_qT", [128, 2, S], BF16)
        c_kvT = self.sb(st, "c_kvT", [128, 2, S], BF16)
        krT = self.sb(st, "krT", [128, S], BF16)
        gains = self.sb(st, "gains", [128, 4], F32)
        Rt = self.sb(st, "Rt", [128, 4, 128], F32)
        with ExitStack() as sth:
            hT, hkeys = _mixer_prologue(self, sth, li)
            w_in = self.sb(sth, "mlawin", [128, KC, 576], BF16)
            ang = self.sb(sth, "ang", [128, NT, 32], F32)
            invf = self.sb(sth, "invf", [128, 32], F32)
            junk = self.sb(sth, "junk", [128, 256], F32)
            ss = [self.sb(sth, f"ss{i}", [128, 4], F32) for i in range(2)]
            cn = [self.sb(sth, f"cn{i}", [128, 512], BF16) for i in range(2)]
            kr = [self.sb(sth, f"kr{i}", [128, 64], BF16) for i in range(2)]
            A(POOL, lambda e: e.dma_start(out=w_in[:], in_=dr["mla_w_in"][0].rearrange("(k p) n -> p k n", p=128)), w=["mlawin"], dma="mlawin")
            A(POOL, lambda e: e.dma_start(out=wuq[:], in_=dr["mla_w_uq"][0].rearrange("(k p) n -> p k n", p=128)), w=["wuq"], dma="wuq")
            for hf in range(2):
                A(POOL, lambda e, hf=hf: e.dma_start(out=wukv[:, :, hf * 1024:(hf + 1) * 1024],
                                                     in_=dr["mla_w_ukv"][0][:, hf * 1024:(hf + 1) * 1024].rearrange("(k p) n -> p k n", p=128)),
                  w=["wukv"], dma="wukv")
            A(SP, lambda e: e.dma_start(out=invf[:], in_=dr["invf"]), w=["invf"], dma="invf")
            A(SP, lambda e: e.dma_start(out=gains[:, 0:2], in_=dr["mla_q_norm"][0:1, :].rearrange("o (j p) -> p (o j)", p=128),
                                        allow_slow_non_contiguous=True), w=["gains"], dma="gains")
            A(SP, lambda e: e.dma_start(out=gains[:, 2:4], in_=dr["mla_kv_norm"][0:1, :].rearrange("o (j p) -> p (o j)", p=128),
                                        allow_slow_non_contiguous=True), w=["gains"], dma="gains")
            for t in range(NT):
                A(DVE, lambda e, t=t: e.tensor_scalar(out=ang[:, t, :], in0=invf[:], scalar1=self.posk[:, t:t + 1], scalar2=None, op0=ALU.mult),
                  r=["invf", "posk"], w=["ang"])
            ki = self.sb(sth, "ki", [128, NT, 32], I32)
            kf = self.sb(sth, "kf", [128, NT, 32], F32)
            mk = self.sb(sth, "mk", [128, NT, 32], F32)
            C1, C2 = 6.28125, 2 * np.pi - 6.28125
            for dst, shift in ((sin_t, 0.0), (cos_t, 0.5 * PI)):
                A(DVE, lambda e, dst=dst, shift=shift: e.tensor_scalar(out=dst[:], in0=ang[:], scalar1=shift, scalar2=None, op0=ALU.add), r=["ang"], w=["rope"])
                A(DVE, lambda e, dst=dst: e.tensor_scalar(out=kf[:], in0=dst[:], scalar1=float(1.0 / (2 * np.pi)), scalar2=None, op0=ALU.mult), r=["rope"], w=["kf"])
                A(DVE, lambda e: e.tensor_copy(out=ki[:], in_=kf[:]), r=["kf"], w=["ki"])
                A(DVE, lambda e: e.tensor_copy(out=kf[:], in_=ki[:]), r=["ki"], w=["kf"])
                A(DVE, lambda e, dst=dst: e.scalar_tensor_tensor(out=dst[:], in0=kf[:], scalar=-C1, in1=dst[:], op0=ALU.mult, op1=ALU.add), r=["kf", "rope"], w=["rope"])
                A(DVE, lambda e, dst=dst: e.scalar_tensor_tensor(out=dst[:], in0=kf[:], scalar=-C2, in1=dst[:], op0=ALU.mult, op1=ALU.add), r=["kf", "rope"], w=["rope"])
                A(DVE, lambda e, dst=dst: e.tensor_scalar(out=mk[:], in0=dst[:], scalar1=PI, scalar2=None, op0=ALU.is_gt), r=["rope"], w=["mk"])
                A(DVE, lambda e, dst=dst: e.scalar_tensor_tensor(out=dst[:], in0=mk[:], scalar=-2 * PI, in1=dst[:], op0=ALU.mult, op1=ALU.add), r=["mk", "rope"], w=["rope"])
                A(DVE, lambda e, dst=dst: e.tensor_scalar(out=mk[:], in0=dst[:], scalar1=-PI, scalar2=None, op0=ALU.is_lt), r=["rope"], w=["mk"])
                A(DVE, lambda e, dst=dst: e.scalar_tensor_tensor(out=dst[:], in0=mk[:], scalar=2 * PI, in1=dst[:], op0=ALU.mult, op1=ALU.add), r=["mk", "rope"], w=["rope"])
                A(DVE, lambda e, dst=dst: e.tensor_scalar(out=dst[:], in0=dst[:], scalar1=-3.1415925, scalar2=3.1415925, op0=ALU.max, op1=ALU.min), r=["rope"], w=["rope"])
            A(ACT, lambda e: e.activation(out=sin_t[:], in_=sin_t[:], func=AF.Sin), r=["rope"], w=["rope"])
            A(ACT, lambda e: e.activation(out=cos_t[:], in_=cos_t[:], func=AF.Sin), r=["rope"], w=["rope"])
            for t in range(NT):
                b = t % 2
                pa, pb2 = self.sring.next(), self.sring.next()
                for k in range(KC):
                    A(PE, lambda e, k=k, t=t, pa=pa: e.matmul(self.bank[pa][:], lhsT=hT[:, k, t * 128:(t + 1) * 128], rhs=w_in[:, k, 0:512],
                                                              start=(k == 0), stop=(k == KC - 1)), r=[hkeys[t], "mlawin"], w=[f"B{pa}"])
                for k in range(KC):
                    A(PE, lambda e, k=k, t=t, pb2=pb2: e.matmul(self.bank[pb2][:, 0:64], lhsT=hT[:, k, t * 128:(t + 1) * 128], rhs=w_in[:, k, 512:576],
                                                                start=(k == 0), stop=(k == KC - 1)), r=[hkeys[t], "mlawin"], w=[f"B{pb2}"])
                for j in range(2):
                    A(ACT, lambda e, j=j, pa=pa, b=b: e.activation(out=junk[:], in_=self.bank[pa][:, j * 256:(j + 1) * 256], func=AF.Square,
                                                                   accum_out=ss[b][:, j:j + 1]), r=[f"B{pa}"], w=["junk", f"ss{b}"])
                A(ACT, lambda e, b=b: e.activation(out=ss[b][:, 2:4], in_=ss[b][:, 0:2], func=AF.Ln, bias=1e-6, scale=1.0 / 256.0), r=[f"ss{b}"], w=[f"ss{b}"])
                A(ACT, lambda e, b=b: e.activation(out=ss[b][:, 2:4], in_=ss[b][:, 2:4], func=AF.Exp, scale=-0.5), r=[f"ss{b}"], w=[f"ss{b}"])
                for j in range(2):
                    A(DVE, lambda e, j=j, pa=pa, b=b: e.tensor_scalar(out=cn[b][:, j * 256:(j + 1) * 256], in0=self.bank[pa][:, j * 256:(j + 1) * 256],
                                                                      scalar1=ss[b][:, 2 + j:3 + j], scalar2=None, op0=ALU.mult),
                      r=[f"B{pa}", f"ss{b}"], w=[f"cn{b}"])
                pt = self.sring.next()
                for j in range(4):
                    A(PE, lambda e, j=j, b=b, pt=pt: e.transpose(out=self.bankbf(pt)[:, j * 128:(j + 1) * 128], in_=cn[b][:, j * 128:(j + 1) * 128],
                                                                 identity=cb[:, 0, :]), r=[f"cn{b}", "cb"], w=[f"B{pt}"])
                for j in range(4):
                    dst = c_qT if j < 2 else c_kvT
                    A(DVE, lambda e, j=j, t=t, pt=pt, dst=dst: e.tensor_scalar(
                        out=dst[:, j % 2, t * 128:(t + 1) * 128], in0=self.bankbf(pt)[:, j * 128:(j + 1) * 128], scalar1=gains[:, j:j + 1],
                        scalar2=None, op0=ALU.mult), r=[f"B{pt}", "gains"], w=["c_qT" if j < 2 else "c_kvT"])
                _rope_ops(self, self.bank[pb2][:, 0:32], self.bank[pb2][:, 32:64], cos_t[:, t, :], sin_t[:, t, :],
                          kr[b][:, 0:32], kr[b][:, 32:64], [Rt[:, i, 0:32] for i in range(4)], [f"Rt{i}" for i in range(4)], [f"B{pb2}"], f"kr{b}")
                pk = self.sring.next()
                A(PE, lambda e, b=b, pk=pk: e.transpose(out=self.bankbf(pk)[0:64, 0:128], in_=kr[b][:], identity=cb[:, 0, :]), r=[f"kr{b}", "cb"], w=[f"B{pk}"])
                A(ACT, lambda e, t=t, pk=pk: e.activation(out=krT[0:64, t * 128:(t + 1) * 128], in_=self.bankbf(pk)[0:64, 0:128], func=AF.Copy),
                  r=[f"B{pk}"], w=["krT"])
            self.P.flush()
        self.sring = Ring("m", [2, 3])
        self.sring_s = Ring("sc", [0, 1])
        qnT = [self.sb(st, f"qnT{i}", [128, S], BF16) for i in range(2)]
        qrT = [self.sb(st, f"qrT{i}", [128, S], BF16) for i in range(2)]
        knT = [self.sb(st, f"knT{i}", [128, S], BF16) for i in range(2)]
        V = [self.sb(st, f"V{i}", [128, NT, 129], BF16) for i in range(2)]
        oT = self.sb(st, "oT", [128, 4, S], BF16)
        wo = self.sb(st, "wo", [128, 4, D], BF16)
        PT = [self.sb(st, f"PT{i}", [128, 512], BF16) for i in range(2)]
        otok = [self.sb(st, f"otok{i}", [128, 128], BF16) for i in range(2)]
        rden = [self.sb(st, f"rden{i}", [128, 1], F32) for i in range(2)]
        qr = [self.sb(st, f"qr{i}", [128, 4, 64], BF16) for i in range(2)]
        for s in range(2):
            A(POOL, lambda e, s=s: e.memset(V[s][:, :, 128:129], 1.0), w=[f"V{s}"])
            A(DVE, lambda e, s=s: e.memset(qrT[s][64:128, :], 0.0), w=[f"qrT{s}"])
        A(DVE, lambda e: e.memset(krT[64:128, :], 0.0), w=["krT"])
        oi = [0]
        qi = 0
        def prep(h):
            nonlocal qi
            s = h % 2
            for c in range(4):
                pb = self.sring.next()
                for k in range(2):
                    A(PE, lambda e, k=k, c=c, pb=pb, h=h: e.matmul(self.bank[pb][:], lhsT=wuq[:, k, h * 192:h * 192 + 128], rhs=c_qT[:, k, c * 512:(c + 1) * 512],
                                                                   start=(k == 0), stop=(k == 1)), r=["wuq", "c_qT"], w=[f"B{pb}"])
                A(ACT, lambda e, c=c, pb=pb, s=s: e.activation(out=qnT[s][:, c * 512:(c + 1) * 512], in_=self.bank[pb][:], func=AF.Copy), r=[f"B{pb}"], w=[f"qnT{s}"])
                pb = self.sring.next()
                for k in range(2):
                    A(PE, lambda e, k=k, c=c, pb=pb, h=h: e.matmul(self.bank[pb][:], lhsT=wukv[:, k, h * 256:h * 256 + 128], rhs=c_kvT[:, k, c * 512:(c + 1) * 512],
                                                                   start=(k == 0), stop=(k == 1)), r=["wukv", "c_kvT"], w=[f"B{pb}"])
                A(DVE, lambda e, c=c, pb=pb, s=s: e.tensor_copy(out=knT[s][:, c * 512:(c + 1) * 512], in_=self.bank[pb][:]), r=[f"B{pb}"], w=[f"knT{s}"])
            for tg in range(4):
                pb = self.sring.next()
                for tt in range(4):
                    t = tg * 4 + tt
                    for k in range(2):
                        A(PE, lambda e, k=k, t=t, tt=tt, pb=pb, h=h: e.matmul(
                            self.bank[pb][:, tt * 128:(tt + 1) * 128], lhsT=c_kvT[:, k, t * 128:(t + 1) * 128], rhs=wukv[:, k, h * 256 + 128:h * 256 + 256],
                            start=(k == 0), stop=(k == 1)), r=["wukv", "c_kvT"], w=[f"B{pb}"])
                A(ACT, lambda e, tg=tg, pb=pb, s=s: e.activation(out=V[s][:, tg * 4:(tg + 1) * 4, 0:128],
                                                                 in_=self.bank[pb][:].rearrange("p (a n) -> p a n", a=4), func=AF.Copy), r=[f"B{pb}"], w=[f"V{s}"])
                pq = self.sring.next()
                for tt in range(4):
                    t = tg * 4 + tt
                    for k in range(2):
                        A(PE, lambda e, k=k, t=t, tt=tt, pq=pq, h=h: e.matmul(
                            self.bank[pq][:, tt * 64:(tt + 1) * 64], lhsT=c_qT[:, k, t * 128:(t + 1) * 128], rhs=wuq[:, k, h * 192 + 128:h * 192 + 192],
                            start=(k == 0), stop=(k == 1)), r=["wuq", "c_qT"], w=[f"B{pq}"])
                qb = qi % 2
                qi += 1
                xv = self.bank[pq][:, 0:256].rearrange("p (a n) -> p a n", a=4)
                _rope_ops(self, xv[:, :, 0:32], xv[:, :, 32:64], cos_t[:, tg * 4:(tg + 1) * 4, :], sin_t[:, tg * 4:(tg + 1) * 4, :],
                          qr[qb][:, :, 0:32], qr[qb][:, :, 32:64], [Rt[:, i, :].rearrange("p (a n) -> p a n", a=4) for i in range(4)],
                          [f"Rt{i}" for i in range(4)], [f"B{pq}"], f"qr{qb}")
                pt = self.sring.next()
                for tt in range(4):
                    A(PE, lambda e, tt=tt, qb=qb, pt=pt: e.transpose(out=self.bankbf(pt)[0:64, tt * 128:(tt + 1) * 128], in_=qr[qb][:, tt, :],
                                                                    identity=cb[:, 0, :]), r=[f"qr{qb}", "cb"], w=[f"B{pt}"])
                A(ACT, lambda e, tg=tg, pt=pt, s=s: e.activation(out=qrT[s][0:64, tg * 512:(tg + 1) * 512], in_=self.bankbf(pt)[0:64, 0:512], func=AF.Copy),
                  r=[f"B{pt}"], w=[f"qrT{s}"])


        prep(0)
        for h in range(8):
            s = h % 2
            def epi(c, i, h=h):
                ob = oi[0] % 2
                oi[0] += 1
                A(DVE, lambda e, ob=ob, i=i: e.reciprocal(out=rden[ob][:], in_=self.bank[4 + i][:, 128:129]), r=[f"B{4 + i}"], w=[f"rden{ob}"])
                _o_to_oT(self, self.bank[4 + i][:, 0:128], [f"B{4 + i}"], otok[ob], f"otok{ob}", oT, h % 4, 4 * c + i,
                         scale_ap=rden[ob][:, 0:1], scale_keys=[f"rden{ob}"])

            _attn_softmax(self, range(4), [(knT[s], qnT[s]), (krT, qrT[s])], [f"knT{s}", f"qnT{s}", "krT", f"qrT{s}"], V[s], f"V{s}",
                          scale, None, _causal_items, None, PT, None, None, epi,
                          mid_hook=((lambda h=h: prep(h + 1)) if h < 7 else None))
            if h % 4 == 3:
                hg = h // 4
                self.out_proj(wo, "wo", oT, 4, dr["mla_w_o"][0, hg * 512:(hg + 1) * 512, :], ["oT"])
        self.P.flush()
    with ExitStack() as st_ln:
        self.layer_norm(st_ln, li, 0)
        self.P.flush()


Builder.mla = _mla


def _proj_fm(self, wcols, wsl, wring, dst, dkey, hT, hkeys, use_act):
    A = self.A
    ws = wring.next()
    A(POOL, lambda e: e.dma_start(out=wsl[ws][:], in_=wcols.rearrange("(k p) n -> p k n", p=128)), w=[f"wsl{ws}"], dma=f"wsl{ws}")
    for c in range(4):
        pb = self.sring.next()
        for k in range(KC):
            A(PE, lambda e, k=k, c=c, pb=pb: e.matmul(self.bank[pb][:], lhsT=wsl[ws][:, k, :], rhs=hT[:, k, c * 512:(c + 1) * 512],
                                                      start=(k == 0), stop=(k == KC - 1)), r=[f"wsl{ws}"] + hkeys[4 * c:4 * c + 4], w=[f"B{pb}"])
        if use_act:
            A(ACT, lambda e, c=c, pb=pb: e.activation(out=dst[:, c * 512:(c + 1) * 512], in_=self.bank[pb][:], func=AF.Copy), r=[f"B{pb}"], w=[dkey])
        else:
            A(DVE, lambda e, c=c, pb=pb: e.tensor_copy(out=dst[:, c * 512:(c + 1) * 512], in_=self.bank[pb][:]), r=[f"B{pb}"], w=[dkey])


def _proj_tm(self, wcols, wsl, wring, V, vkey, hT, hkeys):
    A = self.A
    ws = wring.next()
    A(POOL, lambda e: e.dma_start(out=wsl[ws][:], in_=wcols.rearrange("(k p) n -> p k n", p=128)), w=[f"wsl{ws}"], dma=f"wsl{ws}")
    for tg in range(4):
        pb = self.sring.next()
        for tt in range(4):
            t = tg * 4 + tt
            for k in range(KC):
                A(PE, lambda e, k=k, t=t, tt=tt, pb=pb: e.matmul(self.bank[pb][:, tt * 128:(tt + 1) * 128], lhsT=hT[:, k, t * 128:(t + 1) * 128],
                                                                rhs=wsl[ws][:, k, :], start=(k == 0), stop=(k == KC - 1)),
                  r=[f"wsl{ws}", hkeys[t]], w=[f"B{pb}"])
        if tg % 2 == 0:
            A(ACT, lambda e, tg=tg, pb=pb: e.activation(out=V[:, tg * 4:(tg + 1) * 4, 0:128], in_=self.bank[pb][:].rearrange("p (a n) -> p a n", a=4),
                                                        func=AF.Copy), r=[f"B{pb}"], w=[vkey])
        else:
            A(DVE, lambda e, tg=tg, pb=pb: e.tensor_copy(out=V[:, tg * 4:(tg + 1) * 4, 0:128], in_=self.bank[pb][:].rearrange("p (a n) -> p a n", a=4)),
              r=[f"B{pb}"], w=[vkey])


def _win_items(c):
    out = []
    for kt in range(max(0, 4 * c - 4), 4 * c + 4):
        r_ = kt - 4 * c
        i_lo, i_hi = max(0, r_), min(3, r_ + 4)
        out.append((kt, i_lo * 128, (i_hi + 1) * 128, r_ >= 0, r_ + 4 <= 3))
    return out


def _nsa(self, li):
    from contextlib import ExitStack
    A, dr, cb = self.A, self.dr, self.cb
    scale = 128.0 ** -0.5
    self.atn_it = 0
    w_in = dr["nsa_w_in"][0]
    with ExitStack() as st:
        hT, hkeys = _mixer_prologue(self, st, li)
        oT = self.sb(st, "oT", [128, 4, S], BF16)
        gates = self.sb(st, "gates", [128, NT * 24], F32)
        wring = Ring("w", 2)
        with ExitStack() as sg:
            wg = self.sb(sg, "wg", [128, KC, 24], BF16)
            A(POOL, lambda e: e.dma_start(out=wg[:], in_=w_in[:, 2560:2584].rearrange("(k p) n -> p k n", p=128)), w=["wg"], dma="wg")
            pb = self.sring.next()
            for t in range(NT):
                for k in range(KC):
                    A(PE, lambda e, k=k, t=t, pb=pb: e.matmul(self.bank[pb][:, t * 24:(t + 1) * 24], lhsT=hT[:, k, t * 128:(t + 1) * 128], rhs=wg[:, k, :],
                                                              start=(k == 0), stop=(k == KC - 1)), r=["wg", hkeys[t]], w=[f"B{pb}"])
            A(ACT, lambda e, pb=pb: e.activation(out=gates[:], in_=self.bank[pb][:, 0:NT * 24], func=AF.Sigmoid), r=[f"B{pb}"], w=["gates"])
            self.P.flush()
        for g in range(2):
            with ExitStack() as sgp:
                qT = [self.sb(sgp, f"qT{i}", [128, S], BF16) for i in range(4)]
                kslT = self.sb(sgp, "kslT", [128, S], BF16)
                kwnT = self.sb(sgp, "kwnT", [128, S], BF16)
                vsl = self.sb(sgp, "vsl", [128, NT, 129], BF16)
                vwn = self.sb(sgp, "vwn", [128, NT, 129], BF16)
                ocs = [self.sb(sgp, f"ocs{i}", [128, NT, 128], BF16) for i in range(4)]
                nbT = self.sb(sgp, "nbT", [128, S], BF16)
                A(DVE, lambda e: e.memset(nbT[:], 0.0), w=["nbT"])
                A(POOL, lambda e: e.memset(vsl[:, :, 128:129], 1.0), w=["vsl"])
                A(POOL, lambda e: e.memset(vwn[:, :, 128:129], 1.0), w=["vwn"])
                kvc = lambda which: w_in[:, 1024 + which * 256 + g * 128: 1024 + which * 256 + (g + 1) * 128]
                with ExitStack() as spj:
                    wsl = [self.sb(spj, f"wsl{i}", [128, KC, 128], BF16) for i in range(2)]
                    for r in range(4):
                        hh = g * 4 + r
                        _proj_fm(self, w_in[:, hh * 128:(hh + 1) * 128], wsl, wring, qT[r], f"qT{r}", hT, hkeys, r % 2 == 0)
                    _proj_fm(self, kvc(2), wsl, wring, kslT, "kslT", hT, hkeys, True)
                    _proj_tm(self, kvc(3), wsl, wring, vsl, "vsl", hT, hkeys)
                    _proj_fm(self, kvc(4), wsl, wring, kwnT, "kwnT", hT, hkeys, False)
                    _proj_tm(self, kvc(5), wsl, wring, vwn, "vwn", hT, hkeys)
                    self.P.flush()
                with ExitStack() as sc:
                    kcT = self.sb(sc, "kcT", [128, 128], BF16)
                    vc = self.sb(sc, "vc", [128, 128], BF16)
                    with ExitStack() as sca:
                        wsl = [self.sb(sca, f"wsl{i}", [128, KC, 128], BF16) for i in range(2)]
                        rawT1 = self.sb(sca, "rawT0", [128, S], BF16)
                        rawT = [rawT1, rawT1]
                        w1b = self.sb(sca, "w1b", [128, 32, 128], BF16)
                        w2b = self.sb(sca, "w2b", [128, 128], BF16)
                        peT = self.sb(sca, "peT", [128, 32], F32)
                        peTb = self.sb(sca, "peTb", [128, 32], BF16)
                        b1 = self.sb(sca, "b1", [128, 1], F32)
                        g1 = self.sb(sca, "g1", [128, 128], BF16)
                        for which in range(2):
                            _proj_fm(self, kvc(which), wsl, wring, rawT[which], "rawT0", hT, hkeys, which == 0)
                            A(POOL, lambda e, which=which: e.dma_start(out=w1b[:], in_=dr["nsa_cmp_w1"][0, which].rearrange("(l d) f -> d l f", d=128)),
                              w=["w1b"], dma="w1b")
                            A(POOL, lambda e, which=which: e.dma_start(out=w2b[:], in_=dr["nsa_cmp_w2"][0, which]), w=["w2b"], dma="w2b")
                            A(SP, lambda e, which=which: e.dma_start(out=peT[:], in_=dr["nsa_cmp_pos"][0, which].rearrange("l d -> d l"),
                                                                     allow_slow_non_contiguous=True), w=["peT"], dma="peT")
                            A(DVE, lambda e: e.tensor_copy(out=peTb[:], in_=peT[:]), r=["peT"], w=["peTb"])
                            pz = self.sring.next()
                            for l in range(32):
                                A(PE, lambda e, l=l, pz=pz: e.matmul(self.bank[pz][:, 0:1], lhsT=w1b[:, l, :], rhs=peTb[:, l:l + 1], start=(l == 0), stop=(l == 31)),
                                  r=["w1b", "peTb"], w=[f"B{pz}"])
                            A(DVE, lambda e, pz=pz: e.tensor_copy(out=b1[:], in_=self.bank[pz][:, 0:1]), r=[f"B{pz}"], w=["b1"])
                            ph = self.sring.next()
                            for l in range(32):
                                A(PE, lambda e, l=l, ph=ph, which=which: e.matmul(self.bank[ph][:, 0:127], lhsT=w1b[:, l, :], rhs=rawT[which][:, l:l + 2017:16],
                                                                                 start=(l == 0), stop=(l == 31)), r=["w1b", "rawT0"], w=[f"B{ph}"])
                            A(ACT, lambda e, ph=ph: e.activation(out=g1[:, 0:127], in_=self.bank[ph][:, 0:127], func=AF.Gelu_apprx_tanh, bias=b1[:, 0:1], scale=1.0),
                              r=[f"B{ph}", "b1"], w=["g1"])
                            po = self.sring.next()
                            if which == 0:
                                A(PE, lambda e, po=po: e.matmul(self.bank[po][:, 0:127], lhsT=w2b[:], rhs=g1[:, 0:127], start=True, stop=True), r=["w2b", "g1"], w=[f"B{po}"])
                                A(DVE, lambda e, po=po: e.tensor_copy(out=kcT[:, 0:127], in_=self.bank[po][:, 0:127]), r=[f"B{po}"], w=["kcT"])
                            else:
                                A(PE, lambda e, po=po: e.matmul(self.bank[po][0:127, 0:128], lhsT=g1[:, 0:127], rhs=w2b[:], start=True, stop=True), r=["w2b", "g1"], w=[f"B{po}"])
                                A(DVE, lambda e, po=po: e.tensor_copy(out=vc[0:127, :], in_=self.bank[po][0:127, 0:128]), r=[f"B{po}"], w=["vc"])
                        self.P.flush()
                    with ExitStack() as scl:
                        cmask = self.sb(scl, "cmask", [128, NT * 127], BF16)
                        ovt = self.sb(scl, "ovt", [128, 32], BF16)
                        zer = self.sb(scl, "zer", [128, 512], BF16)
                        Dc = [self.sb(scl, f"Dc{i}", [128, 127], F32) for i in range(2)]
                        tc_ = [self.sb(scl, f"tc{i}", [128, 127], F32) for i in range(2)]
                        pc = [self.sb(scl, f"pc{i}", [128, 127], F32) for i in range(2)]
                        pnb = [self.sb(scl, f"pnb{i}", [128, 127], BF16) for i in range(2)]
                        pnT = [self.sb(scl, f"pnT{i}", [128, 128], BF16) for i in range(2)]
                        sm = [self.sb(scl, f"sm{i}", [128, 8], F32) for i in range(2)]
                        A(SP, lambda e: e.dma_start(out=cmask[:], in_=dr["nsa_cmask"]), w=["cmask"], dma="cmask")
                        A(SP, lambda e: e.dma_start(out=ovt[:], in_=dr["ov"]), w=["ovt"], dma="ovt")
                        A(POOL, lambda e: e.memset(zer[:], 0.0), w=["zer"])
                        A(PE, lambda e: e.matmul(self.bank[4][:], lhsT=cb[:, 4, :], rhs=zer[:], start=True, stop=False, skip_group_check=True), r=["cb", "zer"], w=["B4"])
                        cstages = []
                        ci = 0
                        for r in range(4):
                            hh = g * 4 + r
                            slope = 2.0 ** (-(hh + 1))
                            for t in range(NT):
                                b = ci % 2
                                ci += 1
                                cd = {}

                                def ca1(cd=cd, t=t, r=r, b=b):
                                    pS = self.sring.next()
                                    cd["pS"] = pS
                                    A(PE, lambda e: e.matmul(self.bank[pS][:, 0:127], lhsT=qT[r][:, t * 128:(t + 1) * 128], rhs=kcT[:, 0:127], start=True, stop=True),
                                      r=[f"qT{r}", "kcT"], w=[f"B{pS}"])
                                    A(ACT, lambda e: e.activation(out=Dc[b][:], in_=self.posq[:, 31:2048:16], func=AF.Abs, bias=self.nposk[:, t:t + 1], scale=1.0),
                                      r=["posq", "posk"], w=[f"Dc{b}"])

                                def ca2(cd=cd, t=t, r=r, b=b, slope=slope):
                                    pS = cd["pS"]
                                    A(DVE, lambda e: e.scalar_tensor_tensor(out=tc_[b][:], in0=Dc[b][:], scalar=-slope / scale, in1=self.bank[pS][:, 0:127],
                                                                            op0=ALU.mult, op1=ALU.add), r=[f"Dc{b}", f"B{pS}"], w=[f"tc{b}"])
                                    A(DVE, lambda e: e.reduce_max(out=sm[b][:, 0:1], in_=tc_[b][:], axis=AX.X), r=[f"tc{b}"], w=[f"sm{b}"])
                                    A(DVE, lambda e: e.tensor_scalar(out=sm[b][:, 1:2], in0=sm[b][:, 0:1], scalar1=-scale, scalar2=None, op0=ALU.mult), r=[f"sm{b}"], w=[f"sm{b}"])
                                    A(ACT, lambda e: e.activation(out=pc[b][:], in_=tc_[b][:], func=AF.Exp, bias=sm[b][:, 1:2], scale=scale), r=[f"tc{b}", f"sm{b}"], w=[f"pc{b}"])

                                def cb_(cd=cd, t=t, r=r, b=b, hh=hh):
                                    A(DVE, lambda e: e.tensor_tensor(out=pc[b][:], in0=pc[b][:], in1=cmask[:, t * 127:(t + 1) * 127], op=ALU.mult), r=[f"pc{b}", "cmask"], w=[f"pc{b}"])
                                    A(DVE, lambda e: e.reduce_sum(out=sm[b][:, 2:3], in_=pc[b][:], axis=AX.X), r=[f"pc{b}"], w=[f"sm{b}"])
                                    A(DVE, lambda e: e.tensor_scalar(out=sm[b][:, 2:3], in0=sm[b][:, 2:3], scalar1=1e-30, scalar2=None, op0=ALU.max), r=[f"sm{b}"], w=[f"sm{b}"])
                                    A(DVE, lambda e: e.reciprocal(out=sm[b][:, 3:4], in_=sm[b][:, 2:3]), r=[f"sm{b}"], w=[f"sm{b}"])
                                    A(DVE, lambda e: e.tensor_scalar(out=pnb[b][:], in0=pc[b][:], scalar1=sm[b][:, 3:4], scalar2=None, op0=ALU.mult), r=[f"pc{b}", f"sm{b}"], w=[f"pnb{b}"])
                                    pt = self.sring.next()
                                    A(PE, lambda e: e.transpose(out=self.bankbf(pt)[0:127, 0:128], in_=pnb[b][:], identity=cb[:, 0, :]), r=[f"pnb{b}", "cb"], w=[f"B{pt}"])
                                    A(ACT, lambda e: e.activation(out=pnT[b][0:127, :], in_=self.bankbf(pt)[0:127, 0:128], func=AF.Copy), r=[f"B{pt}"], w=[f"pnT{b}"])
                                    po = self.sring.next()
                                    A(PE, lambda e: e.matmul(self.bank[po][:, 0:128], lhsT=pnT[b][0:127, :], rhs=vc[0:127, :], start=True, stop=True), r=[f"pnT{b}", "vc"], w=[f"B{po}"])
                                    A(PE, lambda e: e.matmul(self.bank[4][:, t * 32:(t + 1) * 32], lhsT=pnT[b][0:127, :], rhs=ovt[0:127, :], start=False, stop=False,
                                                             skip_group_check=True), r=[f"pnT{b}", "ovt"], w=["B4"])
                                    A(ACT, lambda e: e.activation(out=ocs[r][:, t, :], in_=self.bank[po][:, 0:128], func=AF.Copy,
                                                                  scale=gates[:, t * 24 + hh:t * 24 + hh + 1]), r=[f"B{po}", "gates"], w=[f"ocs{r}"])
                                cstages.append((ca1, ca2, cb_))
                        ncs = len(cstages)
                        for step in range(-2, ncs):
                            for si, off in enumerate((2, 1, 0)):
                                k = step + off
                                if 0 <= k < ncs:
                                    cstages[k][si]()
                        self.P.flush()
                    impm = self.sb(sc, "impm", [128, 512], F32)
                    work = self.sb(sc, "work", [128, 512], F32)
                    nval = self.sb(sc, "nval", [128, 512], F32)
                    nbon = self.sb(sc, "nbon", [128, 512], F32)
                    m8a = self.sb(sc, "m8a", [128, 128], F32)
                    m8b = self.sb(sc, "m8b", [128, 128], F32)
                    selt = self.sb(sc, "selt", [128, 512], F32)
                    nbb = self.sb(sc, "nbb", [128, 512], BF16)
                    A(SP, lambda e: e.dma_start(out=nval[:], in_=dr["nsa_valid"]), w=["nval"], dma="nval")
                    A(SP, lambda e: e.dma_start(out=nbon[:], in_=dr["nsa_bonus"]), w=["nbon"], dma="nbon")
                    A(DVE, lambda e: e.tensor_tensor(out=impm[:], in0=self.bank[4][:], in1=nval[:], op=ALU.mult), r=["B4", "nval"], w=["impm"])
                    A(DVE, lambda e: e.tensor_tensor(out=impm[:], in0=impm[:], in1=nbon[:], op=ALU.add), r=["impm", "nbon"], w=["impm"])
                    for t in range(NT):
                        sl = slice(t * 32, (t + 1) * 32)
                        s8 = slice(t * 8, (t + 1) * 8)
                        A(DVE, lambda e, sl=sl, s8=s8: e.max(out=m8a[:, s8], in_=impm[:, sl]), r=["impm"], w=["m8a"])
                        A(DVE, lambda e, sl=sl, s8=s8: e.match_replace(out=work[:, sl], in_to_replace=m8a[:, s8], in_values=impm[:, sl], imm_value=-3.0e38),
                          r=["impm", "m8a"], w=["work"])
                        A(DVE, lambda e, sl=sl, s8=s8: e.max(out=m8b[:, s8], in_=work[:, sl]), r=["work"], w=["m8b"])
                        A(DVE, lambda e, sl=sl, t=t: e.tensor_scalar(out=selt[:, sl], in0=impm[:, sl], scalar1=m8b[:, t * 8 + 7:t * 8 + 8], scalar2=None, op0=ALU.is_ge),
                          r=["impm", "m8b"], w=["selt"])
                    A(DVE, lambda e: e.tensor_tensor(out=selt[:], in0=selt[:], in1=nval[:], op=ALU.mult), r=["selt", "nval"], w=["selt"])
                    A(DVE, lambda e: e.tensor_scalar(out=nbb[:], in0=selt[:], scalar1=-NEGB, scalar2=NEGB, op0=ALU.mult, op1=ALU.add), r=["selt"], w=["nbb"])
                    for q4 in range(2):
                        pb = self.sring.next()
                        for tt in range(8):
                            t = q4 * 8 + tt
                            A(PE, lambda e, t=t, tt=tt, pb=pb: e.transpose(out=self.bankbf(pb)[0:32, tt * 128:(tt + 1) * 128], in_=nbb[:, t * 32:(t + 1) * 32],
                                                                          identity=cb[:, 0, :]), r=["nbb", "cb"], w=[f"B{pb}"])
                        A(ACT, lambda e, q4=q4, pb=pb: e.activation(out=nbT[0:32, q4 * 1024:(q4 + 1) * 1024], in_=self.bankbf(pb)[0:32, :], func=AF.Copy),
                          r=[f"B{pb}"], w=["nbT"])
                    self.P.flush()
                with ExitStack() as sw:
                    e32t = self.sb(sw, "e32t", [128, 2048], BF16)
                    PT = [self.sb(sw, f"PT{i}", [128, 512], BF16) for i in range(2)]
                    TMP = [self.sb(sw, f"TMP{i}", [128, 512], F32) for i in range(2)]
                    DT = [self.sb(sw, f"DT{i}", [128, 512], F32) for i in range(2)]
                    otok = [self.sb(sw, f"otok{i}", [128, 128], BF16) for i in range(2)]
                    rd = [self.sb(sw, f"rd{i}", [128, 2], F32) for i in range(2)]
                    A(SP, lambda e: e.dma_start(out=e32t[:], in_=dr["e32"]), w=["e32t"], dma="e32t")
                    oi = [0]
                    for r in range(4):
                        hh = g * 4 + r
                        slope = 2.0 ** (-(hh + 1))

                        def epi_sel(c, i, r=r, hh=hh):
                            ob = oi[0] % 2
                            oi[0] += 1
                            t = 4 * c + i
                            A(DVE, lambda e: e.reciprocal(out=rd[ob][:, 0:1], in_=self.bank[4 + i][:, 128:129]), r=[f"B{4 + i}"], w=[f"rd{ob}"])
                            A(DVE, lambda e: e.tensor_tensor(out=rd[ob][:, 1:2], in0=rd[ob][:, 0:1], in1=gates[:, t * 24 + 8 + hh:t * 24 + 9 + hh], op=ALU.mult),
                              r=[f"rd{ob}", "gates"], w=[f"rd{ob}"])
                            A(DVE, lambda e: e.scalar_tensor_tensor(out=ocs[r][:, t, :], in0=self.bank[4 + i][:, 0:128], scalar=rd[ob][:, 1:2], in1=ocs[r][:, t, :],
                                                                    op0=ALU.mult, op1=ALU.add), r=[f"B{4 + i}", f"rd{ob}", f"ocs{r}"], w=[f"ocs{r}"])

                        def epi_win(c, i, r=r, hh=hh):
                            ob = oi[0] % 2
                            oi[0] += 1
                            t = 4 * c + i
                            A(DVE, lambda e: e.reciprocal(out=rd[ob][:, 0:1], in_=self.bank[4 + i][:, 128:129]), r=[f"B{4 + i}"], w=[f"rd{ob}"])
                            A(DVE, lambda e: e.tensor_tensor(out=rd[ob][:, 1:2], in0=rd[ob][:, 0:1], in1=gates[:, t * 24 + 16 + hh:t * 24 + 17 + hh], op=ALU.mult),
                              r=[f"rd{ob}", "gates"], w=[f"rd{ob}"])
                            A(DVE, lambda e: e.scalar_tensor_tensor(out=otok[ob][:], in0=self.bank[4 + i][:, 0:128], scalar=rd[ob][:, 1:2], in1=ocs[r][:, t, :],
                                                                    op0=ALU.mult, op1=ALU.add), r=[f"B{4 + i}", f"rd{ob}", f"ocs{r}"], w=[f"otok{ob}"])
                            pb = self.sring.next()
                            A(PE, lambda e, pb=pb: e.transpose(out=self.bankbf(pb)[:, 0:128], in_=otok[ob][:], identity=cb[:, 0, :]), r=[f"otok{ob}", "cb"], w=[f"B{pb}"])
                            A(ACT, lambda e, pb=pb: e.activation(out=oT[:, r, t * 128:(t + 1) * 128], in_=self.bankbf(pb)[:, 0:128], func=AF.Copy), r=[f"B{pb}"], w=["oT"])

                        _attn_softmax(self, range(4), [(kslT, qT[r])], ["kslT", f"qT{r}"], vsl, "vsl", scale, slope, _causal_items,
                                      (lambda kt: e32t[:, kt * 128:(kt + 1) * 128], nbT, ["e32t", "nbT"]), PT, TMP, DT, epi_sel)
                        _attn_softmax(self, range(4), [(kwnT, qT[r])], ["kwnT", f"qT{r}"], vwn, "vwn", scale, slope, _win_items,
                                      None, PT, TMP, DT, epi_win)
                    self.P.flush()
            with ExitStack() as so:
                wo = self.sb(so, "wo", [128, 4, D], BF16)
                self.out_proj(wo, "wo", oT, 4, dr["nsa_w_o"][0, g * 512:(g + 1) * 512, :], ["oT"])
                self.P.flush()
        self.P.flush()
    with ExitStack() as st_ln:
        self.layer_norm(st_ln, li, 0)
        self.P.flush()


Builder.nsa = _nsa
```
